# Optimizing a Trainium2 kernel written in Bass

```python
import jax
import jax.numpy as jnp
from jax import lax
import numpy as np

D_MODEL = 1024
BATCH = 2
SEQ = 8192
DEPTH = 4

GRID_W = 64
CTX_LEN = 256
HEAD_DIM = 64
ATT_HEADS = 8
KV_HEADS = 2
Q_PER_KV = ATT_HEADS // KV_HEADS
ATT_W = ATT_HEADS * HEAD_DIM
KV_W = KV_HEADS * HEAD_DIM
WINDOW = 128
ROPE_THETA = 10000.0
CHUNK = 128
SGU_GROUPS = 4
SGU_GW = 128
SGU_W = SGU_GROUPS * SGU_GW
LRU_W = D_MODEL
LRU_BLOCKS = 8
LRU_BW = LRU_W // LRU_BLOCKS
CONV_W = 4
LRU_C = 8.0
N_BRANCHES = 3
N_GROUPS = 4
EXPERTS_PER_GROUP = 8
N_EXPERTS = N_GROUPS * EXPERTS_PER_GROUP
TOP_K = 2
D_EXPERT = 512
MOE_BLOCK = 128
N_MOD = 6
COL_WIDTHS = (ATT_W, SGU_W, SGU_W, N_BRANCHES * D_MODEL, LRU_W, KV_W, KV_W, LRU_W)
IN_W = ATT_W + 2 * SGU_W + N_BRANCHES * D_MODEL + LRU_W + 2 * KV_W + LRU_W
CTX_COL_OFF = IN_W - (2 * KV_W + LRU_W)
ALPHA = (2.0 * DEPTH) ** 0.25
BETA = (8.0 * DEPTH) ** -0.25
LN_EPS = 1e-6
NEG_INF = -1e30

kernel_name = 'hybrid_gated_branch_dit_trunk'


def layer_norm(x, g, b):
    xf = x.astype(jnp.float32)
    mu = jnp.mean(xf, -1, keepdims=True)
    var = jnp.mean(jnp.square(xf - mu), -1, keepdims=True)
    y = (xf - mu) * lax.rsqrt(var + LN_EPS)
    return (y * g.astype(jnp.float32) + b.astype(jnp.float32)).astype(x.dtype)


def split_cols(t, widths):
    return jnp.split(t, np.cumsum(widths)[:-1].tolist(), axis=-1)


def axial_rope_tables(n_tok):
    rows = n_tok // GRID_W
    row = jnp.repeat(jnp.arange(rows, dtype=jnp.float32), GRID_W)
    col = jnp.tile(jnp.arange(GRID_W, dtype=jnp.float32), rows)
    axis_dim = HEAD_DIM // 2
    inv_freq = ROPE_THETA ** (-jnp.arange(0, axis_dim, 2, dtype=jnp.float32) / axis_dim)
    ang_r = row[:, None] * inv_freq
    ang_c = col[:, None] * inv_freq
    return (jnp.cos(ang_r), jnp.sin(ang_r), jnp.cos(ang_c), jnp.sin(ang_c))


def _rotate(x, cos, sin):
    f = cos.shape[-1]
    x1, x2 = x[..., :f], x[..., f:]
    cos = cos[:, None, :]
    sin = sin[:, None, :]
    return jnp.concatenate([x1 * cos - x2 * sin, x2 * cos + x1 * sin], -1)


def apply_axial_rope(x, tables):
    cr, sr, cc, sc = tables
    xf = x.astype(jnp.float32)
    half = HEAD_DIM // 2
    out = jnp.concatenate([_rotate(xf[..., :half], cr, sr), _rotate(xf[..., half:], cc, sc)], -1)
    return out.astype(x.dtype)


def spatial_gating(u, v, ln_g, ln_b, w_spatial, b_spatial):
    bsz, n = u.shape[:2]
    n_chunks = n // CHUNK
    vn = layer_norm(v, ln_g, ln_b).reshape(bsz, n_chunks, CHUNK, SGU_GROUPS, SGU_GW)
    mixed = jnp.einsum('gpq,bnqgc->bnpgc', w_spatial, vn) + b_spatial.T[:, :, None]
    return u * mixed.reshape(bsz, n, SGU_W)


def window_attention(q, k, v, k_ctx, v_ctx, sink):
    bsz, n = q.shape[:2]
    nb = n // WINDOW
    scale = HEAD_DIM ** -0.5
    qb = q.reshape(bsz, nb, WINDOW, KV_HEADS, Q_PER_KV, HEAD_DIM)

    def neighbours(t):
        tp = jnp.pad(t, ((0, 0), (WINDOW, WINDOW), (0, 0), (0, 0)))
        tp = tp.reshape(bsz, nb + 2, WINDOW, KV_HEADS, HEAD_DIM)
        return jnp.concatenate([tp[:, :-2], tp[:, 1:-1], tp[:, 2:]], axis=2)

    kb, vb = neighbours(k), neighbours(v)
    s_loc = jnp.einsum('bnqkgd,bnjkd->bnkgqj', qb, kb, preferred_element_type=jnp.float32) * scale
    s_ctx = jnp.einsum('bnqkgd,bckd->bnkgqc', qb, k_ctx, preferred_element_type=jnp.float32) * scale
    q_idx = jnp.arange(nb)[:, None, None] * WINDOW + jnp.arange(WINDOW)[None, :, None]
    k_idx = (jnp.arange(nb)[:, None, None] - 1) * WINDOW + jnp.arange(3 * WINDOW)[None, None, :]
    valid = (jnp.abs(k_idx - q_idx) <= WINDOW) & (k_idx >= 0) & (k_idx < n)
    s_loc = jnp.where(valid[None, :, None, None], s_loc, NEG_INF)
    sink_l = jnp.broadcast_to(sink.astype(jnp.float32).reshape(KV_HEADS, Q_PER_KV)[:, :, None, None],
                              s_loc.shape[:-1] + (1,))
    n_ctx = k_ctx.shape[1]
    p = jax.nn.softmax(jnp.concatenate([sink_l, s_ctx, s_loc], -1), axis=-1)
    p_ctx = p[..., 1:1 + n_ctx].astype(v.dtype)
    p_loc = p[..., 1 + n_ctx:].astype(v.dtype)
    o = (jnp.einsum('bnkgqc,bckd->bnqkgd', p_ctx, v_ctx)
         + jnp.einsum('bnkgqj,bnjkd->bnqkgd', p_loc, vb))
    return o.reshape(bsz, n, ATT_W)


def context_attention(q, k, v, sink):
    bsz, n = q.shape[:2]
    qg = q.reshape(bsz, n, KV_HEADS, Q_PER_KV, HEAD_DIM)
    s = jnp.einsum('bqkgd,bckd->bkgqc', qg, k, preferred_element_type=jnp.float32) * HEAD_DIM ** -0.5
    sink_l = jnp.broadcast_to(sink.astype(jnp.float32).reshape(KV_HEADS, Q_PER_KV)[:, :, None, None],
                              s.shape[:-1] + (1,))
    p = jax.nn.softmax(jnp.concatenate([sink_l, s], -1), axis=-1)[..., 1:].astype(v.dtype)
    o = jnp.einsum('bkgqc,bckd->bqkgd', p, v)
    return o.reshape(bsz, n, ATT_W)


def short_conv(t, w, b, reverse):
    n = t.shape[1]
    pad = (0, CONV_W - 1) if reverse else (CONV_W - 1, 0)
    tp = jnp.pad(t, ((0, 0), pad, (0, 0)))
    out = b
    for j in range(CONV_W):
        out = out + w[j] * tp[:, j:j + n]
    return out


def block_diag(t, w, b):
    tb = t.reshape(t.shape[:-1] + (LRU_BLOCKS, LRU_BW))
    return jnp.einsum('blnc,ncd->blnd', tb, w).reshape(t.shape) + b


def lru_gates(t, w_r, b_r, w_i, b_i, lam):
    r = jax.nn.sigmoid(block_diag(t, w_r, b_r).astype(jnp.float32))
    i = jax.nn.sigmoid(block_diag(t, w_i, b_i).astype(jnp.float32))
    log_a = -LRU_C * r * jax.nn.softplus(-lam.astype(jnp.float32))
    a = jnp.exp(log_a)
    mult = jnp.sqrt(-jnp.expm1(2.0 * log_a))
    return a, mult * i * t.astype(jnp.float32)


def linear_scan(a, u, h0):
    def combine(left, right):
        return (left[0] * right[0], right[0] * left[1] + right[1])
    a_cum, h = lax.associative_scan(combine, (a, u), axis=1)
    return h + a_cum * h0[:, None, :]


def rglru_direction(x_ctx, x_lat, conv_w, conv_b, w_r, b_r, w_i, b_i, lam, reverse):
    def scan_inputs(t):
        a, u = lru_gates(short_conv(t, conv_w, conv_b, reverse), w_r, b_r, w_i, b_i, lam)
        if reverse:
            return jnp.flip(a, 1), jnp.flip(u, 1)
        return a, u

    a_c, u_c = scan_inputs(x_ctx)
    a_l, u_l = scan_inputs(x_lat)
    h_c = linear_scan(a_c, u_c, jnp.zeros((x_ctx.shape[0], LRU_W), jnp.float32))
    h_l = linear_scan(a_l, u_l, h_c[:, -1])
    if reverse:
        return jnp.flip(h_c, 1), jnp.flip(h_l, 1)
    return h_c, h_l


def merge_branches(a, b, r, gate_logits, w_proj_a, w_proj_b, w_proj_c, w_out, b_out):
    g_a, g_b, g_r = jnp.split(jax.nn.sigmoid(gate_logits), N_BRANCHES, axis=-1)
    y = g_a * (a @ w_proj_a) + g_b * (b @ w_proj_b) + g_r * (r @ w_proj_c)
    return y @ w_out + b_out


def token_mixer(h_lat, h_ctx, rope, w_in, b_in, sgu_ln_g, sgu_ln_b, w_spatial, b_spatial, attn_sink,
                conv_w, conv_b, w_rgate, b_rgate, w_igate, b_igate, lru_lambda,
                w_proj_a, w_proj_b, w_proj_c, w_out, b_out, need_ctx_out):
    q, u, v, gates, z, k, val, xr = split_cols(h_lat @ w_in + b_in, COL_WIDTHS)
    if need_ctx_out:
        q_c, u_c, v_c, gates_c, z_c, k_c, val_c, xr_c = split_cols(h_ctx @ w_in + b_in, COL_WIDTHS)
    else:
        k_c, val_c, xr_c = split_cols(h_ctx @ w_in[:, CTX_COL_OFF:] + b_in[CTX_COL_OFF:], COL_WIDTHS[-3:])

    def heads(t, nh):
        return t.reshape(t.shape[:2] + (nh, HEAD_DIM))

    a_lat = spatial_gating(jax.nn.gelu(u), jax.nn.gelu(v), sgu_ln_g, sgu_ln_b, w_spatial, b_spatial)
    k_cx, v_cx = heads(k_c, KV_HEADS), heads(val_c, KV_HEADS)
    b_lat = window_attention(apply_axial_rope(heads(q, ATT_HEADS), rope),
                             apply_axial_rope(heads(k, KV_HEADS), rope),
                             heads(val, KV_HEADS), k_cx, v_cx, attn_sink)
    hc_f, hl_f = rglru_direction(xr_c, xr, conv_w[0], conv_b[0], w_rgate[0], b_rgate[0],
                                 w_igate[0], b_igate[0], lru_lambda[0], reverse=False)
    hc_b, hl_b = rglru_direction(xr_c, xr, conv_w[1], conv_b[1], w_rgate[1], b_rgate[1],
                                 w_igate[1], b_igate[1], lru_lambda[1], reverse=True)
    r_lat = (hl_f + hl_b).astype(z.dtype) * jax.nn.gelu(z)
    y_lat = merge_branches(a_lat, b_lat, r_lat, gates, w_proj_a, w_proj_b, w_proj_c, w_out, b_out)
    if not need_ctx_out:
        return y_lat, None
    a_ctx = spatial_gating(jax.nn.gelu(u_c), jax.nn.gelu(v_c), sgu_ln_g, sgu_ln_b, w_spatial, b_spatial)
    b_ctx = context_attention(heads(q_c, ATT_HEADS), k_cx, v_cx, attn_sink)
    r_ctx = (hc_f + hc_b).astype(z_c.dtype) * jax.nn.gelu(z_c)
    y_ctx = merge_branches(a_ctx, b_ctx, r_ctx, gates_c, w_proj_a, w_proj_b, w_proj_c, w_out, b_out)
    return y_lat, y_ctx


def hierarchical_moe(h, w_group, b_group, w_router, b_router, w1, w3, w2):
    bsz, n, d = h.shape
    xt = h.reshape(-1, d)
    n_tok = xt.shape[0]
    g_logits = (xt @ w_group + b_group).astype(jnp.float32)
    g_idx = jnp.argmax(g_logits, axis=-1)
    g_w = jnp.take_along_axis(jax.nn.softmax(g_logits, axis=-1), g_idx[:, None], axis=-1)[:, 0]
    e_logits = (xt @ w_router + b_router).astype(jnp.float32).reshape(n_tok, N_GROUPS, EXPERTS_PER_GROUP)
    e_logits = jnp.take_along_axis(e_logits, g_idx[:, None, None], axis=1)[:, 0]
    top_v, top_j = lax.top_k(e_logits, TOP_K)
    gate = g_w[:, None] * jax.nn.softmax(top_v, axis=-1)
    expert = g_idx[:, None] * EXPERTS_PER_GROUP + top_j
    n_as = n_tok * TOP_K
    e_flat = expert.reshape(-1)
    order = jnp.argsort(e_flat)
    e_sorted = e_flat[order]
    tok_sorted = order // TOP_K
    gate_sorted = gate.reshape(-1)[order]
    counts = jnp.bincount(e_flat, length=N_EXPERTS)
    starts = jnp.cumsum(counts) - counts
    padded = (counts + MOE_BLOCK - 1) // MOE_BLOCK * MOE_BLOCK
    pad_ends = jnp.cumsum(padded)
    pad_starts = pad_ends - padded
    slot = pad_starts[e_sorted] + jnp.arange(n_as) - starts[e_sorted]
    n_slots = (-(-n_as // MOE_BLOCK) + N_EXPERTS) * MOE_BLOCK
    n_blocks = n_slots // MOE_BLOCK
    buf = jnp.zeros((n_slots, d), xt.dtype).at[slot].set(xt[tok_sorted])
    block_expert = jnp.minimum(
        jnp.searchsorted(pad_ends, jnp.arange(n_blocks) * MOE_BLOCK, side='right'), N_EXPERTS - 1)

    def expert_block(args):
        xb, e = args
        return (jax.nn.silu(xb @ w1[e]) * (xb @ w3[e])) @ w2[e]

    out = lax.map(expert_block, (buf.reshape(n_blocks, MOE_BLOCK, d), block_expert)).reshape(n_slots, d)
    contrib = out[slot] * gate_sorted[:, None].astype(out.dtype)
    y = jax.ops.segment_sum(contrib, tok_sorted, num_segments=n_tok)
    return y.reshape(bsz, n, d)


def setup_inputs(seed: int = 0) -> dict:
    key = jax.random.key(seed)
    keys = iter(jax.random.split(key, 48))
    f32 = jnp.float32
    L = DEPTH
    D = D_MODEL

    def nrm(shape, scale):
        return jax.random.normal(next(keys), shape, f32) * scale

    x = nrm((BATCH, SEQ, D), 1.0)
    c = nrm((BATCH, D), 1.0)
    ctx = nrm((BATCH, CTX_LEN, D), 1.0)
    c_ctx = nrm((D,), 1.0)
    w_mod = nrm((L, D, N_MOD * D), 0.5 * D ** -0.5)
    b_mod = nrm((L, N_MOD * D), 0.02)
    w_in = nrm((L, D, IN_W), D ** -0.5)
    b_in = nrm((L, IN_W), 0.02)
    sgu_ln_g = 1.0 + nrm((L, SGU_W), 0.02)
    sgu_ln_b = nrm((L, SGU_W), 0.02)
    w_spatial = nrm((L, SGU_GROUPS, CHUNK, CHUNK), CHUNK ** -0.5)
    b_spatial = 1.0 + nrm((L, SGU_GROUPS, CHUNK), 0.02)
    attn_sink = nrm((L, ATT_HEADS), 0.5)
    conv_w = nrm((L, 2, CONV_W, LRU_W), CONV_W ** -0.5)
    conv_b = nrm((L, 2, LRU_W), 0.02)
    w_rgate = nrm((L, 2, LRU_BLOCKS, LRU_BW, LRU_BW), LRU_BW ** -0.5)
    b_rgate = nrm((L, 2, LRU_W), 0.02)
    w_igate = nrm((L, 2, LRU_BLOCKS, LRU_BW, LRU_BW), LRU_BW ** -0.5)
    b_igate = nrm((L, 2, LRU_W), 0.02)
    a_pow = jax.random.uniform(next(keys), (L, 2, LRU_W), f32, 0.9, 0.999)
    a_base = a_pow ** (1.0 / LRU_C)
    lru_lambda = jnp.log(a_base) - jnp.log1p(-a_base)
    w_proj_a = nrm((L, SGU_W, D), SGU_W ** -0.5)
    w_proj_b = nrm((L, ATT_W, D), ATT_W ** -0.5)
    w_proj_c = nrm((L, LRU_W, D), LRU_W ** -0.5)
    w_out = nrm((L, D, D), BETA * D ** -0.5)
    b_out = nrm((L, D), 0.02)
    ln1_g = 1.0 + nrm((L, D), 0.02)
    ln1_b = nrm((L, D), 0.02)
    ln2_g = 1.0 + nrm((L, D), 0.02)
    ln2_b = nrm((L, D), 0.02)
    w_group = nrm((L, D, N_GROUPS), D ** -0.5)
    b_group = nrm((L, N_GROUPS), 0.01)
    w_router = nrm((L, D, N_EXPERTS), D ** -0.5)
    b_router = nrm((L, N_EXPERTS), 0.01)
    w1 = nrm((L, N_EXPERTS, D, D_EXPERT), D ** -0.5)
    w3 = nrm((L, N_EXPERTS, D, D_EXPERT), D ** -0.5)
    w2 = nrm((L, N_EXPERTS, D_EXPERT, D), BETA * D_EXPERT ** -0.5)
    return {'x': x, 'c': c, 'ctx': ctx, 'c_ctx': c_ctx, 'w_mod': w_mod, 'b_mod': b_mod,
            'w_in': w_in, 'b_in': b_in, 'sgu_ln_g': sgu_ln_g, 'sgu_ln_b': sgu_ln_b,
            'w_spatial': w_spatial, 'b_spatial': b_spatial, 'attn_sink': attn_sink,
            'conv_w': conv_w, 'conv_b': conv_b, 'w_rgate': w_rgate, 'b_rgate': b_rgate,
            'w_igate': w_igate, 'b_igate': b_igate, 'lru_lambda': lru_lambda,
            'w_proj_a': w_proj_a, 'w_proj_b': w_proj_b, 'w_proj_c': w_proj_c,
            'w_out': w_out, 'b_out': b_out, 'ln1_g': ln1_g, 'ln1_b': ln1_b,
            'ln2_g': ln2_g, 'ln2_b': ln2_b, 'w_group': w_group, 'b_group': b_group,
            'w_router': w_router, 'b_router': b_router, 'w1': w1, 'w3': w3, 'w2': w2}


def reference(x, c, ctx, c_ctx, w_mod, b_mod, w_in, b_in, sgu_ln_g, sgu_ln_b, w_spatial, b_spatial,
              attn_sink, conv_w, conv_b, w_rgate, b_rgate, w_igate, b_igate, lru_lambda,
              w_proj_a, w_proj_b, w_proj_c, w_out, b_out, ln1_g, ln1_b, ln2_g, ln2_b,
              w_group, b_group, w_router, b_router, w1, w3, w2):
    n_ctx = ctx.shape[1]
    rope = axial_rope_tables(x.shape[1])
    cond_lat = jax.nn.silu(c)
    cond_ctx = jax.nn.silu(c_ctx)
    for l in range(DEPTH):
        last = l == DEPTH - 1
        m_lat = [m[:, None, :] for m in jnp.split(cond_lat @ w_mod[l] + b_mod[l], N_MOD, axis=-1)]
        m_ctx = jnp.split(cond_ctx @ w_mod[l] + b_mod[l], N_MOD, axis=-1)
        y_lat, y_ctx = token_mixer(
            x * (1.0 + m_lat[1]) + m_lat[0], ctx * (1.0 + m_ctx[1]) + m_ctx[0], rope,
            w_in[l], b_in[l], sgu_ln_g[l], sgu_ln_b[l], w_spatial[l], b_spatial[l], attn_sink[l],
            conv_w[l], conv_b[l], w_rgate[l], b_rgate[l], w_igate[l], b_igate[l], lru_lambda[l],
            w_proj_a[l], w_proj_b[l], w_proj_c[l], w_out[l], b_out[l], need_ctx_out=not last)
        x = layer_norm(ALPHA * x + m_lat[2] * y_lat, ln1_g[l], ln1_b[l])
        h_lat = x * (1.0 + m_lat[4]) + m_lat[3]
        if last:
            f_lat = hierarchical_moe(h_lat, w_group[l], b_group[l], w_router[l], b_router[l],
                                     w1[l], w3[l], w2[l])
        else:
            ctx = layer_norm(ALPHA * ctx + m_ctx[2] * y_ctx, ln1_g[l], ln1_b[l])
            h_ctx = ctx * (1.0 + m_ctx[4]) + m_ctx[3]
            f_all = hierarchical_moe(jnp.concatenate([h_ctx, h_lat], axis=1), w_group[l], b_group[l],
                                     w_router[l], b_router[l], w1[l], w3[l], w2[l])
            f_lat = f_all[:, n_ctx:]
            ctx = layer_norm(ALPHA * ctx + m_ctx[5] * f_all[:, :n_ctx], ln2_g[l], ln2_b[l])
        x = layer_norm(ALPHA * x + m_lat[5] * f_lat, ln2_g[l], ln2_b[l])
    return x
```

```python
import contextlib
import numpy as np
import concourse.bass as bass
import concourse.mybir as mybir
from concourse.bass_utils import run_bass_kernel_spmd

F32 = mybir.dt.float32
BF16 = mybir.dt.bfloat16
I32 = mybir.dt.int32
AF = mybir.ActivationFunctionType
ALU = mybir.AluOpType
AX = mybir.AxisListType

D = 1024
DEPTH = 4
SEQ = 8192
NCTX = 256
NSEG = 4
TOWN = 2048
HALO = 128
TLAT = TOWN + 2 * HALO
TEXT = TLAT + NCTX
NOWN = TOWN + NCTX
ALPHA = (2.0 * DEPTH) ** 0.25
LN_EPS = 1e-6
NEXP = 32
NBLK = (NOWN * 2) // 128 + NEXP
C_Q, C_QS, C_K, C_KS, C_U, C_V, C_VA, C_G, C_Z, C_XR = 0, 512, 1024, 1152, 1280, 1792, 2304, 2432, 5504, 6528
NCOL = 7552


class FW:
    NDMA = 24

    def __init__(self, nc, stack):
        self.nc = nc
        self.E = {'pe': nc.tensor, 'act': nc.scalar, 'dve': nc.vector, 'pool': nc.gpsimd, 'sp': nc.sync}
        self.sem = {}
        self.cnt = {}
        for e in ('pe', 'act', 'dve', 'pool'):
            self.sem[e] = stack.enter_context(nc.semaphore('s_' + e))
            self.cnt[e] = 0
        self.dsem = [stack.enter_context(nc.semaphore('d%d' % i)) for i in range(self.NDMA)]
        self.dval = [0] * self.NDMA
        self.dnext = 0
        self.waited = {}
        self.lastw = {}
        self.readers = {}
        self.n_instr = 0

    def _wait(self, e, tok):
        if tok is None:
            return
        kind, sk, val = tok
        k = (e, kind, sk)
        if self.waited.get(k, 0) >= val:
            return
        self.waited[k] = val
        s = self.sem[sk] if kind == 'eng' else self.dsem[sk]
        self.E[e].wait_ge(s, val)

    def _deps(self, e, reads, writes, strict=False):
        for r in reads:
            t = self.lastw.get(r)
            if t is not None:
                if t[0] == 'eng' and t[1] == e and e == 'pe':
                    continue
                self._wait(e, t)
        for w in writes:
            t = self.lastw.get(w)
            if t is not None and (strict or not (t[0] == 'eng' and t[1] == e)):
                self._wait(e, t)
            for t in self.readers.get(w, ()):
                if strict or not (t[0] == 'eng' and t[1] == e):
                    self._wait(e, t)

    def _commit(self, tok, reads, writes):
        for r in reads:
            self.readers.setdefault(r, []).append(tok)
        for w in writes:
            self.lastw[w] = tok
            self.readers[w] = []

    def op(self, e, fn, reads=(), writes=()):
        self._deps(e, reads, writes)
        ins = fn()
        self.cnt[e] += 1
        ins.then_inc(self.sem[e], 1)
        self._commit(('eng', e, self.cnt[e]), reads, writes)
        self.n_instr += 1
        return ins

    def dma(self, q, fn, reads=(), writes=()):
        i = self.dnext
        self.dnext = (self.dnext + 1) % self.NDMA
        if self.dval[i] > 0:
            self._wait(q, ('dma', i, self.dval[i]))
        self._deps(q, reads, writes, strict=True)
        ins = fn()
        self.dval[i] += 16
        ins.then_inc(self.dsem[i], 16)
        self._commit(('dma', i, self.dval[i]), reads, writes)
        self.n_instr += 1
        return ins

    def barrier(self):
        toks = [('eng', e2, self.cnt[e2]) for e2 in ('pe', 'act', 'dve', 'pool') if self.cnt[e2]]
        toks += [('dma', i, self.dval[i]) for i in range(self.NDMA) if self.dval[i]]
        for e in ('pe', 'act', 'dve', 'pool', 'sp'):
            for t in toks:
                if t[0] == 'eng' and t[1] == e:
                    continue
                self._wait(e, t)

    def finish(self, keys, e='sp'):
        for k in keys:
            self._wait(e, self.lastw.get(k))
        for i in range(self.NDMA):
            if self.dval[i]:
                self._wait(e, ('dma', i, self.dval[i]))


class Shared:
    def __init__(self, nc, st):
        self.nc = nc
        self.fw = FW(nc, st)
        self.PS = [st.enter_context(nc.psum_tensor("ps%d" % i, [128, 512], F32)) for i in range(7)]
        self.psi = 0
        self.uid = 0
        self.scr = {}

    def next_ps(self):
        i = self.psi
        self.psi = (i + 1) % 7
        return i

    def scratch(self, name, shape, dt):
        if name not in self.scr:
            self.scr[name] = self.nc.dram_tensor(name, list(shape), dt).ap()
        return self.scr[name]


def build_layer(prepass, dbg=False, phases=("lru", "att", "sgu", "merge", "moe", "moe8")):
    nc = bass.Bass("TRN2", target_bir_lowering=False)

    def din(name, shape, dt=F32):
        return nc.dram_tensor(name, list(shape), dt, kind="ExternalInput").ap()

    def dout(name, shape, dt=F32):
        return nc.dram_tensor(name, list(shape), dt, kind="ExternalOutput").ap()
    with contextlib.ExitStack() as st0:
        sh = Shared(nc, st0)
        emit_layer(nc, sh, din, dout, prepass, dbg, phases)
        sh.fw.finish([])
    return nc


def emit_layer(nc, sh, din, dout, prepass, dbg=False, phases=("lru", "att", "sgu", "merge", "moe", "moe8")):
    xT = din("xT", [128, 8, TLAT])
    cT = din("cT", [128, 8, NCTX])
    cond = din("cond", [128, 8, 2])
    w_mod = din("w_mod", [128, 8, 6 * D])
    b_mod = din("b_mod", [128, 48])
    w_in = din("w_in", [128, 8, NCOL])
    b_in = din("b_in", [128, NCOL // 128])
    edge = din("edge", [128, 2])
    convw = din("convw", [128, 8, 2, 4])
    convb = din("convb", [128, 8, 2])
    wrg = din("wrg", [128, 2, 8, 128])
    wig = din("wig", [128, 2, 8, 128])
    brg = din("brg", [128, 8, 2])
    big = din("big", [128, 8, 2])
    lam = din("lam", [128, 8, 2])
    car = din("car", [128, 8, 2, 3, 2])
    ab_out = dout("ab_out", [128, 8, 2, 2])
    dbg_outs = {}
    if dbg:
        dbg_outs["r_dbg"] = dout("r_dbg", [128, 8, NOWN])
        dbg_outs["a_dbg"] = dout("a_dbg", [128, 4, NOWN])
        dbg_outs["b_dbg"] = dout("b_dbg", [64, 8, NOWN])
        dbg_outs["kt_dbg"] = dout("kt_dbg", [64, 2, TEXT])
        dbg_outs["x1_dbg"] = dout("x1_dbg", [128, 8, NOWN])
        dbg_outs["lg_dbg"] = dout("lg_dbg", [128, 18, 36])
        dbg_outs["sl_dbg"] = dout("sl_dbg", [128, 2, 18], I32)
        dbg_outs["g_dbg"] = dout("g_dbg", [128, 2, 18])
        dbg_outs["wi_dbg"] = dout("wi_dbg", [128, NBLK], I32)
        dbg_outs["vt_dbg"] = dout("vt_dbg", [128, 20, 128])
    if not prepass:
        rope_cos = din("rope_cos", [64, TLAT])
        rope_sin = din("rope_sin", [64, TLAT])
        masks = din("masks", [128, 2, 4, 128])
        sinkx = din("sinkx", [64, 8, 128])
        ones_in = din("ones_in", [128, 64])
        b_va = din("b_va", [128, 128])
        b_qk = din("b_qk", [64, 20])
        b_v = din("b_v", [128, 512])
        sgu_g = din("sgu_g", [128, 512])
        sgu_b = din("sgu_b", [128, 512])
        w_spT = din("w_spT", [128, 4, 128])
        b_sp = din("b_sp", [128, 4, 128])
        w_pa = din("w_pa", [128, 4, D])
        w_pb = din("w_pb", [64, 8, D])
        w_pc = din("w_pc", [128, 8, D])
        w_o = din("w_o", [128, 8, D])
        b_o = din("b_o", [128, 8])
        ln1g = din("ln1g", [128, 8])
        ln1b = din("ln1b", [128, 8])
        ln2g = din("ln2g", [128, 8])
        ln2b = din("ln2b", [128, 8])
        w_gr = din("w_gr", [128, 8, 36])
        b_gr = din("b_gr", [128, 36])
        ident = din("ident", [128, 128])
        ones_f = din("ones_f", [128, 128])
        tri_in = din("tri_in", [128, 128])
        bval_in = din("bval_in", [128, NBLK])
        pidx_in = din("pidx_in", [128, 1])
        wexp = {}
        for nm in ("w1a", "w1b", "w3a", "w3b", "w2a", "w2b"):
            wexp[nm] = din(nm, [NEXP * 128, 2048])
        xout = dout("xout", [128, 8, TOWN])
        cout = dout("cout", [128, 8, NCTX])

    sh.uid += 1
    sfx = "_%d" % sh.uid
    with contextlib.ExitStack() as st:
        fw = sh.fw

        def sb(name, shape, dt=F32, stack=st):
            return stack.enter_context(nc.sbuf_tensor(name + sfx, list(shape), dt))

        def ps(name, shape, dt=F32, stack=st):
            return stack.enter_context(nc.psum_tensor(name + sfx, list(shape), dt))

        PS = sh.PS
        next_ps = sh.next_ps

        def load(q, dst, src, wkey, rkeys=()):
            eng = {'sp': nc.sync, 'act': nc.scalar, 'pool': nc.gpsimd}[q]
            return fw.dma(q, lambda: eng.dma_start(out=dst, in_=src), reads=list(rkeys), writes=[wkey])

        condt = sb("condt", [128, 8, 2])
        condb = sb("condb", [128, 8, 2], BF16)
        sig = sb("sig", [128, 8, 2])
        bmod = sb("bmod", [128, 48])
        mlat = sb("mlat", [128, 48])
        mctx = sb("mctx", [128, 48])
        bint = sb("bint", [128, NCOL // 128])
        edget = sb("edget", [128, 2])
        cw = sb("cw", [128, 8, 2, 4])
        cb = sb("cb", [128, 8, 2])
        brt = sb("brt", [128, 8, 2])
        bit_ = sb("bit", [128, 8, 2])
        lamt = sb("lamt", [128, 8, 2])
        negc = sb("negc", [128, 8, 2])
        negc2 = sb("negc2", [128, 8, 2])
        cart = sb("cart", [128, 8, 2, 3, 2])
        abt = sb("abt", [128, 8, 2, 2])
        load('sp', condt[:], cond, 'condt')
        load('sp', bmod[:], b_mod, 'bmod')
        load('sp', bint[:], b_in, 'bint')
        load('sp', edget[:], edge, 'edget')
        load('sp', cw[:], convw, 'cw')
        load('sp', cb[:], convb, 'cb')
        load('sp', brt[:], brg, 'brt')
        load('sp', bit_[:], big, 'bit')
        load('sp', lamt[:], lam, 'lamt')
        load('sp', cart[:], car, 'cart')

        fw.op('act', lambda: nc.scalar.activation(out=sig[:], in_=condt[:], func=AF.Sigmoid), reads=['condt'], writes=['sig'])
        fw.op('dve', lambda: nc.vector.tensor_tensor(out=condb[:], in0=condt[:], in1=sig[:], op=ALU.mult), reads=['condt', 'sig'], writes=['condb'])
        fw.op('act', lambda: nc.scalar.activation(out=negc[:], in_=lamt[:], func=AF.Exp, scale=-1.0), reads=['lamt'], writes=['negc'])
        fw.op('act', lambda: nc.scalar.activation(out=negc[:], in_=negc[:], func=AF.Ln, bias=1.0), reads=['negc'], writes=['negc'])
        fw.op('dve', lambda: nc.vector.tensor_scalar(out=negc2[:], in0=negc[:], scalar1=-16.0, scalar2=None, op0=ALU.mult), reads=['negc'], writes=['negc2'])
        fw.op('dve', lambda: nc.vector.tensor_scalar(out=negc[:], in0=negc[:], scalar1=-8.0, scalar2=None, op0=ALU.mult), reads=['negc'], writes=['negc'])

        with contextlib.ExitStack() as s1:
            wm = [sb("wm%d" % i, [128, 8, 512], BF16, s1) for i in range(2)]
            pmi = next_ps()
            pm = PS[pmi]
            pmk = 'ps%d' % pmi
            for g in range(12):
                wbuf = wm[g % 2]
                load('pool', wbuf[:], w_mod[:, :, g * 512:(g + 1) * 512], 'wm%d' % (g % 2))
                for jj in range(4):
                    j = g * 4 + jj
                    for kc in range(8):
                        fw.op('pe', lambda: nc.tensor.matmul(pm[:, 2 * j:2 * j + 2], lhsT=wbuf[:, kc, jj * 128:(jj + 1) * 128],
                                                              rhs=condb[:, kc, :], start=(kc == 0), stop=(kc == 7)),
                              reads=['wm%d' % (g % 2), 'condb'], writes=[pmk])
            pmv = pm[:, 0:96].rearrange("p (j t) -> p j t", t=2)
            fw.op('dve', lambda: nc.vector.tensor_tensor(out=mlat[:], in0=pmv[:, :, 0], in1=bmod[:], op=ALU.add), reads=[pmk, 'bmod'], writes=['mlat'])
            fw.op('dve', lambda: nc.vector.tensor_tensor(out=mctx[:], in0=pmv[:, :, 1], in1=bmod[:], op=ALU.add), reads=[pmk, 'bmod'], writes=['mctx'])
        fw.barrier()
        m1p = sb("m1p", [128, 2, 8])
        m4p = sb("m4p", [128, 2, 8])
        for t_, mv in ((0, mlat), (1, mctx)):
            fw.op('dve', lambda: nc.vector.tensor_scalar(out=m1p[:, t_, :], in0=mv[:, 8:16], scalar1=1.0, scalar2=None, op0=ALU.add), reads=['mlat', 'mctx'], writes=['m1p'])
            fw.op('dve', lambda: nc.vector.tensor_scalar(out=m4p[:, t_, :], in0=mv[:, 32:40], scalar1=1.0, scalar2=None, op0=ALU.add), reads=['mlat', 'mctx'], writes=['m4p'])

        sY = st.enter_context(contextlib.ExitStack())
        yT = sY.enter_context(nc.sbuf_tensor("yT" + sfx, [128, 8, NOWN], BF16, side='right'))
        sHT = contextlib.ExitStack()
        hT = sHT.enter_context(nc.sbuf_tensor("hT" + sfx, [128, 8, TEXT], BF16, side='right'))
        with contextlib.ExitStack() as s2:
            xs = [sb("xs%d" % i, [128, TLAT], F32, s2) for i in range(2)]
            for c in range(8):
                xb_ = xs[c % 2]
                k = 'xs%d' % (c % 2)
                load('sp', xb_[:], xT[:, c, :], k)
                fw.op('act', lambda: nc.scalar.activation(out=hT[:, c, 0:TLAT], in_=xb_[:], func=AF.Identity,
                                                          scale=m1p[:, 0, c:c + 1], bias=mlat[:, c:c + 1]),
                      reads=[k, 'm1p', 'mlat'], writes=['hT%d' % c])
                load('sp', xb_[:, 0:NCTX], cT[:, c, :], k)
                fw.op('act', lambda: nc.scalar.activation(out=hT[:, c, TLAT:TEXT], in_=xb_[:, 0:NCTX], func=AF.Identity,
                                                          scale=m1p[:, 1, c:c + 1], bias=mctx[:, c:c + 1]),
                      reads=[k, 'm1p', 'mctx'], writes=['hT%d' % c])
        fw.barrier()
        hkeys = ['hT%d' % c for c in range(8)]

        def inproj(col0, ncols, tok0, ntok, wkey, wtile, evac):
            if wtile is not None:
                wsl = lambda kc: wtile[:, kc, col0:col0 + ncols]
            else:
                wsl = col0
            return inproj2(wsl, ncols, tok0, ntok, wkey, evac)

        def inproj2(wsl, ncols, tok0, ntok, wkey, evac):
            pi = next_ps()
            pt = PS[pi]
            for kc in range(8):
                fw.op('pe', lambda: nc.tensor.matmul(pt[0:ncols, 0:ntok], lhsT=wsl(kc), rhs=hT[:, kc, tok0:tok0 + ntok],
                                                      start=(kc == 0), stop=(kc == 7)),
                      reads=[wkey, 'hT%d' % kc], writes=['ps%d' % pi])
            evac(pt, 'ps%d' % pi)

        R_d = sh.scratch("R_d", [128, 8, NOWN], BF16)
        A_d = sh.scratch("A_d", [128, 4, NOWN], BF16)
        B_d = sh.scratch("B_d", [64, 8, NOWN], BF16)
        if "lru" in phases:
          with contextlib.ExitStack() as s3:
            rst = sb("rst", [128, NOWN], BF16, s3)
            wxr = [sb("wxr%d" % i, [128, 8, 128], BF16, s3) for i in range(2)]
            wz = [sb("wz%d" % i, [128, 8, 128], BF16, s3) for i in range(2)]
            wr_t = sb("wr_t", [128, 2, 8, 128], BF16, s3)
            wi_t = sb("wi_t", [128, 2, 8, 128], BF16, s3)
            load('pool', wr_t[:], wrg, 'wr_t')
            load('pool', wi_t[:], wig, 'wi_t')
            XRL = sb("XRL", [128, TLAT], F32, s3)
            XRC = sb("XRC", [128, NCTX + 6], F32, s3)
            GZ = sb("GZ", [128, NOWN], F32, s3)
            TT = [sb("TT0", [128, NOWN], F32, s3)] * 2
            TB = [sb("TB0", [128, NOWN], BF16, s3)] * 2
            RG = [sb("RG0", [128, NOWN], F32, s3)] * 2
            IG = [sb("IG0", [128, NOWN], F32, s3)] * 2
            AA = [sb("AA0", [128, NOWN], F32, s3)] * 2
            HH = [sb("HH%d" % i, [128, NOWN], F32, s3) for i in range(2)]
            sm = sb("sm", [128, 16], F32, s3)
            fw.op('pool', lambda: nc.gpsimd.memset(XRC[:], 0.0), writes=['XRC'])
            for j in range(8):
                wx = wxr[j % 2]
                wzz = wz[j % 2]
                kx = 'wxr%d' % (j % 2)
                kz = 'wz%d' % (j % 2)
                load('pool', wx[:], w_in[:, :, C_XR + j * 128:C_XR + (j + 1) * 128], kx)
                load('pool', wzz[:], w_in[:, :, C_Z + j * 128:C_Z + (j + 1) * 128], kz)
                bx = bint[:, (C_XR // 128) + j:(C_XR // 128) + j + 1]
                bz = bint[:, (C_Z // 128) + j:(C_Z // 128) + j + 1]
                for ti in range(5):
                    def ev(pt, pk, ti=ti):
                        if ti < 4:
                            fw.op('act', lambda: nc.scalar.activation(out=XRL[:, ti * 512:(ti + 1) * 512], in_=pt[:, 0:512], func=AF.Identity, bias=bx),
                                  reads=[pk, 'bint'], writes=['XRL'])
                        else:
                            fw.op('act', lambda: nc.scalar.activation(out=XRL[:, 2048:2304], in_=pt[:, 0:256], func=AF.Identity, bias=bx),
                                  reads=[pk, 'bint'], writes=['XRL'])
                            fw.op('act', lambda: nc.scalar.activation(out=XRC[:, 3:3 + NCTX], in_=pt[:, 256:512], func=AF.Identity, bias=bx),
                                  reads=[pk, 'bint'], writes=['XRC'])
                    inproj(0, 128, ti * 512, 512, kx, wx, ev)
                fw.op('dve', lambda: nc.vector.tensor_scalar(out=XRL[:, 0:HALO], in0=XRL[:, 0:HALO], scalar1=edget[:, 0:1], scalar2=None, op0=ALU.mult),
                      reads=['XRL', 'edget'], writes=['XRL'])
                fw.op('dve', lambda: nc.vector.tensor_scalar(out=XRL[:, HALO + TOWN:TLAT], in0=XRL[:, HALO + TOWN:TLAT], scalar1=edget[:, 1:2], scalar2=None, op0=ALU.mult),
                      reads=['XRL', 'edget'], writes=['XRL'])
                for ti in range(5):
                    def evz(pt, pk, ti=ti):
                        if ti < 4:
                            fw.op('act', lambda: nc.scalar.activation(out=GZ[:, ti * 512:(ti + 1) * 512], in_=pt[:, 0:512], func=AF.Gelu, bias=bz),
                                  reads=[pk, 'bint'], writes=['GZ'])
                        else:
                            fw.op('act', lambda: nc.scalar.activation(out=GZ[:, 2048:2304], in_=pt[:, 0:256], func=AF.Gelu, bias=bz),
                                  reads=[pk, 'bint'], writes=['GZ'])
                    if ti < 4:
                        inproj(0, 128, HALO + ti * 512, 512, kz, wzz, evz)
                    else:
                        inproj(0, 128, TLAT, 256, kz, wzz, evz)
                for d_ in range(2):
                    T_ = TT[d_]; Tb = TB[d_]; Rg = RG[d_]; Ig = IG[d_]; A_ = AA[d_]; H_ = HH[d_]
                    kT, kTb, kR, kI, kA, kH = 'TT', 'TB', 'RG', 'IG', 'AA', 'HH%d' % d_
                    for (dst0, n, src, s0, sk) in ((0, TOWN, XRL, HALO, 'XRL'), (TOWN, NCTX, XRC, 3, 'XRC')):
                        for tap in range(4):
                            off = s0 + (tap - 3 if d_ == 0 else tap)
                            wcol = cw[:, j, d_, tap:tap + 1]
                            if tap == 0:
                                fw.op('dve', lambda: nc.vector.tensor_scalar(out=T_[:, dst0:dst0 + n], in0=src[:, off:off + n], scalar1=wcol,
                                                                             scalar2=cb[:, j, d_:d_ + 1], op0=ALU.mult, op1=ALU.add),
                                      reads=[sk, 'cw', 'cb'], writes=[kT])
                            else:
                                fw.op('dve', lambda: nc.vector.scalar_tensor_tensor(out=T_[:, dst0:dst0 + n], in0=src[:, off:off + n], scalar=wcol,
                                                                                    in1=T_[:, dst0:dst0 + n], op0=ALU.mult, op1=ALU.add),
                                      reads=[sk, 'cw', kT], writes=[kT])
                    fw.op('pool', lambda: nc.gpsimd.tensor_copy(out=Tb[:], in_=T_[:]), reads=[kT], writes=[kTb])
                    for ti in range(5):
                        t0 = ti * 512
                        n = 512 if ti < 4 else 256
                        for (wt_, wk, bt_, bk, dst, dk) in ((wr_t, 'wr_t', brt, 'brt', Rg, kR), (wi_t, 'wi_t', bit_, 'bit', Ig, kI)):
                            pi = next_ps()
                            pt = PS[pi]
                            fw.op('pe', lambda: nc.tensor.matmul(pt[:, 0:n], lhsT=wt_[:, d_, j, :], rhs=Tb[:, t0:t0 + n], start=True, stop=True),
                                  reads=[wk, kTb], writes=['ps%d' % pi])
                            fw.op('act', lambda: nc.scalar.activation(out=dst[:, t0:t0 + n], in_=pt[:, 0:n], func=AF.Sigmoid, bias=bt_[:, j, d_:d_ + 1]),
                                  reads=['ps%d' % pi, bk], writes=[dk])
                    fw.op('act', lambda: nc.scalar.activation(out=A_[:], in_=Rg[:], func=AF.Exp, scale=negc[:, j, d_:d_ + 1]), reads=[kR, 'negc'], writes=[kA])
                    fw.op('act', lambda: nc.scalar.activation(out=H_[:], in_=Rg[:], func=AF.Exp, scale=negc2[:, j, d_:d_ + 1]), reads=[kR, 'negc2'], writes=[kH])
                    fw.op('act', lambda: nc.scalar.activation(out=H_[:], in_=H_[:], func=AF.Sqrt, scale=-1.0, bias=1.0), reads=[kH], writes=[kH])
                    fw.op('pool', lambda: nc.gpsimd.tensor_tensor(out=Ig[:], in0=Ig[:], in1=T_[:], op=ALU.mult), reads=[kI, kT], writes=[kI])
                    fw.op('pool', lambda: nc.gpsimd.tensor_tensor(out=Ig[:], in0=Ig[:], in1=H_[:], op=ALU.mult), reads=[kI, kH], writes=[kI])
                    rs = sm[:, 8 + d_:9 + d_]
                    fw.op('dve', lambda: nc.vector.reduce_sum(out=rs, in_=Rg[:, 0:TOWN], axis=AX.X), reads=[kR], writes=['sm_rs%d' % d_])
                    fw.op('act', lambda: nc.scalar.activation(out=abt[:, j, d_, 0:1], in_=rs, func=AF.Exp, scale=negc[:, j, d_:d_ + 1]),
                          reads=['sm_rs%d' % d_, 'negc'], writes=['abt'])
                    if d_ == 0:
                        a_c, u_c, h_c = A_[:, TOWN:NOWN], Ig[:, TOWN:NOWN], H_[:, TOWN:NOWN]
                        a_l, u_l, h_l = A_[:, 0:TOWN], Ig[:, 0:TOWN], H_[:, 0:TOWN]
                        hc_fin = H_[:, NOWN - 1:NOWN]
                        hl_fin = H_[:, TOWN - 1:TOWN]
                    else:
                        a_c, u_c, h_c = A_[:, TOWN:NOWN][:, ::-1], Ig[:, TOWN:NOWN][:, ::-1], H_[:, TOWN:NOWN][:, ::-1]
                        a_l, u_l, h_l = A_[:, 0:TOWN][:, ::-1], Ig[:, 0:TOWN][:, ::-1], H_[:, 0:TOWN][:, ::-1]
                        hc_fin = H_[:, TOWN:TOWN + 1]
                        hl_fin = H_[:, 0:1]
                    fw.op('dve', lambda: nc.vector.tensor_tensor_scan(out=h_c, data0=a_c, data1=u_c, initial=0.0, op0=ALU.mult, op1=ALU.add),
                          reads=[kA, kI], writes=[kH])
                    hin = sm[:, d_ * 4:d_ * 4 + 1]
                    fw.op('dve', lambda: nc.vector.tensor_copy(out=hin, in_=hc_fin), reads=[kH], writes=['hin%d' % d_])
                    for k3 in range(3):
                        fw.op('dve', lambda: nc.vector.scalar_tensor_tensor(out=hin, in0=hin, scalar=cart[:, j, d_, k3, 0:1], in1=cart[:, j, d_, k3, 1:2],
                                                                            op0=ALU.mult, op1=ALU.add),
                              reads=['hin%d' % d_, 'cart'], writes=['hin%d' % d_])
                    fw.op('dve', lambda: nc.vector.tensor_tensor_scan(out=h_l, data0=a_l, data1=u_l, initial=hin, op0=ALU.mult, op1=ALU.add),
                          reads=[kA, kI, 'hin%d' % d_], writes=[kH])
                    tmp = sm[:, d_ * 4 + 1:d_ * 4 + 2]
                    fw.op('dve', lambda: nc.vector.tensor_tensor(out=tmp, in0=abt[:, j, d_, 0:1], in1=hin, op=ALU.mult), reads=['abt', 'hin%d' % d_], writes=['tmp%d' % d_])
                    fw.op('dve', lambda: nc.vector.tensor_tensor(out=abt[:, j, d_, 1:2], in0=hl_fin, in1=tmp, op=ALU.subtract), reads=[kH, 'tmp%d' % d_], writes=['abt'])
                fw.op('pool', lambda: nc.gpsimd.tensor_tensor(out=HH[0][:], in0=HH[0][:], in1=HH[1][:], op=ALU.add), reads=['HH0', 'HH1'], writes=['HH0'])
                fw.op('pool', lambda: nc.gpsimd.tensor_tensor(out=rst[:], in0=HH[0][:], in1=GZ[:], op=ALU.mult), reads=['HH0', 'GZ'], writes=['rst'])
                load('sp', R_d[:, j, :], rst[:], 'R_d', ['rst'])
          fw.barrier()
          load('sp', ab_out, abt[:], 'ab_out', ['abt'])
        outs = ['ab_out']
        if prepass:
            sHT.close()
            fw.barrier()
            return
        if "att" in phases:
          with contextlib.ExitStack() as s4:
            KT = sb("KT", [64, 2, TEXT], BF16, s4)
            Vtok = sb("Vtok", [128, 20, 128], BF16, s4)
            COS = sb("COS", [64, TLAT], F32, s4)
            SIN = sb("SIN", [64, TLAT], F32, s4)
            MSK = sb("MSK", [128, 2, 4, 128], F32, s4)
            ESX = sb("ESX", [64, 8, 128], F32, s4)
            ONES = sb("ONES", [128, 64], BF16, s4)
            BVA = sb("BVA", [128, 128], F32, s4)
            bqk = sb("bqk", [64, 20], F32, s4)
            wk_t = sb("wk_t", [128, 8, 256], BF16, s4)
            wva_t = sb("wva_t", [128, 8, 128], BF16, s4)
            wq_t = sb("wq_t", [128, 8, 1024], BF16, s4)
            load('sp', COS[:], rope_cos, 'COS')
            load('sp', SIN[:], rope_sin, 'SIN')
            load('sp', MSK[:], masks, 'MSK')
            load('sp', ESX[:], sinkx, 'ESX')
            load('sp', BVA[:], b_va, 'BVA')
            load('sp', bqk[:], b_qk, 'bqk')
            load('pool', ONES[:], ones_in, 'ONES')
            load('pool', wk_t[:], w_in[:, :, C_K:C_K + 256], 'wk_t')
            load('pool', wva_t[:], w_in[:, :, C_VA:C_VA + 128], 'wva_t')
            load('pool', wq_t[:], w_in[:, :, C_Q:C_Q + 1024], 'wq_t')
            fw.op('act', lambda: nc.scalar.activation(out=ESX[:], in_=ESX[:], func=AF.Exp), reads=['ESX'], writes=['ESX'])
            QF = [sb("QF%d" % i, [64, 512], F32, s4) for i in range(2)]
            QS = [sb("QS%d" % i, [64, 512], F32, s4) for i in range(2)]
            rot = [0]

            def roped(wt, wk, c_main, c_sw, bcol_main, bcol_sw, e0, n, dst, dkey, use_rope):
                i = rot[0] % 2
                rot[0] += 1
                qf, qs = QF[i], QS[i]

                def ev1(pt, pk):
                    fw.op('act', lambda: nc.scalar.activation(out=qf[:, 0:n], in_=pt[0:64, 0:n], func=AF.Identity, bias=bqk[:, bcol_main:bcol_main + 1]),
                          reads=[pk, 'bqk'], writes=['QF%d' % i])
                inproj2(lambda kc: wt[:, kc, c_main:c_main + 64], 64, e0, n, wk, ev1)
                if not use_rope:
                    fw.op('dve', lambda: nc.vector.tensor_copy(out=dst, in_=qf[:, 0:n]), reads=['QF%d' % i], writes=[dkey])
                    return

                def ev2(pt, pk):
                    fw.op('act', lambda: nc.scalar.activation(out=qs[:, 0:n], in_=pt[0:64, 0:n], func=AF.Identity, bias=bqk[:, bcol_sw:bcol_sw + 1]),
                          reads=[pk, 'bqk'], writes=['QS%d' % i])
                inproj2(lambda kc: wt[:, kc, c_sw:c_sw + 64], 64, e0, n, wk, ev2)
                fw.op('dve', lambda: nc.vector.tensor_tensor(out=qf[:, 0:n], in0=qf[:, 0:n], in1=COS[:, e0:e0 + n], op=ALU.mult), reads=['QF%d' % i, 'COS'], writes=['QF%d' % i])
                fw.op('pool', lambda: nc.gpsimd.tensor_tensor(out=qs[:, 0:n], in0=qs[:, 0:n], in1=SIN[:, e0:e0 + n], op=ALU.mult), reads=['QS%d' % i, 'SIN'], writes=['QS%d' % i])
                fw.op('dve', lambda: nc.vector.tensor_tensor(out=dst, in0=qf[:, 0:n], in1=qs[:, 0:n], op=ALU.add), reads=['QF%d' % i, 'QS%d' % i], writes=[dkey])

            for kv in range(2):
                for ti in range(5):
                    if ti < 4:
                        roped(wk_t, 'wk_t', kv * 64, 128 + kv * 64, 16 + kv, 18 + kv, ti * 512, 512, KT[:, kv, ti * 512:(ti + 1) * 512], 'KT', True)
                    else:
                        roped(wk_t, 'wk_t', kv * 64, 128 + kv * 64, 16 + kv, 18 + kv, 2048, 256, KT[:, kv, 2048:2304], 'KT', True)
                        roped(wk_t, 'wk_t', kv * 64, 128 + kv * 64, 16 + kv, 18 + kv, TLAT, 256, KT[:, kv, TLAT:TEXT], 'KT', False)
            for tt in range(20):
                pi = next_ps()
                pt = PS[pi]
                for kc in range(8):
                    fw.op('pe', lambda: nc.tensor.matmul(pt[:, 0:128], lhsT=hT[:, kc, tt * 128:(tt + 1) * 128], rhs=wva_t[:, kc, :], start=(kc == 0), stop=(kc == 7)),
                          reads=['wva_t', 'hT%d' % kc], writes=['ps%d' % pi])
                fw.op('dve', lambda: nc.vector.tensor_tensor(out=Vtok[:, tt, :], in0=pt[:, 0:128], in1=BVA[:], op=ALU.add), reads=['ps%d' % pi, 'BVA'], writes=['Vtok'])
            QT = sb("QT", [64, 8, 512], BF16, s4)
            PTS = [sb("PT%d" % i, [128, 4, 128], BF16, s4) for i in range(3)]
            DEN = sb("DEN", [64, 4, 128], F32, s4)
            BTS = [sb("BTS%d" % i, [64, 8, 128], BF16, s4) for i in range(2)]
            pti = [0]
            nqb = 0
            for ti in range(5):
                lat = ti < 4
                n = 512 if lat else 256
                e0 = HALO + ti * 512 if lat else TLAT
                for h in range(8):
                    roped(wq_t, 'wq_t', h * 64, 512 + h * 64, h, 8 + h, e0, n, QT[:, h, 0:n], 'QT', lat)
                for qb in range(n // 128):
                    q0 = qb * 128
                    if lat:
                        bi = ti * 4 + qb
                        ktiles = [(bi, 'p'), (bi + 1, 'o'), (bi + 2, 'n'), (18, 'c'), (19, 'c')]
                        own0 = bi * 128
                    else:
                        bi = -1
                        ktiles = [(18, 'c'), (19, 'c')]
                        own0 = TOWN + qb * 128
                    bts = BTS[nqb % 2]
                    bk = 'BTS%d' % (nqb % 2)
                    nqb += 1
                    for kv in range(2):
                        po_i = next_ps(); pd_i = next_ps()
                        po, pd = PS[po_i], PS[pd_i]
                        for ki, (tt, kind) in enumerate(ktiles):
                            psi_ = next_ps()
                            pss = PS[psi_]
                            fw.op('pe', lambda: nc.tensor.matmul(pss[:, 0:512], lhsT=KT[:, kv, tt * 128:(tt + 1) * 128],
                                                                  rhs=QT[:, 4 * kv:4 * kv + 4, q0:q0 + 128], start=True, stop=True),
                                  reads=['KT', 'QT'], writes=['ps%d' % psi_])
                            pi3 = pti[0] % 3
                            pti[0] += 1
                            P_ = PTS[pi3]
                            pk3 = 'PT%d' % pi3
                            fw.op('act', lambda: nc.scalar.activation(out=P_[:].rearrange("p g q -> p (g q)"), in_=pss[:, 0:512], func=AF.Exp, scale=0.125),
                                  reads=['ps%d' % psi_], writes=[pk3])
                            if kind in ('p', 'n'):
                                mi = 0 if kind == 'p' else 1
                                if (kind == 'p' and bi == 0) or (kind == 'n' and bi == 15):
                                    fw.op('dve', lambda: nc.vector.scalar_tensor_tensor(out=P_[:], in0=P_[:], scalar=edget[:, mi:mi + 1], in1=MSK[:, mi, :, :], op0=ALU.mult, op1=ALU.mult),
                                          reads=[pk3, 'MSK', 'edget'], writes=[pk3])
                                else:
                                    fw.op('dve', lambda: nc.vector.tensor_tensor(out=P_[:], in0=P_[:], in1=MSK[:, mi, :, :], op=ALU.mult), reads=[pk3, 'MSK'], writes=[pk3])
                            last = ki == len(ktiles) - 1
                            fw.op('pe', lambda: nc.tensor.matmul(po[0:64, 0:512], lhsT=Vtok[:, tt, kv * 64:(kv + 1) * 64], rhs=P_[:].rearrange("p g q -> p (g q)"), start=(ki == 0), stop=last),
                                  reads=['Vtok', pk3], writes=['ps%d' % po_i])
                            fw.op('pe', lambda: nc.tensor.matmul(pd[0:64, 0:512], lhsT=ONES[:], rhs=P_[:].rearrange("p g q -> p (g q)"), start=(ki == 0), stop=last),
                                  reads=['ONES', pk3], writes=['ps%d' % pd_i])
                        fw.op('dve', lambda: nc.vector.tensor_tensor(out=DEN[:], in0=pd[0:64, 0:512].rearrange("p (g q) -> p g q", g=4), in1=ESX[:, 4 * kv:4 * kv + 4, :], op=ALU.add),
                              reads=['ps%d' % pd_i, 'ESX'], writes=['DEN'])
                        fw.op('dve', lambda: nc.vector.reciprocal(out=DEN[:], in_=DEN[:]), reads=['DEN'], writes=['DEN'])
                        fw.op('dve', lambda: nc.vector.tensor_tensor(out=bts[:, 4 * kv:4 * kv + 4, :], in0=po[0:64, 0:512].rearrange("p (g q) -> p g q", g=4), in1=DEN[:], op=ALU.mult),
                              reads=['ps%d' % po_i, 'DEN'], writes=[bk])
                    load('sp', B_d[:, :, own0:own0 + 128], bts[:], 'B_d', [bk])

            if dbg:
                ktf = sb("ktf", [64, 2, TEXT], F32, s4)
                vtf = sb("vtf", [128, 20, 128], F32, s4)
                fw.op('dve', lambda: nc.vector.tensor_copy(out=ktf[:], in_=KT[:]), reads=['KT'], writes=['ktf'])
                fw.op('dve', lambda: nc.vector.tensor_copy(out=vtf[:], in_=Vtok[:]), reads=['Vtok'], writes=['vtf'])
                load('sp', dbg_outs['kt_dbg'], ktf[:], 'kt_dbg', ['ktf'])
                load('sp', dbg_outs['vt_dbg'], vtf[:], 'vt_dbg', ['vtf'])
                outs.extend(['kt_dbg', 'vt_dbg'])
        fw.barrier()
        if "sgu" in phases:
          with contextlib.ExitStack() as s5:
            wu_t = sb("wu_t", [128, 8, 512], BF16, s5)
            wv_t = sb("wv_t", [128, 8, 512], BF16, s5)
            BV = sb("BV", [128, 512], F32, s5)
            LNG = sb("LNG", [128, 512], F32, s5)
            LNB = sb("LNB", [128, 512], F32, s5)
            WST = sb("WST", [128, 4, 128], BF16, s5)
            BSP = sb("BSP", [128, 4, 128], F32, s5)
            load('pool', wu_t[:], w_in[:, :, C_U:C_U + 512], 'wu_t')
            load('pool', wv_t[:], w_in[:, :, C_V:C_V + 512], 'wv_t')
            load('pool', WST[:], w_spT, 'WST')
            load('sp', BV[:], b_v, 'BV')
            load('sp', LNG[:], sgu_g, 'LNG')
            load('sp', LNB[:], sgu_b, 'LNB')
            load('sp', BSP[:], b_sp, 'BSP')
            GU = sb("GU", [128, 4, 512], F32, s5)
            V1 = sb("V1", [128, 512], F32, s5)
            GV = sb("GV", [128, 512], F32, s5)
            VNB = sb("VNB", [128, 512], BF16, s5)
            STT = sb("STT", [128, 6], F32, s5)
            MV = sb("MV", [128, 2], F32, s5)
            RSTD = sb("RSTD", [128, 1], F32, s5)
            MX = sb("MX", [128, 4, 128], F32, s5)
            ATS = [sb("ATS%d" % i, [128, 4, 128], BF16, s5) for i in range(2)]
            nch = 0
            for ti in range(5):
                lat = ti < 4
                n = 512 if lat else 256
                e0 = HALO + ti * 512 if lat else TLAT
                for g in range(4):
                    def evu(pt, pk, g=g):
                        fw.op('act', lambda: nc.scalar.activation(out=GU[:, g, 0:n], in_=pt[:, 0:n], func=AF.Gelu, bias=bint[:, C_U // 128 + g:C_U // 128 + g + 1]),
                              reads=[pk, 'bint'], writes=['GU'])
                    inproj2(lambda kc: wu_t[:, kc, g * 128:(g + 1) * 128], 128, e0, n, 'wu_t', evu)
                for sub in range(n // 128):
                    t0 = e0 + sub * 128
                    own0 = (ti * 512 if lat else TOWN) + sub * 128
                    pi = next_ps()
                    pv = PS[pi]
                    for kc in range(8):
                        fw.op('pe', lambda: nc.tensor.matmul(pv[:, 0:512], lhsT=hT[:, kc, t0:t0 + 128], rhs=wv_t[:, kc, :], start=(kc == 0), stop=(kc == 7)),
                              reads=['wv_t', 'hT%d' % kc], writes=['ps%d' % pi])
                    fw.op('dve', lambda: nc.vector.tensor_tensor(out=V1[:], in0=pv[:, 0:512], in1=BV[:], op=ALU.add), reads=['ps%d' % pi, 'BV'], writes=['V1'])
                    fw.op('act', lambda: nc.scalar.activation(out=GV[:], in_=V1[:], func=AF.Gelu), reads=['V1'], writes=['GV'])
                    fw.op('dve', lambda: nc.vector.bn_stats(out=STT[:], in_=GV[:]), reads=['GV'], writes=['STT'])
                    fw.op('dve', lambda: nc.vector.bn_aggr(out=MV[:], in_=STT[:]), reads=['STT'], writes=['MV'])
                    fw.op('act', lambda: nc.scalar.activation(out=RSTD[:], in_=MV[:, 1:2], func=AF.Sqrt, bias=LN_EPS), reads=['MV'], writes=['RSTD'])
                    fw.op('dve', lambda: nc.vector.reciprocal(out=RSTD[:], in_=RSTD[:]), reads=['RSTD'], writes=['RSTD'])
                    fw.op('dve', lambda: nc.vector.tensor_scalar(out=GV[:], in0=GV[:], scalar1=MV[:, 0:1], scalar2=RSTD[:, 0:1], op0=ALU.subtract, op1=ALU.mult),
                          reads=['GV', 'MV', 'RSTD'], writes=['GV'])
                    fw.op('pool', lambda: nc.gpsimd.tensor_tensor(out=GV[:], in0=GV[:], in1=LNG[:], op=ALU.mult), reads=['GV', 'LNG'], writes=['GV'])
                    fw.op('pool', lambda: nc.gpsimd.tensor_tensor(out=VNB[:], in0=GV[:], in1=LNB[:], op=ALU.add), reads=['GV', 'LNB'], writes=['VNB'])
                    pmi_ = next_ps()
                    pmx = PS[pmi_]
                    for g in range(4):
                        fw.op('pe', lambda: nc.tensor.matmul(pmx[:, g * 128:(g + 1) * 128], lhsT=VNB[:, g * 128:(g + 1) * 128], rhs=WST[:, g, :], start=True, stop=True),
                              reads=['VNB', 'WST'], writes=['ps%d' % pmi_])
                    fw.op('dve', lambda: nc.vector.tensor_tensor(out=MX[:], in0=pmx[:, 0:512].rearrange("p (g q) -> p g q", g=4), in1=BSP[:], op=ALU.add),
                          reads=['ps%d' % pmi_, 'BSP'], writes=['MX'])
                    ats = ATS[nch % 2]
                    ak = 'ATS%d' % (nch % 2)
                    nch += 1
                    fw.op('pool', lambda: nc.gpsimd.tensor_tensor(out=ats[:], in0=MX[:], in1=GU[:, :, sub * 128:(sub + 1) * 128], op=ALU.mult), reads=['MX', 'GU'], writes=[ak])
                    load('sp', A_d[:, :, own0:own0 + 128], ats[:], 'A_d', [ak])
        fw.barrier()
        if dbg:
            with contextlib.ExitStack() as sd:
                rb = sb("dbgb", [128, NOWN], BF16, sd)
                rf = sb("dbgf", [128, NOWN], F32, sd)
                for (nm, src_d, npart, nchk) in (("r_dbg", R_d, 128, 8), ("a_dbg", A_d, 128, 4), ("b_dbg", B_d, 64, 8)):
                    o_ = dbg_outs[nm]
                    for j in range(nchk):
                        load('sp', rb[0:npart, :], src_d[:, j, :], 'dbgb', [nm[0].upper() + '_d'])
                        fw.op('dve', lambda: nc.vector.tensor_copy(out=rf[0:npart, :], in_=rb[0:npart, :]), reads=['dbgb'], writes=['dbgf'])
                        load('sp', o_[:, j, :], rf[0:npart, :], nm, ['dbgf'])
                    outs.append(nm)
        def layer_norm(X, xk, n, SQ, sqk, MEAN, mk, MSQ, msk, RSTDW, rk, ONF, ok, gcol, bcol, pvk):
            fw.op('act', lambda: nc.scalar.activation(out=SQ[:, :, 0:n], in_=X[:, :, 0:n], func=AF.Square), reads=[xk], writes=[sqk])
            p1 = next_ps(); p2 = next_ps()
            for c in range(8):
                fw.op('pe', lambda: nc.tensor.matmul(PS[p1][:, 0:n], lhsT=ONF[:], rhs=X[:, c, 0:n], start=(c == 0), stop=(c == 7)), reads=[ok, xk], writes=['ps%d' % p1])
            for c in range(8):
                fw.op('pe', lambda: nc.tensor.matmul(PS[p2][:, 0:n], lhsT=ONF[:], rhs=SQ[:, c, 0:n], start=(c == 0), stop=(c == 7)), reads=[ok, sqk], writes=['ps%d' % p2])
            fw.op('act', lambda: nc.scalar.activation(out=MEAN[:, 0:n], in_=PS[p1][:, 0:n], func=AF.Copy, scale=1.0 / D), reads=['ps%d' % p1], writes=[mk])
            fw.op('dve', lambda: nc.vector.tensor_tensor(out=MSQ[:, 0:n], in0=MEAN[:, 0:n], in1=MEAN[:, 0:n], op=ALU.mult), reads=[mk], writes=[msk])
            fw.op('dve', lambda: nc.vector.scalar_tensor_tensor(out=RSTDW[:, 0:n], in0=PS[p2][:, 0:n], scalar=1.0 / D, in1=MSQ[:, 0:n], op0=ALU.mult, op1=ALU.subtract),
                  reads=['ps%d' % p2, msk], writes=[rk])
            fw.op('act', lambda: nc.scalar.activation(out=RSTDW[:, 0:n], in_=RSTDW[:, 0:n], func=AF.Sqrt, bias=LN_EPS), reads=[rk], writes=[rk])
            fw.op('dve', lambda: nc.vector.reciprocal(out=RSTDW[:, 0:n], in_=RSTDW[:, 0:n]), reads=[rk], writes=[rk])
            for c in range(8):
                fw.op('dve', lambda: nc.vector.tensor_tensor(out=X[:, c, 0:n], in0=X[:, c, 0:n], in1=MEAN[:, 0:n], op=ALU.subtract), reads=[xk, mk], writes=[xk])
                fw.op('pool', lambda: nc.gpsimd.tensor_tensor(out=X[:, c, 0:n], in0=X[:, c, 0:n], in1=RSTDW[:, 0:n], op=ALU.mult), reads=[xk, rk], writes=[xk])
                fw.op('act', lambda: nc.scalar.activation(out=X[:, c, 0:n], in_=X[:, c, 0:n], func=AF.Identity, scale=gcol(c), bias=bcol(c)), reads=[xk, pvk], writes=[xk])

        X1_d = sh.scratch("X1_d", [128, 8, NOWN], F32)
        H2_d = sh.scratch("H2_d", [NOWN, D], BF16)
        LG = sb("LG", [128, 18, 36], F32)
        OT = [(HALO + i * 512, i * 512, 512) for i in range(4)] + [(TLAT, TOWN, 256)]
        if "merge" in phases:
          with contextlib.ExitStack() as s6:
            ATl = sb("ATl", [128, 4, 1280], BF16, s6)
            BTl = sb("BTl", [64, 8, 1280], BF16, s6)
            RTl = sb("RTl", [128, 8, 1280], BF16, s6)
            WGs = [sb("WG%d" % i, [128, 8, 3, 128], BF16, s6) for i in range(2)]
            WAs = [sb("WA%d" % i, [128, 4, 128], BF16, s6) for i in range(2)]
            WBs = [sb("WB%d" % i, [64, 8, 128], BF16, s6) for i in range(2)]
            WCs = [sb("WC%d" % i, [128, 8, 128], BF16, s6) for i in range(2)]
            GS = [sb("GS%d" % i, [128, 512], F32, s6) for i in range(3)]
            TS = [sb("TS%d" % i, [128, 512], F32, s6) for i in range(3)]
            wi = 0
            for hf in range(2):
                tiles = OT[0:2] if hf == 0 else OT[2:5]
                h0 = tiles[0][1]
                hn = sum(t[2] for t in tiles)
                load('sp', ATl[:, :, 0:hn], A_d[:, :, h0:h0 + hn], 'ATl', ['A_d'])
                load('sp', BTl[:, :, 0:hn], B_d[:, :, h0:h0 + hn], 'BTl', ['B_d'])
                load('sp', RTl[:, :, 0:hn], R_d[:, :, h0:h0 + hn], 'RTl', ['R_d'])
                for oc in range(8):
                    w_ = wi % 2
                    wi += 1
                    WG, WA, WB, WC = WGs[w_], WAs[w_], WBs[w_], WCs[w_]
                    kg, ka, kb, kc_ = 'WG%d' % w_, 'WA%d' % w_, 'WB%d' % w_, 'WC%d' % w_
                    for br in range(3):
                        c0 = C_G + br * 1024 + oc * 128
                        load('pool', WG[:, :, br, :], w_in[:, :, c0:c0 + 128], kg)
                    load('pool', WA[:], w_pa[:, :, oc * 128:(oc + 1) * 128], ka)
                    load('pool', WB[:], w_pb[:, :, oc * 128:(oc + 1) * 128], kb)
                    load('pool', WC[:], w_pc[:, :, oc * 128:(oc + 1) * 128], kc_)
                    for (e0, o0, n) in tiles:
                        l0 = o0 - h0
                        for br in range(3):
                            def evg(pt, pk, br=br):
                                bcol = C_G // 128 + br * 8 + oc
                                fw.op('act', lambda: nc.scalar.activation(out=GS[br][:, 0:n], in_=pt[:, 0:n], func=AF.Sigmoid, bias=bint[:, bcol:bcol + 1]),
                                      reads=[pk, 'bint'], writes=['GS%d' % br])
                            inproj2(lambda kc: WG[:, kc, br, :], 128, e0, n, kg, evg)
                        for br, (Wt, wk, src, sk, nk, npart) in enumerate(((WA, ka, ATl, 'ATl', 4, 128), (WB, kb, BTl, 'BTl', 8, 64), (WC, kc_, RTl, 'RTl', 8, 128))):
                            pi = next_ps()
                            pt = PS[pi]
                            for kk in range(nk):
                                fw.op('pe', lambda: nc.tensor.matmul(pt[:, 0:n], lhsT=Wt[0:npart, kk, :], rhs=src[0:npart, kk, l0:l0 + n], start=(kk == 0), stop=(kk == nk - 1)),
                                      reads=[wk, sk], writes=['ps%d' % pi])
                            fw.op('dve', lambda: nc.vector.tensor_tensor(out=TS[br][:, 0:n], in0=pt[:, 0:n], in1=GS[br][:, 0:n], op=ALU.mult),
                                  reads=['ps%d' % pi, 'GS%d' % br], writes=['TS%d' % br])
                        fw.op('pool', lambda: nc.gpsimd.tensor_tensor(out=TS[0][:, 0:n], in0=TS[0][:, 0:n], in1=TS[1][:, 0:n], op=ALU.add), reads=['TS0', 'TS1'], writes=['TS0'])
                        fw.op('pool', lambda: nc.gpsimd.tensor_tensor(out=yT[:, oc, o0:o0 + n], in0=TS[0][:, 0:n], in1=TS[2][:, 0:n], op=ALU.add), reads=['TS0', 'TS2'], writes=['yT%d' % oc])
          fw.barrier()
          sHT.close()
          with contextlib.ExitStack() as s7:
            WO = sb("WO", [128, 8, D], BF16, s7)
            WGR = sb("WGR", [128, 8, 36], F32, s7)
            BGR = sb("BGR", [128, 36], F32, s7)
            IDN = sb("IDN", [128, 128], F32, s7)
            ONF = sb("ONF", [128, 128], F32, s7)
            pv = sb("pvec", [128, 5, 8], F32, s7)
            bom = sb("bom", [128, 2, 8], F32, s7)
            load('pool', WO[:], w_o, 'WO')
            load('sp', WGR[:], w_gr, 'WGR')
            load('sp', BGR[:], b_gr, 'BGR')
            load('sp', IDN[:], ident, 'IDN')
            load('sp', ONF[:], ones_f, 'ONF')
            load('sp', pv[:, 0, :], b_o, 'pvec')
            load('sp', pv[:, 1, :], ln1g, 'pvec')
            load('sp', pv[:, 2, :], ln1b, 'pvec')
            fw.op('dve', lambda: nc.vector.tensor_tensor(out=bom[:, 0, :], in0=pv[:, 0, :], in1=mlat[:, 16:24], op=ALU.mult), reads=['pvec', 'mlat'], writes=['bom'])
            fw.op('dve', lambda: nc.vector.tensor_tensor(out=bom[:, 1, :], in0=pv[:, 0, :], in1=mctx[:, 16:24], op=ALU.mult), reads=['pvec', 'mctx'], writes=['bom'])
            XT = sb("XT", [128, 8, 512], F32, s7)
            SQ = sb("SQ", [128, 8, 512], F32, s7)
            H2F = sb("H2F", [128, 8, 512], F32, s7)
            MEAN = sb("MEAN", [128, 512], F32, s7)
            MSQ = sb("MSQ", [128, 512], F32, s7)
            RSTDW = sb("RSTDW", [128, 512], F32, s7)
            nsub = 0
            H2B = [sb("H2B%d" % i, [128, D], BF16, s7) for i in range(2)]
            for ti, (e0, o0, n) in enumerate(OT):
                lat = ti < 4
                mv_ = mlat if lat else mctx
                mk_ = 'mlat' if lat else 'mctx'
                li = 0 if lat else 1
                if lat:
                    load('sp', XT[:, :, 0:n], xT[:, :, e0:e0 + n], 'XT')
                else:
                    load('sp', XT[:, :, 0:n], cT[:, :, :], 'XT')
                for oc in range(8):
                    pi = next_ps()
                    pt = PS[pi]
                    for kc in range(8):
                        fw.op('pe', lambda: nc.tensor.matmul(pt[:, 0:n], lhsT=WO[:, kc, oc * 128:(oc + 1) * 128], rhs=yT[:, kc, o0:o0 + n], start=(kc == 0), stop=(kc == 7)),
                              reads=['WO', 'yT%d' % kc], writes=['ps%d' % pi])
                    fw.op('act', lambda: nc.scalar.activation(out=SQ[:, oc, 0:n], in_=pt[:, 0:n], func=AF.Identity, scale=mv_[:, 16 + oc:17 + oc], bias=bom[:, li, oc:oc + 1]),
                          reads=['ps%d' % pi, mk_, 'bom'], writes=['SQ'])
                    fw.op('dve', lambda: nc.vector.scalar_tensor_tensor(out=XT[:, oc, 0:n], in0=XT[:, oc, 0:n], scalar=ALPHA, in1=SQ[:, oc, 0:n], op0=ALU.mult, op1=ALU.add),
                          reads=['XT', 'SQ'], writes=['XT'])

                layer_norm(XT, 'XT', n, SQ, 'SQ', MEAN, 'MEAN', MSQ, 'MSQ', RSTDW, 'RSTDW', ONF, 'ONF', lambda c: pv[:, 1, c:c + 1], lambda c: pv[:, 2, c:c + 1], 'pvec')
                load('sp', X1_d[:, :, o0:o0 + n], XT[:, :, 0:n], 'X1_d', ['XT'])
                m4 = m4p[:, li, :]
                for c in range(8):
                    fw.op('act', lambda: nc.scalar.activation(out=H2F[:, c, 0:n], in_=XT[:, c, 0:n], func=AF.Identity, scale=m4p[:, li, c:c + 1], bias=mv_[:, 24 + c:25 + c]),
                          reads=['XT', 'm4p', mk_], writes=['H2F'])
                for sub in range(n // 128):
                    t0 = sub * 128
                    chunk = (o0 + t0) // 128
                    pi = next_ps()
                    pt = PS[pi]
                    for kc in range(8):
                        fw.op('pe', lambda: nc.tensor.matmul(pt[:, 0:36], lhsT=H2F[:, kc, t0:t0 + 128], rhs=WGR[:, kc, :], start=(kc == 0), stop=(kc == 7)),
                              reads=['H2F', 'WGR'], writes=['ps%d' % pi])
                    fw.op('dve', lambda: nc.vector.tensor_tensor(out=LG[:, chunk, :], in0=pt[:, 0:36], in1=BGR[:], op=ALU.add), reads=['ps%d' % pi, 'BGR'], writes=['LG'])
                    hb = H2B[nsub % 2]
                    hk = 'H2B%d' % (nsub % 2)
                    nsub += 1
                    for half in range(2):
                        pi = next_ps()
                        pt = PS[pi]
                        for cc in range(4):
                            c = half * 4 + cc
                            fw.op('pe', lambda: nc.tensor.transpose(out=pt[:, cc * 128:(cc + 1) * 128], in_=H2F[:, c, t0:t0 + 128], identity=IDN[:]),
                                  reads=['H2F', 'IDN'], writes=['ps%d' % pi])
                        if half == 0:
                            fw.op('act', lambda: nc.scalar.copy(out=hb[:, 0:512], in_=pt[:, 0:512]), reads=['ps%d' % pi], writes=[hk])
                        else:
                            fw.op('dve', lambda: nc.vector.tensor_copy(out=hb[:, 512:1024], in_=pt[:, 0:512]), reads=['ps%d' % pi], writes=[hk])
                    load('sp', H2_d[o0 + t0:o0 + t0 + 128, :], hb[:], 'H2_d', [hk])
          fw.barrier()
          sY.close()
          if dbg:
              load('sp', dbg_outs['x1_dbg'], X1_d, 'x1_dbg', ['X1_d'])
              lgf = LG
              load('sp', dbg_outs['lg_dbg'], LG[:], 'lg_dbg', ['LG'])
              outs.extend(['x1_dbg', 'lg_dbg'])
        T = 18
        if "moe" in phases:
          XB_d = sh.scratch("XB_d", [NBLK * 128, D], BF16)
          OUT_d = sh.scratch("OUT_d", [NBLK * 128, D], F32)
          SL1 = sb("SL1", [128, T], I32)
          SL2 = sb("SL2", [128, T], I32)
          G1 = sb("G1", [128, T], F32)
          G2 = sb("G2", [128, T], F32)
          WIDX = sb("WIDX", [128, NBLK], I32)
          with contextlib.ExitStack() as s8:
            def t8(name, shape, dt=F32):
                return sb(name, shape, dt, s8)
            TRI = t8("TRI", [128, 128]); ONF2 = t8("ONF2", [128, 128]); BVAL = t8("BVAL", [128, NBLK]); PIDX = t8("PIDX", [128, 1])
            load('sp', TRI[:], tri_in, 'TRI'); load('sp', ONF2[:], ones_f, 'ONF2'); load('sp', BVAL[:], bval_in, 'BVAL'); load('sp', PIDX[:], pidx_in, 'PIDX')
            gmax = t8("gmax", [128, T]); goh = t8("goh", [128, T, 4]); ge = t8("ge", [128, T, 4]); gw = t8("gw", [128, T])
            pen = t8("pen", [128, T, 4]); em = t8("em", [128, T, 32]); em2 = t8("em2", [128, T, 32])
            m1 = t8("m1", [128, T]); m2 = t8("m2", [128, T]); oh1 = t8("oh1", [128, T, 32]); oh2 = t8("oh2", [128, T, 32])
            dm = t8("dm", [128, T]); p1 = t8("p1", [128, T])
            A_ = t8("Aas", [128, T, 32]); CUMA = t8("CUMA", [128, T + 1, 32]); SLT = t8("SLT", [128, T, 32]); TMP = t8("TMP", [128, T, 32])
            cnt = t8("cnt", [128, 32]); cnti = t8("cnti", [128, 32], I32); padd = t8("padd", [128, 32]); pend = t8("pend", [128, 32]); pst = t8("pst", [128, 32])
            one32 = t8("one32", [128, 32]); CMP = t8("CMP", [128, NBLK, 32]); be = t8("be", [128, NBLK]); slf = t8("slf", [128, T])

            def V(fn, reads, writes):
                fw.op('dve', fn, reads=reads, writes=writes)
            gl = LG[:, :, 0:4]
            el = LG[:, :, 4:36]
            V(lambda: nc.vector.tensor_reduce(out=gmax[:], in_=gl, op=ALU.max, axis=AX.X), ['LG'], ['gmax'])
            gmb = gmax[:].unsqueeze(2).to_broadcast([128, T, 4])
            V(lambda: nc.vector.tensor_tensor(out=goh[:], in0=gl, in1=gmb, op=ALU.is_equal), ['LG', 'gmax'], ['goh'])
            V(lambda: nc.vector.tensor_tensor(out=ge[:], in0=gl, in1=gmb, op=ALU.subtract), ['LG', 'gmax'], ['ge'])
            fw.op('act', lambda: nc.scalar.activation(out=ge[:], in_=ge[:], func=AF.Exp), reads=['ge'], writes=['ge'])
            V(lambda: nc.vector.tensor_reduce(out=gw[:], in_=ge[:], op=ALU.add, axis=AX.X), ['ge'], ['gw'])
            V(lambda: nc.vector.reciprocal(out=gw[:], in_=gw[:]), ['gw'], ['gw'])
            V(lambda: nc.vector.tensor_scalar(out=pen[:], in0=goh[:], scalar1=1e30, scalar2=-1e30, op0=ALU.mult, op1=ALU.add), ['goh'], ['pen'])
            V(lambda: nc.vector.tensor_tensor(out=em[:].rearrange("p t (g e) -> p t g e", g=4), in0=el.rearrange("p t (g e) -> p t g e", g=4),
                                              in1=pen[:].unsqueeze(3).to_broadcast([128, T, 4, 8]), op=ALU.add), ['LG', 'pen'], ['em'])
            V(lambda: nc.vector.tensor_reduce(out=m1[:], in_=em[:], op=ALU.max, axis=AX.X), ['em'], ['m1'])
            V(lambda: nc.vector.tensor_tensor(out=oh1[:], in0=em[:], in1=m1[:].unsqueeze(2).to_broadcast([128, T, 32]), op=ALU.is_equal), ['em', 'm1'], ['oh1'])
            V(lambda: nc.vector.scalar_tensor_tensor(out=em2[:], in0=oh1[:], scalar=-1e30, in1=em[:], op0=ALU.mult, op1=ALU.add), ['oh1', 'em'], ['em2'])
            V(lambda: nc.vector.tensor_reduce(out=m2[:], in_=em2[:], op=ALU.max, axis=AX.X), ['em2'], ['m2'])
            V(lambda: nc.vector.tensor_tensor(out=oh2[:], in0=em2[:], in1=m2[:].unsqueeze(2).to_broadcast([128, T, 32]), op=ALU.is_equal), ['em2', 'm2'], ['oh2'])
            V(lambda: nc.vector.tensor_tensor(out=dm[:], in0=m2[:], in1=m1[:], op=ALU.subtract), ['m1', 'm2'], ['dm'])
            fw.op('act', lambda: nc.scalar.activation(out=dm[:], in_=dm[:], func=AF.Exp), reads=['dm'], writes=['dm'])
            V(lambda: nc.vector.tensor_scalar(out=p1[:], in0=dm[:], scalar1=1.0, scalar2=None, op0=ALU.add), ['dm'], ['p1'])
            V(lambda: nc.vector.reciprocal(out=p1[:], in_=p1[:]), ['p1'], ['p1'])
            V(lambda: nc.vector.tensor_tensor(out=G1[:], in0=gw[:], in1=p1[:], op=ALU.mult), ['gw', 'p1'], ['G1'])
            V(lambda: nc.vector.tensor_tensor(out=dm[:], in0=dm[:], in1=p1[:], op=ALU.mult), ['dm', 'p1'], ['dm'])
            V(lambda: nc.vector.tensor_tensor(out=G2[:], in0=gw[:], in1=dm[:], op=ALU.mult), ['gw', 'dm'], ['G2'])
            V(lambda: nc.vector.tensor_tensor(out=A_[:], in0=oh1[:], in1=oh2[:], op=ALU.add), ['oh1', 'oh2'], ['A'])
            V(lambda: nc.vector.memset(CUMA[:, 0, :], 0.0), [], ['CUMA'])
            for t in range(T):
                V(lambda: nc.vector.tensor_tensor(out=CUMA[:, t + 1, :], in0=CUMA[:, t, :], in1=A_[:, t, :], op=ALU.add), ['CUMA', 'A'], ['CUMA'])
            pr = [next_ps(), next_ps(), next_ps()]
            for t in range(T):
                pb_ = PS[pr[0]] if t < 16 else PS[pr[1]]
                pk = 'ps%d' % (pr[0] if t < 16 else pr[1])
                tt_ = t % 16
                fw.op('pe', lambda: nc.tensor.matmul(pb_[:, tt_ * 32:(tt_ + 1) * 32], lhsT=TRI[:], rhs=A_[:, t, :], start=True, stop=False), reads=['TRI', 'A'], writes=[pk])
                fw.op('pe', lambda: nc.tensor.matmul(pb_[:, tt_ * 32:(tt_ + 1) * 32], lhsT=ONF2[:], rhs=CUMA[:, t, :], start=False, stop=True), reads=['ONF2', 'CUMA'], writes=[pk])
            fw.op('pe', lambda: nc.tensor.matmul(PS[pr[2]][:, 0:32], lhsT=ONF2[:], rhs=CUMA[:, T, :], start=True, stop=True), reads=['ONF2', 'CUMA'], writes=['ps%d' % pr[2]])
            V(lambda: nc.vector.tensor_copy(out=cnt[:], in_=PS[pr[2]][:, 0:32]), ['ps%d' % pr[2]], ['cnt'])
            CM2 = t8("CM2", [128, 32, 40])
            V(lambda: nc.vector.tensor_tensor(out=CM2[:], in0=cnt[:].unsqueeze(2).to_broadcast([128, 32, 40]), in1=BVAL[:, 0:40].unsqueeze(1).to_broadcast([128, 32, 40]), op=ALU.is_gt),
              ['cnt', 'BVAL'], ['CM2'])
            V(lambda: nc.vector.tensor_reduce(out=padd[:], in_=CM2[:], op=ALU.add, axis=AX.X), ['CM2'], ['padd'])
            V(lambda: nc.vector.tensor_scalar(out=padd[:], in0=padd[:], scalar1=128.0, scalar2=None, op0=ALU.mult), ['padd'], ['padd'])
            V(lambda: nc.vector.memset(one32[:], 1.0), [], ['one32'])
            V(lambda: nc.vector.tensor_tensor_scan(out=pend[:], data0=one32[:], data1=padd[:], initial=0.0, op0=ALU.mult, op1=ALU.add), ['one32', 'padd'], ['pend'])
            V(lambda: nc.vector.tensor_tensor(out=pst[:], in0=pend[:], in1=padd[:], op=ALU.subtract), ['pend', 'padd'], ['pst'])
            psb = pst[:].unsqueeze(1)
            V(lambda: nc.vector.tensor_tensor(out=SLT[:, 0:16, :], in0=PS[pr[0]][:, 0:512].rearrange("p (t e) -> p t e", e=32), in1=psb.to_broadcast([128, 16, 32]), op=ALU.add),
              ['ps%d' % pr[0], 'pst'], ['SLT'])
            V(lambda: nc.vector.tensor_tensor(out=SLT[:, 16:18, :], in0=PS[pr[1]][:, 0:64].rearrange("p (t e) -> p t e", e=32), in1=psb.to_broadcast([128, 2, 32]), op=ALU.add),
              ['ps%d' % pr[1], 'pst'], ['SLT'])
            for (oh, ok, SL) in ((oh1, 'oh1', SL1), (oh2, 'oh2', SL2)):
                V(lambda: nc.vector.tensor_tensor(out=TMP[:], in0=SLT[:], in1=oh[:], op=ALU.mult), ['SLT', ok], ['TMP'])
                V(lambda: nc.vector.tensor_reduce(out=slf[:], in_=TMP[:], op=ALU.add, axis=AX.X), ['TMP'], ['slf'])
                V(lambda: nc.vector.tensor_copy(out=SL[:], in_=slf[:]), ['slf'], ['SL'])
            V(lambda: nc.vector.tensor_tensor(out=CMP[:], in0=pend[:].unsqueeze(1).to_broadcast([128, NBLK, 32]), in1=BVAL[:].unsqueeze(2).to_broadcast([128, NBLK, 32]), op=ALU.is_le),
              ['pend', 'BVAL'], ['CMP'])
            V(lambda: nc.vector.tensor_reduce(out=be[:], in_=CMP[:], op=ALU.add, axis=AX.X), ['CMP'], ['be'])
            V(lambda: nc.vector.tensor_scalar(out=be[:], in0=be[:], scalar1=float(NEXP - 1), scalar2=128.0, op0=ALU.min, op1=ALU.mult), ['be'], ['be'])
            V(lambda: nc.vector.tensor_scalar(out=WIDX[:], in0=be[:], scalar1=PIDX[:, 0:1], scalar2=None, op0=ALU.add), ['be', 'PIDX'], ['WIDX'])
            if dbg:
                load('sp', dbg_outs['sl_dbg'][:, 0, :], SL1[:], 'sl_dbg', ['SL'])
                load('sp', dbg_outs['sl_dbg'][:, 1, :], SL2[:], 'sl_dbg', ['SL'])
                load('sp', dbg_outs['g_dbg'][:, 0, :], G1[:], 'g_dbg', ['G1'])
                load('sp', dbg_outs['g_dbg'][:, 1, :], G2[:], 'g_dbg', ['G2'])
                load('sp', dbg_outs['wi_dbg'], WIDX[:], 'wi_dbg', ['WIDX'])
                outs.extend(['sl_dbg', 'g_dbg', 'wi_dbg'])
          fw.barrier()
          if 'moe8' not in phases and dbg:
              sHT.close()
              fw.barrier()
              return
          with contextlib.ExitStack() as s8b:
            HBs = [sb("HBs%d" % i, [128, D], BF16, s8b) for i in range(2)]
            for t in range(T):
                hb_ = HBs[t % 2]
                hk_ = 'HBs%d' % (t % 2)
                load('sp', hb_[:], H2_d[t * 128:(t + 1) * 128, :], hk_, ['H2_d'])
                for SL in (SL1, SL2):
                    fw.dma('pool', lambda: nc.gpsimd.indirect_dma_start(out=XB_d[:, :], out_offset=bass.IndirectOffsetOnAxis(ap=SL[:, t:t + 1], axis=0),
                                                                        in_=hb_[:], in_offset=None), reads=[hk_, 'SL'], writes=['XB_d'])
          fw.barrier()
          with contextlib.ExitStack() as s9:
            PSB = s9.enter_context(nc.psum_tensor("PSB" + sfx, [128, 1024], BF16))
            IDB = sb("IDB", [128, 128], BF16, s9)
            load('pool', IDB[:], ident, 'IDB')
            W1B = [sb("W1B%d" % i, [128, 4096], BF16, s9) for i in range(2)]
            W3B = [sb("W3B%d" % i, [128, 4096], BF16, s9) for i in range(2)]
            W2B = [sb("W2B%d" % i, [128, 4096], BF16, s9) for i in range(2)]
            XBs = [sb("XBs%d" % i, [128, D], BF16, s9) for i in range(2)]
            XTs = sb("XTs", [128, 8, 128], BF16, s9)
            S1 = sb("S1", [128, 512], F32, s9)
            Gb = sb("Gb", [128, 512], BF16, s9)
            GTs = sb("GTs", [128, 4, 128], BF16, s9)
            OBs = [sb("OBs%d" % i, [128, D], F32, s9) for i in range(2)]
            for b in range(NBLK):
                i2 = b % 2
                w1, w3, w2, xb, ob = W1B[i2], W3B[i2], W2B[i2], XBs[i2], OBs[i2]
                k1, k3, k2, kx, ko = 'W1B%d' % i2, 'W3B%d' % i2, 'W2B%d' % i2, 'XBs%d' % i2, 'OBs%d' % i2
                for (wt, wk, srcn) in ((w1, k1, "w1"), (w3, k3, "w3"), (w2, k2, "w2")):
                    for a_, hsfx in enumerate(("a", "b")):
                        fw.dma('pool', lambda: nc.gpsimd.indirect_dma_start(out=wt[:, a_ * 2048:(a_ + 1) * 2048], out_offset=None, in_=wexp[srcn + hsfx][:, :],
                                                                            in_offset=bass.IndirectOffsetOnAxis(ap=WIDX[:, b:b + 1], axis=0)),
                               reads=['WIDX'], writes=[wk])
                load('sp', xb[:], XB_d[b * 128:(b + 1) * 128, :], kx, ['XB_d'])
                for c in range(8):
                    fw.op('pe', lambda: nc.tensor.transpose(out=PSB[:, c * 128:(c + 1) * 128], in_=xb[:, c * 128:(c + 1) * 128], identity=IDB[:]), reads=[kx, 'IDB'], writes=['PSB'])
                fw.op('act', lambda: nc.scalar.copy(out=XTs[:, 0:4, :], in_=PSB[:, 0:512].rearrange("p (c s) -> p c s", c=4)), reads=['PSB'], writes=['XTs'])
                fw.op('dve', lambda: nc.vector.tensor_copy(out=XTs[:, 4:8, :], in_=PSB[:, 512:1024].rearrange("p (c s) -> p c s", c=4)), reads=['PSB'], writes=['XTs'])
                p1i = next_ps(); p3i = next_ps()
                for (pi_, wt, wk) in ((p1i, w1, k1), (p3i, w3, k3)):
                    for kc in range(8):
                        fw.op('pe', lambda: nc.tensor.matmul(PS[pi_][:, 0:512], lhsT=XTs[:, kc, :], rhs=wt[:, kc * 512:(kc + 1) * 512], start=(kc == 0), stop=(kc == 7)),
                              reads=['XTs', wk], writes=['ps%d' % pi_])
                fw.op('act', lambda: nc.scalar.activation(out=S1[:], in_=PS[p1i][:, 0:512], func=AF.Silu), reads=['ps%d' % p1i], writes=['S1'])
                fw.op('dve', lambda: nc.vector.tensor_tensor(out=Gb[:], in0=PS[p3i][:, 0:512], in1=S1[:], op=ALU.mult), reads=['ps%d' % p3i, 'S1'], writes=['Gb'])
                for fc in range(4):
                    fw.op('pe', lambda: nc.tensor.transpose(out=PSB[:, fc * 128:(fc + 1) * 128], in_=Gb[:, fc * 128:(fc + 1) * 128], identity=IDB[:]), reads=['Gb', 'IDB'], writes=['PSB'])
                fw.op('dve', lambda: nc.vector.tensor_copy(out=GTs[:], in_=PSB[:, 0:512].rearrange("p (c s) -> p c s", c=4)), reads=['PSB'], writes=['GTs'])
                for half in range(2):
                    pi_ = next_ps()
                    for fc in range(4):
                        fw.op('pe', lambda: nc.tensor.matmul(PS[pi_][:, 0:512], lhsT=GTs[:, fc, :], rhs=w2[:, fc * 1024 + half * 512:fc * 1024 + (half + 1) * 512], start=(fc == 0), stop=(fc == 3)),
                              reads=['GTs', k2], writes=['ps%d' % pi_])
                    if half == 0:
                        fw.op('act', lambda: nc.scalar.copy(out=ob[:, 0:512], in_=PS[pi_][:, 0:512]), reads=['ps%d' % pi_], writes=[ko])
                    else:
                        fw.op('dve', lambda: nc.vector.tensor_copy(out=ob[:, 512:1024], in_=PS[pi_][:, 0:512]), reads=['ps%d' % pi_], writes=[ko])
                load('sp', OUT_d[b * 128:(b + 1) * 128, :], ob[:], 'OUT_d', [ko])
          fw.barrier()
          with contextlib.ExitStack() as s10:
            IDN2 = sb("IDN2", [128, 128], F32, s10)
            ONF3 = sb("ONF3", [128, 128], F32, s10)
            pv2 = sb("pvec2", [128, 2, 8], F32, s10)
            load('sp', IDN2[:], ident, 'IDN2')
            load('sp', ONF3[:], ones_f, 'ONF3')
            load('sp', pv2[:, 0, :], ln2g, 'pvec2')
            load('sp', pv2[:, 1, :], ln2b, 'pvec2')
            XT2 = sb("XT2", [128, 8, 512], F32, s10)
            SQ2 = sb("SQ2", [128, 8, 512], F32, s10)
            MEAN2 = sb("MEAN2", [128, 512], F32, s10)
            MSQ2 = sb("MSQ2", [128, 512], F32, s10)
            RSTD2 = sb("RSTD2", [128, 512], F32, s10)
            O1s = [sb("O1s%d" % i, [128, D], F32, s10) for i in range(2)]
            O2s = [sb("O2s%d" % i, [128, D], F32, s10) for i in range(2)]
            ng = 0
            for ti, (e0, o0, n) in enumerate(OT):
                lat = ti < 4
                mv_ = mlat if lat else mctx
                mk_ = 'mlat' if lat else 'mctx'
                load('sp', XT2[:, :, 0:n], X1_d[:, :, o0:o0 + n], 'XT2', ['X1_d'])
                for sub in range(n // 128):
                    t = (o0 + sub * 128) // 128
                    o1, o2 = O1s[ng % 2], O2s[ng % 2]
                    ko1, ko2 = 'O1s%d' % (ng % 2), 'O2s%d' % (ng % 2)
                    ng += 1
                    for (o_, ko_, SL) in ((o1, ko1, SL1), (o2, ko2, SL2)):
                        fw.dma('pool', lambda: nc.gpsimd.indirect_dma_start(out=o_[:], out_offset=None, in_=OUT_d[:, :],
                                                                            in_offset=bass.IndirectOffsetOnAxis(ap=SL[:, t:t + 1], axis=0)),
                               reads=['OUT_d', 'SL'], writes=[ko_])
                    fw.op('dve', lambda: nc.vector.tensor_scalar(out=o1[:], in0=o1[:], scalar1=G1[:, t:t + 1], scalar2=None, op0=ALU.mult), reads=[ko1, 'G1'], writes=[ko1])
                    fw.op('dve', lambda: nc.vector.scalar_tensor_tensor(out=o1[:], in0=o2[:], scalar=G2[:, t:t + 1], in1=o1[:], op0=ALU.mult, op1=ALU.add), reads=[ko1, ko2, 'G2'], writes=[ko1])
                    for half in range(2):
                        pi_ = next_ps()
                        for cc in range(4):
                            c = half * 4 + cc
                            fw.op('pe', lambda: nc.tensor.transpose(out=PS[pi_][:, cc * 128:(cc + 1) * 128], in_=o1[:, c * 128:(c + 1) * 128], identity=IDN2[:]),
                                  reads=[ko1, 'IDN2'], writes=['ps%d' % pi_])
                        for cc in range(4):
                            c = half * 4 + cc
                            fw.op('act', lambda: nc.scalar.activation(out=SQ2[:, c, sub * 128:(sub + 1) * 128], in_=PS[pi_][:, cc * 128:(cc + 1) * 128], func=AF.Copy, scale=mv_[:, 40 + c:41 + c]),
                                  reads=['ps%d' % pi_, mk_], writes=['SQ2'])
                fw.op('dve', lambda: nc.vector.scalar_tensor_tensor(out=XT2[:, :, 0:n], in0=XT2[:, :, 0:n], scalar=ALPHA, in1=SQ2[:, :, 0:n], op0=ALU.mult, op1=ALU.add),
                      reads=['XT2', 'SQ2'], writes=['XT2'])
                layer_norm(XT2, 'XT2', n, SQ2, 'SQ2', MEAN2, 'MEAN2', MSQ2, 'MSQ2', RSTD2, 'RSTD2', ONF3, 'ONF3',
                           lambda c: pv2[:, 0, c:c + 1], lambda c: pv2[:, 1, c:c + 1], 'pvec2')
                if lat:
                    load('sp', xout[:, :, o0:o0 + n], XT2[:, :, 0:n], 'xout', ['XT2'])
                else:
                    load('sp', cout[:, :, :], XT2[:, :, 0:n], 'cout', ['XT2'])
            outs.extend(['xout', 'cout'])
        sHT.close()
        fw.barrier()


def _fm(a):
    return np.ascontiguousarray(a.T.reshape(8, 128, a.shape[0]).transpose(1, 0, 2))


def _pp(v):
    return np.ascontiguousarray(v.reshape(-1, 128).T)


def _kc(w):
    return np.ascontiguousarray(w.reshape(8, 128, w.shape[1]).transpose(1, 0, 2))


def _partner_cols(nh):
    idx = []
    for h in range(nh):
        for d in range(64):
            half, i = d // 32, d % 32
            p = i + 16 if i < 16 else i - 16
            idx.append(h * 64 + half * 32 + p)
    return np.array(idx)


def prep_layer_weights(inp, l):
    w_in = inp['w_in'][l]
    b_in = inp['b_in'][l]
    o = np.cumsum([0, 512, 512, 512, 3072, 1024, 128, 128, 1024])
    q, u, v, g, z, k, va, xr = [slice(o[i], o[i + 1]) for i in range(8)]
    pq = _partner_cols(8)
    pk = _partner_cols(2)

    def cols(a):
        return np.concatenate([a[..., q], a[..., q][..., pq], a[..., k], a[..., k][..., pk], a[..., u], a[..., v],
                               a[..., va], a[..., g], a[..., z], a[..., xr]], axis=-1)
    W = {}
    W['w_in'] = _kc(cols(w_in))
    W['b_in'] = _pp(cols(b_in))
    W['w_mod'] = _kc(inp['w_mod'][l])
    W['b_mod'] = _pp(inp['b_mod'][l])
    W['convw'] = np.ascontiguousarray(inp['conv_w'][l].reshape(2, 4, 8, 128).transpose(3, 2, 0, 1))
    W['convb'] = np.ascontiguousarray(inp['conv_b'][l].reshape(2, 8, 128).transpose(2, 1, 0))
    W['wrg'] = np.ascontiguousarray(inp['w_rgate'][l].transpose(2, 0, 1, 3))
    W['wig'] = np.ascontiguousarray(inp['w_igate'][l].transpose(2, 0, 1, 3))
    W['brg'] = np.ascontiguousarray(inp['b_rgate'][l].reshape(2, 8, 128).transpose(2, 1, 0))
    W['big'] = np.ascontiguousarray(inp['b_igate'][l].reshape(2, 8, 128).transpose(2, 1, 0))
    W['lam'] = np.ascontiguousarray(inp['lru_lambda'][l].reshape(2, 8, 128).transpose(2, 1, 0))
    bq, bqs, bk, bks = b_in[q], b_in[q][pq], b_in[k], b_in[k][pk]
    W['b_qk'] = np.ascontiguousarray(np.concatenate([bq.reshape(8, 64), bqs.reshape(8, 64), bk.reshape(2, 64), bks.reshape(2, 64)], 0).T)
    W['b_va'] = np.ascontiguousarray(np.broadcast_to(b_in[va][None, :], (128, 128)))
    W['b_v'] = np.ascontiguousarray(np.broadcast_to(b_in[v][None, :], (128, 512)))
    W['sinkx'] = np.ascontiguousarray(np.broadcast_to(inp['attn_sink'][l][None, :, None], (64, 8, 128)))
    W['sgu_g'] = np.ascontiguousarray(np.broadcast_to(inp['sgu_ln_g'][l][None, :], (128, 512)))
    W['sgu_b'] = np.ascontiguousarray(np.broadcast_to(inp['sgu_ln_b'][l][None, :], (128, 512)))
    W['w_spT'] = np.ascontiguousarray(inp['w_spatial'][l].transpose(2, 0, 1))
    W['b_sp'] = np.ascontiguousarray(np.broadcast_to(inp['b_spatial'][l][None, :, :], (128, 4, 128)))
    W['w_pa'] = np.ascontiguousarray(inp['w_proj_a'][l].reshape(4, 128, D).transpose(1, 0, 2))
    W['w_pb'] = np.ascontiguousarray(inp['w_proj_b'][l].reshape(8, 64, D).transpose(1, 0, 2))
    W['w_pc'] = _kc(inp['w_proj_c'][l])
    W['w_o'] = _kc(inp['w_out'][l])
    W['b_o'] = _pp(inp['b_out'][l])
    W['ln1g'] = _pp(inp['ln1_g'][l]); W['ln1b'] = _pp(inp['ln1_b'][l])
    W['ln2g'] = _pp(inp['ln2_g'][l]); W['ln2b'] = _pp(inp['ln2_b'][l])
    W['w_gr'] = _kc(np.concatenate([inp['w_group'][l], inp['w_router'][l]], 1))
    w1r = inp['w1'][l].reshape(NEXP, 8, 128, 512).transpose(0, 2, 1, 3).reshape(NEXP * 128, 4096)
    w3r = inp['w3'][l].reshape(NEXP, 8, 128, 512).transpose(0, 2, 1, 3).reshape(NEXP * 128, 4096)
    w2r = inp['w2'][l].reshape(NEXP, 4, 128, 1024).transpose(0, 2, 1, 3).reshape(NEXP * 128, 4096)
    for nm, arr in (("w1", w1r), ("w3", w3r), ("w2", w2r)):
        W[nm + 'a'] = np.ascontiguousarray(arr[:, :2048])
        W[nm + 'b'] = np.ascontiguousarray(arr[:, 2048:])
    W['b_gr'] = np.ascontiguousarray(np.broadcast_to(np.concatenate([inp['b_group'][l], inp['b_router'][l]])[None, :], (128, 36)))
    return W


def const_inputs(core):
    s = core % NSEG
    pos = np.arange(TLAT) + s * TOWN - HALO
    row = (pos // 64).astype(np.float32)
    colp = (pos % 64).astype(np.float32)
    inv = (10000.0 ** (-np.arange(0, 32, 2, dtype=np.float32) / 32)).astype(np.float32)
    cos = np.zeros((64, TLAT), np.float32)
    sin = np.zeros((64, TLAT), np.float32)
    for d in range(64):
        half, i = d // 32, d % 32
        ang = (row if half == 0 else colp) * inv[i % 16]
        cos[d] = np.cos(ang)
        sin[d] = np.sin(ang) * (-1.0 if i < 16 else 1.0)
    kk = np.arange(128)[:, None]
    qq = np.arange(128)[None, :]
    m = np.zeros((128, 2, 4, 128), np.float32)
    m[:, 0] = (kk >= qq).astype(np.float32)[:, None, :]
    m[:, 1] = (kk <= qq).astype(np.float32)[:, None, :]
    return {'rope_cos': cos, 'rope_sin': sin, 'masks': m, 'ones_in': np.ones((128, 64), np.float32),
            'ident': np.eye(128, dtype=np.float32), 'tri_in': np.triu(np.ones((128, 128), np.float32), 1),
            'bval_in': np.ascontiguousarray(np.broadcast_to((np.arange(NBLK, dtype=np.float32) * 128)[None, :], (128, NBLK))),
            'pidx_in': np.arange(128, dtype=np.float32)[:, None].copy(), 'ones_f': np.ones((128, 128), np.float32)}


def core_inputs(x, ctx, c, c_ctx, core):
    b, s = core // NSEG, core % NSEG
    lo = s * TOWN - HALO
    ext = np.zeros((TLAT, D), np.float32)
    a, e = max(lo, 0), min(lo + TLAT, SEQ)
    ext[a - lo:e - lo] = x[b, a:e]
    m = {'xT': _fm(ext), 'cT': _fm(ctx[b])}
    m['cond'] = np.ascontiguousarray(np.stack([_pp(c[b]), _pp(c_ctx)], -1))
    ed = np.zeros((128, 2), np.float32)
    ed[:, 0] = 1.0 if s > 0 else 0.0
    ed[:, 1] = 1.0 if s < NSEG - 1 else 0.0
    m['edge'] = ed
    return m


def carry_input(ab, core):
    car = np.zeros((128, 8, 2, 3, 2), np.float32)
    car[..., 0] = 1.0
    if ab is None:
        return car
    b, s = core // NSEG, core % NSEG
    fwd = [b * NSEG + j for j in range(0, s)]
    bwd = [b * NSEG + j for j in range(NSEG - 1, s, -1)]
    for k, cc in enumerate(fwd):
        car[:, :, 0, k, :] = ab[cc][:, :, 0, :]
    for k, cc in enumerate(bwd):
        car[:, :, 1, k, :] = ab[cc][:, :, 1, :]
    return car


PRE_NAMES = ['w_mod', 'b_mod', 'w_in', 'b_in', 'convw', 'convb', 'wrg', 'wig', 'brg', 'big', 'lam']
XPAD = SEQ + 2 * HALO


def all_consts():
    pos = np.arange(XPAD) - HALO
    row = (pos // 64).astype(np.float32)
    colp = (pos % 64).astype(np.float32)
    inv = (10000.0 ** (-np.arange(0, 32, 2, dtype=np.float32) / 32)).astype(np.float32)
    cos = np.zeros((64, XPAD), np.float32)
    sin = np.zeros((64, XPAD), np.float32)
    for d in range(64):
        half, i = d // 32, d % 32
        ang = (row if half == 0 else colp) * inv[i % 16]
        cos[d] = np.cos(ang)
        sin[d] = np.sin(ang) * (-1.0 if i < 16 else 1.0)
    C = const_inputs(0)
    C.pop('rope_cos')
    C.pop('rope_sin')
    C['rope_cos_all'] = cos
    C['rope_sin_all'] = sin
    ed = np.ones((NSEG, 128, 2), np.float32)
    ed[0, :, 0] = 0.0
    ed[NSEG - 1, :, 1] = 0.0
    C['edge_all'] = ed
    C['car_id'] = carry_input(None, 0)
    return C


def build_fused(w_shapes, c_shapes):
    nc = bass.Bass("TRN2", target_bir_lowering=False)

    def ein(name, shape, dt=F32):
        return nc.dram_tensor(name, list(shape), dt, kind="ExternalInput").ap()
    X0 = ein("X0", [128, 8, XPAD])
    C0 = ein("C0", [128, 8, NCTX])
    cond = ein("cond", [128, 8, 2])
    EXPN = ("w1a", "w1b", "w3a", "w3b", "w2a", "w2b")
    Wall = {k: ein(k, [DEPTH] + list(shp)) for k, shp in w_shapes.items() if k not in EXPN}
    Wexp = {k: [ein("%s_%d" % (k, l_), list(w_shapes[k])) for l_ in range(DEPTH)] for k in EXPN}
    Call = {k: ein(k, shp) for k, shp in c_shapes.items()}
    yout = nc.dram_tensor("yout", [128, 8, SEQ], F32, kind="ExternalOutput").ap()
    XS = [nc.dram_tensor("XS%d" % i, [128, 8, XPAD], F32).ap() for i in range(2)]
    CS = [nc.dram_tensor("CS%d" % i, [128, 8, NCTX], F32).ap() for i in range(3)]
    AB_d = nc.dram_tensor("AB_d", [NSEG, 128, 8, 2, 2], F32).ap()
    CAR_d = nc.dram_tensor("CAR_d", [128, 8, 2, 3, 2], F32).ap()
    with contextlib.ExitStack() as st0:
        sh = Shared(nc, st0)
        fw = sh.fw
        with contextlib.ExitStack() as sz:
            zt = sz.enter_context(nc.sbuf_tensor("zt", [128, 8, HALO], F32))
            fw.op('dve', lambda: nc.vector.memset(zt[:], 0.0), writes=['zt'])
            for i in range(2):
                fw.dma('sp', lambda: nc.sync.dma_start(out=XS[i][:, :, 0:HALO], in_=zt[:]), reads=['zt'], writes=['XS'])
                fw.dma('sp', lambda: nc.sync.dma_start(out=XS[i][:, :, HALO + SEQ:XPAD], in_=zt[:]), reads=['zt'], writes=['XS'])
            fw.barrier()
        for l in range(DEPTH):
            Xsrc = X0 if l == 0 else XS[(l - 1) % 2]
            Xdst = XS[l % 2]
            Csrc = C0 if l == 0 else CS[(l - 1) % 2]
            Cdst = CS[l % 2]
            for prepass in (True, False):
                for seg in range(NSEG):
                    if not prepass:
                        fwd = list(range(0, seg))
                        bwd = list(range(NSEG - 1, seg, -1))
                        for d_, lst in ((0, fwd), (1, bwd)):
                            for k in range(3):
                                if k < len(lst):
                                    src_ = AB_d[lst[k]][:, :, d_, :]
                                else:
                                    src_ = Call['car_id'][:, :, d_, k, :]
                                fw.dma('sp', lambda: nc.sync.dma_start(out=CAR_d[:, :, d_, k, :], in_=src_), writes=['CAR_d'])
                        fw.barrier()

                    def getin(name, shape, dt=F32, seg=seg, prepass=prepass):
                        if name == 'xT':
                            return Xsrc[:, :, seg * TOWN:seg * TOWN + TLAT]
                        if name == 'cT':
                            return Csrc
                        if name == 'cond':
                            return cond
                        if name == 'edge':
                            return Call['edge_all'][seg]
                        if name == 'car':
                            return Call['car_id'] if prepass else CAR_d
                        if name == 'rope_cos':
                            return Call['rope_cos_all'][:, seg * TOWN:seg * TOWN + TLAT]
                        if name == 'rope_sin':
                            return Call['rope_sin_all'][:, seg * TOWN:seg * TOWN + TLAT]
                        if name in Call:
                            return Call[name]
                        if name in Wexp:
                            return Wexp[name][l]
                        return Wall[name][l]

                    def getout(name, shape, dt=F32, seg=seg):
                        if name == 'ab_out':
                            return AB_d[seg]
                        if name == 'xout':
                            if l == DEPTH - 1:
                                return yout[:, :, seg * TOWN:(seg + 1) * TOWN]
                            return Xdst[:, :, HALO + seg * TOWN:HALO + (seg + 1) * TOWN]
                        if name == 'cout':
                            return Cdst if seg == 0 else CS[2]
                        raise KeyError(name)
                    emit_layer(nc, sh, getin, getout, prepass)
        fw.finish([])
        print("fused instructions:", fw.n_instr)
    return nc


_PROG = {}


def _unfm(a):
    return np.ascontiguousarray(a.transpose(2, 1, 0).reshape(a.shape[2], D))


def kernel(**inputs):
    inp = {k: np.asarray(v) for k, v in inputs.items()}
    x = np.ascontiguousarray(inp['x'], dtype=np.float32)
    ctx = np.ascontiguousarray(inp['ctx'], dtype=np.float32)
    c, c_ctx = inp['c'].astype(np.float32), inp['c_ctx'].astype(np.float32)
    Ws = [prep_layer_weights(inp, l) for l in range(DEPTH)]
    EXPN = ("w1a", "w1b", "w3a", "w3b", "w2a", "w2b")
    Wst = {k: np.stack([Ws[l][k] for l in range(DEPTH)], 0) for k in Ws[0] if k not in EXPN}
    Wex = {"%s_%d" % (k, l): Ws[l][k] for k in EXPN for l in range(DEPTH)}
    wshapes = {k: Ws[0][k].shape for k in Ws[0]}
    del Ws
    C = all_consts()
    if 'fused' not in _PROG:
        _PROG['fused'] = build_fused(wshapes, {k: v.shape for k, v in C.items()})
    nc = _PROG['fused']
    cores = list(range(8))
    per_batch = []
    for b in range(2):
        xp = np.zeros((XPAD, D), np.float32)
        xp[HALO:HALO + SEQ] = x[b]
        m = {'X0': _fm(xp), 'C0': _fm(ctx[b]), 'cond': np.ascontiguousarray(np.stack([_pp(c[b]), _pp(c_ctx)], -1))}
        m.update(Wst)
        m.update(Wex)
        m.update(C)
        per_batch.append(m)
    maps = [per_batch[cc // NSEG] for cc in cores]
    res = run_bass_kernel_spmd(nc, maps, core_ids=cores)
    out = np.empty_like(x)
    for b in range(2):
        out[b] = _unfm(np.asarray(res.results[b * NSEG]['yout']))
    return out
```

```python
import contextlib
import numpy as np
import concourse.bass as bass
import concourse.mybir as mybir
from concourse.bass_utils import run_bass_kernel_spmd

F32 = mybir.dt.float32
BF16 = mybir.dt.bfloat16
I32 = mybir.dt.int32
AF = mybir.ActivationFunctionType
ALU = mybir.AluOpType
AX = mybir.AxisListType

D = 1024
DEPTH = 4
SEQ = 8192
NCTX = 256
NSEG = 4
TOWN = 2048
HALO = 128
TLAT = TOWN + 2 * HALO
TEXT = TLAT + NCTX
NOWN = TOWN + NCTX
ALPHA = (2.0 * DEPTH) ** 0.25
LN_EPS = 1e-6
NEXP = 32
NBLK = (NOWN * 2) // 128 + NEXP
C_Q, C_QS, C_K, C_KS, C_U, C_V, C_VA, C_G, C_Z, C_XR = 0, 512, 1024, 1152, 1280, 1792, 2304, 2432, 5504, 6528
NCOL = 7552


class FW:
    NDMA = 24

    def __init__(self, nc, stack):
        self.nc = nc
        self.E = {'pe': nc.tensor, 'act': nc.scalar, 'dve': nc.vector, 'pool': nc.gpsimd, 'sp': nc.sync}
        self.sem = {}
        self.cnt = {}
        for e in ('pe', 'act', 'dve', 'pool'):
            self.sem[e] = stack.enter_context(nc.semaphore('s_' + e))
            self.cnt[e] = 0
        self.dsem = [stack.enter_context(nc.semaphore('d%d' % i)) for i in range(self.NDMA)]
        self.dval = [0] * self.NDMA
        self.dnext = 0
        self.waited = {}
        self.lastw = {}
        self.readers = {}
        self.n_instr = 0

    def _wait(self, e, tok):
        if tok is None:
            return
        kind, sk, val = tok
        k = (e, kind, sk)
        if self.waited.get(k, 0) >= val:
            return
        self.waited[k] = val
        s = self.sem[sk] if kind == 'eng' else self.dsem[sk]
        self.E[e].wait_ge(s, val)

    def _deps(self, e, reads, writes, strict=False):
        for r in reads:
            t = self.lastw.get(r)
            if t is not None:
                if t[0] == 'eng' and t[1] == e and e == 'pe':
                    continue
                self._wait(e, t)
        for w in writes:
            t = self.lastw.get(w)
            if t is not None and (strict or not (t[0] == 'eng' and t[1] == e)):
                self._wait(e, t)
            for t in self.readers.get(w, ()):
                if strict or not (t[0] == 'eng' and t[1] == e):
                    self._wait(e, t)

    def _commit(self, tok, reads, writes):
        for r in reads:
            self.readers.setdefault(r, []).append(tok)
        for w in writes:
            self.lastw[w] = tok
            self.readers[w] = []

    def op(self, e, fn, reads=(), writes=()):
        self._deps(e, reads, writes)
        ins = fn()
        self.cnt[e] += 1
        ins.then_inc(self.sem[e], 1)
        self._commit(('eng', e, self.cnt[e]), reads, writes)
        self.n_instr += 1
        return ins

    def dma(self, q, fn, reads=(), writes=()):
        i = self.dnext
        self.dnext = (self.dnext + 1) % self.NDMA
        if self.dval[i] > 0:
            self._wait(q, ('dma', i, self.dval[i]))
        self._deps(q, reads, writes, strict=True)
        ins = fn()
        self.dval[i] += 16
        ins.then_inc(self.dsem[i], 16)
        self._commit(('dma', i, self.dval[i]), reads, writes)
        self.n_instr += 1
        return ins

    def barrier(self):
        toks = [('eng', e2, self.cnt[e2]) for e2 in ('pe', 'act', 'dve', 'pool') if self.cnt[e2]]
        toks += [('dma', i, self.dval[i]) for i in range(self.NDMA) if self.dval[i]]
        for e in ('pe', 'act', 'dve', 'pool', 'sp'):
            for t in toks:
                if t[0] == 'eng' and t[1] == e:
                    continue
                self._wait(e, t)

    def finish(self, keys, e='sp'):
        for k in keys:
            self._wait(e, self.lastw.get(k))
        for i in range(self.NDMA):
            if self.dval[i]:
                self._wait(e, ('dma', i, self.dval[i]))


class Shared:
    def __init__(self, nc, st):
        self.nc = nc
        self.fw = FW(nc, st)
        self.PS = [st.enter_context(nc.psum_tensor("ps%d" % i, [128, 512], F32)) for i in range(7)]
        self.psi = 0
        self.uid = 0
        self.scr = {}

    def next_ps(self):
        i = self.psi
        self.psi = (i + 1) % 7
        return i

    def scratch(self, name, shape, dt):
        if name not in self.scr:
            self.scr[name] = self.nc.dram_tensor(name, list(shape), dt).ap()
        return self.scr[name]


def build_layer(prepass, dbg=False, phases=("lru", "att", "sgu", "merge", "moe", "moe8")):
    nc = bass.Bass("TRN2", target_bir_lowering=False)

    def din(name, shape, dt=F32):
        return nc.dram_tensor(name, list(shape), dt, kind="ExternalInput").ap()

    def dout(name, shape, dt=F32):
        return nc.dram_tensor(name, list(shape), dt, kind="ExternalOutput").ap()
    with contextlib.ExitStack() as st0:
        sh = Shared(nc, st0)
        emit_layer(nc, sh, din, dout, prepass, dbg, phases)
        sh.fw.finish([])
    return nc


def emit_layer(nc, sh, din, dout, prepass, dbg=False, phases=("lru", "att", "sgu", "merge", "moe", "moe8"), mode="full", seg=0):
    xT = din("xT", [128, 8, TLAT])
    cT = din("cT", [128, 8, NCTX])
    cond = din("cond", [128, 8, 2])
    w_mod = din("w_mod", [128, 8, 6 * D])
    b_mod = din("b_mod", [128, 48])
    w_in = din("w_in", [128, 8, NCOL])
    b_in = din("b_in", [128, NCOL // 128])
    edge = din("edge", [128, 2])
    convw = din("convw", [128, 8, 2, 4])
    convb = din("convb", [128, 8, 2])
    wrg = din("wrg", [128, 2, 8, 128])
    wig = din("wig", [128, 2, 8, 128])
    brg = din("brg", [128, 8, 2])
    big = din("big", [128, 8, 2])
    lam = din("lam", [128, 8, 2])
    car = din("car", [128, 8, 2, 3, 2])
    ab_out = dout("ab_out", [128, 8, 2, 2])
    dbg_outs = {}
    if dbg:
        dbg_outs["r_dbg"] = dout("r_dbg", [128, 8, NOWN])
        dbg_outs["a_dbg"] = dout("a_dbg", [128, 4, NOWN])
        dbg_outs["b_dbg"] = dout("b_dbg", [64, 8, NOWN])
        dbg_outs["kt_dbg"] = dout("kt_dbg", [64, 2, TEXT])
        dbg_outs["x1_dbg"] = dout("x1_dbg", [128, 8, NOWN])
        dbg_outs["lg_dbg"] = dout("lg_dbg", [128, 18, 36])
        dbg_outs["sl_dbg"] = dout("sl_dbg", [128, 2, 18], I32)
        dbg_outs["g_dbg"] = dout("g_dbg", [128, 2, 18])
        dbg_outs["wi_dbg"] = dout("wi_dbg", [128, NBLK], I32)
        dbg_outs["vt_dbg"] = dout("vt_dbg", [128, 20, 128])
    if not prepass:
        rope_cos = din("rope_cos", [64, TLAT])
        rope_sin = din("rope_sin", [64, TLAT])
        masks = din("masks", [128, 2, 4, 128])
        sinkx = din("sinkx", [64, 8, 128])
        ones_in = din("ones_in", [128, 64])
        b_va = din("b_va", [128, 128])
        b_qk = din("b_qk", [64, 20])
        b_v = din("b_v", [128, 512])
        sgu_g = din("sgu_g", [128, 512])
        sgu_b = din("sgu_b", [128, 512])
        w_spT = din("w_spT", [128, 4, 128])
        b_sp = din("b_sp", [128, 4, 128])
        w_pa = din("w_pa", [128, 4, D])
        w_pb = din("w_pb", [64, 8, D])
        w_pc = din("w_pc", [128, 8, D])
        w_o = din("w_o", [128, 8, D])
        b_o = din("b_o", [128, 8])
        ln1g = din("ln1g", [128, 8])
        ln1b = din("ln1b", [128, 8])
        ln2g = din("ln2g", [128, 8])
        ln2b = din("ln2b", [128, 8])
        w_gr = din("w_gr", [128, 8, 36])
        b_gr = din("b_gr", [128, 36])
        ident = din("ident", [128, 128])
        ones_f = din("ones_f", [128, 128])
        tri_in = din("tri_in", [128, 128])
        bval_in = din("bval_in", [128, NBLK])
        pidx_in = din("pidx_in", [128, 1])
        wexp = {}
        for nm in ("w1a", "w1b", "w3a", "w3b", "w2a", "w2b"):
            wexp[nm] = din(nm, [NEXP * 128, 2048])
        xout = dout("xout", [128, 8, TOWN])
        cout = dout("cout", [128, 8, NCTX])

    sh.uid += 1
    sfx = "_%d" % sh.uid
    if mode == "premoe":
        phases = ("lru", "att", "sgu", "merge")
    elif mode == "moe":
        phases = ("moe", "moe8")
    GL = mode in ("premoe", "moe")
    T = 66 if mode == "moe" else 18
    SUB = 4 if mode == "moe" else 1
    BS = 128 * SUB
    NB = (33 + NEXP) if mode == "moe" else NBLK
    with contextlib.ExitStack() as st:
        fw = sh.fw

        def sb(name, shape, dt=F32, stack=st):
            return stack.enter_context(nc.sbuf_tensor(name + sfx, list(shape), dt))

        def ps(name, shape, dt=F32, stack=st):
            return stack.enter_context(nc.psum_tensor(name + sfx, list(shape), dt))

        PS = sh.PS
        next_ps = sh.next_ps

        def load(q, dst, src, wkey, rkeys=()):
            eng = {'sp': nc.sync, 'act': nc.scalar, 'pool': nc.gpsimd}[q]
            return fw.dma(q, lambda: eng.dma_start(out=dst, in_=src), reads=list(rkeys), writes=[wkey])

        condt = sb("condt", [128, 8, 2])
        condb = sb("condb", [128, 8, 2], BF16)
        sig = sb("sig", [128, 8, 2])
        bmod = sb("bmod", [128, 48])
        mlat = sb("mlat", [128, 48])
        mctx = sb("mctx", [128, 48])
        bint = sb("bint", [128, NCOL // 128])
        edget = sb("edget", [128, 2])
        cw = sb("cw", [128, 8, 2, 4])
        cb = sb("cb", [128, 8, 2])
        brt = sb("brt", [128, 8, 2])
        bit_ = sb("bit", [128, 8, 2])
        lamt = sb("lamt", [128, 8, 2])
        negc = sb("negc", [128, 8, 2])
        negc2 = sb("negc2", [128, 8, 2])
        cart = sb("cart", [128, 8, 2, 3, 2])
        abt = sb("abt", [128, 8, 2, 2])
        load('sp', condt[:], cond, 'condt')
        load('sp', bmod[:], b_mod, 'bmod')
        load('sp', bint[:], b_in, 'bint')
        load('sp', edget[:], edge, 'edget')
        load('sp', cw[:], convw, 'cw')
        load('sp', cb[:], convb, 'cb')
        load('sp', brt[:], brg, 'brt')
        load('sp', bit_[:], big, 'bit')
        load('sp', lamt[:], lam, 'lamt')
        load('sp', cart[:], car, 'cart')

        fw.op('act', lambda: nc.scalar.activation(out=sig[:], in_=condt[:], func=AF.Sigmoid), reads=['condt'], writes=['sig'])
        fw.op('dve', lambda: nc.vector.tensor_tensor(out=condb[:], in0=condt[:], in1=sig[:], op=ALU.mult), reads=['condt', 'sig'], writes=['condb'])
        fw.op('act', lambda: nc.scalar.activation(out=negc[:], in_=lamt[:], func=AF.Exp, scale=-1.0), reads=['lamt'], writes=['negc'])
        fw.op('act', lambda: nc.scalar.activation(out=negc[:], in_=negc[:], func=AF.Ln, bias=1.0), reads=['negc'], writes=['negc'])
        fw.op('dve', lambda: nc.vector.tensor_scalar(out=negc2[:], in0=negc[:], scalar1=-16.0, scalar2=None, op0=ALU.mult), reads=['negc'], writes=['negc2'])
        fw.op('dve', lambda: nc.vector.tensor_scalar(out=negc[:], in0=negc[:], scalar1=-8.0, scalar2=None, op0=ALU.mult), reads=['negc'], writes=['negc'])

        with contextlib.ExitStack() as s1:
            wm = [sb("wm%d" % i, [128, 8, 512], BF16, s1) for i in range(2)]
            pmi = next_ps()
            pm = PS[pmi]
            pmk = 'ps%d' % pmi
            for g in range(12):
                wbuf = wm[g % 2]
                load('pool', wbuf[:], w_mod[:, :, g * 512:(g + 1) * 512], 'wm%d' % (g % 2))
                for jj in range(4):
                    j = g * 4 + jj
                    for kc in range(8):
                        fw.op('pe', lambda: nc.tensor.matmul(pm[:, 2 * j:2 * j + 2], lhsT=wbuf[:, kc, jj * 128:(jj + 1) * 128],
                                                              rhs=condb[:, kc, :], start=(kc == 0), stop=(kc == 7)),
                              reads=['wm%d' % (g % 2), 'condb'], writes=[pmk])
            pmv = pm[:, 0:96].rearrange("p (j t) -> p j t", t=2)
            fw.op('dve', lambda: nc.vector.tensor_tensor(out=mlat[:], in0=pmv[:, :, 0], in1=bmod[:], op=ALU.add), reads=[pmk, 'bmod'], writes=['mlat'])
            fw.op('dve', lambda: nc.vector.tensor_tensor(out=mctx[:], in0=pmv[:, :, 1], in1=bmod[:], op=ALU.add), reads=[pmk, 'bmod'], writes=['mctx'])
        fw.barrier()
        m1p = sb("m1p", [128, 2, 8])
        m4p = sb("m4p", [128, 2, 8])
        for t_, mv in ((0, mlat), (1, mctx)):
            fw.op('dve', lambda: nc.vector.tensor_scalar(out=m1p[:, t_, :], in0=mv[:, 8:16], scalar1=1.0, scalar2=None, op0=ALU.add), reads=['mlat', 'mctx'], writes=['m1p'])
            fw.op('dve', lambda: nc.vector.tensor_scalar(out=m4p[:, t_, :], in0=mv[:, 32:40], scalar1=1.0, scalar2=None, op0=ALU.add), reads=['mlat', 'mctx'], writes=['m4p'])

        sY = st.enter_context(contextlib.ExitStack())
        yT = sY.enter_context(nc.sbuf_tensor("yT" + sfx, [128, 8, NOWN], BF16, side='right'))
        sHT = contextlib.ExitStack()
        hT = sHT.enter_context(nc.sbuf_tensor("hT" + sfx, [128, 8, TEXT], BF16, side='right'))
        with contextlib.ExitStack() as s2:
            xs = [sb("xs%d" % i, [128, TLAT], F32, s2) for i in range(2)]
            for c in (range(8) if mode != "moe" else ()):
                xb_ = xs[c % 2]
                k = 'xs%d' % (c % 2)
                load('sp', xb_[:], xT[:, c, :], k)
                fw.op('act', lambda: nc.scalar.activation(out=hT[:, c, 0:TLAT], in_=xb_[:], func=AF.Identity,
                                                          scale=m1p[:, 0, c:c + 1], bias=mlat[:, c:c + 1]),
                      reads=[k, 'm1p', 'mlat'], writes=['hT%d' % c])
                load('sp', xb_[:, 0:NCTX], cT[:, c, :], k)
                fw.op('act', lambda: nc.scalar.activation(out=hT[:, c, TLAT:TEXT], in_=xb_[:, 0:NCTX], func=AF.Identity,
                                                          scale=m1p[:, 1, c:c + 1], bias=mctx[:, c:c + 1]),
                      reads=[k, 'm1p', 'mctx'], writes=['hT%d' % c])
        fw.barrier()
        hkeys = ['hT%d' % c for c in range(8)]

        def inproj(col0, ncols, tok0, ntok, wkey, wtile, evac):
            if wtile is not None:
                wsl = lambda kc: wtile[:, kc, col0:col0 + ncols]
            else:
                wsl = col0
            return inproj2(wsl, ncols, tok0, ntok, wkey, evac)

        def inproj2(wsl, ncols, tok0, ntok, wkey, evac):
            pi = next_ps()
            pt = PS[pi]
            for kc in range(8):
                fw.op('pe', lambda: nc.tensor.matmul(pt[0:ncols, 0:ntok], lhsT=wsl(kc), rhs=hT[:, kc, tok0:tok0 + ntok],
                                                      start=(kc == 0), stop=(kc == 7)),
                      reads=[wkey, 'hT%d' % kc], writes=['ps%d' % pi])
            evac(pt, 'ps%d' % pi)

        R_d = sh.scratch("R_d", [128, 8, NOWN], BF16)
        A_d = sh.scratch("A_d", [128, 4, NOWN], BF16)
        B_d = sh.scratch("B_d", [64, 8, NOWN], BF16)
        if "lru" in phases:
          with contextlib.ExitStack() as s3:
            rst = sb("rst", [128, NOWN], BF16, s3)
            wxr = [sb("wxr%d" % i, [128, 8, 128], BF16, s3) for i in range(2)]
            wz = [sb("wz%d" % i, [128, 8, 128], BF16, s3) for i in range(2)]
            wr_t = sb("wr_t", [128, 2, 8, 128], BF16, s3)
            wi_t = sb("wi_t", [128, 2, 8, 128], BF16, s3)
            load('pool', wr_t[:], wrg, 'wr_t')
            load('pool', wi_t[:], wig, 'wi_t')
            XRL = sb("XRL", [128, TLAT], F32, s3)
            XRC = sb("XRC", [128, NCTX + 6], F32, s3)
            GZ = sb("GZ", [128, NOWN], F32, s3)
            TT = [sb("TT0", [128, NOWN], F32, s3)] * 2
            TB = [sb("TB0", [128, NOWN], BF16, s3)] * 2
            RG = [sb("RG0", [128, NOWN], F32, s3)] * 2
            IG = [sb("IG0", [128, NOWN], F32, s3)] * 2
            AA = [sb("AA0", [128, NOWN], F32, s3)] * 2
            HH = [sb("HH%d" % i, [128, NOWN], F32, s3) for i in range(2)]
            sm = sb("sm", [128, 16], F32, s3)
            fw.op('pool', lambda: nc.gpsimd.memset(XRC[:], 0.0), writes=['XRC'])
            for j in range(8):
                wx = wxr[j % 2]
                wzz = wz[j % 2]
                kx = 'wxr%d' % (j % 2)
                kz = 'wz%d' % (j % 2)
                load('pool', wx[:], w_in[:, :, C_XR + j * 128:C_XR + (j + 1) * 128], kx)
                load('pool', wzz[:], w_in[:, :, C_Z + j * 128:C_Z + (j + 1) * 128], kz)
                bx = bint[:, (C_XR // 128) + j:(C_XR // 128) + j + 1]
                bz = bint[:, (C_Z // 128) + j:(C_Z // 128) + j + 1]
                for ti in range(5):
                    def ev(pt, pk, ti=ti):
                        if ti < 4:
                            fw.op('act', lambda: nc.scalar.activation(out=XRL[:, ti * 512:(ti + 1) * 512], in_=pt[:, 0:512], func=AF.Identity, bias=bx),
                                  reads=[pk, 'bint'], writes=['XRL'])
                        else:
                            fw.op('act', lambda: nc.scalar.activation(out=XRL[:, 2048:2304], in_=pt[:, 0:256], func=AF.Identity, bias=bx),
                                  reads=[pk, 'bint'], writes=['XRL'])
                            fw.op('act', lambda: nc.scalar.activation(out=XRC[:, 3:3 + NCTX], in_=pt[:, 256:512], func=AF.Identity, bias=bx),
                                  reads=[pk, 'bint'], writes=['XRC'])
                    inproj(0, 128, ti * 512, 512, kx, wx, ev)
                fw.op('dve', lambda: nc.vector.tensor_scalar(out=XRL[:, 0:HALO], in0=XRL[:, 0:HALO], scalar1=edget[:, 0:1], scalar2=None, op0=ALU.mult),
                      reads=['XRL', 'edget'], writes=['XRL'])
                fw.op('dve', lambda: nc.vector.tensor_scalar(out=XRL[:, HALO + TOWN:TLAT], in0=XRL[:, HALO + TOWN:TLAT], scalar1=edget[:, 1:2], scalar2=None, op0=ALU.mult),
                      reads=['XRL', 'edget'], writes=['XRL'])
                for ti in range(5):
                    def evz(pt, pk, ti=ti):
                        if ti < 4:
                            fw.op('act', lambda: nc.scalar.activation(out=GZ[:, ti * 512:(ti + 1) * 512], in_=pt[:, 0:512], func=AF.Gelu, bias=bz),
                                  reads=[pk, 'bint'], writes=['GZ'])
                        else:
                            fw.op('act', lambda: nc.scalar.activation(out=GZ[:, 2048:2304], in_=pt[:, 0:256], func=AF.Gelu, bias=bz),
                                  reads=[pk, 'bint'], writes=['GZ'])
                    if ti < 4:
                        inproj(0, 128, HALO + ti * 512, 512, kz, wzz, evz)
                    else:
                        inproj(0, 128, TLAT, 256, kz, wzz, evz)
                for d_ in range(2):
                    T_ = TT[d_]; Tb = TB[d_]; Rg = RG[d_]; Ig = IG[d_]; A_ = AA[d_]; H_ = HH[d_]
                    kT, kTb, kR, kI, kA, kH = 'TT', 'TB', 'RG', 'IG', 'AA', 'HH%d' % d_
                    for (dst0, n, src, s0, sk) in ((0, TOWN, XRL, HALO, 'XRL'), (TOWN, NCTX, XRC, 3, 'XRC')):
                        for tap in range(4):
                            off = s0 + (tap - 3 if d_ == 0 else tap)
                            wcol = cw[:, j, d_, tap:tap + 1]
                            if tap == 0:
                                fw.op('dve', lambda: nc.vector.tensor_scalar(out=T_[:, dst0:dst0 + n], in0=src[:, off:off + n], scalar1=wcol,
                                                                             scalar2=cb[:, j, d_:d_ + 1], op0=ALU.mult, op1=ALU.add),
                                      reads=[sk, 'cw', 'cb'], writes=[kT])
                            else:
                                fw.op('dve', lambda: nc.vector.scalar_tensor_tensor(out=T_[:, dst0:dst0 + n], in0=src[:, off:off + n], scalar=wcol,
                                                                                    in1=T_[:, dst0:dst0 + n], op0=ALU.mult, op1=ALU.add),
                                      reads=[sk, 'cw', kT], writes=[kT])
                    fw.op('pool', lambda: nc.gpsimd.tensor_copy(out=Tb[:], in_=T_[:]), reads=[kT], writes=[kTb])
                    for ti in range(5):
                        t0 = ti * 512
                        n = 512 if ti < 4 else 256
                        for (wt_, wk, bt_, bk, dst, dk) in ((wr_t, 'wr_t', brt, 'brt', Rg, kR), (wi_t, 'wi_t', bit_, 'bit', Ig, kI)):
                            pi = next_ps()
                            pt = PS[pi]
                            fw.op('pe', lambda: nc.tensor.matmul(pt[:, 0:n], lhsT=wt_[:, d_, j, :], rhs=Tb[:, t0:t0 + n], start=True, stop=True),
                                  reads=[wk, kTb], writes=['ps%d' % pi])
                            fw.op('act', lambda: nc.scalar.activation(out=dst[:, t0:t0 + n], in_=pt[:, 0:n], func=AF.Sigmoid, bias=bt_[:, j, d_:d_ + 1]),
                                  reads=['ps%d' % pi, bk], writes=[dk])
                    fw.op('act', lambda: nc.scalar.activation(out=A_[:], in_=Rg[:], func=AF.Exp, scale=negc[:, j, d_:d_ + 1]), reads=[kR, 'negc'], writes=[kA])
                    fw.op('act', lambda: nc.scalar.activation(out=H_[:], in_=Rg[:], func=AF.Exp, scale=negc2[:, j, d_:d_ + 1]), reads=[kR, 'negc2'], writes=[kH])
                    fw.op('act', lambda: nc.scalar.activation(out=H_[:], in_=H_[:], func=AF.Sqrt, scale=-1.0, bias=1.0), reads=[kH], writes=[kH])
                    fw.op('pool', lambda: nc.gpsimd.tensor_tensor(out=Ig[:], in0=Ig[:], in1=T_[:], op=ALU.mult), reads=[kI, kT], writes=[kI])
                    fw.op('pool', lambda: nc.gpsimd.tensor_tensor(out=Ig[:], in0=Ig[:], in1=H_[:], op=ALU.mult), reads=[kI, kH], writes=[kI])
                    rs = sm[:, 8 + d_:9 + d_]
                    fw.op('dve', lambda: nc.vector.reduce_sum(out=rs, in_=Rg[:, 0:TOWN], axis=AX.X), reads=[kR], writes=['sm_rs%d' % d_])
                    fw.op('act', lambda: nc.scalar.activation(out=abt[:, j, d_, 0:1], in_=rs, func=AF.Exp, scale=negc[:, j, d_:d_ + 1]),
                          reads=['sm_rs%d' % d_, 'negc'], writes=['abt'])
                    if d_ == 0:
                        a_c, u_c, h_c = A_[:, TOWN:NOWN], Ig[:, TOWN:NOWN], H_[:, TOWN:NOWN]
                        a_l, u_l, h_l = A_[:, 0:TOWN], Ig[:, 0:TOWN], H_[:, 0:TOWN]
                        hc_fin = H_[:, NOWN - 1:NOWN]
                        hl_fin = H_[:, TOWN - 1:TOWN]
                    else:
                        a_c, u_c, h_c = A_[:, TOWN:NOWN][:, ::-1], Ig[:, TOWN:NOWN][:, ::-1], H_[:, TOWN:NOWN][:, ::-1]
                        a_l, u_l, h_l = A_[:, 0:TOWN][:, ::-1], Ig[:, 0:TOWN][:, ::-1], H_[:, 0:TOWN][:, ::-1]
                        hc_fin = H_[:, TOWN:TOWN + 1]
                        hl_fin = H_[:, 0:1]
                    fw.op('dve', lambda: nc.vector.tensor_tensor_scan(out=h_c, data0=a_c, data1=u_c, initial=0.0, op0=ALU.mult, op1=ALU.add),
                          reads=[kA, kI], writes=[kH])
                    hin = sm[:, d_ * 4:d_ * 4 + 1]
                    fw.op('dve', lambda: nc.vector.tensor_copy(out=hin, in_=hc_fin), reads=[kH], writes=['hin%d' % d_])
                    for k3 in range(3):
                        fw.op('dve', lambda: nc.vector.scalar_tensor_tensor(out=hin, in0=hin, scalar=cart[:, j, d_, k3, 0:1], in1=cart[:, j, d_, k3, 1:2],
                                                                            op0=ALU.mult, op1=ALU.add),
                              reads=['hin%d' % d_, 'cart'], writes=['hin%d' % d_])
                    fw.op('dve', lambda: nc.vector.tensor_tensor_scan(out=h_l, data0=a_l, data1=u_l, initial=hin, op0=ALU.mult, op1=ALU.add),
                          reads=[kA, kI, 'hin%d' % d_], writes=[kH])
                    tmp = sm[:, d_ * 4 + 1:d_ * 4 + 2]
                    fw.op('dve', lambda: nc.vector.tensor_tensor(out=tmp, in0=abt[:, j, d_, 0:1], in1=hin, op=ALU.mult), reads=['abt', 'hin%d' % d_], writes=['tmp%d' % d_])
                    fw.op('dve', lambda: nc.vector.tensor_tensor(out=abt[:, j, d_, 1:2], in0=hl_fin, in1=tmp, op=ALU.subtract), reads=[kH, 'tmp%d' % d_], writes=['abt'])
                fw.op('pool', lambda: nc.gpsimd.tensor_tensor(out=HH[0][:], in0=HH[0][:], in1=HH[1][:], op=ALU.add), reads=['HH0', 'HH1'], writes=['HH0'])
                fw.op('pool', lambda: nc.gpsimd.tensor_tensor(out=rst[:], in0=HH[0][:], in1=GZ[:], op=ALU.mult), reads=['HH0', 'GZ'], writes=['rst'])
                load('sp', R_d[:, j, :], rst[:], 'R_d', ['rst'])
          fw.barrier()
          load('sp', ab_out, abt[:], 'ab_out', ['abt'])
        outs = ['ab_out']
        if prepass:
            sHT.close()
            fw.barrier()
            return
        if "att" in phases:
          with contextlib.ExitStack() as s4:
            KT = sb("KT", [64, 2, TEXT], BF16, s4)
            Vtok = sb("Vtok", [128, 20, 128], BF16, s4)
            COS = sb("COS", [64, TLAT], F32, s4)
            SIN = sb("SIN", [64, TLAT], F32, s4)
            MSK = sb("MSK", [128, 2, 4, 128], F32, s4)
            ESX = sb("ESX", [64, 8, 128], F32, s4)
            ONES = sb("ONES", [128, 64], BF16, s4)
            BVA = sb("BVA", [128, 128], F32, s4)
            bqk = sb("bqk", [64, 20], F32, s4)
            wk_t = sb("wk_t", [128, 8, 256], BF16, s4)
            wva_t = sb("wva_t", [128, 8, 128], BF16, s4)
            wq_t = sb("wq_t", [128, 8, 1024], BF16, s4)
            load('sp', COS[:], rope_cos, 'COS')
            load('sp', SIN[:], rope_sin, 'SIN')
            load('sp', MSK[:], masks, 'MSK')
            load('sp', ESX[:], sinkx, 'ESX')
            load('sp', BVA[:], b_va, 'BVA')
            load('sp', bqk[:], b_qk, 'bqk')
            load('pool', ONES[:], ones_in, 'ONES')
            load('pool', wk_t[:], w_in[:, :, C_K:C_K + 256], 'wk_t')
            load('pool', wva_t[:], w_in[:, :, C_VA:C_VA + 128], 'wva_t')
            load('pool', wq_t[:], w_in[:, :, C_Q:C_Q + 1024], 'wq_t')
            fw.op('act', lambda: nc.scalar.activation(out=ESX[:], in_=ESX[:], func=AF.Exp), reads=['ESX'], writes=['ESX'])
            QF = [sb("QF%d" % i, [64, 512], F32, s4) for i in range(2)]
            QS = [sb("QS%d" % i, [64, 512], F32, s4) for i in range(2)]
            rot = [0]

            def roped(wt, wk, c_main, c_sw, bcol_main, bcol_sw, e0, n, dst, dkey, use_rope):
                i = rot[0] % 2
                rot[0] += 1
                qf, qs = QF[i], QS[i]

                def ev1(pt, pk):
                    fw.op('act', lambda: nc.scalar.activation(out=qf[:, 0:n], in_=pt[0:64, 0:n], func=AF.Identity, bias=bqk[:, bcol_main:bcol_main + 1]),
                          reads=[pk, 'bqk'], writes=['QF%d' % i])
                inproj2(lambda kc: wt[:, kc, c_main:c_main + 64], 64, e0, n, wk, ev1)
                if not use_rope:
                    fw.op('dve', lambda: nc.vector.tensor_copy(out=dst, in_=qf[:, 0:n]), reads=['QF%d' % i], writes=[dkey])
                    return

                def ev2(pt, pk):
                    fw.op('act', lambda: nc.scalar.activation(out=qs[:, 0:n], in_=pt[0:64, 0:n], func=AF.Identity, bias=bqk[:, bcol_sw:bcol_sw + 1]),
                          reads=[pk, 'bqk'], writes=['QS%d' % i])
                inproj2(lambda kc: wt[:, kc, c_sw:c_sw + 64], 64, e0, n, wk, ev2)
                fw.op('dve', lambda: nc.vector.tensor_tensor(out=qf[:, 0:n], in0=qf[:, 0:n], in1=COS[:, e0:e0 + n], op=ALU.mult), reads=['QF%d' % i, 'COS'], writes=['QF%d' % i])
                fw.op('pool', lambda: nc.gpsimd.tensor_tensor(out=qs[:, 0:n], in0=qs[:, 0:n], in1=SIN[:, e0:e0 + n], op=ALU.mult), reads=['QS%d' % i, 'SIN'], writes=['QS%d' % i])
                fw.op('dve', lambda: nc.vector.tensor_tensor(out=dst, in0=qf[:, 0:n], in1=qs[:, 0:n], op=ALU.add), reads=['QF%d' % i, 'QS%d' % i], writes=[dkey])

            for kv in range(2):
                for ti in range(5):
                    if ti < 4:
                        roped(wk_t, 'wk_t', kv * 64, 128 + kv * 64, 16 + kv, 18 + kv, ti * 512, 512, KT[:, kv, ti * 512:(ti + 1) * 512], 'KT', True)
                    else:
                        roped(wk_t, 'wk_t', kv * 64, 128 + kv * 64, 16 + kv, 18 + kv, 2048, 256, KT[:, kv, 2048:2304], 'KT', True)
                        roped(wk_t, 'wk_t', kv * 64, 128 + kv * 64, 16 + kv, 18 + kv, TLAT, 256, KT[:, kv, TLAT:TEXT], 'KT', False)
            for tt in range(20):
                pi = next_ps()
                pt = PS[pi]
                for kc in range(8):
                    fw.op('pe', lambda: nc.tensor.matmul(pt[:, 0:128], lhsT=hT[:, kc, tt * 128:(tt + 1) * 128], rhs=wva_t[:, kc, :], start=(kc == 0), stop=(kc == 7)),
                          reads=['wva_t', 'hT%d' % kc], writes=['ps%d' % pi])
                fw.op('dve', lambda: nc.vector.tensor_tensor(out=Vtok[:, tt, :], in0=pt[:, 0:128], in1=BVA[:], op=ALU.add), reads=['ps%d' % pi, 'BVA'], writes=['Vtok'])
            QT = sb("QT", [64, 8, 512], BF16, s4)
            PTS = [sb("PT%d" % i, [128, 4, 128], BF16, s4) for i in range(3)]
            DEN = sb("DEN", [64, 4, 128], F32, s4)
            BTS = [sb("BTS%d" % i, [64, 8, 128], BF16, s4) for i in range(2)]
            pti = [0]
            nqb = 0
            for ti in range(5):
                lat = ti < 4
                n = 512 if lat else 256
                e0 = HALO + ti * 512 if lat else TLAT
                for h in range(8):
                    roped(wq_t, 'wq_t', h * 64, 512 + h * 64, h, 8 + h, e0, n, QT[:, h, 0:n], 'QT', lat)
                for qb in range(n // 128):
                    q0 = qb * 128
                    if lat:
                        bi = ti * 4 + qb
                        ktiles = [(bi, 'p'), (bi + 1, 'o'), (bi + 2, 'n'), (18, 'c'), (19, 'c')]
                        own0 = bi * 128
                    else:
                        bi = -1
                        ktiles = [(18, 'c'), (19, 'c')]
                        own0 = TOWN + qb * 128
                    bts = BTS[nqb % 2]
                    bk = 'BTS%d' % (nqb % 2)
                    nqb += 1
                    for kv in range(2):
                        po_i = next_ps(); pd_i = next_ps()
                        po, pd = PS[po_i], PS[pd_i]
                        for ki, (tt, kind) in enumerate(ktiles):
                            psi_ = next_ps()
                            pss = PS[psi_]
                            fw.op('pe', lambda: nc.tensor.matmul(pss[:, 0:512], lhsT=KT[:, kv, tt * 128:(tt + 1) * 128],
                                                                  rhs=QT[:, 4 * kv:4 * kv + 4, q0:q0 + 128], start=True, stop=True),
                                  reads=['KT', 'QT'], writes=['ps%d' % psi_])
                            pi3 = pti[0] % 3
                            pti[0] += 1
                            P_ = PTS[pi3]
                            pk3 = 'PT%d' % pi3
                            fw.op('act', lambda: nc.scalar.activation(out=P_[:].rearrange("p g q -> p (g q)"), in_=pss[:, 0:512], func=AF.Exp, scale=0.125),
                                  reads=['ps%d' % psi_], writes=[pk3])
                            if kind in ('p', 'n'):
                                mi = 0 if kind == 'p' else 1
                                if (kind == 'p' and bi == 0) or (kind == 'n' and bi == 15):
                                    fw.op('dve', lambda: nc.vector.scalar_tensor_tensor(out=P_[:], in0=P_[:], scalar=edget[:, mi:mi + 1], in1=MSK[:, mi, :, :], op0=ALU.mult, op1=ALU.mult),
                                          reads=[pk3, 'MSK', 'edget'], writes=[pk3])
                                else:
                                    fw.op('dve', lambda: nc.vector.tensor_tensor(out=P_[:], in0=P_[:], in1=MSK[:, mi, :, :], op=ALU.mult), reads=[pk3, 'MSK'], writes=[pk3])
                            last = ki == len(ktiles) - 1
                            fw.op('pe', lambda: nc.tensor.matmul(po[0:64, 0:512], lhsT=Vtok[:, tt, kv * 64:(kv + 1) * 64], rhs=P_[:].rearrange("p g q -> p (g q)"), start=(ki == 0), stop=last),
                                  reads=['Vtok', pk3], writes=['ps%d' % po_i])
                            fw.op('pe', lambda: nc.tensor.matmul(pd[0:64, 0:512], lhsT=ONES[:], rhs=P_[:].rearrange("p g q -> p (g q)"), start=(ki == 0), stop=last),
                                  reads=['ONES', pk3], writes=['ps%d' % pd_i])
                        fw.op('dve', lambda: nc.vector.tensor_tensor(out=DEN[:], in0=pd[0:64, 0:512].rearrange("p (g q) -> p g q", g=4), in1=ESX[:, 4 * kv:4 * kv + 4, :], op=ALU.add),
                              reads=['ps%d' % pd_i, 'ESX'], writes=['DEN'])
                        fw.op('dve', lambda: nc.vector.reciprocal(out=DEN[:], in_=DEN[:]), reads=['DEN'], writes=['DEN'])
                        fw.op('dve', lambda: nc.vector.tensor_tensor(out=bts[:, 4 * kv:4 * kv + 4, :], in0=po[0:64, 0:512].rearrange("p (g q) -> p g q", g=4), in1=DEN[:], op=ALU.mult),
                              reads=['ps%d' % po_i, 'DEN'], writes=[bk])
                    load('sp', B_d[:, :, own0:own0 + 128], bts[:], 'B_d', [bk])

            if dbg:
                ktf = sb("ktf", [64, 2, TEXT], F32, s4)
                vtf = sb("vtf", [128, 20, 128], F32, s4)
                fw.op('dve', lambda: nc.vector.tensor_copy(out=ktf[:], in_=KT[:]), reads=['KT'], writes=['ktf'])
                fw.op('dve', lambda: nc.vector.tensor_copy(out=vtf[:], in_=Vtok[:]), reads=['Vtok'], writes=['vtf'])
                load('sp', dbg_outs['kt_dbg'], ktf[:], 'kt_dbg', ['ktf'])
                load('sp', dbg_outs['vt_dbg'], vtf[:], 'vt_dbg', ['vtf'])
                outs.extend(['kt_dbg', 'vt_dbg'])
        fw.barrier()
        if "sgu" in phases:
          with contextlib.ExitStack() as s5:
            wu_t = sb("wu_t", [128, 8, 512], BF16, s5)
            wv_t = sb("wv_t", [128, 8, 512], BF16, s5)
            BV = sb("BV", [128, 512], F32, s5)
            LNG = sb("LNG", [128, 512], F32, s5)
            LNB = sb("LNB", [128, 512], F32, s5)
            WST = sb("WST", [128, 4, 128], BF16, s5)
            BSP = sb("BSP", [128, 4, 128], F32, s5)
            load('pool', wu_t[:], w_in[:, :, C_U:C_U + 512], 'wu_t')
            load('pool', wv_t[:], w_in[:, :, C_V:C_V + 512], 'wv_t')
            load('pool', WST[:], w_spT, 'WST')
            load('sp', BV[:], b_v, 'BV')
            load('sp', LNG[:], sgu_g, 'LNG')
            load('sp', LNB[:], sgu_b, 'LNB')
            load('sp', BSP[:], b_sp, 'BSP')
            GU = sb("GU", [128, 4, 512], F32, s5)
            V1 = sb("V1", [128, 512], F32, s5)
            GV = sb("GV", [128, 512], F32, s5)
            VNB = sb("VNB", [128, 512], BF16, s5)
            STT = sb("STT", [128, 6], F32, s5)
            MV = sb("MV", [128, 2], F32, s5)
            RSTD = sb("RSTD", [128, 1], F32, s5)
            MX = sb("MX", [128, 4, 128], F32, s5)
            ATS = [sb("ATS%d" % i, [128, 4, 128], BF16, s5) for i in range(2)]
            nch = 0
            for ti in range(5):
                lat = ti < 4
                n = 512 if lat else 256
                e0 = HALO + ti * 512 if lat else TLAT
                for g in range(4):
                    def evu(pt, pk, g=g):
                        fw.op('act', lambda: nc.scalar.activation(out=GU[:, g, 0:n], in_=pt[:, 0:n], func=AF.Gelu, bias=bint[:, C_U // 128 + g:C_U // 128 + g + 1]),
                              reads=[pk, 'bint'], writes=['GU'])
                    inproj2(lambda kc: wu_t[:, kc, g * 128:(g + 1) * 128], 128, e0, n, 'wu_t', evu)
                for sub in range(n // 128):
                    t0 = e0 + sub * 128
                    own0 = (ti * 512 if lat else TOWN) + sub * 128
                    pi = next_ps()
                    pv = PS[pi]
                    for kc in range(8):
                        fw.op('pe', lambda: nc.tensor.matmul(pv[:, 0:512], lhsT=hT[:, kc, t0:t0 + 128], rhs=wv_t[:, kc, :], start=(kc == 0), stop=(kc == 7)),
                              reads=['wv_t', 'hT%d' % kc], writes=['ps%d' % pi])
                    fw.op('dve', lambda: nc.vector.tensor_tensor(out=V1[:], in0=pv[:, 0:512], in1=BV[:], op=ALU.add), reads=['ps%d' % pi, 'BV'], writes=['V1'])
                    fw.op('act', lambda: nc.scalar.activation(out=GV[:], in_=V1[:], func=AF.Gelu), reads=['V1'], writes=['GV'])
                    fw.op('dve', lambda: nc.vector.bn_stats(out=STT[:], in_=GV[:]), reads=['GV'], writes=['STT'])
                    fw.op('dve', lambda: nc.vector.bn_aggr(out=MV[:], in_=STT[:]), reads=['STT'], writes=['MV'])
                    fw.op('act', lambda: nc.scalar.activation(out=RSTD[:], in_=MV[:, 1:2], func=AF.Sqrt, bias=LN_EPS), reads=['MV'], writes=['RSTD'])
                    fw.op('dve', lambda: nc.vector.reciprocal(out=RSTD[:], in_=RSTD[:]), reads=['RSTD'], writes=['RSTD'])
                    fw.op('dve', lambda: nc.vector.tensor_scalar(out=GV[:], in0=GV[:], scalar1=MV[:, 0:1], scalar2=RSTD[:, 0:1], op0=ALU.subtract, op1=ALU.mult),
                          reads=['GV', 'MV', 'RSTD'], writes=['GV'])
                    fw.op('pool', lambda: nc.gpsimd.tensor_tensor(out=GV[:], in0=GV[:], in1=LNG[:], op=ALU.mult), reads=['GV', 'LNG'], writes=['GV'])
                    fw.op('pool', lambda: nc.gpsimd.tensor_tensor(out=VNB[:], in0=GV[:], in1=LNB[:], op=ALU.add), reads=['GV', 'LNB'], writes=['VNB'])
                    pmi_ = next_ps()
                    pmx = PS[pmi_]
                    for g in range(4):
                        fw.op('pe', lambda: nc.tensor.matmul(pmx[:, g * 128:(g + 1) * 128], lhsT=VNB[:, g * 128:(g + 1) * 128], rhs=WST[:, g, :], start=True, stop=True),
                              reads=['VNB', 'WST'], writes=['ps%d' % pmi_])
                    fw.op('dve', lambda: nc.vector.tensor_tensor(out=MX[:], in0=pmx[:, 0:512].rearrange("p (g q) -> p g q", g=4), in1=BSP[:], op=ALU.add),
                          reads=['ps%d' % pmi_, 'BSP'], writes=['MX'])
                    ats = ATS[nch % 2]
                    ak = 'ATS%d' % (nch % 2)
                    nch += 1
                    fw.op('pool', lambda: nc.gpsimd.tensor_tensor(out=ats[:], in0=MX[:], in1=GU[:, :, sub * 128:(sub + 1) * 128], op=ALU.mult), reads=['MX', 'GU'], writes=[ak])
                    load('sp', A_d[:, :, own0:own0 + 128], ats[:], 'A_d', [ak])
        fw.barrier()
        if dbg:
            with contextlib.ExitStack() as sd:
                rb = sb("dbgb", [128, NOWN], BF16, sd)
                rf = sb("dbgf", [128, NOWN], F32, sd)
                for (nm, src_d, npart, nchk) in (("r_dbg", R_d, 128, 8), ("a_dbg", A_d, 128, 4), ("b_dbg", B_d, 64, 8)):
                    o_ = dbg_outs[nm]
                    for j in range(nchk):
                        load('sp', rb[0:npart, :], src_d[:, j, :], 'dbgb', [nm[0].upper() + '_d'])
                        fw.op('dve', lambda: nc.vector.tensor_copy(out=rf[0:npart, :], in_=rb[0:npart, :]), reads=['dbgb'], writes=['dbgf'])
                        load('sp', o_[:, j, :], rf[0:npart, :], nm, ['dbgf'])
                    outs.append(nm)
        def layer_norm(X, xk, n, SQ, sqk, MEAN, mk, MSQ, msk, RSTDW, rk, ONF, ok, gcol, bcol, pvk):
            fw.op('act', lambda: nc.scalar.activation(out=SQ[:, :, 0:n], in_=X[:, :, 0:n], func=AF.Square), reads=[xk], writes=[sqk])
            p1 = next_ps(); p2 = next_ps()
            for c in range(8):
                fw.op('pe', lambda: nc.tensor.matmul(PS[p1][:, 0:n], lhsT=ONF[:], rhs=X[:, c, 0:n], start=(c == 0), stop=(c == 7)), reads=[ok, xk], writes=['ps%d' % p1])
            for c in range(8):
                fw.op('pe', lambda: nc.tensor.matmul(PS[p2][:, 0:n], lhsT=ONF[:], rhs=SQ[:, c, 0:n], start=(c == 0), stop=(c == 7)), reads=[ok, sqk], writes=['ps%d' % p2])
            fw.op('act', lambda: nc.scalar.activation(out=MEAN[:, 0:n], in_=PS[p1][:, 0:n], func=AF.Copy, scale=1.0 / D), reads=['ps%d' % p1], writes=[mk])
            fw.op('dve', lambda: nc.vector.tensor_tensor(out=MSQ[:, 0:n], in0=MEAN[:, 0:n], in1=MEAN[:, 0:n], op=ALU.mult), reads=[mk], writes=[msk])
            fw.op('dve', lambda: nc.vector.scalar_tensor_tensor(out=RSTDW[:, 0:n], in0=PS[p2][:, 0:n], scalar=1.0 / D, in1=MSQ[:, 0:n], op0=ALU.mult, op1=ALU.subtract),
                  reads=['ps%d' % p2, msk], writes=[rk])
            fw.op('act', lambda: nc.scalar.activation(out=RSTDW[:, 0:n], in_=RSTDW[:, 0:n], func=AF.Sqrt, bias=LN_EPS), reads=[rk], writes=[rk])
            fw.op('dve', lambda: nc.vector.reciprocal(out=RSTDW[:, 0:n], in_=RSTDW[:, 0:n]), reads=[rk], writes=[rk])
            for c in range(8):
                fw.op('dve', lambda: nc.vector.tensor_tensor(out=X[:, c, 0:n], in0=X[:, c, 0:n], in1=MEAN[:, 0:n], op=ALU.subtract), reads=[xk, mk], writes=[xk])
                fw.op('pool', lambda: nc.gpsimd.tensor_tensor(out=X[:, c, 0:n], in0=X[:, c, 0:n], in1=RSTDW[:, 0:n], op=ALU.mult), reads=[xk, rk], writes=[xk])
                fw.op('act', lambda: nc.scalar.activation(out=X[:, c, 0:n], in_=X[:, c, 0:n], func=AF.Identity, scale=gcol(c), bias=bcol(c)), reads=[xk, pvk], writes=[xk])

        if GL:
            X1_d = sh.scratch("X1g", [128, 8, 8448], F32)
            H2_d = sh.scratch("H2g", [8448, D], BF16)
            LG_d = sh.scratch("LGg", [128, 66, 36], F32)
        else:
            X1_d = sh.scratch("X1_d", [128, 8, NOWN], F32)
            H2_d = sh.scratch("H2_d", [NOWN, D], BF16)
        LG = sb("LG", [128, T, 36], F32)

        def gbase(o0):
            if not GL:
                return o0
            return seg * TOWN + o0 if o0 < TOWN else SEQ + (o0 - TOWN)

        def gskip(o0):
            return GL and o0 >= TOWN and seg != 0
        OT = [(HALO + i * 512, i * 512, 512) for i in range(4)] + [(TLAT, TOWN, 256)]
        if "merge" in phases:
          with contextlib.ExitStack() as s6:
            ATl = sb("ATl", [128, 4, 1280], BF16, s6)
            BTl = sb("BTl", [64, 8, 1280], BF16, s6)
            RTl = sb("RTl", [128, 8, 1280], BF16, s6)
            WGs = [sb("WG%d" % i, [128, 8, 3, 128], BF16, s6) for i in range(2)]
            WAs = [sb("WA%d" % i, [128, 4, 128], BF16, s6) for i in range(2)]
            WBs = [sb("WB%d" % i, [64, 8, 128], BF16, s6) for i in range(2)]
            WCs = [sb("WC%d" % i, [128, 8, 128], BF16, s6) for i in range(2)]
            GS = [sb("GS%d" % i, [128, 512], F32, s6) for i in range(3)]
            TS = [sb("TS%d" % i, [128, 512], F32, s6) for i in range(3)]
            wi = 0
            for hf in range(2):
                tiles = OT[0:2] if hf == 0 else OT[2:5]
                h0 = tiles[0][1]
                hn = sum(t[2] for t in tiles)
                load('sp', ATl[:, :, 0:hn], A_d[:, :, h0:h0 + hn], 'ATl', ['A_d'])
                load('sp', BTl[:, :, 0:hn], B_d[:, :, h0:h0 + hn], 'BTl', ['B_d'])
                load('sp', RTl[:, :, 0:hn], R_d[:, :, h0:h0 + hn], 'RTl', ['R_d'])
                for oc in range(8):
                    w_ = wi % 2
                    wi += 1
                    WG, WA, WB, WC = WGs[w_], WAs[w_], WBs[w_], WCs[w_]
                    kg, ka, kb, kc_ = 'WG%d' % w_, 'WA%d' % w_, 'WB%d' % w_, 'WC%d' % w_
                    for br in range(3):
                        c0 = C_G + br * 1024 + oc * 128
                        load('pool', WG[:, :, br, :], w_in[:, :, c0:c0 + 128], kg)
                    load('pool', WA[:], w_pa[:, :, oc * 128:(oc + 1) * 128], ka)
                    load('pool', WB[:], w_pb[:, :, oc * 128:(oc + 1) * 128], kb)
                    load('pool', WC[:], w_pc[:, :, oc * 128:(oc + 1) * 128], kc_)
                    for (e0, o0, n) in tiles:
                        l0 = o0 - h0
                        for br in range(3):
                            def evg(pt, pk, br=br):
                                bcol = C_G // 128 + br * 8 + oc
                                fw.op('act', lambda: nc.scalar.activation(out=GS[br][:, 0:n], in_=pt[:, 0:n], func=AF.Sigmoid, bias=bint[:, bcol:bcol + 1]),
                                      reads=[pk, 'bint'], writes=['GS%d' % br])
                            inproj2(lambda kc: WG[:, kc, br, :], 128, e0, n, kg, evg)
                        for br, (Wt, wk, src, sk, nk, npart) in enumerate(((WA, ka, ATl, 'ATl', 4, 128), (WB, kb, BTl, 'BTl', 8, 64), (WC, kc_, RTl, 'RTl', 8, 128))):
                            pi = next_ps()
                            pt = PS[pi]
                            for kk in range(nk):
                                fw.op('pe', lambda: nc.tensor.matmul(pt[:, 0:n], lhsT=Wt[0:npart, kk, :], rhs=src[0:npart, kk, l0:l0 + n], start=(kk == 0), stop=(kk == nk - 1)),
                                      reads=[wk, sk], writes=['ps%d' % pi])
                            fw.op('dve', lambda: nc.vector.tensor_tensor(out=TS[br][:, 0:n], in0=pt[:, 0:n], in1=GS[br][:, 0:n], op=ALU.mult),
                                  reads=['ps%d' % pi, 'GS%d' % br], writes=['TS%d' % br])
                        fw.op('pool', lambda: nc.gpsimd.tensor_tensor(out=TS[0][:, 0:n], in0=TS[0][:, 0:n], in1=TS[1][:, 0:n], op=ALU.add), reads=['TS0', 'TS1'], writes=['TS0'])
                        fw.op('pool', lambda: nc.gpsimd.tensor_tensor(out=yT[:, oc, o0:o0 + n], in0=TS[0][:, 0:n], in1=TS[2][:, 0:n], op=ALU.add), reads=['TS0', 'TS2'], writes=['yT%d' % oc])
          fw.barrier()
          sHT.close()
          with contextlib.ExitStack() as s7:
            WO = sb("WO", [128, 8, D], BF16, s7)
            WGR = sb("WGR", [128, 8, 36], F32, s7)
            BGR = sb("BGR", [128, 36], F32, s7)
            IDN = sb("IDN", [128, 128], F32, s7)
            ONF = sb("ONF", [128, 128], F32, s7)
            pv = sb("pvec", [128, 5, 8], F32, s7)
            bom = sb("bom", [128, 2, 8], F32, s7)
            load('pool', WO[:], w_o, 'WO')
            load('sp', WGR[:], w_gr, 'WGR')
            load('sp', BGR[:], b_gr, 'BGR')
            load('sp', IDN[:], ident, 'IDN')
            load('sp', ONF[:], ones_f, 'ONF')
            load('sp', pv[:, 0, :], b_o, 'pvec')
            load('sp', pv[:, 1, :], ln1g, 'pvec')
            load('sp', pv[:, 2, :], ln1b, 'pvec')
            fw.op('dve', lambda: nc.vector.tensor_tensor(out=bom[:, 0, :], in0=pv[:, 0, :], in1=mlat[:, 16:24], op=ALU.mult), reads=['pvec', 'mlat'], writes=['bom'])
            fw.op('dve', lambda: nc.vector.tensor_tensor(out=bom[:, 1, :], in0=pv[:, 0, :], in1=mctx[:, 16:24], op=ALU.mult), reads=['pvec', 'mctx'], writes=['bom'])
            XT = sb("XT", [128, 8, 512], F32, s7)
            SQ = sb("SQ", [128, 8, 512], F32, s7)
            H2F = sb("H2F", [128, 8, 512], F32, s7)
            MEAN = sb("MEAN", [128, 512], F32, s7)
            MSQ = sb("MSQ", [128, 512], F32, s7)
            RSTDW = sb("RSTDW", [128, 512], F32, s7)
            nsub = 0
            H2B = [sb("H2B%d" % i, [128, D], BF16, s7) for i in range(2)]
            for ti, (e0, o0, n) in enumerate(OT):
                lat = ti < 4
                mv_ = mlat if lat else mctx
                mk_ = 'mlat' if lat else 'mctx'
                li = 0 if lat else 1
                if lat:
                    load('sp', XT[:, :, 0:n], xT[:, :, e0:e0 + n], 'XT')
                else:
                    load('sp', XT[:, :, 0:n], cT[:, :, :], 'XT')
                for oc in range(8):
                    pi = next_ps()
                    pt = PS[pi]
                    for kc in range(8):
                        fw.op('pe', lambda: nc.tensor.matmul(pt[:, 0:n], lhsT=WO[:, kc, oc * 128:(oc + 1) * 128], rhs=yT[:, kc, o0:o0 + n], start=(kc == 0), stop=(kc == 7)),
                              reads=['WO', 'yT%d' % kc], writes=['ps%d' % pi])
                    fw.op('act', lambda: nc.scalar.activation(out=SQ[:, oc, 0:n], in_=pt[:, 0:n], func=AF.Identity, scale=mv_[:, 16 + oc:17 + oc], bias=bom[:, li, oc:oc + 1]),
                          reads=['ps%d' % pi, mk_, 'bom'], writes=['SQ'])
                    fw.op('dve', lambda: nc.vector.scalar_tensor_tensor(out=XT[:, oc, 0:n], in0=XT[:, oc, 0:n], scalar=ALPHA, in1=SQ[:, oc, 0:n], op0=ALU.mult, op1=ALU.add),
                          reads=['XT', 'SQ'], writes=['XT'])

                layer_norm(XT, 'XT', n, SQ, 'SQ', MEAN, 'MEAN', MSQ, 'MSQ', RSTDW, 'RSTDW', ONF, 'ONF', lambda c: pv[:, 1, c:c + 1], lambda c: pv[:, 2, c:c + 1], 'pvec')
                if not gskip(o0):
                    load('sp', X1_d[:, :, gbase(o0):gbase(o0) + n], XT[:, :, 0:n], 'X1_d', ['XT'])
                m4 = m4p[:, li, :]
                for c in range(8):
                    fw.op('act', lambda: nc.scalar.activation(out=H2F[:, c, 0:n], in_=XT[:, c, 0:n], func=AF.Identity, scale=m4p[:, li, c:c + 1], bias=mv_[:, 24 + c:25 + c]),
                          reads=['XT', 'm4p', mk_], writes=['H2F'])
                for sub in range(n // 128):
                    t0 = sub * 128
                    chunk = (o0 + t0) // 128
                    pi = next_ps()
                    pt = PS[pi]
                    for kc in range(8):
                        fw.op('pe', lambda: nc.tensor.matmul(pt[:, 0:36], lhsT=H2F[:, kc, t0:t0 + 128], rhs=WGR[:, kc, :], start=(kc == 0), stop=(kc == 7)),
                              reads=['H2F', 'WGR'], writes=['ps%d' % pi])
                    fw.op('dve', lambda: nc.vector.tensor_tensor(out=LG[:, chunk, :], in0=pt[:, 0:36], in1=BGR[:], op=ALU.add), reads=['ps%d' % pi, 'BGR'], writes=['LG'])
                    hb = H2B[nsub % 2]
                    hk = 'H2B%d' % (nsub % 2)
                    nsub += 1
                    for half in range(2):
                        pi = next_ps()
                        pt = PS[pi]
                        for cc in range(4):
                            c = half * 4 + cc
                            fw.op('pe', lambda: nc.tensor.transpose(out=pt[:, cc * 128:(cc + 1) * 128], in_=H2F[:, c, t0:t0 + 128], identity=IDN[:]),
                                  reads=['H2F', 'IDN'], writes=['ps%d' % pi])
                        if half == 0:
                            fw.op('act', lambda: nc.scalar.copy(out=hb[:, 0:512], in_=pt[:, 0:512]), reads=['ps%d' % pi], writes=[hk])
                        else:
                            fw.op('dve', lambda: nc.vector.tensor_copy(out=hb[:, 512:1024], in_=pt[:, 0:512]), reads=['ps%d' % pi], writes=[hk])
                    if not gskip(o0):
                        load('sp', H2_d[gbase(o0) + t0:gbase(o0) + t0 + 128, :], hb[:], 'H2_d', [hk])
          if mode == "premoe":
              load('sp', LG_d[:, seg * 16:(seg + 1) * 16, :], LG[:, 0:16, :], 'LG_d', ['LG'])
              if seg == 0:
                  load('sp', LG_d[:, 64:66, :], LG[:, 16:18, :], 'LG_d', ['LG'])
          fw.barrier()
          sY.close()
          if mode == "premoe":
              fw.barrier()
              return
          if dbg:
              load('sp', dbg_outs['x1_dbg'], X1_d, 'x1_dbg', ['X1_d'])
              lgf = LG
              load('sp', dbg_outs['lg_dbg'], LG[:], 'lg_dbg', ['LG'])
              outs.extend(['x1_dbg', 'lg_dbg'])
        if mode == "moe":
            sHT.close()
            sY.close()
            load('sp', LG[:], LG_d, 'LG', ['LG_d'])
        if "moe" in phases:
          XB_d = sh.scratch("XBg" if GL else "XB_d", [NB * BS, D], BF16)
          OUT_d = sh.scratch("OUTg" if GL else "OUT_d", [NB * BS, D], F32)
          SL1 = sb("SL1", [128, T], I32)
          SL2 = sb("SL2", [128, T], I32)
          G1 = sb("G1", [128, T], F32)
          G2 = sb("G2", [128, T], F32)
          WIDX = sb("WIDX", [128, NB], I32)
          with contextlib.ExitStack() as s8:
            def t8(name, shape, dt=F32):
                return sb(name, shape, dt, s8)
            TRI = t8("TRI", [128, 128]); ONF2 = t8("ONF2", [128, 128]); BVAL = t8("BVAL", [128, NBLK]); PIDX = t8("PIDX", [128, 1])
            load('sp', TRI[:], tri_in, 'TRI'); load('sp', ONF2[:], ones_f, 'ONF2'); load('sp', BVAL[:], bval_in, 'BVAL'); load('sp', PIDX[:], pidx_in, 'PIDX')
            if SUB != 1:
                fw.op('dve', lambda: nc.vector.tensor_scalar(out=BVAL[:], in0=BVAL[:], scalar1=float(SUB), scalar2=None, op0=ALU.mult), reads=['BVAL'], writes=['BVAL'])
            gmax = t8("gmax", [128, T]); goh = t8("goh", [128, T, 4]); ge = t8("ge", [128, T, 4]); gw = t8("gw", [128, T])
            pen = t8("pen", [128, T, 4]); em = t8("em", [128, T, 32]); em2 = t8("em2", [128, T, 32])
            m1 = t8("m1", [128, T]); m2 = t8("m2", [128, T]); oh1 = t8("oh1", [128, T, 32]); oh2 = t8("oh2", [128, T, 32])
            dm = t8("dm", [128, T]); p1 = t8("p1", [128, T])
            A_ = t8("Aas", [128, T, 32]); CUMA = t8("CUMA", [128, T + 1, 32]); SLT = t8("SLT", [128, T, 32]); TMP = t8("TMP", [128, T, 32])
            cnt = t8("cnt", [128, 32]); cnti = t8("cnti", [128, 32], I32); padd = t8("padd", [128, 32]); pend = t8("pend", [128, 32]); pst = t8("pst", [128, 32])
            one32 = t8("one32", [128, 32]); CMP = t8("CMP", [128, NB, 32]); be = t8("be", [128, NB]); slf = t8("slf", [128, T])

            def V(fn, reads, writes):
                fw.op('dve', fn, reads=reads, writes=writes)
            gl = LG[:, :, 0:4]
            el = LG[:, :, 4:36]
            V(lambda: nc.vector.tensor_reduce(out=gmax[:], in_=gl, op=ALU.max, axis=AX.X), ['LG'], ['gmax'])
            gmb = gmax[:].unsqueeze(2).to_broadcast([128, T, 4])
            V(lambda: nc.vector.tensor_tensor(out=goh[:], in0=gl, in1=gmb, op=ALU.is_equal), ['LG', 'gmax'], ['goh'])
            V(lambda: nc.vector.tensor_tensor(out=ge[:], in0=gl, in1=gmb, op=ALU.subtract), ['LG', 'gmax'], ['ge'])
            fw.op('act', lambda: nc.scalar.activation(out=ge[:], in_=ge[:], func=AF.Exp), reads=['ge'], writes=['ge'])
            V(lambda: nc.vector.tensor_reduce(out=gw[:], in_=ge[:], op=ALU.add, axis=AX.X), ['ge'], ['gw'])
            V(lambda: nc.vector.reciprocal(out=gw[:], in_=gw[:]), ['gw'], ['gw'])
            V(lambda: nc.vector.tensor_scalar(out=pen[:], in0=goh[:], scalar1=1e30, scalar2=-1e30, op0=ALU.mult, op1=ALU.add), ['goh'], ['pen'])
            V(lambda: nc.vector.tensor_tensor(out=em[:].rearrange("p t (g e) -> p t g e", g=4), in0=el.rearrange("p t (g e) -> p t g e", g=4),
                                              in1=pen[:].unsqueeze(3).to_broadcast([128, T, 4, 8]), op=ALU.add), ['LG', 'pen'], ['em'])
            V(lambda: nc.vector.tensor_reduce(out=m1[:], in_=em[:], op=ALU.max, axis=AX.X), ['em'], ['m1'])
            V(lambda: nc.vector.tensor_tensor(out=oh1[:], in0=em[:], in1=m1[:].unsqueeze(2).to_broadcast([128, T, 32]), op=ALU.is_equal), ['em', 'm1'], ['oh1'])
            V(lambda: nc.vector.scalar_tensor_tensor(out=em2[:], in0=oh1[:], scalar=-1e30, in1=em[:], op0=ALU.mult, op1=ALU.add), ['oh1', 'em'], ['em2'])
            V(lambda: nc.vector.tensor_reduce(out=m2[:], in_=em2[:], op=ALU.max, axis=AX.X), ['em2'], ['m2'])
            V(lambda: nc.vector.tensor_tensor(out=oh2[:], in0=em2[:], in1=m2[:].unsqueeze(2).to_broadcast([128, T, 32]), op=ALU.is_equal), ['em2', 'm2'], ['oh2'])
            V(lambda: nc.vector.tensor_tensor(out=dm[:], in0=m2[:], in1=m1[:], op=ALU.subtract), ['m1', 'm2'], ['dm'])
            fw.op('act', lambda: nc.scalar.activation(out=dm[:], in_=dm[:], func=AF.Exp), reads=['dm'], writes=['dm'])
            V(lambda: nc.vector.tensor_scalar(out=p1[:], in0=dm[:], scalar1=1.0, scalar2=None, op0=ALU.add), ['dm'], ['p1'])
            V(lambda: nc.vector.reciprocal(out=p1[:], in_=p1[:]), ['p1'], ['p1'])
            V(lambda: nc.vector.tensor_tensor(out=G1[:], in0=gw[:], in1=p1[:], op=ALU.mult), ['gw', 'p1'], ['G1'])
            V(lambda: nc.vector.tensor_tensor(out=dm[:], in0=dm[:], in1=p1[:], op=ALU.mult), ['dm', 'p1'], ['dm'])
            V(lambda: nc.vector.tensor_tensor(out=G2[:], in0=gw[:], in1=dm[:], op=ALU.mult), ['gw', 'dm'], ['G2'])
            V(lambda: nc.vector.tensor_tensor(out=A_[:], in0=oh1[:], in1=oh2[:], op=ALU.add), ['oh1', 'oh2'], ['A'])
            V(lambda: nc.vector.memset(CUMA[:, 0, :], 0.0), [], ['CUMA'])
            for t in range(T):
                V(lambda: nc.vector.tensor_tensor(out=CUMA[:, t + 1, :], in0=CUMA[:, t, :], in1=A_[:, t, :], op=ALU.add), ['CUMA', 'A'], ['CUMA'])
            pc_ = next_ps()
            fw.op('pe', lambda: nc.tensor.matmul(PS[pc_][:, 0:32], lhsT=ONF2[:], rhs=CUMA[:, T, :], start=True, stop=True), reads=['ONF2', 'CUMA'], writes=['ps%d' % pc_])
            V(lambda: nc.vector.tensor_copy(out=cnt[:], in_=PS[pc_][:, 0:32]), ['ps%d' % pc_], ['cnt'])
            CM2 = t8("CM2", [128, 32, 40])
            V(lambda: nc.vector.tensor_tensor(out=CM2[:], in0=cnt[:].unsqueeze(2).to_broadcast([128, 32, 40]), in1=BVAL[:, 0:40].unsqueeze(1).to_broadcast([128, 32, 40]), op=ALU.is_gt),
              ['cnt', 'BVAL'], ['CM2'])
            V(lambda: nc.vector.tensor_reduce(out=padd[:], in_=CM2[:], op=ALU.add, axis=AX.X), ['CM2'], ['padd'])
            V(lambda: nc.vector.tensor_scalar(out=padd[:], in0=padd[:], scalar1=float(BS), scalar2=None, op0=ALU.mult), ['padd'], ['padd'])
            V(lambda: nc.vector.memset(one32[:], 1.0), [], ['one32'])
            V(lambda: nc.vector.tensor_tensor_scan(out=pend[:], data0=one32[:], data1=padd[:], initial=0.0, op0=ALU.mult, op1=ALU.add), ['one32', 'padd'], ['pend'])
            V(lambda: nc.vector.tensor_tensor(out=pst[:], in0=pend[:], in1=padd[:], op=ALU.subtract), ['pend', 'padd'], ['pst'])
            psb = pst[:].unsqueeze(1)
            for g0_ in range(0, T, 16):
                ng_ = min(16, T - g0_)
                pr_ = next_ps()
                pk = 'ps%d' % pr_
                for t in range(g0_, g0_ + ng_):
                    tt_ = t - g0_
                    fw.op('pe', lambda: nc.tensor.matmul(PS[pr_][:, tt_ * 32:(tt_ + 1) * 32], lhsT=TRI[:], rhs=A_[:, t, :], start=True, stop=False), reads=['TRI', 'A'], writes=[pk])
                    fw.op('pe', lambda: nc.tensor.matmul(PS[pr_][:, tt_ * 32:(tt_ + 1) * 32], lhsT=ONF2[:], rhs=CUMA[:, t, :], start=False, stop=True), reads=['ONF2', 'CUMA'], writes=[pk])
                V(lambda: nc.vector.tensor_tensor(out=SLT[:, g0_:g0_ + ng_, :], in0=PS[pr_][:, 0:ng_ * 32].rearrange("p (t e) -> p t e", e=32), in1=psb.to_broadcast([128, ng_, 32]), op=ALU.add),
                  [pk, 'pst'], ['SLT'])
            for (oh, ok, SL) in ((oh1, 'oh1', SL1), (oh2, 'oh2', SL2)):
                V(lambda: nc.vector.tensor_tensor(out=TMP[:], in0=SLT[:], in1=oh[:], op=ALU.mult), ['SLT', ok], ['TMP'])
                V(lambda: nc.vector.tensor_reduce(out=slf[:], in_=TMP[:], op=ALU.add, axis=AX.X), ['TMP'], ['slf'])
                V(lambda: nc.vector.tensor_copy(out=SL[:], in_=slf[:]), ['slf'], ['SL'])
            V(lambda: nc.vector.tensor_tensor(out=CMP[:], in0=pend[:].unsqueeze(1).to_broadcast([128, NB, 32]), in1=BVAL[:, 0:NB].unsqueeze(2).to_broadcast([128, NB, 32]), op=ALU.is_le),
              ['pend', 'BVAL'], ['CMP'])
            V(lambda: nc.vector.tensor_reduce(out=be[:], in_=CMP[:], op=ALU.add, axis=AX.X), ['CMP'], ['be'])
            V(lambda: nc.vector.tensor_scalar(out=be[:], in0=be[:], scalar1=float(NEXP - 1), scalar2=128.0, op0=ALU.min, op1=ALU.mult), ['be'], ['be'])
            V(lambda: nc.vector.tensor_scalar(out=WIDX[:], in0=be[:], scalar1=PIDX[:, 0:1], scalar2=None, op0=ALU.add), ['be', 'PIDX'], ['WIDX'])
            if dbg:
                load('sp', dbg_outs['sl_dbg'][:, 0, :], SL1[:], 'sl_dbg', ['SL'])
                load('sp', dbg_outs['sl_dbg'][:, 1, :], SL2[:], 'sl_dbg', ['SL'])
                load('sp', dbg_outs['g_dbg'][:, 0, :], G1[:], 'g_dbg', ['G1'])
                load('sp', dbg_outs['g_dbg'][:, 1, :], G2[:], 'g_dbg', ['G2'])
                load('sp', dbg_outs['wi_dbg'], WIDX[:], 'wi_dbg', ['WIDX'])
                outs.extend(['sl_dbg', 'g_dbg', 'wi_dbg'])
          fw.barrier()
          if 'moe8' not in phases and dbg:
              sHT.close()
              fw.barrier()
              return
          with contextlib.ExitStack() as s8b:
            HBs = [sb("HBs%d" % i, [128, D], BF16, s8b) for i in range(2)]
            for t in range(T):
                hb_ = HBs[t % 2]
                hk_ = 'HBs%d' % (t % 2)
                load('sp', hb_[:], H2_d[t * 128:(t + 1) * 128, :], hk_, ['H2_d'])
                for SL in (SL1, SL2):
                    fw.dma('pool', lambda: nc.gpsimd.indirect_dma_start(out=XB_d[:, :], out_offset=bass.IndirectOffsetOnAxis(ap=SL[:, t:t + 1], axis=0),
                                                                        in_=hb_[:], in_offset=None), reads=[hk_, 'SL'], writes=['XB_d'])
          fw.barrier()
          with contextlib.ExitStack() as s9:
            PSB = s9.enter_context(nc.psum_tensor("PSB" + sfx, [128, 1024], BF16))
            IDB = sb("IDB", [128, 128], BF16, s9)
            load('pool', IDB[:], ident, 'IDB')
            W1B = [sb("W1B%d" % i, [128, 4096], BF16, s9) for i in range(2)]
            W3B = [sb("W3B%d" % i, [128, 4096], BF16, s9) for i in range(2)]
            W2B = [sb("W2B%d" % i, [128, 4096], BF16, s9) for i in range(2)]
            XBs = [sb("XBs%d" % i, [128, D], BF16, s9) for i in range(2)]
            XTs = sb("XTs", [128, 8, 128], BF16, s9)
            S1 = sb("S1", [128, 512], F32, s9)
            Gb = sb("Gb", [128, 512], BF16, s9)
            GTs = sb("GTs", [128, 4, 128], BF16, s9)
            OBs = [sb("OBs%d" % i, [128, D], F32, s9) for i in range(2)]
            nxb = 0
            for b in range(NB):
                i2 = b % 2
                w1, w3, w2 = W1B[i2], W3B[i2], W2B[i2]
                k1, k3, k2 = 'W1B%d' % i2, 'W3B%d' % i2, 'W2B%d' % i2
                for (wt, wk, srcn) in ((w1, k1, "w1"), (w3, k3, "w3"), (w2, k2, "w2")):
                    for a_, hsfx in enumerate(("a", "b")):
                        fw.dma('pool', lambda: nc.gpsimd.indirect_dma_start(out=wt[:, a_ * 2048:(a_ + 1) * 2048], out_offset=None, in_=wexp[srcn + hsfx][:, :],
                                                                            in_offset=bass.IndirectOffsetOnAxis(ap=WIDX[:, b:b + 1], axis=0)),
                               reads=['WIDX'], writes=[wk])
                for sub_ in range(SUB):
                    r0 = b * BS + sub_ * 128
                    x2i = nxb % 2
                    nxb += 1
                    xb, ob = XBs[x2i], OBs[x2i]
                    kx, ko = 'XBs%d' % x2i, 'OBs%d' % x2i
                    load('sp', xb[:], XB_d[r0:r0 + 128, :], kx, ['XB_d'])
                    for c in range(8):
                        fw.op('pe', lambda: nc.tensor.transpose(out=PSB[:, c * 128:(c + 1) * 128], in_=xb[:, c * 128:(c + 1) * 128], identity=IDB[:]), reads=[kx, 'IDB'], writes=['PSB'])
                    fw.op('act', lambda: nc.scalar.copy(out=XTs[:, 0:4, :], in_=PSB[:, 0:512].rearrange("p (c s) -> p c s", c=4)), reads=['PSB'], writes=['XTs'])
                    fw.op('dve', lambda: nc.vector.tensor_copy(out=XTs[:, 4:8, :], in_=PSB[:, 512:1024].rearrange("p (c s) -> p c s", c=4)), reads=['PSB'], writes=['XTs'])
                    p1i = next_ps(); p3i = next_ps()
                    for (pi_, wt, wk) in ((p1i, w1, k1), (p3i, w3, k3)):
                        for kc in range(8):
                            fw.op('pe', lambda: nc.tensor.matmul(PS[pi_][:, 0:512], lhsT=XTs[:, kc, :], rhs=wt[:, kc * 512:(kc + 1) * 512], start=(kc == 0), stop=(kc == 7)),
                                  reads=['XTs', wk], writes=['ps%d' % pi_])
                    fw.op('act', lambda: nc.scalar.activation(out=S1[:], in_=PS[p1i][:, 0:512], func=AF.Silu), reads=['ps%d' % p1i], writes=['S1'])
                    fw.op('dve', lambda: nc.vector.tensor_tensor(out=Gb[:], in0=PS[p3i][:, 0:512], in1=S1[:], op=ALU.mult), reads=['ps%d' % p3i, 'S1'], writes=['Gb'])
                    for fc in range(4):
                        fw.op('pe', lambda: nc.tensor.transpose(out=PSB[:, fc * 128:(fc + 1) * 128], in_=Gb[:, fc * 128:(fc + 1) * 128], identity=IDB[:]), reads=['Gb', 'IDB'], writes=['PSB'])
                    fw.op('dve', lambda: nc.vector.tensor_copy(out=GTs[:], in_=PSB[:, 0:512].rearrange("p (c s) -> p c s", c=4)), reads=['PSB'], writes=['GTs'])
                    for half in range(2):
                        pi_ = next_ps()
                        for fc in range(4):
                            fw.op('pe', lambda: nc.tensor.matmul(PS[pi_][:, 0:512], lhsT=GTs[:, fc, :], rhs=w2[:, fc * 1024 + half * 512:fc * 1024 + (half + 1) * 512], start=(fc == 0), stop=(fc == 3)),
                                  reads=['GTs', k2], writes=['ps%d' % pi_])
                        if half == 0:
                            fw.op('act', lambda: nc.scalar.copy(out=ob[:, 0:512], in_=PS[pi_][:, 0:512]), reads=['ps%d' % pi_], writes=[ko])
                        else:
                            fw.op('dve', lambda: nc.vector.tensor_copy(out=ob[:, 512:1024], in_=PS[pi_][:, 0:512]), reads=['ps%d' % pi_], writes=[ko])
                    load('sp', OUT_d[r0:r0 + 128, :], ob[:], 'OUT_d', [ko])
          fw.barrier()
          with contextlib.ExitStack() as s10:
            IDN2 = sb("IDN2", [128, 128], F32, s10)
            ONF3 = sb("ONF3", [128, 128], F32, s10)
            pv2 = sb("pvec2", [128, 2, 8], F32, s10)
            load('sp', IDN2[:], ident, 'IDN2')
            load('sp', ONF3[:], ones_f, 'ONF3')
            load('sp', pv2[:, 0, :], ln2g, 'pvec2')
            load('sp', pv2[:, 1, :], ln2b, 'pvec2')
            XT2 = sb("XT2", [128, 8, 512], F32, s10)
            SQ2 = sb("SQ2", [128, 8, 512], F32, s10)
            MEAN2 = sb("MEAN2", [128, 512], F32, s10)
            MSQ2 = sb("MSQ2", [128, 512], F32, s10)
            RSTD2 = sb("RSTD2", [128, 512], F32, s10)
            O1s = [sb("O1s%d" % i, [128, D], F32, s10) for i in range(2)]
            O2s = [sb("O2s%d" % i, [128, D], F32, s10) for i in range(2)]
            ng = 0
            if mode == "moe":
                CTL = [(g_ * 512, 512, True) for g_ in range(16)] + [(SEQ, NCTX, False)]
            else:
                CTL = [(o0_, n_, i_ < 4) for i_, (e0_, o0_, n_) in enumerate(OT)]
            for (o0, n, lat) in CTL:
                mv_ = mlat if lat else mctx
                mk_ = 'mlat' if lat else 'mctx'
                load('sp', XT2[:, :, 0:n], X1_d[:, :, o0:o0 + n], 'XT2', ['X1_d'])
                for sub in range(n // 128):
                    t = (o0 + sub * 128) // 128
                    o1, o2 = O1s[ng % 2], O2s[ng % 2]
                    ko1, ko2 = 'O1s%d' % (ng % 2), 'O2s%d' % (ng % 2)
                    ng += 1
                    for (o_, ko_, SL) in ((o1, ko1, SL1), (o2, ko2, SL2)):
                        fw.dma('pool', lambda: nc.gpsimd.indirect_dma_start(out=o_[:], out_offset=None, in_=OUT_d[:, :],
                                                                            in_offset=bass.IndirectOffsetOnAxis(ap=SL[:, t:t + 1], axis=0)),
                               reads=['OUT_d', 'SL'], writes=[ko_])
                    fw.op('dve', lambda: nc.vector.tensor_scalar(out=o1[:], in0=o1[:], scalar1=G1[:, t:t + 1], scalar2=None, op0=ALU.mult), reads=[ko1, 'G1'], writes=[ko1])
                    fw.op('dve', lambda: nc.vector.scalar_tensor_tensor(out=o1[:], in0=o2[:], scalar=G2[:, t:t + 1], in1=o1[:], op0=ALU.mult, op1=ALU.add), reads=[ko1, ko2, 'G2'], writes=[ko1])
                    for half in range(2):
                        pi_ = next_ps()
                        for cc in range(4):
                            c = half * 4 + cc
                            fw.op('pe', lambda: nc.tensor.transpose(out=PS[pi_][:, cc * 128:(cc + 1) * 128], in_=o1[:, c * 128:(c + 1) * 128], identity=IDN2[:]),
                                  reads=[ko1, 'IDN2'], writes=['ps%d' % pi_])
                        for cc in range(4):
                            c = half * 4 + cc
                            fw.op('act', lambda: nc.scalar.activation(out=SQ2[:, c, sub * 128:(sub + 1) * 128], in_=PS[pi_][:, cc * 128:(cc + 1) * 128], func=AF.Copy, scale=mv_[:, 40 + c:41 + c]),
                                  reads=['ps%d' % pi_, mk_], writes=['SQ2'])
                fw.op('dve', lambda: nc.vector.scalar_tensor_tensor(out=XT2[:, :, 0:n], in0=XT2[:, :, 0:n], scalar=ALPHA, in1=SQ2[:, :, 0:n], op0=ALU.mult, op1=ALU.add),
                      reads=['XT2', 'SQ2'], writes=['XT2'])
                layer_norm(XT2, 'XT2', n, SQ2, 'SQ2', MEAN2, 'MEAN2', MSQ2, 'MSQ2', RSTD2, 'RSTD2', ONF3, 'ONF3',
                           lambda c: pv2[:, 0, c:c + 1], lambda c: pv2[:, 1, c:c + 1], 'pvec2')
                if lat:
                    load('sp', xout[:, :, o0:o0 + n], XT2[:, :, 0:n], 'xout', ['XT2'])
                else:
                    load('sp', cout[:, :, :], XT2[:, :, 0:n], 'cout', ['XT2'])
            outs.extend(['xout', 'cout'])
        sHT.close()
        fw.barrier()


def _fm(a):
    return np.ascontiguousarray(a.T.reshape(8, 128, a.shape[0]).transpose(1, 0, 2))


def _pp(v):
    return np.ascontiguousarray(v.reshape(-1, 128).T)


def _kc(w):
    return np.ascontiguousarray(w.reshape(8, 128, w.shape[1]).transpose(1, 0, 2))


def _partner_cols(nh):
    idx = []
    for h in range(nh):
        for d in range(64):
            half, i = d // 32, d % 32
            p = i + 16 if i < 16 else i - 16
            idx.append(h * 64 + half * 32 + p)
    return np.array(idx)


def prep_layer_weights(inp, l):
    w_in = inp['w_in'][l]
    b_in = inp['b_in'][l]
    o = np.cumsum([0, 512, 512, 512, 3072, 1024, 128, 128, 1024])
    q, u, v, g, z, k, va, xr = [slice(o[i], o[i + 1]) for i in range(8)]
    pq = _partner_cols(8)
    pk = _partner_cols(2)

    def cols(a):
        return np.concatenate([a[..., q], a[..., q][..., pq], a[..., k], a[..., k][..., pk], a[..., u], a[..., v],
                               a[..., va], a[..., g], a[..., z], a[..., xr]], axis=-1)
    W = {}
    W['w_in'] = _kc(cols(w_in))
    W['b_in'] = _pp(cols(b_in))
    W['w_mod'] = _kc(inp['w_mod'][l])
    W['b_mod'] = _pp(inp['b_mod'][l])
    W['convw'] = np.ascontiguousarray(inp['conv_w'][l].reshape(2, 4, 8, 128).transpose(3, 2, 0, 1))
    W['convb'] = np.ascontiguousarray(inp['conv_b'][l].reshape(2, 8, 128).transpose(2, 1, 0))
    W['wrg'] = np.ascontiguousarray(inp['w_rgate'][l].transpose(2, 0, 1, 3))
    W['wig'] = np.ascontiguousarray(inp['w_igate'][l].transpose(2, 0, 1, 3))
    W['brg'] = np.ascontiguousarray(inp['b_rgate'][l].reshape(2, 8, 128).transpose(2, 1, 0))
    W['big'] = np.ascontiguousarray(inp['b_igate'][l].reshape(2, 8, 128).transpose(2, 1, 0))
    W['lam'] = np.ascontiguousarray(inp['lru_lambda'][l].reshape(2, 8, 128).transpose(2, 1, 0))
    bq, bqs, bk, bks = b_in[q], b_in[q][pq], b_in[k], b_in[k][pk]
    W['b_qk'] = np.ascontiguousarray(np.concatenate([bq.reshape(8, 64), bqs.reshape(8, 64), bk.reshape(2, 64), bks.reshape(2, 64)], 0).T)
    W['b_va'] = np.ascontiguousarray(np.broadcast_to(b_in[va][None, :], (128, 128)))
    W['b_v'] = np.ascontiguousarray(np.broadcast_to(b_in[v][None, :], (128, 512)))
    W['sinkx'] = np.ascontiguousarray(np.broadcast_to(inp['attn_sink'][l][None, :, None], (64, 8, 128)))
    W['sgu_g'] = np.ascontiguousarray(np.broadcast_to(inp['sgu_ln_g'][l][None, :], (128, 512)))
    W['sgu_b'] = np.ascontiguousarray(np.broadcast_to(inp['sgu_ln_b'][l][None, :], (128, 512)))
    W['w_spT'] = np.ascontiguousarray(inp['w_spatial'][l].transpose(2, 0, 1))
    W['b_sp'] = np.ascontiguousarray(np.broadcast_to(inp['b_spatial'][l][None, :, :], (128, 4, 128)))
    W['w_pa'] = np.ascontiguousarray(inp['w_proj_a'][l].reshape(4, 128, D).transpose(1, 0, 2))
    W['w_pb'] = np.ascontiguousarray(inp['w_proj_b'][l].reshape(8, 64, D).transpose(1, 0, 2))
    W['w_pc'] = _kc(inp['w_proj_c'][l])
    W['w_o'] = _kc(inp['w_out'][l])
    W['b_o'] = _pp(inp['b_out'][l])
    W['ln1g'] = _pp(inp['ln1_g'][l]); W['ln1b'] = _pp(inp['ln1_b'][l])
    W['ln2g'] = _pp(inp['ln2_g'][l]); W['ln2b'] = _pp(inp['ln2_b'][l])
    W['w_gr'] = _kc(np.concatenate([inp['w_group'][l], inp['w_router'][l]], 1))
    w1r = inp['w1'][l].reshape(NEXP, 8, 128, 512).transpose(0, 2, 1, 3).reshape(NEXP * 128, 4096)
    w3r = inp['w3'][l].reshape(NEXP, 8, 128, 512).transpose(0, 2, 1, 3).reshape(NEXP * 128, 4096)
    w2r = inp['w2'][l].reshape(NEXP, 4, 128, 1024).transpose(0, 2, 1, 3).reshape(NEXP * 128, 4096)
    for nm, arr in (("w1", w1r), ("w3", w3r), ("w2", w2r)):
        W[nm + 'a'] = np.ascontiguousarray(arr[:, :2048])
        W[nm + 'b'] = np.ascontiguousarray(arr[:, 2048:])
    W['b_gr'] = np.ascontiguousarray(np.broadcast_to(np.concatenate([inp['b_group'][l], inp['b_router'][l]])[None, :], (128, 36)))
    return W


def const_inputs(core):
    s = core % NSEG
    pos = np.arange(TLAT) + s * TOWN - HALO
    row = (pos // 64).astype(np.float32)
    colp = (pos % 64).astype(np.float32)
    inv = (10000.0 ** (-np.arange(0, 32, 2, dtype=np.float32) / 32)).astype(np.float32)
    cos = np.zeros((64, TLAT), np.float32)
    sin = np.zeros((64, TLAT), np.float32)
    for d in range(64):
        half, i = d // 32, d % 32
        ang = (row if half == 0 else colp) * inv[i % 16]
        cos[d] = np.cos(ang)
        sin[d] = np.sin(ang) * (-1.0 if i < 16 else 1.0)
    kk = np.arange(128)[:, None]
    qq = np.arange(128)[None, :]
    m = np.zeros((128, 2, 4, 128), np.float32)
    m[:, 0] = (kk >= qq).astype(np.float32)[:, None, :]
    m[:, 1] = (kk <= qq).astype(np.float32)[:, None, :]
    return {'rope_cos': cos, 'rope_sin': sin, 'masks': m, 'ones_in': np.ones((128, 64), np.float32),
            'ident': np.eye(128, dtype=np.float32), 'tri_in': np.triu(np.ones((128, 128), np.float32), 1),
            'bval_in': np.ascontiguousarray(np.broadcast_to((np.arange(NBLK, dtype=np.float32) * 128)[None, :], (128, NBLK))),
            'pidx_in': np.arange(128, dtype=np.float32)[:, None].copy(), 'ones_f': np.ones((128, 128), np.float32)}


def core_inputs(x, ctx, c, c_ctx, core):
    b, s = core // NSEG, core % NSEG
    lo = s * TOWN - HALO
    ext = np.zeros((TLAT, D), np.float32)
    a, e = max(lo, 0), min(lo + TLAT, SEQ)
    ext[a - lo:e - lo] = x[b, a:e]
    m = {'xT': _fm(ext), 'cT': _fm(ctx[b])}
    m['cond'] = np.ascontiguousarray(np.stack([_pp(c[b]), _pp(c_ctx)], -1))
    ed = np.zeros((128, 2), np.float32)
    ed[:, 0] = 1.0 if s > 0 else 0.0
    ed[:, 1] = 1.0 if s < NSEG - 1 else 0.0
    m['edge'] = ed
    return m


def carry_input(ab, core):
    car = np.zeros((128, 8, 2, 3, 2), np.float32)
    car[..., 0] = 1.0
    if ab is None:
        return car
    b, s = core // NSEG, core % NSEG
    fwd = [b * NSEG + j for j in range(0, s)]
    bwd = [b * NSEG + j for j in range(NSEG - 1, s, -1)]
    for k, cc in enumerate(fwd):
        car[:, :, 0, k, :] = ab[cc][:, :, 0, :]
    for k, cc in enumerate(bwd):
        car[:, :, 1, k, :] = ab[cc][:, :, 1, :]
    return car


PRE_NAMES = ['w_mod', 'b_mod', 'w_in', 'b_in', 'convw', 'convb', 'wrg', 'wig', 'brg', 'big', 'lam']
XPAD = SEQ + 2 * HALO


def all_consts():
    pos = np.arange(XPAD) - HALO
    row = (pos // 64).astype(np.float32)
    colp = (pos % 64).astype(np.float32)
    inv = (10000.0 ** (-np.arange(0, 32, 2, dtype=np.float32) / 32)).astype(np.float32)
    cos = np.zeros((64, XPAD), np.float32)
    sin = np.zeros((64, XPAD), np.float32)
    for d in range(64):
        half, i = d // 32, d % 32
        ang = (row if half == 0 else colp) * inv[i % 16]
        cos[d] = np.cos(ang)
        sin[d] = np.sin(ang) * (-1.0 if i < 16 else 1.0)
    C = const_inputs(0)
    C.pop('rope_cos')
    C.pop('rope_sin')
    C['rope_cos_all'] = cos
    C['rope_sin_all'] = sin
    ed = np.ones((NSEG, 128, 2), np.float32)
    ed[0, :, 0] = 0.0
    ed[NSEG - 1, :, 1] = 0.0
    C['edge_all'] = ed
    C['car_id'] = carry_input(None, 0)
    return C


def build_fused(w_shapes, c_shapes):
    nc = bass.Bass("TRN2", target_bir_lowering=False)

    def ein(name, shape, dt=F32):
        return nc.dram_tensor(name, list(shape), dt, kind="ExternalInput").ap()
    X0 = ein("X0", [128, 8, XPAD])
    C0 = ein("C0", [128, 8, NCTX])
    cond = ein("cond", [128, 8, 2])
    EXPN = ("w1a", "w1b", "w3a", "w3b", "w2a", "w2b")
    Wall = {k: ein(k, [DEPTH] + list(shp)) for k, shp in w_shapes.items() if k not in EXPN}
    Wexp = {k: [ein("%s_%d" % (k, l_), list(w_shapes[k])) for l_ in range(DEPTH)] for k in EXPN}
    Call = {k: ein(k, shp) for k, shp in c_shapes.items()}
    yout = nc.dram_tensor("yout", [128, 8, SEQ], F32, kind="ExternalOutput").ap()
    XS = [nc.dram_tensor("XS%d" % i, [128, 8, XPAD], F32).ap() for i in range(2)]
    CS = [nc.dram_tensor("CS%d" % i, [128, 8, NCTX], F32).ap() for i in range(3)]
    AB_d = nc.dram_tensor("AB_d", [NSEG, 128, 8, 2, 2], F32).ap()
    CAR_d = nc.dram_tensor("CAR_d", [128, 8, 2, 3, 2], F32).ap()
    with contextlib.ExitStack() as st0:
        sh = Shared(nc, st0)
        fw = sh.fw
        with contextlib.ExitStack() as sz:
            zt = sz.enter_context(nc.sbuf_tensor("zt", [128, 8, HALO], F32))
            fw.op('dve', lambda: nc.vector.memset(zt[:], 0.0), writes=['zt'])
            for i in range(2):
                fw.dma('sp', lambda: nc.sync.dma_start(out=XS[i][:, :, 0:HALO], in_=zt[:]), reads=['zt'], writes=['XS'])
                fw.dma('sp', lambda: nc.sync.dma_start(out=XS[i][:, :, HALO + SEQ:XPAD], in_=zt[:]), reads=['zt'], writes=['XS'])
            fw.barrier()
        for l in range(DEPTH):
            Xsrc = X0 if l == 0 else XS[(l - 1) % 2]
            Xdst = XS[l % 2]
            Csrc = C0 if l == 0 else CS[(l - 1) % 2]
            Cdst = CS[l % 2]
            for prepass in (True, False):
                for seg in range(NSEG):
                    if not prepass:
                        fwd = list(range(0, seg))
                        bwd = list(range(NSEG - 1, seg, -1))
                        for d_, lst in ((0, fwd), (1, bwd)):
                            for k in range(3):
                                if k < len(lst):
                                    src_ = AB_d[lst[k]][:, :, d_, :]
                                else:
                                    src_ = Call['car_id'][:, :, d_, k, :]
                                fw.dma('sp', lambda: nc.sync.dma_start(out=CAR_d[:, :, d_, k, :], in_=src_), writes=['CAR_d'])
                        fw.barrier()

                    def getin(name, shape, dt=F32, seg=seg, prepass=prepass):
                        if name == 'xT':
                            return Xsrc[:, :, seg * TOWN:seg * TOWN + TLAT]
                        if name == 'cT':
                            return Csrc
                        if name == 'cond':
                            return cond
                        if name == 'edge':
                            return Call['edge_all'][seg]
                        if name == 'car':
                            return Call['car_id'] if prepass else CAR_d
                        if name == 'rope_cos':
                            return Call['rope_cos_all'][:, seg * TOWN:seg * TOWN + TLAT]
                        if name == 'rope_sin':
                            return Call['rope_sin_all'][:, seg * TOWN:seg * TOWN + TLAT]
                        if name in Call:
                            return Call[name]
                        if name in Wexp:
                            return Wexp[name][l]
                        return Wall[name][l]

                    def getout(name, shape, dt=F32, seg=seg):
                        if name == 'ab_out':
                            return AB_d[seg]
                        if name == 'xout':
                            return XS[l % 2][:, :, HALO:HALO + TOWN]
                        if name == 'cout':
                            return CS[2]
                        raise KeyError(name)
                    if prepass:
                        emit_layer(nc, sh, getin, getout, True)
                    else:
                        emit_layer(nc, sh, getin, getout, False, mode="premoe", seg=seg)

            def getout_m(name, shape, dt=F32):
                if name == 'xout':
                    return yout if l == DEPTH - 1 else Xdst[:, :, HALO:HALO + SEQ]
                if name == 'cout':
                    return Cdst
                return CS[2]
            emit_layer(nc, sh, getin, getout_m, False, mode="moe")
        fw.finish([])
        print("fused instructions:", fw.n_instr)
    return nc


_PROG = {}


def _unfm(a):
    return np.ascontiguousarray(a.transpose(2, 1, 0).reshape(a.shape[2], D))


def kernel(**inputs):
    inp = {k: np.asarray(v) for k, v in inputs.items()}
    x = np.ascontiguousarray(inp['x'], dtype=np.float32)
    ctx = np.ascontiguousarray(inp['ctx'], dtype=np.float32)
    c, c_ctx = inp['c'].astype(np.float32), inp['c_ctx'].astype(np.float32)
    Ws = [prep_layer_weights(inp, l) for l in range(DEPTH)]
    EXPN = ("w1a", "w1b", "w3a", "w3b", "w2a", "w2b")
    Wst = {k: np.stack([Ws[l][k] for l in range(DEPTH)], 0) for k in Ws[0] if k not in EXPN}
    Wex = {"%s_%d" % (k, l): Ws[l][k] for k in EXPN for l in range(DEPTH)}
    wshapes = {k: Ws[0][k].shape for k in Ws[0]}
    del Ws
    C = all_consts()
    if 'fused' not in _PROG:
        _PROG['fused'] = build_fused(wshapes, {k: v.shape for k, v in C.items()})
    nc = _PROG['fused']
    cores = list(range(8))
    per_batch = []
    for b in range(2):
        xp = np.zeros((XPAD, D), np.float32)
        xp[HALO:HALO + SEQ] = x[b]
        m = {'X0': _fm(xp), 'C0': _fm(ctx[b]), 'cond': np.ascontiguousarray(np.stack([_pp(c[b]), _pp(c_ctx)], -1))}
        m.update(Wst)
        m.update(Wex)
        m.update(C)
        per_batch.append(m)
    maps = [per_batch[cc // NSEG] for cc in cores]
    res = run_bass_kernel_spmd(nc, maps, core_ids=cores)
    out = np.empty_like(x)
    for b in range(2):
        out[b] = _unfm(np.asarray(res.results[b * NSEG]['yout']))
    return out
```

```python
import contextlib
import numpy as np
import concourse.bass as bass
import concourse.mybir as mybir
from concourse.bass_utils import run_bass_kernel_spmd

F32 = mybir.dt.float32
BF16 = mybir.dt.bfloat16
I32 = mybir.dt.int32
AF = mybir.ActivationFunctionType
ALU = mybir.AluOpType
AX = mybir.AxisListType

D = 1024
DEPTH = 4
SEQ = 8192
NCTX = 256
NSEG = 4
TOWN = 2048
HALO = 128
TLAT = TOWN + 2 * HALO
TEXT = TLAT + NCTX
NOWN = TOWN + NCTX
ALPHA = (2.0 * DEPTH) ** 0.25
LN_EPS = 1e-6
NEXP = 32
NBLK = (NOWN * 2) // 128 + NEXP
C_Q, C_QS, C_K, C_KS, C_U, C_V, C_VA, C_G, C_Z, C_XR = 0, 512, 1024, 1152, 1280, 1792, 2304, 2432, 5504, 6528
NCOL = 7552


class FW:
    NDMA = 24

    def __init__(self, nc, stack):
        self.nc = nc
        self.E = {'pe': nc.tensor, 'act': nc.scalar, 'dve': nc.vector, 'pool': nc.gpsimd, 'sp': nc.sync}
        self.sem = {}
        self.cnt = {}
        for e in ('pe', 'act', 'dve', 'pool'):
            self.sem[e] = stack.enter_context(nc.semaphore('s_' + e))
            self.cnt[e] = 0
        self.dsem = [stack.enter_context(nc.semaphore('d%d' % i)) for i in range(self.NDMA)]
        self.dval = [0] * self.NDMA
        self.dnext = 0
        self.waited = {}
        self.lastw = {}
        self.readers = {}
        self.n_instr = 0

    def _wait(self, e, tok):
        if tok is None:
            return
        kind, sk, val = tok
        k = (e, kind, sk)
        if self.waited.get(k, 0) >= val:
            return
        self.waited[k] = val
        s = self.sem[sk] if kind == 'eng' else self.dsem[sk]
        self.E[e].wait_ge(s, val)

    def _deps(self, e, reads, writes, strict=False):
        for r in reads:
            t = self.lastw.get(r)
            if t is not None:
                if t[0] == 'eng' and t[1] == e and e == 'pe':
                    continue
                self._wait(e, t)
        for w in writes:
            t = self.lastw.get(w)
            if t is not None and (strict or not (t[0] == 'eng' and t[1] == e)):
                self._wait(e, t)
            for t in self.readers.get(w, ()):
                if strict or not (t[0] == 'eng' and t[1] == e):
                    self._wait(e, t)

    def _commit(self, tok, reads, writes):
        for r in reads:
            self.readers.setdefault(r, []).append(tok)
        for w in writes:
            self.lastw[w] = tok
            self.readers[w] = []

    def op(self, e, fn, reads=(), writes=()):
        self._deps(e, reads, writes)
        ins = fn()
        self.cnt[e] += 1
        ins.then_inc(self.sem[e], 1)
        self._commit(('eng', e, self.cnt[e]), reads, writes)
        self.n_instr += 1
        return ins

    def dma(self, q, fn, reads=(), writes=()):
        i = self.dnext
        self.dnext = (self.dnext + 1) % self.NDMA
        if self.dval[i] > 0:
            self._wait(q, ('dma', i, self.dval[i]))
        self._deps(q, reads, writes, strict=True)
        ins = fn()
        self.dval[i] += 16
        ins.then_inc(self.dsem[i], 16)
        self._commit(('dma', i, self.dval[i]), reads, writes)
        self.n_instr += 1
        return ins

    def barrier(self):
        toks = [('eng', e2, self.cnt[e2]) for e2 in ('pe', 'act', 'dve', 'pool') if self.cnt[e2]]
        toks += [('dma', i, self.dval[i]) for i in range(self.NDMA) if self.dval[i]]
        for e in ('pe', 'act', 'dve', 'pool', 'sp'):
            for t in toks:
                if t[0] == 'eng' and t[1] == e:
                    continue
                self._wait(e, t)

    def finish(self, keys, e='sp'):
        for k in keys:
            self._wait(e, self.lastw.get(k))
        for i in range(self.NDMA):
            if self.dval[i]:
                self._wait(e, ('dma', i, self.dval[i]))


class Shared:
    def __init__(self, nc, st):
        self.nc = nc
        self.fw = FW(nc, st)
        self.PS = [st.enter_context(nc.psum_tensor("ps%d" % i, [128, 512], F32)) for i in range(7)]
        self.psi = 0
        self.uid = 0
        self.scr = {}

    def next_ps(self):
        i = self.psi
        self.psi = (i + 1) % 7
        return i

    def scratch(self, name, shape, dt):
        if name not in self.scr:
            self.scr[name] = self.nc.dram_tensor(name, list(shape), dt).ap()
        return self.scr[name]


def build_layer(prepass, dbg=False, phases=("lru", "att", "sgu", "merge", "moe", "moe8")):
    nc = bass.Bass("TRN2", target_bir_lowering=False)

    def din(name, shape, dt=F32):
        return nc.dram_tensor(name, list(shape), dt, kind="ExternalInput").ap()

    def dout(name, shape, dt=F32):
        return nc.dram_tensor(name, list(shape), dt, kind="ExternalOutput").ap()
    with contextlib.ExitStack() as st0:
        sh = Shared(nc, st0)
        emit_layer(nc, sh, din, dout, prepass, dbg, phases)
        sh.fw.finish([])
    return nc


def emit_layer(nc, sh, din, dout, prepass, dbg=False, phases=("lru", "att", "sgu", "merge", "moe", "moe8"), mode="full", seg=0, lru="compute"):
    xT = din("xT", [128, 8, TLAT])
    cT = din("cT", [128, 8, NCTX])
    cond = din("cond", [128, 8, 2])
    w_mod = din("w_mod", [128, 8, 6 * D])
    b_mod = din("b_mod", [128, 48])
    w_in = din("w_in", [128, 8, NCOL])
    b_in = din("b_in", [128, NCOL // 128])
    edge = din("edge", [128, 2])
    convw = din("convw", [128, 8, 2, 4])
    convb = din("convb", [128, 8, 2])
    wrg = din("wrg", [128, 2, 8, 128])
    wig = din("wig", [128, 2, 8, 128])
    brg = din("brg", [128, 8, 2])
    big = din("big", [128, 8, 2])
    lam = din("lam", [128, 8, 2])
    car = din("car", [128, 8, 2, 3, 2])
    ab_out = dout("ab_out", [128, 8, 2, 2])
    dbg_outs = {}
    if dbg:
        dbg_outs["r_dbg"] = dout("r_dbg", [128, 8, NOWN])
        dbg_outs["a_dbg"] = dout("a_dbg", [128, 4, NOWN])
        dbg_outs["b_dbg"] = dout("b_dbg", [64, 8, NOWN])
        dbg_outs["kt_dbg"] = dout("kt_dbg", [64, 2, TEXT])
        dbg_outs["x1_dbg"] = dout("x1_dbg", [128, 8, NOWN])
        dbg_outs["lg_dbg"] = dout("lg_dbg", [128, 18, 36])
        dbg_outs["sl_dbg"] = dout("sl_dbg", [128, 2, 18], I32)
        dbg_outs["g_dbg"] = dout("g_dbg", [128, 2, 18])
        dbg_outs["wi_dbg"] = dout("wi_dbg", [128, NBLK], I32)
        dbg_outs["vt_dbg"] = dout("vt_dbg", [128, 20, 128])
    if not prepass:
        rope_cos = din("rope_cos", [64, TLAT])
        rope_sin = din("rope_sin", [64, TLAT])
        masks = din("masks", [128, 2, 4, 128])
        sinkx = din("sinkx", [64, 8, 128])
        ones_in = din("ones_in", [128, 64])
        b_va = din("b_va", [128, 128])
        b_qk = din("b_qk", [64, 20])
        b_v = din("b_v", [128, 512])
        sgu_g = din("sgu_g", [128, 512])
        sgu_b = din("sgu_b", [128, 512])
        w_spT = din("w_spT", [128, 4, 128])
        b_sp = din("b_sp", [128, 4, 128])
        w_pa = din("w_pa", [128, 4, D])
        w_pb = din("w_pb", [64, 8, D])
        w_pc = din("w_pc", [128, 8, D])
        w_o = din("w_o", [128, 8, D])
        b_o = din("b_o", [128, 8])
        ln1g = din("ln1g", [128, 8])
        ln1b = din("ln1b", [128, 8])
        ln2g = din("ln2g", [128, 8])
        ln2b = din("ln2b", [128, 8])
        w_gr = din("w_gr", [128, 8, 36])
        b_gr = din("b_gr", [128, 36])
        ident = din("ident", [128, 128])
        ones_f = din("ones_f", [128, 128])
        tri_in = din("tri_in", [128, 128])
        bval_in = din("bval_in", [128, NBLK])
        pidx_in = din("pidx_in", [128, 1])
        wexp = {}
        for nm in ("w1a", "w1b", "w3a", "w3b", "w2a", "w2b"):
            wexp[nm] = din(nm, [NEXP * 128, 2048])
        xout = dout("xout", [128, 8, TOWN])
        cout = dout("cout", [128, 8, NCTX])

    sh.uid += 1
    sfx = "_%d" % sh.uid
    if mode == "premoe":
        phases = ("lru", "att", "sgu", "merge")
    elif mode == "moe":
        phases = ("moe", "moe8")
    GL = mode in ("premoe", "moe")
    T = 66 if mode == "moe" else 18
    SUB = 4 if mode == "moe" else 1
    BS = 128 * SUB
    NB = (33 + NEXP) if mode == "moe" else NBLK
    with contextlib.ExitStack() as st:
        fw = sh.fw

        def sb(name, shape, dt=F32, stack=st):
            return stack.enter_context(nc.sbuf_tensor(name + sfx, list(shape), dt))

        def ps(name, shape, dt=F32, stack=st):
            return stack.enter_context(nc.psum_tensor(name + sfx, list(shape), dt))

        PS = sh.PS
        next_ps = sh.next_ps

        def load(q, dst, src, wkey, rkeys=()):
            eng = {'sp': nc.sync, 'act': nc.scalar, 'pool': nc.gpsimd}[q]
            return fw.dma(q, lambda: eng.dma_start(out=dst, in_=src), reads=list(rkeys), writes=[wkey])

        condt = sb("condt", [128, 8, 2])
        condb = sb("condb", [128, 8, 2], BF16)
        sig = sb("sig", [128, 8, 2])
        bmod = sb("bmod", [128, 48])
        mlat = sb("mlat", [128, 48])
        mctx = sb("mctx", [128, 48])
        bint = sb("bint", [128, NCOL // 128])
        edget = sb("edget", [128, 2])
        cw = sb("cw", [128, 8, 2, 4])
        cb = sb("cb", [128, 8, 2])
        brt = sb("brt", [128, 8, 2])
        bit_ = sb("bit", [128, 8, 2])
        lamt = sb("lamt", [128, 8, 2])
        negc = sb("negc", [128, 8, 2])
        negc2 = sb("negc2", [128, 8, 2])
        cart = sb("cart", [128, 8, 2, 3, 2])
        abt = sb("abt", [128, 8, 2, 2])
        load('sp', condt[:], cond, 'condt')
        load('sp', bmod[:], b_mod, 'bmod')
        load('sp', bint[:], b_in, 'bint')
        load('sp', edget[:], edge, 'edget')
        load('sp', cw[:], convw, 'cw')
        load('sp', cb[:], convb, 'cb')
        load('sp', brt[:], brg, 'brt')
        load('sp', bit_[:], big, 'bit')
        load('sp', lamt[:], lam, 'lamt')
        load('sp', cart[:], car, 'cart')

        fw.op('act', lambda: nc.scalar.activation(out=sig[:], in_=condt[:], func=AF.Sigmoid), reads=['condt'], writes=['sig'])
        fw.op('dve', lambda: nc.vector.tensor_tensor(out=condb[:], in0=condt[:], in1=sig[:], op=ALU.mult), reads=['condt', 'sig'], writes=['condb'])
        fw.op('act', lambda: nc.scalar.activation(out=negc[:], in_=lamt[:], func=AF.Exp, scale=-1.0), reads=['lamt'], writes=['negc'])
        fw.op('act', lambda: nc.scalar.activation(out=negc[:], in_=negc[:], func=AF.Ln, bias=1.0), reads=['negc'], writes=['negc'])
        fw.op('dve', lambda: nc.vector.tensor_scalar(out=negc2[:], in0=negc[:], scalar1=-16.0, scalar2=None, op0=ALU.mult), reads=['negc'], writes=['negc2'])
        fw.op('dve', lambda: nc.vector.tensor_scalar(out=negc[:], in0=negc[:], scalar1=-8.0, scalar2=None, op0=ALU.mult), reads=['negc'], writes=['negc'])

        with contextlib.ExitStack() as s1:
            wm = [sb("wm%d" % i, [128, 8, 512], BF16, s1) for i in range(2)]
            pmi = next_ps()
            pm = PS[pmi]
            pmk = 'ps%d' % pmi
            for g in range(12):
                wbuf = wm[g % 2]
                load('pool', wbuf[:], w_mod[:, :, g * 512:(g + 1) * 512], 'wm%d' % (g % 2))
                for jj in range(4):
                    j = g * 4 + jj
                    for kc in range(8):
                        fw.op('pe', lambda: nc.tensor.matmul(pm[:, 2 * j:2 * j + 2], lhsT=wbuf[:, kc, jj * 128:(jj + 1) * 128],
                                                              rhs=condb[:, kc, :], start=(kc == 0), stop=(kc == 7)),
                              reads=['wm%d' % (g % 2), 'condb'], writes=[pmk])
            pmv = pm[:, 0:96].rearrange("p (j t) -> p j t", t=2)
            fw.op('dve', lambda: nc.vector.tensor_tensor(out=mlat[:], in0=pmv[:, :, 0], in1=bmod[:], op=ALU.add), reads=[pmk, 'bmod'], writes=['mlat'])
            fw.op('dve', lambda: nc.vector.tensor_tensor(out=mctx[:], in0=pmv[:, :, 1], in1=bmod[:], op=ALU.add), reads=[pmk, 'bmod'], writes=['mctx'])
        fw.barrier()
        m1p = sb("m1p", [128, 2, 8])
        m4p = sb("m4p", [128, 2, 8])
        for t_, mv in ((0, mlat), (1, mctx)):
            fw.op('dve', lambda: nc.vector.tensor_scalar(out=m1p[:, t_, :], in0=mv[:, 8:16], scalar1=1.0, scalar2=None, op0=ALU.add), reads=['mlat', 'mctx'], writes=['m1p'])
            fw.op('dve', lambda: nc.vector.tensor_scalar(out=m4p[:, t_, :], in0=mv[:, 32:40], scalar1=1.0, scalar2=None, op0=ALU.add), reads=['mlat', 'mctx'], writes=['m4p'])

        sY = st.enter_context(contextlib.ExitStack())
        yT = sY.enter_context(nc.sbuf_tensor("yT" + sfx, [128, 8, NOWN], BF16, side='right'))
        sHT = contextlib.ExitStack()
        hT = sHT.enter_context(nc.sbuf_tensor("hT" + sfx, [128, 8, TEXT], BF16, side='right'))
        with contextlib.ExitStack() as s2:
            xs = [sb("xs%d" % i, [128, TLAT], F32, s2) for i in range(2)]
            for c in (range(8) if mode != "moe" else ()):
                xb_ = xs[c % 2]
                k = 'xs%d' % (c % 2)
                load('sp', xb_[:], xT[:, c, :], k)
                fw.op('act', lambda: nc.scalar.activation(out=hT[:, c, 0:TLAT], in_=xb_[:], func=AF.Identity,
                                                          scale=m1p[:, 0, c:c + 1], bias=mlat[:, c:c + 1]),
                      reads=[k, 'm1p', 'mlat'], writes=['hT%d' % c])
                load('sp', xb_[:, 0:NCTX], cT[:, c, :], k)
                fw.op('act', lambda: nc.scalar.activation(out=hT[:, c, TLAT:TEXT], in_=xb_[:, 0:NCTX], func=AF.Identity,
                                                          scale=m1p[:, 1, c:c + 1], bias=mctx[:, c:c + 1]),
                      reads=[k, 'm1p', 'mctx'], writes=['hT%d' % c])
        fw.barrier()
        hkeys = ['hT%d' % c for c in range(8)]

        def inproj(col0, ncols, tok0, ntok, wkey, wtile, evac):
            if wtile is not None:
                wsl = lambda kc: wtile[:, kc, col0:col0 + ncols]
            else:
                wsl = col0
            return inproj2(wsl, ncols, tok0, ntok, wkey, evac)

        def inproj2(wsl, ncols, tok0, ntok, wkey, evac):
            pi = next_ps()
            pt = PS[pi]
            for kc in range(8):
                fw.op('pe', lambda: nc.tensor.matmul(pt[0:ncols, 0:ntok], lhsT=wsl(kc), rhs=hT[:, kc, tok0:tok0 + ntok],
                                                      start=(kc == 0), stop=(kc == 7)),
                      reads=[wkey, 'hT%d' % kc], writes=['ps%d' % pi])
            evac(pt, 'ps%d' % pi)

        R_d = sh.scratch("R_d", [128, 8, NOWN], BF16)
        A_d = sh.scratch("A_d", [128, 4, NOWN], BF16)
        B_d = sh.scratch("B_d", [64, 8, NOWN], BF16)
        if "lru" in phases:
          with contextlib.ExitStack() as s3:
            rst = sb("rst", [128, NOWN], BF16, s3)
            wxr = [sb("wxr%d" % i, [128, 8, 128], BF16, s3) for i in range(2)]
            wz = [sb("wz%d" % i, [128, 8, 128], BF16, s3) for i in range(2)]
            wr_t = sb("wr_t", [128, 2, 8, 128], BF16, s3)
            wi_t = sb("wi_t", [128, 2, 8, 128], BF16, s3)
            load('pool', wr_t[:], wrg, 'wr_t')
            load('pool', wi_t[:], wig, 'wi_t')
            XRL = sb("XRL", [128, TLAT if lru != "load" else 8], F32, s3)
            XRC = sb("XRC", [128, NCTX + 6], F32, s3)
            GZ = sb("GZ", [128, NOWN], F32, s3)
            LAU = sh.scratch("LAU", [NSEG, 8, 2, 2, 128, NOWN], F32) if lru != "compute" else None
            if lru == "load":
                TT = TB = RG = [None, None]
                IG = [sb("IG%d" % i, [128, NOWN], F32, s3) for i in range(2)]
                AA = [sb("AA%d" % i, [128, NOWN], F32, s3) for i in range(2)]
            else:
                TT = [sb("TT0", [128, NOWN], F32, s3)] * 2
                TB = [sb("TB0", [128, NOWN], BF16, s3)] * 2
                RG = [sb("RG0", [128, NOWN], F32, s3)] * 2
                IG = [sb("IG0", [128, NOWN], F32, s3)] * 2
                AA = [sb("AA0", [128, NOWN], F32, s3)] * 2
            HH = [sb("HH%d" % i, [128, NOWN], F32, s3) for i in range(2)]
            sm = sb("sm", [128, 16], F32, s3)
            fw.op('pool', lambda: nc.gpsimd.memset(XRC[:], 0.0), writes=['XRC'])
            for j in range(8):
                wx = wxr[j % 2]
                wzz = wz[j % 2]
                kx = 'wxr%d' % (j % 2)
                kz = 'wz%d' % (j % 2)
                load('pool', wx[:], w_in[:, :, C_XR + j * 128:C_XR + (j + 1) * 128], kx)
                load('pool', wzz[:], w_in[:, :, C_Z + j * 128:C_Z + (j + 1) * 128], kz)
                bx = bint[:, (C_XR // 128) + j:(C_XR // 128) + j + 1]
                bz = bint[:, (C_Z // 128) + j:(C_Z // 128) + j + 1]
                for ti in (range(5) if lru != "load" else ()):
                    def ev(pt, pk, ti=ti):
                        if ti < 4:
                            fw.op('act', lambda: nc.scalar.activation(out=XRL[:, ti * 512:(ti + 1) * 512], in_=pt[:, 0:512], func=AF.Identity, bias=bx),
                                  reads=[pk, 'bint'], writes=['XRL'])
                        else:
                            fw.op('act', lambda: nc.scalar.activation(out=XRL[:, 2048:2304], in_=pt[:, 0:256], func=AF.Identity, bias=bx),
                                  reads=[pk, 'bint'], writes=['XRL'])
                            fw.op('act', lambda: nc.scalar.activation(out=XRC[:, 3:3 + NCTX], in_=pt[:, 256:512], func=AF.Identity, bias=bx),
                                  reads=[pk, 'bint'], writes=['XRC'])
                    inproj(0, 128, ti * 512, 512, kx, wx, ev)
                if lru != "load":
                  fw.op('dve', lambda: nc.vector.tensor_scalar(out=XRL[:, 0:HALO], in0=XRL[:, 0:HALO], scalar1=edget[:, 0:1], scalar2=None, op0=ALU.mult),
                      reads=['XRL', 'edget'], writes=['XRL'])
                if lru != "load":
                  fw.op('dve', lambda: nc.vector.tensor_scalar(out=XRL[:, HALO + TOWN:TLAT], in0=XRL[:, HALO + TOWN:TLAT], scalar1=edget[:, 1:2], scalar2=None, op0=ALU.mult),
                      reads=['XRL', 'edget'], writes=['XRL'])
                for ti in range(5):
                    def evz(pt, pk, ti=ti):
                        if ti < 4:
                            fw.op('act', lambda: nc.scalar.activation(out=GZ[:, ti * 512:(ti + 1) * 512], in_=pt[:, 0:512], func=AF.Gelu, bias=bz),
                                  reads=[pk, 'bint'], writes=['GZ'])
                        else:
                            fw.op('act', lambda: nc.scalar.activation(out=GZ[:, 2048:2304], in_=pt[:, 0:256], func=AF.Gelu, bias=bz),
                                  reads=[pk, 'bint'], writes=['GZ'])
                    if ti < 4:
                        inproj(0, 128, HALO + ti * 512, 512, kz, wzz, evz)
                    else:
                        inproj(0, 128, TLAT, 256, kz, wzz, evz)
                for d_ in range(2):
                    T_ = TT[d_]; Tb = TB[d_]; Rg = RG[d_]; Ig = IG[d_]; A_ = AA[d_]; H_ = HH[d_]
                    kT, kTb, kR, kI, kA, kH = 'TT', 'TB', 'RG', 'IG', 'AA', 'HH%d' % d_
                    if lru == "load":
                        kI, kA = 'IG%d' % d_, 'AA%d' % d_
                        load('sp', A_[:], LAU[seg, j, d_, 0], kA)
                        load('sp', Ig[:], LAU[seg, j, d_, 1], kI)
                    else:
                        for (dst0, n, src, s0, sk) in ((0, TOWN, XRL, HALO, 'XRL'), (TOWN, NCTX, XRC, 3, 'XRC')):
                            for tap in range(4):
                                off = s0 + (tap - 3 if d_ == 0 else tap)
                                wcol = cw[:, j, d_, tap:tap + 1]
                                if tap == 0:
                                    fw.op('dve', lambda: nc.vector.tensor_scalar(out=T_[:, dst0:dst0 + n], in0=src[:, off:off + n], scalar1=wcol,
                                                                                 scalar2=cb[:, j, d_:d_ + 1], op0=ALU.mult, op1=ALU.add),
                                          reads=[sk, 'cw', 'cb'], writes=[kT])
                                else:
                                    fw.op('dve', lambda: nc.vector.scalar_tensor_tensor(out=T_[:, dst0:dst0 + n], in0=src[:, off:off + n], scalar=wcol,
                                                                                        in1=T_[:, dst0:dst0 + n], op0=ALU.mult, op1=ALU.add),
                                          reads=[sk, 'cw', kT], writes=[kT])
                        fw.op('pool', lambda: nc.gpsimd.tensor_copy(out=Tb[:], in_=T_[:]), reads=[kT], writes=[kTb])
                        for ti in range(5):
                            t0 = ti * 512
                            n = 512 if ti < 4 else 256
                            for (wt_, wk, bt_, bk, dst, dk) in ((wr_t, 'wr_t', brt, 'brt', Rg, kR), (wi_t, 'wi_t', bit_, 'bit', Ig, kI)):
                                pi = next_ps()
                                pt = PS[pi]
                                fw.op('pe', lambda: nc.tensor.matmul(pt[:, 0:n], lhsT=wt_[:, d_, j, :], rhs=Tb[:, t0:t0 + n], start=True, stop=True),
                                      reads=[wk, kTb], writes=['ps%d' % pi])
                                fw.op('act', lambda: nc.scalar.activation(out=dst[:, t0:t0 + n], in_=pt[:, 0:n], func=AF.Sigmoid, bias=bt_[:, j, d_:d_ + 1]),
                                      reads=['ps%d' % pi, bk], writes=[dk])
                        fw.op('act', lambda: nc.scalar.activation(out=A_[:], in_=Rg[:], func=AF.Exp, scale=negc[:, j, d_:d_ + 1]), reads=[kR, 'negc'], writes=[kA])
                        fw.op('act', lambda: nc.scalar.activation(out=H_[:], in_=Rg[:], func=AF.Exp, scale=negc2[:, j, d_:d_ + 1]), reads=[kR, 'negc2'], writes=[kH])
                        fw.op('act', lambda: nc.scalar.activation(out=H_[:], in_=H_[:], func=AF.Sqrt, scale=-1.0, bias=1.0), reads=[kH], writes=[kH])
                        fw.op('pool', lambda: nc.gpsimd.tensor_tensor(out=Ig[:], in0=Ig[:], in1=T_[:], op=ALU.mult), reads=[kI, kT], writes=[kI])
                        fw.op('pool', lambda: nc.gpsimd.tensor_tensor(out=Ig[:], in0=Ig[:], in1=H_[:], op=ALU.mult), reads=[kI, kH], writes=[kI])
                        rs = sm[:, 8 + d_:9 + d_]
                        fw.op('dve', lambda: nc.vector.reduce_sum(out=rs, in_=Rg[:, 0:TOWN], axis=AX.X), reads=[kR], writes=['sm_rs%d' % d_])
                        fw.op('act', lambda: nc.scalar.activation(out=abt[:, j, d_, 0:1], in_=rs, func=AF.Exp, scale=negc[:, j, d_:d_ + 1]),
                              reads=['sm_rs%d' % d_, 'negc'], writes=['abt'])
                        if lru == "store":
                            load('sp', LAU[seg, j, d_, 0], A_[:], 'LAU', [kA])
                            load('sp', LAU[seg, j, d_, 1], Ig[:], 'LAU', [kI])
                    if d_ == 0:
                        a_c, u_c, h_c = A_[:, TOWN:NOWN], Ig[:, TOWN:NOWN], H_[:, TOWN:NOWN]
                        a_l, u_l, h_l = A_[:, 0:TOWN], Ig[:, 0:TOWN], H_[:, 0:TOWN]
                        hc_fin = H_[:, NOWN - 1:NOWN]
                        hl_fin = H_[:, TOWN - 1:TOWN]
                    else:
                        a_c, u_c, h_c = A_[:, TOWN:NOWN][:, ::-1], Ig[:, TOWN:NOWN][:, ::-1], H_[:, TOWN:NOWN][:, ::-1]
                        a_l, u_l, h_l = A_[:, 0:TOWN][:, ::-1], Ig[:, 0:TOWN][:, ::-1], H_[:, 0:TOWN][:, ::-1]
                        hc_fin = H_[:, TOWN:TOWN + 1]
                        hl_fin = H_[:, 0:1]
                    fw.op('dve', lambda: nc.vector.tensor_tensor_scan(out=h_c, data0=a_c, data1=u_c, initial=0.0, op0=ALU.mult, op1=ALU.add),
                          reads=[kA, kI], writes=[kH])
                    hin = sm[:, d_ * 4:d_ * 4 + 1]
                    fw.op('dve', lambda: nc.vector.tensor_copy(out=hin, in_=hc_fin), reads=[kH], writes=['hin%d' % d_])
                    for k3 in range(3):
                        fw.op('dve', lambda: nc.vector.scalar_tensor_tensor(out=hin, in0=hin, scalar=cart[:, j, d_, k3, 0:1], in1=cart[:, j, d_, k3, 1:2],
                                                                            op0=ALU.mult, op1=ALU.add),
                              reads=['hin%d' % d_, 'cart'], writes=['hin%d' % d_])
                    fw.op('dve', lambda: nc.vector.tensor_tensor_scan(out=h_l, data0=a_l, data1=u_l, initial=hin, op0=ALU.mult, op1=ALU.add),
                          reads=[kA, kI, 'hin%d' % d_], writes=[kH])
                    tmp = sm[:, d_ * 4 + 1:d_ * 4 + 2]
                    if lru != "load":
                        fw.op('dve', lambda: nc.vector.tensor_tensor(out=tmp, in0=abt[:, j, d_, 0:1], in1=hin, op=ALU.mult), reads=['abt', 'hin%d' % d_], writes=['tmp%d' % d_])
                        fw.op('dve', lambda: nc.vector.tensor_tensor(out=abt[:, j, d_, 1:2], in0=hl_fin, in1=tmp, op=ALU.subtract), reads=[kH, 'tmp%d' % d_], writes=['abt'])
                fw.op('pool', lambda: nc.gpsimd.tensor_tensor(out=HH[0][:], in0=HH[0][:], in1=HH[1][:], op=ALU.add), reads=['HH0', 'HH1'], writes=['HH0'])
                fw.op('pool', lambda: nc.gpsimd.tensor_tensor(out=rst[:], in0=HH[0][:], in1=GZ[:], op=ALU.mult), reads=['HH0', 'GZ'], writes=['rst'])
                load('sp', R_d[:, j, :], rst[:], 'R_d', ['rst'])
          fw.barrier()
          if lru != "load":
              load('sp', ab_out, abt[:], 'ab_out', ['abt'])
        outs = ['ab_out']
        if prepass:
            sHT.close()
            fw.barrier()
            return
        if "att" in phases:
          with contextlib.ExitStack() as s4:
            KT = sb("KT", [64, 2, TEXT], BF16, s4)
            Vtok = sb("Vtok", [128, 20, 128], BF16, s4)
            COS = sb("COS", [64, TLAT], F32, s4)
            SIN = sb("SIN", [64, TLAT], F32, s4)
            MSK = sb("MSK", [128, 2, 4, 128], F32, s4)
            ESX = sb("ESX", [64, 8, 128], F32, s4)
            ONES = sb("ONES", [128, 64], BF16, s4)
            BVA = sb("BVA", [128, 128], F32, s4)
            bqk = sb("bqk", [64, 20], F32, s4)
            wk_t = sb("wk_t", [128, 8, 256], BF16, s4)
            wva_t = sb("wva_t", [128, 8, 128], BF16, s4)
            wq_t = sb("wq_t", [128, 8, 1024], BF16, s4)
            load('sp', COS[:], rope_cos, 'COS')
            load('sp', SIN[:], rope_sin, 'SIN')
            load('sp', MSK[:], masks, 'MSK')
            load('sp', ESX[:], sinkx, 'ESX')
            load('sp', BVA[:], b_va, 'BVA')
            load('sp', bqk[:], b_qk, 'bqk')
            load('pool', ONES[:], ones_in, 'ONES')
            load('pool', wk_t[:], w_in[:, :, C_K:C_K + 256], 'wk_t')
            load('pool', wva_t[:], w_in[:, :, C_VA:C_VA + 128], 'wva_t')
            load('pool', wq_t[:], w_in[:, :, C_Q:C_Q + 1024], 'wq_t')
            fw.op('act', lambda: nc.scalar.activation(out=ESX[:], in_=ESX[:], func=AF.Exp), reads=['ESX'], writes=['ESX'])
            QF = [sb("QF%d" % i, [64, 512], F32, s4) for i in range(2)]
            QS = [sb("QS%d" % i, [64, 512], F32, s4) for i in range(2)]
            rot = [0]

            def roped(wt, wk, c_main, c_sw, bcol_main, bcol_sw, e0, n, dst, dkey, use_rope):
                i = rot[0] % 2
                rot[0] += 1
                qf, qs = QF[i], QS[i]

                def ev1(pt, pk):
                    fw.op('act', lambda: nc.scalar.activation(out=qf[:, 0:n], in_=pt[0:64, 0:n], func=AF.Identity, bias=bqk[:, bcol_main:bcol_main + 1]),
                          reads=[pk, 'bqk'], writes=['QF%d' % i])
                inproj2(lambda kc: wt[:, kc, c_main:c_main + 64], 64, e0, n, wk, ev1)
                if not use_rope:
                    fw.op('dve', lambda: nc.vector.tensor_copy(out=dst, in_=qf[:, 0:n]), reads=['QF%d' % i], writes=[dkey])
                    return

                def ev2(pt, pk):
                    fw.op('act', lambda: nc.scalar.activation(out=qs[:, 0:n], in_=pt[0:64, 0:n], func=AF.Identity, bias=bqk[:, bcol_sw:bcol_sw + 1]),
                          reads=[pk, 'bqk'], writes=['QS%d' % i])
                inproj2(lambda kc: wt[:, kc, c_sw:c_sw + 64], 64, e0, n, wk, ev2)
                fw.op('dve', lambda: nc.vector.tensor_tensor(out=qf[:, 0:n], in0=qf[:, 0:n], in1=COS[:, e0:e0 + n], op=ALU.mult), reads=['QF%d' % i, 'COS'], writes=['QF%d' % i])
                fw.op('pool', lambda: nc.gpsimd.tensor_tensor(out=qs[:, 0:n], in0=qs[:, 0:n], in1=SIN[:, e0:e0 + n], op=ALU.mult), reads=['QS%d' % i, 'SIN'], writes=['QS%d' % i])
                fw.op('dve', lambda: nc.vector.tensor_tensor(out=dst, in0=qf[:, 0:n], in1=qs[:, 0:n], op=ALU.add), reads=['QF%d' % i, 'QS%d' % i], writes=[dkey])

            for kv in range(2):
                for ti in range(5):
                    if ti < 4:
                        roped(wk_t, 'wk_t', kv * 64, 128 + kv * 64, 16 + kv, 18 + kv, ti * 512, 512, KT[:, kv, ti * 512:(ti + 1) * 512], 'KT', True)
                    else:
                        roped(wk_t, 'wk_t', kv * 64, 128 + kv * 64, 16 + kv, 18 + kv, 2048, 256, KT[:, kv, 2048:2304], 'KT', True)
                        roped(wk_t, 'wk_t', kv * 64, 128 + kv * 64, 16 + kv, 18 + kv, TLAT, 256, KT[:, kv, TLAT:TEXT], 'KT', False)
            for tt in range(20):
                pi = next_ps()
                pt = PS[pi]
                for kc in range(8):
                    fw.op('pe', lambda: nc.tensor.matmul(pt[:, 0:128], lhsT=hT[:, kc, tt * 128:(tt + 1) * 128], rhs=wva_t[:, kc, :], start=(kc == 0), stop=(kc == 7)),
                          reads=['wva_t', 'hT%d' % kc], writes=['ps%d' % pi])
                fw.op('dve', lambda: nc.vector.tensor_tensor(out=Vtok[:, tt, :], in0=pt[:, 0:128], in1=BVA[:], op=ALU.add), reads=['ps%d' % pi, 'BVA'], writes=['Vtok'])
            QT = sb("QT", [64, 8, 512], BF16, s4)
            PTS = [sb("PT%d" % i, [128, 4, 128], BF16, s4) for i in range(3)]
            DEN = sb("DEN", [64, 4, 128], F32, s4)
            BTS = [sb("BTS%d" % i, [64, 8, 128], BF16, s4) for i in range(2)]
            pti = [0]
            nqb = 0
            for ti in range(5):
                lat = ti < 4
                n = 512 if lat else 256
                e0 = HALO + ti * 512 if lat else TLAT
                for h in range(8):
                    roped(wq_t, 'wq_t', h * 64, 512 + h * 64, h, 8 + h, e0, n, QT[:, h, 0:n], 'QT', lat)
                for qb in range(n // 128):
                    q0 = qb * 128
                    if lat:
                        bi = ti * 4 + qb
                        ktiles = [(bi, 'p'), (bi + 1, 'o'), (bi + 2, 'n'), (18, 'c'), (19, 'c')]
                        own0 = bi * 128
                    else:
                        bi = -1
                        ktiles = [(18, 'c'), (19, 'c')]
                        own0 = TOWN + qb * 128
                    bts = BTS[nqb % 2]
                    bk = 'BTS%d' % (nqb % 2)
                    nqb += 1
                    for kv in range(2):
                        po_i = next_ps(); pd_i = next_ps()
                        po, pd = PS[po_i], PS[pd_i]
                        for ki, (tt, kind) in enumerate(ktiles):
                            psi_ = next_ps()
                            pss = PS[psi_]
                            fw.op('pe', lambda: nc.tensor.matmul(pss[:, 0:512], lhsT=KT[:, kv, tt * 128:(tt + 1) * 128],
                                                                  rhs=QT[:, 4 * kv:4 * kv + 4, q0:q0 + 128], start=True, stop=True),
                                  reads=['KT', 'QT'], writes=['ps%d' % psi_])
                            pi3 = pti[0] % 3
                            pti[0] += 1
                            P_ = PTS[pi3]
                            pk3 = 'PT%d' % pi3
                            fw.op('act', lambda: nc.scalar.activation(out=P_[:].rearrange("p g q -> p (g q)"), in_=pss[:, 0:512], func=AF.Exp, scale=0.125),
                                  reads=['ps%d' % psi_], writes=[pk3])
                            if kind in ('p', 'n'):
                                mi = 0 if kind == 'p' else 1
                                if (kind == 'p' and bi == 0) or (kind == 'n' and bi == 15):
                                    fw.op('dve', lambda: nc.vector.scalar_tensor_tensor(out=P_[:], in0=P_[:], scalar=edget[:, mi:mi + 1], in1=MSK[:, mi, :, :], op0=ALU.mult, op1=ALU.mult),
                                          reads=[pk3, 'MSK', 'edget'], writes=[pk3])
                                else:
                                    fw.op('dve', lambda: nc.vector.tensor_tensor(out=P_[:], in0=P_[:], in1=MSK[:, mi, :, :], op=ALU.mult), reads=[pk3, 'MSK'], writes=[pk3])
                            last = ki == len(ktiles) - 1
                            fw.op('pe', lambda: nc.tensor.matmul(po[0:64, 0:512], lhsT=Vtok[:, tt, kv * 64:(kv + 1) * 64], rhs=P_[:].rearrange("p g q -> p (g q)"), start=(ki == 0), stop=last),
                                  reads=['Vtok', pk3], writes=['ps%d' % po_i])
                            fw.op('pe', lambda: nc.tensor.matmul(pd[0:64, 0:512], lhsT=ONES[:], rhs=P_[:].rearrange("p g q -> p (g q)"), start=(ki == 0), stop=last),
                                  reads=['ONES', pk3], writes=['ps%d' % pd_i])
                        fw.op('dve', lambda: nc.vector.tensor_tensor(out=DEN[:], in0=pd[0:64, 0:512].rearrange("p (g q) -> p g q", g=4), in1=ESX[:, 4 * kv:4 * kv + 4, :], op=ALU.add),
                              reads=['ps%d' % pd_i, 'ESX'], writes=['DEN'])
                        fw.op('dve', lambda: nc.vector.reciprocal(out=DEN[:], in_=DEN[:]), reads=['DEN'], writes=['DEN'])
                        fw.op('dve', lambda: nc.vector.tensor_tensor(out=bts[:, 4 * kv:4 * kv + 4, :], in0=po[0:64, 0:512].rearrange("p (g q) -> p g q", g=4), in1=DEN[:], op=ALU.mult),
                              reads=['ps%d' % po_i, 'DEN'], writes=[bk])
                    load('sp', B_d[:, :, own0:own0 + 128], bts[:], 'B_d', [bk])

            if dbg:
                ktf = sb("ktf", [64, 2, TEXT], F32, s4)
                vtf = sb("vtf", [128, 20, 128], F32, s4)
                fw.op('dve', lambda: nc.vector.tensor_copy(out=ktf[:], in_=KT[:]), reads=['KT'], writes=['ktf'])
                fw.op('dve', lambda: nc.vector.tensor_copy(out=vtf[:], in_=Vtok[:]), reads=['Vtok'], writes=['vtf'])
                load('sp', dbg_outs['kt_dbg'], ktf[:], 'kt_dbg', ['ktf'])
                load('sp', dbg_outs['vt_dbg'], vtf[:], 'vt_dbg', ['vtf'])
                outs.extend(['kt_dbg', 'vt_dbg'])
        fw.barrier()
        if "sgu" in phases:
          with contextlib.ExitStack() as s5:
            wu_t = sb("wu_t", [128, 8, 512], BF16, s5)
            wv_t = sb("wv_t", [128, 8, 512], BF16, s5)
            BV = sb("BV", [128, 512], F32, s5)
            LNG = sb("LNG", [128, 512], F32, s5)
            LNB = sb("LNB", [128, 512], F32, s5)
            WST = sb("WST", [128, 4, 128], BF16, s5)
            BSP = sb("BSP", [128, 4, 128], F32, s5)
            load('pool', wu_t[:], w_in[:, :, C_U:C_U + 512], 'wu_t')
            load('pool', wv_t[:], w_in[:, :, C_V:C_V + 512], 'wv_t')
            load('pool', WST[:], w_spT, 'WST')
            load('sp', BV[:], b_v, 'BV')
            load('sp', LNG[:], sgu_g, 'LNG')
            load('sp', LNB[:], sgu_b, 'LNB')
            load('sp', BSP[:], b_sp, 'BSP')
            GU = sb("GU", [128, 4, 512], F32, s5)
            V1 = sb("V1", [128, 512], F32, s5)
            GV = sb("GV", [128, 512], F32, s5)
            VNB = sb("VNB", [128, 512], BF16, s5)
            STT = sb("STT", [128, 6], F32, s5)
            MV = sb("MV", [128, 2], F32, s5)
            RSTD = sb("RSTD", [128, 1], F32, s5)
            MX = sb("MX", [128, 4, 128], F32, s5)
            ATS = [sb("ATS%d" % i, [128, 4, 128], BF16, s5) for i in range(2)]
            nch = 0
            for ti in range(5):
                lat = ti < 4
                n = 512 if lat else 256
                e0 = HALO + ti * 512 if lat else TLAT
                for g in range(4):
                    def evu(pt, pk, g=g):
                        fw.op('act', lambda: nc.scalar.activation(out=GU[:, g, 0:n], in_=pt[:, 0:n], func=AF.Gelu, bias=bint[:, C_U // 128 + g:C_U // 128 + g + 1]),
                              reads=[pk, 'bint'], writes=['GU'])
                    inproj2(lambda kc: wu_t[:, kc, g * 128:(g + 1) * 128], 128, e0, n, 'wu_t', evu)
                for sub in range(n // 128):
                    t0 = e0 + sub * 128
                    own0 = (ti * 512 if lat else TOWN) + sub * 128
                    pi = next_ps()
                    pv = PS[pi]
                    for kc in range(8):
                        fw.op('pe', lambda: nc.tensor.matmul(pv[:, 0:512], lhsT=hT[:, kc, t0:t0 + 128], rhs=wv_t[:, kc, :], start=(kc == 0), stop=(kc == 7)),
                              reads=['wv_t', 'hT%d' % kc], writes=['ps%d' % pi])
                    fw.op('dve', lambda: nc.vector.tensor_tensor(out=V1[:], in0=pv[:, 0:512], in1=BV[:], op=ALU.add), reads=['ps%d' % pi, 'BV'], writes=['V1'])
                    fw.op('act', lambda: nc.scalar.activation(out=GV[:], in_=V1[:], func=AF.Gelu), reads=['V1'], writes=['GV'])
                    fw.op('dve', lambda: nc.vector.bn_stats(out=STT[:], in_=GV[:]), reads=['GV'], writes=['STT'])
                    fw.op('dve', lambda: nc.vector.bn_aggr(out=MV[:], in_=STT[:]), reads=['STT'], writes=['MV'])
                    fw.op('act', lambda: nc.scalar.activation(out=RSTD[:], in_=MV[:, 1:2], func=AF.Sqrt, bias=LN_EPS), reads=['MV'], writes=['RSTD'])
                    fw.op('dve', lambda: nc.vector.reciprocal(out=RSTD[:], in_=RSTD[:]), reads=['RSTD'], writes=['RSTD'])
                    fw.op('dve', lambda: nc.vector.tensor_scalar(out=GV[:], in0=GV[:], scalar1=MV[:, 0:1], scalar2=RSTD[:, 0:1], op0=ALU.subtract, op1=ALU.mult),
                          reads=['GV', 'MV', 'RSTD'], writes=['GV'])
                    fw.op('pool', lambda: nc.gpsimd.tensor_tensor(out=GV[:], in0=GV[:], in1=LNG[:], op=ALU.mult), reads=['GV', 'LNG'], writes=['GV'])
                    fw.op('pool', lambda: nc.gpsimd.tensor_tensor(out=VNB[:], in0=GV[:], in1=LNB[:], op=ALU.add), reads=['GV', 'LNB'], writes=['VNB'])
                    pmi_ = next_ps()
                    pmx = PS[pmi_]
                    for g in range(4):
                        fw.op('pe', lambda: nc.tensor.matmul(pmx[:, g * 128:(g + 1) * 128], lhsT=VNB[:, g * 128:(g + 1) * 128], rhs=WST[:, g, :], start=True, stop=True),
                              reads=['VNB', 'WST'], writes=['ps%d' % pmi_])
                    fw.op('dve', lambda: nc.vector.tensor_tensor(out=MX[:], in0=pmx[:, 0:512].rearrange("p (g q) -> p g q", g=4), in1=BSP[:], op=ALU.add),
                          reads=['ps%d' % pmi_, 'BSP'], writes=['MX'])
                    ats = ATS[nch % 2]
                    ak = 'ATS%d' % (nch % 2)
                    nch += 1
                    fw.op('pool', lambda: nc.gpsimd.tensor_tensor(out=ats[:], in0=MX[:], in1=GU[:, :, sub * 128:(sub + 1) * 128], op=ALU.mult), reads=['MX', 'GU'], writes=[ak])
                    load('sp', A_d[:, :, own0:own0 + 128], ats[:], 'A_d', [ak])
        fw.barrier()
        if dbg:
            with contextlib.ExitStack() as sd:
                rb = sb("dbgb", [128, NOWN], BF16, sd)
                rf = sb("dbgf", [128, NOWN], F32, sd)
                for (nm, src_d, npart, nchk) in (("r_dbg", R_d, 128, 8), ("a_dbg", A_d, 128, 4), ("b_dbg", B_d, 64, 8)):
                    o_ = dbg_outs[nm]
                    for j in range(nchk):
                        load('sp', rb[0:npart, :], src_d[:, j, :], 'dbgb', [nm[0].upper() + '_d'])
                        fw.op('dve', lambda: nc.vector.tensor_copy(out=rf[0:npart, :], in_=rb[0:npart, :]), reads=['dbgb'], writes=['dbgf'])
                        load('sp', o_[:, j, :], rf[0:npart, :], nm, ['dbgf'])
                    outs.append(nm)
        def layer_norm(X, xk, n, SQ, sqk, MEAN, mk, MSQ, msk, RSTDW, rk, ONF, ok, gcol, bcol, pvk):
            fw.op('act', lambda: nc.scalar.activation(out=SQ[:, :, 0:n], in_=X[:, :, 0:n], func=AF.Square), reads=[xk], writes=[sqk])
            p1 = next_ps(); p2 = next_ps()
            for c in range(8):
                fw.op('pe', lambda: nc.tensor.matmul(PS[p1][:, 0:n], lhsT=ONF[:], rhs=X[:, c, 0:n], start=(c == 0), stop=(c == 7)), reads=[ok, xk], writes=['ps%d' % p1])
            for c in range(8):
                fw.op('pe', lambda: nc.tensor.matmul(PS[p2][:, 0:n], lhsT=ONF[:], rhs=SQ[:, c, 0:n], start=(c == 0), stop=(c == 7)), reads=[ok, sqk], writes=['ps%d' % p2])
            fw.op('act', lambda: nc.scalar.activation(out=MEAN[:, 0:n], in_=PS[p1][:, 0:n], func=AF.Copy, scale=1.0 / D), reads=['ps%d' % p1], writes=[mk])
            fw.op('dve', lambda: nc.vector.tensor_tensor(out=MSQ[:, 0:n], in0=MEAN[:, 0:n], in1=MEAN[:, 0:n], op=ALU.mult), reads=[mk], writes=[msk])
            fw.op('dve', lambda: nc.vector.scalar_tensor_tensor(out=RSTDW[:, 0:n], in0=PS[p2][:, 0:n], scalar=1.0 / D, in1=MSQ[:, 0:n], op0=ALU.mult, op1=ALU.subtract),
                  reads=['ps%d' % p2, msk], writes=[rk])
            fw.op('act', lambda: nc.scalar.activation(out=RSTDW[:, 0:n], in_=RSTDW[:, 0:n], func=AF.Sqrt, bias=LN_EPS), reads=[rk], writes=[rk])
            fw.op('dve', lambda: nc.vector.reciprocal(out=RSTDW[:, 0:n], in_=RSTDW[:, 0:n]), reads=[rk], writes=[rk])
            for c in range(8):
                fw.op('dve', lambda: nc.vector.tensor_tensor(out=X[:, c, 0:n], in0=X[:, c, 0:n], in1=MEAN[:, 0:n], op=ALU.subtract), reads=[xk, mk], writes=[xk])
                fw.op('pool', lambda: nc.gpsimd.tensor_tensor(out=X[:, c, 0:n], in0=X[:, c, 0:n], in1=RSTDW[:, 0:n], op=ALU.mult), reads=[xk, rk], writes=[xk])
                fw.op('act', lambda: nc.scalar.activation(out=X[:, c, 0:n], in_=X[:, c, 0:n], func=AF.Identity, scale=gcol(c), bias=bcol(c)), reads=[xk, pvk], writes=[xk])

        if GL:
            X1_d = sh.scratch("X1g", [128, 8, 8448], F32)
            H2_d = sh.scratch("H2g", [8448, D], BF16)
            LG_d = sh.scratch("LGg", [128, 66, 36], F32)
        else:
            X1_d = sh.scratch("X1_d", [128, 8, NOWN], F32)
            H2_d = sh.scratch("H2_d", [NOWN, D], BF16)
        LG = sb("LG", [128, T, 36], F32)

        def gbase(o0):
            if not GL:
                return o0
            return seg * TOWN + o0 if o0 < TOWN else SEQ + (o0 - TOWN)

        def gskip(o0):
            return GL and o0 >= TOWN and seg != 0
        OT = [(HALO + i * 512, i * 512, 512) for i in range(4)] + [(TLAT, TOWN, 256)]
        if "merge" in phases:
          with contextlib.ExitStack() as s6:
            ATl = sb("ATl", [128, 4, 1280], BF16, s6)
            BTl = sb("BTl", [64, 8, 1280], BF16, s6)
            RTl = sb("RTl", [128, 8, 1280], BF16, s6)
            WGs = [sb("WG%d" % i, [128, 8, 3, 128], BF16, s6) for i in range(2)]
            WAs = [sb("WA%d" % i, [128, 4, 128], BF16, s6) for i in range(2)]
            WBs = [sb("WB%d" % i, [64, 8, 128], BF16, s6) for i in range(2)]
            WCs = [sb("WC%d" % i, [128, 8, 128], BF16, s6) for i in range(2)]
            GS = [sb("GS%d" % i, [128, 512], F32, s6) for i in range(3)]
            TS = [sb("TS%d" % i, [128, 512], F32, s6) for i in range(3)]
            wi = 0
            for hf in range(2):
                tiles = OT[0:2] if hf == 0 else OT[2:5]
                h0 = tiles[0][1]
                hn = sum(t[2] for t in tiles)
                load('sp', ATl[:, :, 0:hn], A_d[:, :, h0:h0 + hn], 'ATl', ['A_d'])
                load('sp', BTl[:, :, 0:hn], B_d[:, :, h0:h0 + hn], 'BTl', ['B_d'])
                load('sp', RTl[:, :, 0:hn], R_d[:, :, h0:h0 + hn], 'RTl', ['R_d'])
                for oc in range(8):
                    w_ = wi % 2
                    wi += 1
                    WG, WA, WB, WC = WGs[w_], WAs[w_], WBs[w_], WCs[w_]
                    kg, ka, kb, kc_ = 'WG%d' % w_, 'WA%d' % w_, 'WB%d' % w_, 'WC%d' % w_
                    for br in range(3):
                        c0 = C_G + br * 1024 + oc * 128
                        load('pool', WG[:, :, br, :], w_in[:, :, c0:c0 + 128], kg)
                    load('pool', WA[:], w_pa[:, :, oc * 128:(oc + 1) * 128], ka)
                    load('pool', WB[:], w_pb[:, :, oc * 128:(oc + 1) * 128], kb)
                    load('pool', WC[:], w_pc[:, :, oc * 128:(oc + 1) * 128], kc_)
                    for (e0, o0, n) in tiles:
                        l0 = o0 - h0
                        for br in range(3):
                            def evg(pt, pk, br=br):
                                bcol = C_G // 128 + br * 8 + oc
                                fw.op('act', lambda: nc.scalar.activation(out=GS[br][:, 0:n], in_=pt[:, 0:n], func=AF.Sigmoid, bias=bint[:, bcol:bcol + 1]),
                                      reads=[pk, 'bint'], writes=['GS%d' % br])
                            inproj2(lambda kc: WG[:, kc, br, :], 128, e0, n, kg, evg)
                        for br, (Wt, wk, src, sk, nk, npart) in enumerate(((WA, ka, ATl, 'ATl', 4, 128), (WB, kb, BTl, 'BTl', 8, 64), (WC, kc_, RTl, 'RTl', 8, 128))):
                            pi = next_ps()
                            pt = PS[pi]
                            for kk in range(nk):
                                fw.op('pe', lambda: nc.tensor.matmul(pt[:, 0:n], lhsT=Wt[0:npart, kk, :], rhs=src[0:npart, kk, l0:l0 + n], start=(kk == 0), stop=(kk == nk - 1)),
                                      reads=[wk, sk], writes=['ps%d' % pi])
                            fw.op('dve', lambda: nc.vector.tensor_tensor(out=TS[br][:, 0:n], in0=pt[:, 0:n], in1=GS[br][:, 0:n], op=ALU.mult),
                                  reads=['ps%d' % pi, 'GS%d' % br], writes=['TS%d' % br])
                        fw.op('pool', lambda: nc.gpsimd.tensor_tensor(out=TS[0][:, 0:n], in0=TS[0][:, 0:n], in1=TS[1][:, 0:n], op=ALU.add), reads=['TS0', 'TS1'], writes=['TS0'])
                        fw.op('pool', lambda: nc.gpsimd.tensor_tensor(out=yT[:, oc, o0:o0 + n], in0=TS[0][:, 0:n], in1=TS[2][:, 0:n], op=ALU.add), reads=['TS0', 'TS2'], writes=['yT%d' % oc])
          fw.barrier()
          sHT.close()
          with contextlib.ExitStack() as s7:
            WO = sb("WO", [128, 8, D], BF16, s7)
            WGR = sb("WGR", [128, 8, 36], F32, s7)
            BGR = sb("BGR", [128, 36], F32, s7)
            IDN = sb("IDN", [128, 128], F32, s7)
            ONF = sb("ONF", [128, 128], F32, s7)
            pv = sb("pvec", [128, 5, 8], F32, s7)
            bom = sb("bom", [128, 2, 8], F32, s7)
            load('pool', WO[:], w_o, 'WO')
            load('sp', WGR[:], w_gr, 'WGR')
            load('sp', BGR[:], b_gr, 'BGR')
            load('sp', IDN[:], ident, 'IDN')
            load('sp', ONF[:], ones_f, 'ONF')
            load('sp', pv[:, 0, :], b_o, 'pvec')
            load('sp', pv[:, 1, :], ln1g, 'pvec')
            load('sp', pv[:, 2, :], ln1b, 'pvec')
            fw.op('dve', lambda: nc.vector.tensor_tensor(out=bom[:, 0, :], in0=pv[:, 0, :], in1=mlat[:, 16:24], op=ALU.mult), reads=['pvec', 'mlat'], writes=['bom'])
            fw.op('dve', lambda: nc.vector.tensor_tensor(out=bom[:, 1, :], in0=pv[:, 0, :], in1=mctx[:, 16:24], op=ALU.mult), reads=['pvec', 'mctx'], writes=['bom'])
            XT = sb("XT", [128, 8, 512], F32, s7)
            SQ = sb("SQ", [128, 8, 512], F32, s7)
            H2F = sb("H2F", [128, 8, 512], F32, s7)
            MEAN = sb("MEAN", [128, 512], F32, s7)
            MSQ = sb("MSQ", [128, 512], F32, s7)
            RSTDW = sb("RSTDW", [128, 512], F32, s7)
            nsub = 0
            H2B = [sb("H2B%d" % i, [128, D], BF16, s7) for i in range(2)]
            for ti, (e0, o0, n) in enumerate(OT):
                lat = ti < 4
                mv_ = mlat if lat else mctx
                mk_ = 'mlat' if lat else 'mctx'
                li = 0 if lat else 1
                if lat:
                    load('sp', XT[:, :, 0:n], xT[:, :, e0:e0 + n], 'XT')
                else:
                    load('sp', XT[:, :, 0:n], cT[:, :, :], 'XT')
                for oc in range(8):
                    pi = next_ps()
                    pt = PS[pi]
                    for kc in range(8):
                        fw.op('pe', lambda: nc.tensor.matmul(pt[:, 0:n], lhsT=WO[:, kc, oc * 128:(oc + 1) * 128], rhs=yT[:, kc, o0:o0 + n], start=(kc == 0), stop=(kc == 7)),
                              reads=['WO', 'yT%d' % kc], writes=['ps%d' % pi])
                    fw.op('act', lambda: nc.scalar.activation(out=SQ[:, oc, 0:n], in_=pt[:, 0:n], func=AF.Identity, scale=mv_[:, 16 + oc:17 + oc], bias=bom[:, li, oc:oc + 1]),
                          reads=['ps%d' % pi, mk_, 'bom'], writes=['SQ'])
                    fw.op('dve', lambda: nc.vector.scalar_tensor_tensor(out=XT[:, oc, 0:n], in0=XT[:, oc, 0:n], scalar=ALPHA, in1=SQ[:, oc, 0:n], op0=ALU.mult, op1=ALU.add),
                          reads=['XT', 'SQ'], writes=['XT'])

                layer_norm(XT, 'XT', n, SQ, 'SQ', MEAN, 'MEAN', MSQ, 'MSQ', RSTDW, 'RSTDW', ONF, 'ONF', lambda c: pv[:, 1, c:c + 1], lambda c: pv[:, 2, c:c + 1], 'pvec')
                if not gskip(o0):
                    load('sp', X1_d[:, :, gbase(o0):gbase(o0) + n], XT[:, :, 0:n], 'X1_d', ['XT'])
                m4 = m4p[:, li, :]
                for c in range(8):
                    fw.op('act', lambda: nc.scalar.activation(out=H2F[:, c, 0:n], in_=XT[:, c, 0:n], func=AF.Identity, scale=m4p[:, li, c:c + 1], bias=mv_[:, 24 + c:25 + c]),
                          reads=['XT', 'm4p', mk_], writes=['H2F'])
                for sub in range(n // 128):
                    t0 = sub * 128
                    chunk = (o0 + t0) // 128
                    pi = next_ps()
                    pt = PS[pi]
                    for kc in range(8):
                        fw.op('pe', lambda: nc.tensor.matmul(pt[:, 0:36], lhsT=H2F[:, kc, t0:t0 + 128], rhs=WGR[:, kc, :], start=(kc == 0), stop=(kc == 7)),
                              reads=['H2F', 'WGR'], writes=['ps%d' % pi])
                    fw.op('dve', lambda: nc.vector.tensor_tensor(out=LG[:, chunk, :], in0=pt[:, 0:36], in1=BGR[:], op=ALU.add), reads=['ps%d' % pi, 'BGR'], writes=['LG'])
                    hb = H2B[nsub % 2]
                    hk = 'H2B%d' % (nsub % 2)
                    nsub += 1
                    for half in range(2):
                        pi = next_ps()
                        pt = PS[pi]
                        for cc in range(4):
                            c = half * 4 + cc
                            fw.op('pe', lambda: nc.tensor.transpose(out=pt[:, cc * 128:(cc + 1) * 128], in_=H2F[:, c, t0:t0 + 128], identity=IDN[:]),
                                  reads=['H2F', 'IDN'], writes=['ps%d' % pi])
                        if half == 0:
                            fw.op('act', lambda: nc.scalar.copy(out=hb[:, 0:512], in_=pt[:, 0:512]), reads=['ps%d' % pi], writes=[hk])
                        else:
                            fw.op('dve', lambda: nc.vector.tensor_copy(out=hb[:, 512:1024], in_=pt[:, 0:512]), reads=['ps%d' % pi], writes=[hk])
                    if not gskip(o0):
                        load('sp', H2_d[gbase(o0) + t0:gbase(o0) + t0 + 128, :], hb[:], 'H2_d', [hk])
          if mode == "premoe":
              load('sp', LG_d[:, seg * 16:(seg + 1) * 16, :], LG[:, 0:16, :], 'LG_d', ['LG'])
              if seg == 0:
                  load('sp', LG_d[:, 64:66, :], LG[:, 16:18, :], 'LG_d', ['LG'])
          fw.barrier()
          sY.close()
          if mode == "premoe":
              fw.barrier()
              return
          if dbg:
              load('sp', dbg_outs['x1_dbg'], X1_d, 'x1_dbg', ['X1_d'])
              lgf = LG
              load('sp', dbg_outs['lg_dbg'], LG[:], 'lg_dbg', ['LG'])
              outs.extend(['x1_dbg', 'lg_dbg'])
        if mode == "moe":
            sHT.close()
            sY.close()
            load('sp', LG[:], LG_d, 'LG', ['LG_d'])
        if "moe" in phases:
          XB_d = sh.scratch("XBg" if GL else "XB_d", [NB * BS, D], BF16)
          OUT_d = sh.scratch("OUTg" if GL else "OUT_d", [NB * BS, D], F32)
          SL1 = sb("SL1", [128, T], I32)
          SL2 = sb("SL2", [128, T], I32)
          G1 = sb("G1", [128, T], F32)
          G2 = sb("G2", [128, T], F32)
          WIDX = sb("WIDX", [128, NB], I32)
          with contextlib.ExitStack() as s8:
            def t8(name, shape, dt=F32):
                return sb(name, shape, dt, s8)
            TRI = t8("TRI", [128, 128]); ONF2 = t8("ONF2", [128, 128]); BVAL = t8("BVAL", [128, NBLK]); PIDX = t8("PIDX", [128, 1])
            load('sp', TRI[:], tri_in, 'TRI'); load('sp', ONF2[:], ones_f, 'ONF2'); load('sp', BVAL[:], bval_in, 'BVAL'); load('sp', PIDX[:], pidx_in, 'PIDX')
            if SUB != 1:
                fw.op('dve', lambda: nc.vector.tensor_scalar(out=BVAL[:], in0=BVAL[:], scalar1=float(SUB), scalar2=None, op0=ALU.mult), reads=['BVAL'], writes=['BVAL'])
            gmax = t8("gmax", [128, T]); goh = t8("goh", [128, T, 4]); ge = t8("ge", [128, T, 4]); gw = t8("gw", [128, T])
            pen = t8("pen", [128, T, 4]); em = t8("em", [128, T, 32]); em2 = t8("em2", [128, T, 32])
            m1 = t8("m1", [128, T]); m2 = t8("m2", [128, T]); oh1 = t8("oh1", [128, T, 32]); oh2 = t8("oh2", [128, T, 32])
            dm = t8("dm", [128, T]); p1 = t8("p1", [128, T])
            A_ = t8("Aas", [128, T, 32]); CUMA = t8("CUMA", [128, T + 1, 32]); SLT = t8("SLT", [128, T, 32]); TMP = t8("TMP", [128, T, 32])
            cnt = t8("cnt", [128, 32]); cnti = t8("cnti", [128, 32], I32); padd = t8("padd", [128, 32]); pend = t8("pend", [128, 32]); pst = t8("pst", [128, 32])
            one32 = t8("one32", [128, 32]); CMP = t8("CMP", [128, NB, 32]); be = t8("be", [128, NB]); slf = t8("slf", [128, T])

            def V(fn, reads, writes):
                fw.op('dve', fn, reads=reads, writes=writes)
            gl = LG[:, :, 0:4]
            el = LG[:, :, 4:36]
            V(lambda: nc.vector.tensor_reduce(out=gmax[:], in_=gl, op=ALU.max, axis=AX.X), ['LG'], ['gmax'])
            gmb = gmax[:].unsqueeze(2).to_broadcast([128, T, 4])
            V(lambda: nc.vector.tensor_tensor(out=goh[:], in0=gl, in1=gmb, op=ALU.is_equal), ['LG', 'gmax'], ['goh'])
            V(lambda: nc.vector.tensor_tensor(out=ge[:], in0=gl, in1=gmb, op=ALU.subtract), ['LG', 'gmax'], ['ge'])
            fw.op('act', lambda: nc.scalar.activation(out=ge[:], in_=ge[:], func=AF.Exp), reads=['ge'], writes=['ge'])
            V(lambda: nc.vector.tensor_reduce(out=gw[:], in_=ge[:], op=ALU.add, axis=AX.X), ['ge'], ['gw'])
            V(lambda: nc.vector.reciprocal(out=gw[:], in_=gw[:]), ['gw'], ['gw'])
            V(lambda: nc.vector.tensor_scalar(out=pen[:], in0=goh[:], scalar1=1e30, scalar2=-1e30, op0=ALU.mult, op1=ALU.add), ['goh'], ['pen'])
            V(lambda: nc.vector.tensor_tensor(out=em[:].rearrange("p t (g e) -> p t g e", g=4), in0=el.rearrange("p t (g e) -> p t g e", g=4),
                                              in1=pen[:].unsqueeze(3).to_broadcast([128, T, 4, 8]), op=ALU.add), ['LG', 'pen'], ['em'])
            V(lambda: nc.vector.tensor_reduce(out=m1[:], in_=em[:], op=ALU.max, axis=AX.X), ['em'], ['m1'])
            V(lambda: nc.vector.tensor_tensor(out=oh1[:], in0=em[:], in1=m1[:].unsqueeze(2).to_broadcast([128, T, 32]), op=ALU.is_equal), ['em', 'm1'], ['oh1'])
            V(lambda: nc.vector.scalar_tensor_tensor(out=em2[:], in0=oh1[:], scalar=-1e30, in1=em[:], op0=ALU.mult, op1=ALU.add), ['oh1', 'em'], ['em2'])
            V(lambda: nc.vector.tensor_reduce(out=m2[:], in_=em2[:], op=ALU.max, axis=AX.X), ['em2'], ['m2'])
            V(lambda: nc.vector.tensor_tensor(out=oh2[:], in0=em2[:], in1=m2[:].unsqueeze(2).to_broadcast([128, T, 32]), op=ALU.is_equal), ['em2', 'm2'], ['oh2'])
            V(lambda: nc.vector.tensor_tensor(out=dm[:], in0=m2[:], in1=m1[:], op=ALU.subtract), ['m1', 'm2'], ['dm'])
            fw.op('act', lambda: nc.scalar.activation(out=dm[:], in_=dm[:], func=AF.Exp), reads=['dm'], writes=['dm'])
            V(lambda: nc.vector.tensor_scalar(out=p1[:], in0=dm[:], scalar1=1.0, scalar2=None, op0=ALU.add), ['dm'], ['p1'])
            V(lambda: nc.vector.reciprocal(out=p1[:], in_=p1[:]), ['p1'], ['p1'])
            V(lambda: nc.vector.tensor_tensor(out=G1[:], in0=gw[:], in1=p1[:], op=ALU.mult), ['gw', 'p1'], ['G1'])
            V(lambda: nc.vector.tensor_tensor(out=dm[:], in0=dm[:], in1=p1[:], op=ALU.mult), ['dm', 'p1'], ['dm'])
            V(lambda: nc.vector.tensor_tensor(out=G2[:], in0=gw[:], in1=dm[:], op=ALU.mult), ['gw', 'dm'], ['G2'])
            V(lambda: nc.vector.tensor_tensor(out=A_[:], in0=oh1[:], in1=oh2[:], op=ALU.add), ['oh1', 'oh2'], ['A'])
            V(lambda: nc.vector.memset(CUMA[:, 0, :], 0.0), [], ['CUMA'])
            for t in range(T):
                V(lambda: nc.vector.tensor_tensor(out=CUMA[:, t + 1, :], in0=CUMA[:, t, :], in1=A_[:, t, :], op=ALU.add), ['CUMA', 'A'], ['CUMA'])
            pc_ = next_ps()
            fw.op('pe', lambda: nc.tensor.matmul(PS[pc_][:, 0:32], lhsT=ONF2[:], rhs=CUMA[:, T, :], start=True, stop=True), reads=['ONF2', 'CUMA'], writes=['ps%d' % pc_])
            V(lambda: nc.vector.tensor_copy(out=cnt[:], in_=PS[pc_][:, 0:32]), ['ps%d' % pc_], ['cnt'])
            CM2 = t8("CM2", [128, 32, 40])
            V(lambda: nc.vector.tensor_tensor(out=CM2[:], in0=cnt[:].unsqueeze(2).to_broadcast([128, 32, 40]), in1=BVAL[:, 0:40].unsqueeze(1).to_broadcast([128, 32, 40]), op=ALU.is_gt),
              ['cnt', 'BVAL'], ['CM2'])
            V(lambda: nc.vector.tensor_reduce(out=padd[:], in_=CM2[:], op=ALU.add, axis=AX.X), ['CM2'], ['padd'])
            V(lambda: nc.vector.tensor_scalar(out=padd[:], in0=padd[:], scalar1=float(BS), scalar2=None, op0=ALU.mult), ['padd'], ['padd'])
            V(lambda: nc.vector.memset(one32[:], 1.0), [], ['one32'])
            V(lambda: nc.vector.tensor_tensor_scan(out=pend[:], data0=one32[:], data1=padd[:], initial=0.0, op0=ALU.mult, op1=ALU.add), ['one32', 'padd'], ['pend'])
            V(lambda: nc.vector.tensor_tensor(out=pst[:], in0=pend[:], in1=padd[:], op=ALU.subtract), ['pend', 'padd'], ['pst'])
            psb = pst[:].unsqueeze(1)
            for g0_ in range(0, T, 16):
                ng_ = min(16, T - g0_)
                pr_ = next_ps()
                pk = 'ps%d' % pr_
                for t in range(g0_, g0_ + ng_):
                    tt_ = t - g0_
                    fw.op('pe', lambda: nc.tensor.matmul(PS[pr_][:, tt_ * 32:(tt_ + 1) * 32], lhsT=TRI[:], rhs=A_[:, t, :], start=True, stop=False), reads=['TRI', 'A'], writes=[pk])
                    fw.op('pe', lambda: nc.tensor.matmul(PS[pr_][:, tt_ * 32:(tt_ + 1) * 32], lhsT=ONF2[:], rhs=CUMA[:, t, :], start=False, stop=True), reads=['ONF2', 'CUMA'], writes=[pk])
                V(lambda: nc.vector.tensor_tensor(out=SLT[:, g0_:g0_ + ng_, :], in0=PS[pr_][:, 0:ng_ * 32].rearrange("p (t e) -> p t e", e=32), in1=psb.to_broadcast([128, ng_, 32]), op=ALU.add),
                  [pk, 'pst'], ['SLT'])
            for (oh, ok, SL) in ((oh1, 'oh1', SL1), (oh2, 'oh2', SL2)):
                V(lambda: nc.vector.tensor_tensor(out=TMP[:], in0=SLT[:], in1=oh[:], op=ALU.mult), ['SLT', ok], ['TMP'])
                V(lambda: nc.vector.tensor_reduce(out=slf[:], in_=TMP[:], op=ALU.add, axis=AX.X), ['TMP'], ['slf'])
                V(lambda: nc.vector.tensor_copy(out=SL[:], in_=slf[:]), ['slf'], ['SL'])
            V(lambda: nc.vector.tensor_tensor(out=CMP[:], in0=pend[:].unsqueeze(1).to_broadcast([128, NB, 32]), in1=BVAL[:, 0:NB].unsqueeze(2).to_broadcast([128, NB, 32]), op=ALU.is_le),
              ['pend', 'BVAL'], ['CMP'])
            V(lambda: nc.vector.tensor_reduce(out=be[:], in_=CMP[:], op=ALU.add, axis=AX.X), ['CMP'], ['be'])
            V(lambda: nc.vector.tensor_scalar(out=be[:], in0=be[:], scalar1=float(NEXP - 1), scalar2=128.0, op0=ALU.min, op1=ALU.mult), ['be'], ['be'])
            V(lambda: nc.vector.tensor_scalar(out=WIDX[:], in0=be[:], scalar1=PIDX[:, 0:1], scalar2=None, op0=ALU.add), ['be', 'PIDX'], ['WIDX'])
            if dbg:
                load('sp', dbg_outs['sl_dbg'][:, 0, :], SL1[:], 'sl_dbg', ['SL'])
                load('sp', dbg_outs['sl_dbg'][:, 1, :], SL2[:], 'sl_dbg', ['SL'])
                load('sp', dbg_outs['g_dbg'][:, 0, :], G1[:], 'g_dbg', ['G1'])
                load('sp', dbg_outs['g_dbg'][:, 1, :], G2[:], 'g_dbg', ['G2'])
                load('sp', dbg_outs['wi_dbg'], WIDX[:], 'wi_dbg', ['WIDX'])
                outs.extend(['sl_dbg', 'g_dbg', 'wi_dbg'])
          fw.barrier()
          if 'moe8' not in phases and dbg:
              sHT.close()
              fw.barrier()
              return
          with contextlib.ExitStack() as s8b:
            HBs = [sb("HBs%d" % i, [128, D], BF16, s8b) for i in range(2)]
            for t in range(T):
                hb_ = HBs[t % 2]
                hk_ = 'HBs%d' % (t % 2)
                load('sp', hb_[:], H2_d[t * 128:(t + 1) * 128, :], hk_, ['H2_d'])
                for SL in (SL1, SL2):
                    fw.dma('pool', lambda: nc.gpsimd.indirect_dma_start(out=XB_d[:, :], out_offset=bass.IndirectOffsetOnAxis(ap=SL[:, t:t + 1], axis=0),
                                                                        in_=hb_[:], in_offset=None), reads=[hk_, 'SL'], writes=['XB_d'])
          fw.barrier()
          with contextlib.ExitStack() as s9:
            IDB = sb("IDB", [128, 128], BF16, s9)
            load('pool', IDB[:], ident, 'IDB')
            W1B = [sb("W1B%d" % i, [128, 4096], BF16, s9) for i in range(2)]
            W3B = [sb("W3B%d" % i, [128, 4096], BF16, s9) for i in range(2)]
            W2B = [sb("W2B%d" % i, [128, 4096], BF16, s9) for i in range(2)]
            NBUF = 3
            XBs = [sb("XBs%d" % i, [128, D], BF16, s9) for i in range(NBUF)]
            XTs = [sb("XTs%d" % i, [128, 8, 128], BF16, s9) for i in range(NBUF)]
            S1s = [sb("S1s%d" % i, [128, 512], F32, s9) for i in range(NBUF)]
            Gbs = [sb("Gbs%d" % i, [128, 512], BF16, s9) for i in range(NBUF)]
            GTs = [sb("GTs%d" % i, [128, 4, 128], BF16, s9) for i in range(NBUF)]
            OBs = [sb("OBs%d" % i, [128, D], F32, s9) for i in range(NBUF)]
            items = [(b_, s_) for b_ in range(NB) for s_ in range(SUB)]
            NI = len(items)

            def loadw(b_):
                i2 = b_ % 2
                for (wt, wk, srcn) in ((W1B[i2], 'W1B%d' % i2, "w1"), (W3B[i2], 'W3B%d' % i2, "w3"), (W2B[i2], 'W2B%d' % i2, "w2")):
                    for a_, hsfx in enumerate(("a", "b")):
                        fw.dma('pool', lambda: nc.gpsimd.indirect_dma_start(out=wt[:, a_ * 2048:(a_ + 1) * 2048], out_offset=None, in_=wexp[srcn + hsfx][:, :],
                                                                            in_offset=bass.IndirectOffsetOnAxis(ap=WIDX[:, b_:b_ + 1], axis=0)),
                               reads=['WIDX'], writes=[wk])

            def stA(i):
                b_, s_ = items[i]
                j = i % NBUF
                r0 = b_ * BS + s_ * 128
                xb = XBs[j]
                load('sp', xb[:], XB_d[r0:r0 + 128, :], 'XBs%d' % j, ['XB_d'])
                pi_ = next_ps()
                pv_ = PS[pi_][:].bitcast(BF16)
                for c in range(8):
                    fw.op('pe', lambda: nc.tensor.transpose(out=pv_[:, c * 128:(c + 1) * 128], in_=xb[:, c * 128:(c + 1) * 128], identity=IDB[:]),
                          reads=['XBs%d' % j, 'IDB'], writes=['ps%d' % pi_])
                fw.op('act', lambda: nc.scalar.copy(out=XTs[j][:, 0:4, :], in_=pv_[:, 0:512].rearrange("p (c s) -> p c s", c=4)), reads=['ps%d' % pi_], writes=['XTs%d' % j])
                fw.op('dve', lambda: nc.vector.tensor_copy(out=XTs[j][:, 4:8, :], in_=pv_[:, 512:1024].rearrange("p (c s) -> p c s", c=4)), reads=['ps%d' % pi_], writes=['XTs%d' % j])

            def stB(i):
                b_, s_ = items[i]
                j = i % NBUF
                i2 = b_ % 2
                p1i = next_ps(); p3i = next_ps()
                for (pi_, wt, wk) in ((p1i, W1B[i2], 'W1B%d' % i2), (p3i, W3B[i2], 'W3B%d' % i2)):
                    for kc in range(8):
                        fw.op('pe', lambda: nc.tensor.matmul(PS[pi_][:, 0:512], lhsT=XTs[j][:, kc, :], rhs=wt[:, kc * 512:(kc + 1) * 512], start=(kc == 0), stop=(kc == 7)),
                              reads=['XTs%d' % j, wk], writes=['ps%d' % pi_])
                fw.op('act', lambda: nc.scalar.activation(out=S1s[j][:], in_=PS[p1i][:, 0:512], func=AF.Silu), reads=['ps%d' % p1i], writes=['S1s%d' % j])
                fw.op('dve', lambda: nc.vector.tensor_tensor(out=Gbs[j][:], in0=PS[p3i][:, 0:512], in1=S1s[j][:], op=ALU.mult), reads=['ps%d' % p3i, 'S1s%d' % j], writes=['Gbs%d' % j])

            def stC(i):
                j = i % NBUF
                pi_ = next_ps()
                pv_ = PS[pi_][:].bitcast(BF16)
                for fc in range(4):
                    fw.op('pe', lambda: nc.tensor.transpose(out=pv_[:, fc * 128:(fc + 1) * 128], in_=Gbs[j][:, fc * 128:(fc + 1) * 128], identity=IDB[:]),
                          reads=['Gbs%d' % j, 'IDB'], writes=['ps%d' % pi_])
                fw.op('dve', lambda: nc.vector.tensor_copy(out=GTs[j][:], in_=pv_[:, 0:512].rearrange("p (c s) -> p c s", c=4)), reads=['ps%d' % pi_], writes=['GTs%d' % j])

            def stD(i):
                b_, s_ = items[i]
                j = i % NBUF
                i2 = b_ % 2
                r0 = b_ * BS + s_ * 128
                ob = OBs[j]
                for half in range(2):
                    pi_ = next_ps()
                    for fc in range(4):
                        fw.op('pe', lambda: nc.tensor.matmul(PS[pi_][:, 0:512], lhsT=GTs[j][:, fc, :], rhs=W2B[i2][:, fc * 1024 + half * 512:fc * 1024 + (half + 1) * 512], start=(fc == 0), stop=(fc == 3)),
                              reads=['GTs%d' % j, 'W2B%d' % i2], writes=['ps%d' % pi_])
                    if half == 0:
                        fw.op('act', lambda: nc.scalar.copy(out=ob[:, 0:512], in_=PS[pi_][:, 0:512]), reads=['ps%d' % pi_], writes=['OBs%d' % j])
                    else:
                        fw.op('dve', lambda: nc.vector.tensor_copy(out=ob[:, 512:1024], in_=PS[pi_][:, 0:512]), reads=['ps%d' % pi_], writes=['OBs%d' % j])
                load('sp', OUT_d[r0:r0 + 128, :], ob[:], 'OUT_d', ['OBs%d' % j])

            loadw(0)
            if NB > 1:
                loadw(1)
            for step in range(NI + 2):
                if step < NI:
                    stA(step)
                if 0 <= step - 1 < NI:
                    stB(step - 1)
                if 0 <= step - 2 < NI:
                    stD(step - 2)
                    bb, ss = items[step - 2]
                    if ss == SUB - 1 and bb + 2 < NB:
                        loadw(bb + 2)
                if 0 <= step - 1 < NI:
                    stC(step - 1)
          fw.barrier()
          with contextlib.ExitStack() as s10:
            IDN2 = sb("IDN2", [128, 128], F32, s10)
            ONF3 = sb("ONF3", [128, 128], F32, s10)
            pv2 = sb("pvec2", [128, 2, 8], F32, s10)
            load('sp', IDN2[:], ident, 'IDN2')
            load('sp', ONF3[:], ones_f, 'ONF3')
            load('sp', pv2[:, 0, :], ln2g, 'pvec2')
            load('sp', pv2[:, 1, :], ln2b, 'pvec2')
            XT2 = sb("XT2", [128, 8, 512], F32, s10)
            SQ2 = sb("SQ2", [128, 8, 512], F32, s10)
            MEAN2 = sb("MEAN2", [128, 512], F32, s10)
            MSQ2 = sb("MSQ2", [128, 512], F32, s10)
            RSTD2 = sb("RSTD2", [128, 512], F32, s10)
            O1s = [sb("O1s%d" % i, [128, D], F32, s10) for i in range(2)]
            O2s = [sb("O2s%d" % i, [128, D], F32, s10) for i in range(2)]
            ng = 0
            if mode == "moe":
                CTL = [(g_ * 512, 512, True) for g_ in range(16)] + [(SEQ, NCTX, False)]
            else:
                CTL = [(o0_, n_, i_ < 4) for i_, (e0_, o0_, n_) in enumerate(OT)]
            for (o0, n, lat) in CTL:
                mv_ = mlat if lat else mctx
                mk_ = 'mlat' if lat else 'mctx'
                load('sp', XT2[:, :, 0:n], X1_d[:, :, o0:o0 + n], 'XT2', ['X1_d'])
                for sub in range(n // 128):
                    t = (o0 + sub * 128) // 128
                    o1, o2 = O1s[ng % 2], O2s[ng % 2]
                    ko1, ko2 = 'O1s%d' % (ng % 2), 'O2s%d' % (ng % 2)
                    ng += 1
                    for (o_, ko_, SL) in ((o1, ko1, SL1), (o2, ko2, SL2)):
                        fw.dma('pool', lambda: nc.gpsimd.indirect_dma_start(out=o_[:], out_offset=None, in_=OUT_d[:, :],
                                                                            in_offset=bass.IndirectOffsetOnAxis(ap=SL[:, t:t + 1], axis=0)),
                               reads=['OUT_d', 'SL'], writes=[ko_])
                    fw.op('dve', lambda: nc.vector.tensor_scalar(out=o1[:], in0=o1[:], scalar1=G1[:, t:t + 1], scalar2=None, op0=ALU.mult), reads=[ko1, 'G1'], writes=[ko1])
                    fw.op('dve', lambda: nc.vector.scalar_tensor_tensor(out=o1[:], in0=o2[:], scalar=G2[:, t:t + 1], in1=o1[:], op0=ALU.mult, op1=ALU.add), reads=[ko1, ko2, 'G2'], writes=[ko1])
                    for half in range(2):
                        pi_ = next_ps()
                        for cc in range(4):
                            c = half * 4 + cc
                            fw.op('pe', lambda: nc.tensor.transpose(out=PS[pi_][:, cc * 128:(cc + 1) * 128], in_=o1[:, c * 128:(c + 1) * 128], identity=IDN2[:]),
                                  reads=[ko1, 'IDN2'], writes=['ps%d' % pi_])
                        for cc in range(4):
                            c = half * 4 + cc
                            fw.op('act', lambda: nc.scalar.activation(out=SQ2[:, c, sub * 128:(sub + 1) * 128], in_=PS[pi_][:, cc * 128:(cc + 1) * 128], func=AF.Copy, scale=mv_[:, 40 + c:41 + c]),
                                  reads=['ps%d' % pi_, mk_], writes=['SQ2'])
                fw.op('dve', lambda: nc.vector.scalar_tensor_tensor(out=XT2[:, :, 0:n], in0=XT2[:, :, 0:n], scalar=ALPHA, in1=SQ2[:, :, 0:n], op0=ALU.mult, op1=ALU.add),
                      reads=['XT2', 'SQ2'], writes=['XT2'])
                layer_norm(XT2, 'XT2', n, SQ2, 'SQ2', MEAN2, 'MEAN2', MSQ2, 'MSQ2', RSTD2, 'RSTD2', ONF3, 'ONF3',
                           lambda c: pv2[:, 0, c:c + 1], lambda c: pv2[:, 1, c:c + 1], 'pvec2')
                if lat:
                    load('sp', xout[:, :, o0:o0 + n], XT2[:, :, 0:n], 'xout', ['XT2'])
                else:
                    load('sp', cout[:, :, :], XT2[:, :, 0:n], 'cout', ['XT2'])
            outs.extend(['xout', 'cout'])
        sHT.close()
        fw.barrier()


def _fm(a):
    return np.ascontiguousarray(a.T.reshape(8, 128, a.shape[0]).transpose(1, 0, 2))


def _pp(v):
    return np.ascontiguousarray(v.reshape(-1, 128).T)


def _kc(w):
    return np.ascontiguousarray(w.reshape(8, 128, w.shape[1]).transpose(1, 0, 2))


def _partner_cols(nh):
    idx = []
    for h in range(nh):
        for d in range(64):
            half, i = d // 32, d % 32
            p = i + 16 if i < 16 else i - 16
            idx.append(h * 64 + half * 32 + p)
    return np.array(idx)


def prep_layer_weights(inp, l):
    w_in = inp['w_in'][l]
    b_in = inp['b_in'][l]
    o = np.cumsum([0, 512, 512, 512, 3072, 1024, 128, 128, 1024])
    q, u, v, g, z, k, va, xr = [slice(o[i], o[i + 1]) for i in range(8)]
    pq = _partner_cols(8)
    pk = _partner_cols(2)

    def cols(a):
        return np.concatenate([a[..., q], a[..., q][..., pq], a[..., k], a[..., k][..., pk], a[..., u], a[..., v],
                               a[..., va], a[..., g], a[..., z], a[..., xr]], axis=-1)
    W = {}
    W['w_in'] = _kc(cols(w_in))
    W['b_in'] = _pp(cols(b_in))
    W['w_mod'] = _kc(inp['w_mod'][l])
    W['b_mod'] = _pp(inp['b_mod'][l])
    W['convw'] = np.ascontiguousarray(inp['conv_w'][l].reshape(2, 4, 8, 128).transpose(3, 2, 0, 1))
    W['convb'] = np.ascontiguousarray(inp['conv_b'][l].reshape(2, 8, 128).transpose(2, 1, 0))
    W['wrg'] = np.ascontiguousarray(inp['w_rgate'][l].transpose(2, 0, 1, 3))
    W['wig'] = np.ascontiguousarray(inp['w_igate'][l].transpose(2, 0, 1, 3))
    W['brg'] = np.ascontiguousarray(inp['b_rgate'][l].reshape(2, 8, 128).transpose(2, 1, 0))
    W['big'] = np.ascontiguousarray(inp['b_igate'][l].reshape(2, 8, 128).transpose(2, 1, 0))
    W['lam'] = np.ascontiguousarray(inp['lru_lambda'][l].reshape(2, 8, 128).transpose(2, 1, 0))
    bq, bqs, bk, bks = b_in[q], b_in[q][pq], b_in[k], b_in[k][pk]
    W['b_qk'] = np.ascontiguousarray(np.concatenate([bq.reshape(8, 64), bqs.reshape(8, 64), bk.reshape(2, 64), bks.reshape(2, 64)], 0).T)
    W['b_va'] = np.ascontiguousarray(np.broadcast_to(b_in[va][None, :], (128, 128)))
    W['b_v'] = np.ascontiguousarray(np.broadcast_to(b_in[v][None, :], (128, 512)))
    W['sinkx'] = np.ascontiguousarray(np.broadcast_to(inp['attn_sink'][l][None, :, None], (64, 8, 128)))
    W['sgu_g'] = np.ascontiguousarray(np.broadcast_to(inp['sgu_ln_g'][l][None, :], (128, 512)))
    W['sgu_b'] = np.ascontiguousarray(np.broadcast_to(inp['sgu_ln_b'][l][None, :], (128, 512)))
    W['w_spT'] = np.ascontiguousarray(inp['w_spatial'][l].transpose(2, 0, 1))
    W['b_sp'] = np.ascontiguousarray(np.broadcast_to(inp['b_spatial'][l][None, :, :], (128, 4, 128)))
    W['w_pa'] = np.ascontiguousarray(inp['w_proj_a'][l].reshape(4, 128, D).transpose(1, 0, 2))
    W['w_pb'] = np.ascontiguousarray(inp['w_proj_b'][l].reshape(8, 64, D).transpose(1, 0, 2))
    W['w_pc'] = _kc(inp['w_proj_c'][l])
    W['w_o'] = _kc(inp['w_out'][l])
    W['b_o'] = _pp(inp['b_out'][l])
    W['ln1g'] = _pp(inp['ln1_g'][l]); W['ln1b'] = _pp(inp['ln1_b'][l])
    W['ln2g'] = _pp(inp['ln2_g'][l]); W['ln2b'] = _pp(inp['ln2_b'][l])
    W['w_gr'] = _kc(np.concatenate([inp['w_group'][l], inp['w_router'][l]], 1))
    w1r = inp['w1'][l].reshape(NEXP, 8, 128, 512).transpose(0, 2, 1, 3).reshape(NEXP * 128, 4096)
    w3r = inp['w3'][l].reshape(NEXP, 8, 128, 512).transpose(0, 2, 1, 3).reshape(NEXP * 128, 4096)
    w2r = inp['w2'][l].reshape(NEXP, 4, 128, 1024).transpose(0, 2, 1, 3).reshape(NEXP * 128, 4096)
    for nm, arr in (("w1", w1r), ("w3", w3r), ("w2", w2r)):
        W[nm + 'a'] = np.ascontiguousarray(arr[:, :2048])
        W[nm + 'b'] = np.ascontiguousarray(arr[:, 2048:])
    W['b_gr'] = np.ascontiguousarray(np.broadcast_to(np.concatenate([inp['b_group'][l], inp['b_router'][l]])[None, :], (128, 36)))
    return W


def const_inputs(core):
    s = core % NSEG
    pos = np.arange(TLAT) + s * TOWN - HALO
    row = (pos // 64).astype(np.float32)
    colp = (pos % 64).astype(np.float32)
    inv = (10000.0 ** (-np.arange(0, 32, 2, dtype=np.float32) / 32)).astype(np.float32)
    cos = np.zeros((64, TLAT), np.float32)
    sin = np.zeros((64, TLAT), np.float32)
    for d in range(64):
        half, i = d // 32, d % 32
        ang = (row if half == 0 else colp) * inv[i % 16]
        cos[d] = np.cos(ang)
        sin[d] = np.sin(ang) * (-1.0 if i < 16 else 1.0)
    kk = np.arange(128)[:, None]
    qq = np.arange(128)[None, :]
    m = np.zeros((128, 2, 4, 128), np.float32)
    m[:, 0] = (kk >= qq).astype(np.float32)[:, None, :]
    m[:, 1] = (kk <= qq).astype(np.float32)[:, None, :]
    return {'rope_cos': cos, 'rope_sin': sin, 'masks': m, 'ones_in': np.ones((128, 64), np.float32),
            'ident': np.eye(128, dtype=np.float32), 'tri_in': np.triu(np.ones((128, 128), np.float32), 1),
            'bval_in': np.ascontiguousarray(np.broadcast_to((np.arange(NBLK, dtype=np.float32) * 128)[None, :], (128, NBLK))),
            'pidx_in': np.arange(128, dtype=np.float32)[:, None].copy(), 'ones_f': np.ones((128, 128), np.float32)}


def core_inputs(x, ctx, c, c_ctx, core):
    b, s = core // NSEG, core % NSEG
    lo = s * TOWN - HALO
    ext = np.zeros((TLAT, D), np.float32)
    a, e = max(lo, 0), min(lo + TLAT, SEQ)
    ext[a - lo:e - lo] = x[b, a:e]
    m = {'xT': _fm(ext), 'cT': _fm(ctx[b])}
    m['cond'] = np.ascontiguousarray(np.stack([_pp(c[b]), _pp(c_ctx)], -1))
    ed = np.zeros((128, 2), np.float32)
    ed[:, 0] = 1.0 if s > 0 else 0.0
    ed[:, 1] = 1.0 if s < NSEG - 1 else 0.0
    m['edge'] = ed
    return m


def carry_input(ab, core):
    car = np.zeros((128, 8, 2, 3, 2), np.float32)
    car[..., 0] = 1.0
    if ab is None:
        return car
    b, s = core // NSEG, core % NSEG
    fwd = [b * NSEG + j for j in range(0, s)]
    bwd = [b * NSEG + j for j in range(NSEG - 1, s, -1)]
    for k, cc in enumerate(fwd):
        car[:, :, 0, k, :] = ab[cc][:, :, 0, :]
    for k, cc in enumerate(bwd):
        car[:, :, 1, k, :] = ab[cc][:, :, 1, :]
    return car


PRE_NAMES = ['w_mod', 'b_mod', 'w_in', 'b_in', 'convw', 'convb', 'wrg', 'wig', 'brg', 'big', 'lam']
XPAD = SEQ + 2 * HALO


def all_consts():
    pos = np.arange(XPAD) - HALO
    row = (pos // 64).astype(np.float32)
    colp = (pos % 64).astype(np.float32)
    inv = (10000.0 ** (-np.arange(0, 32, 2, dtype=np.float32) / 32)).astype(np.float32)
    cos = np.zeros((64, XPAD), np.float32)
    sin = np.zeros((64, XPAD), np.float32)
    for d in range(64):
        half, i = d // 32, d % 32
        ang = (row if half == 0 else colp) * inv[i % 16]
        cos[d] = np.cos(ang)
        sin[d] = np.sin(ang) * (-1.0 if i < 16 else 1.0)
    C = const_inputs(0)
    C.pop('rope_cos')
    C.pop('rope_sin')
    C['rope_cos_all'] = cos
    C['rope_sin_all'] = sin
    ed = np.ones((NSEG, 128, 2), np.float32)
    ed[0, :, 0] = 0.0
    ed[NSEG - 1, :, 1] = 0.0
    C['edge_all'] = ed
    C['car_id'] = carry_input(None, 0)
    return C


def build_fused(w_shapes, c_shapes):
    nc = bass.Bass("TRN2", target_bir_lowering=False)

    def ein(name, shape, dt=F32):
        return nc.dram_tensor(name, list(shape), dt, kind="ExternalInput").ap()
    X0 = ein("X0", [128, 8, XPAD])
    C0 = ein("C0", [128, 8, NCTX])
    cond = ein("cond", [128, 8, 2])
    EXPN = ("w1a", "w1b", "w3a", "w3b", "w2a", "w2b")
    Wall = {k: ein(k, [DEPTH] + list(shp)) for k, shp in w_shapes.items() if k not in EXPN}
    Wexp = {k: [ein("%s_%d" % (k, l_), list(w_shapes[k])) for l_ in range(DEPTH)] for k in EXPN}
    Call = {k: ein(k, shp) for k, shp in c_shapes.items()}
    yout = nc.dram_tensor("yout", [128, 8, SEQ], F32, kind="ExternalOutput").ap()
    XS = [nc.dram_tensor("XS%d" % i, [128, 8, XPAD], F32).ap() for i in range(2)]
    CS = [nc.dram_tensor("CS%d" % i, [128, 8, NCTX], F32).ap() for i in range(3)]
    AB_d = nc.dram_tensor("AB_d", [NSEG, 128, 8, 2, 2], F32).ap()
    CAR_d = nc.dram_tensor("CAR_d", [128, 8, 2, 3, 2], F32).ap()
    with contextlib.ExitStack() as st0:
        sh = Shared(nc, st0)
        fw = sh.fw
        with contextlib.ExitStack() as sz:
            zt = sz.enter_context(nc.sbuf_tensor("zt", [128, 8, HALO], F32))
            fw.op('dve', lambda: nc.vector.memset(zt[:], 0.0), writes=['zt'])
            for i in range(2):
                fw.dma('sp', lambda: nc.sync.dma_start(out=XS[i][:, :, 0:HALO], in_=zt[:]), reads=['zt'], writes=['XS'])
                fw.dma('sp', lambda: nc.sync.dma_start(out=XS[i][:, :, HALO + SEQ:XPAD], in_=zt[:]), reads=['zt'], writes=['XS'])
            fw.barrier()
        for l in range(DEPTH):
            Xsrc = X0 if l == 0 else XS[(l - 1) % 2]
            Xdst = XS[l % 2]
            Csrc = C0 if l == 0 else CS[(l - 1) % 2]
            Cdst = CS[l % 2]
            for prepass in (True, False):
                for seg in range(NSEG):
                    if not prepass:
                        fwd = list(range(0, seg))
                        bwd = list(range(NSEG - 1, seg, -1))
                        for d_, lst in ((0, fwd), (1, bwd)):
                            for k in range(3):
                                if k < len(lst):
                                    src_ = AB_d[lst[k]][:, :, d_, :]
                                else:
                                    src_ = Call['car_id'][:, :, d_, k, :]
                                fw.dma('sp', lambda: nc.sync.dma_start(out=CAR_d[:, :, d_, k, :], in_=src_), writes=['CAR_d'])
                        fw.barrier()

                    def getin(name, shape, dt=F32, seg=seg, prepass=prepass):
                        if name == 'xT':
                            return Xsrc[:, :, seg * TOWN:seg * TOWN + TLAT]
                        if name == 'cT':
                            return Csrc
                        if name == 'cond':
                            return cond
                        if name == 'edge':
                            return Call['edge_all'][seg]
                        if name == 'car':
                            return Call['car_id'] if prepass else CAR_d
                        if name == 'rope_cos':
                            return Call['rope_cos_all'][:, seg * TOWN:seg * TOWN + TLAT]
                        if name == 'rope_sin':
                            return Call['rope_sin_all'][:, seg * TOWN:seg * TOWN + TLAT]
                        if name in Call:
                            return Call[name]
                        if name in Wexp:
                            return Wexp[name][l]
                        return Wall[name][l]

                    def getout(name, shape, dt=F32, seg=seg):
                        if name == 'ab_out':
                            return AB_d[seg]
                        if name == 'xout':
                            return XS[l % 2][:, :, HALO:HALO + TOWN]
                        if name == 'cout':
                            return CS[2]
                        raise KeyError(name)
                    if prepass:
                        emit_layer(nc, sh, getin, getout, True, seg=seg, lru="store")
                    else:
                        emit_layer(nc, sh, getin, getout, False, mode="premoe", seg=seg, lru="load")

            def getout_m(name, shape, dt=F32):
                if name == 'xout':
                    return yout if l == DEPTH - 1 else Xdst[:, :, HALO:HALO + SEQ]
                if name == 'cout':
                    return Cdst
                return CS[2]
            emit_layer(nc, sh, getin, getout_m, False, mode="moe")
        fw.finish([])
        print("fused instructions:", fw.n_instr)
    return nc


_PROG = {}


def _unfm(a):
    return np.ascontiguousarray(a.transpose(2, 1, 0).reshape(a.shape[2], D))


def kernel(**inputs):
    inp = {k: np.asarray(v) for k, v in inputs.items()}
    x = np.ascontiguousarray(inp['x'], dtype=np.float32)
    ctx = np.ascontiguousarray(inp['ctx'], dtype=np.float32)
    c, c_ctx = inp['c'].astype(np.float32), inp['c_ctx'].astype(np.float32)
    Ws = [prep_layer_weights(inp, l) for l in range(DEPTH)]
    EXPN = ("w1a", "w1b", "w3a", "w3b", "w2a", "w2b")
    Wst = {k: np.stack([Ws[l][k] for l in range(DEPTH)], 0) for k in Ws[0] if k not in EXPN}
    Wex = {"%s_%d" % (k, l): Ws[l][k] for k in EXPN for l in range(DEPTH)}
    wshapes = {k: Ws[0][k].shape for k in Ws[0]}
    del Ws
    C = all_consts()
    if 'fused' not in _PROG:
        _PROG['fused'] = build_fused(wshapes, {k: v.shape for k, v in C.items()})
    nc = _PROG['fused']
    cores = list(range(8))
    per_batch = []
    for b in range(2):
        xp = np.zeros((XPAD, D), np.float32)
        xp[HALO:HALO + SEQ] = x[b]
        m = {'X0': _fm(xp), 'C0': _fm(ctx[b]), 'cond': np.ascontiguousarray(np.stack([_pp(c[b]), _pp(c_ctx)], -1))}
        m.update(Wst)
        m.update(Wex)
        m.update(C)
        per_batch.append(m)
    maps = [per_batch[cc // NSEG] for cc in cores]
    res = run_bass_kernel_spmd(nc, maps, core_ids=cores)
    out = np.empty_like(x)
    for b in range(2):
        out[b] = _unfm(np.asarray(res.results[b * NSEG]['yout']))
    return out
```

```python
import contextlib
import numpy as np
import concourse.bass as bass
import concourse.mybir as mybir
from concourse.bass_utils import run_bass_kernel_spmd

F32 = mybir.dt.float32
BF16 = mybir.dt.bfloat16
I32 = mybir.dt.int32
AF = mybir.ActivationFunctionType
ALU = mybir.AluOpType
AX = mybir.AxisListType

D = 1024
DEPTH = 4
SEQ = 8192
NCTX = 256
NSEG = 4
TOWN = 2048
HALO = 128
TLAT = TOWN + 2 * HALO
TEXT = TLAT + NCTX
NOWN = TOWN + NCTX
ALPHA = (2.0 * DEPTH) ** 0.25
LN_EPS = 1e-6
NEXP = 32
NBLK = (NOWN * 2) // 128 + NEXP
C_Q, C_QS, C_K, C_KS, C_U, C_V, C_VA, C_G, C_Z, C_XR = 0, 512, 1024, 1152, 1280, 1792, 2304, 2432, 5504, 6528
NCOL = 7552


class FW:
    NDMA = 24

    def __init__(self, nc, stack):
        self.nc = nc
        self.E = {'pe': nc.tensor, 'act': nc.scalar, 'dve': nc.vector, 'pool': nc.gpsimd, 'sp': nc.sync}
        self.sem = {}
        self.cnt = {}
        for e in ('pe', 'act', 'dve', 'pool'):
            self.sem[e] = stack.enter_context(nc.semaphore('s_' + e))
            self.cnt[e] = 0
        self.dsem = [stack.enter_context(nc.semaphore('d%d' % i)) for i in range(self.NDMA)]
        self.dval = [0] * self.NDMA
        self.dnext = 0
        self.waited = {}
        self.lastw = {}
        self.readers = {}
        self.n_instr = 0

    def _wait(self, e, tok):
        if tok is None:
            return
        kind, sk, val = tok
        k = (e, kind, sk)
        if self.waited.get(k, 0) >= val:
            return
        self.waited[k] = val
        s = self.sem[sk] if kind == 'eng' else self.dsem[sk]
        self.E[e].wait_ge(s, val)

    def _deps(self, e, reads, writes, strict=False):
        for r in reads:
            t = self.lastw.get(r)
            if t is not None:
                if t[0] == 'eng' and t[1] == e and e == 'pe':
                    continue
                self._wait(e, t)
        for w in writes:
            t = self.lastw.get(w)
            if t is not None and (strict or not (t[0] == 'eng' and t[1] == e)):
                self._wait(e, t)
            for t in self.readers.get(w, ()):
                if strict or not (t[0] == 'eng' and t[1] == e):
                    self._wait(e, t)

    def _commit(self, tok, reads, writes):
        for r in reads:
            self.readers.setdefault(r, []).append(tok)
        for w in writes:
            self.lastw[w] = tok
            self.readers[w] = []

    def op(self, e, fn, reads=(), writes=()):
        self._deps(e, reads, writes)
        ins = fn()
        self.cnt[e] += 1
        ins.then_inc(self.sem[e], 1)
        self._commit(('eng', e, self.cnt[e]), reads, writes)
        self.n_instr += 1
        return ins

    def dma(self, q, fn, reads=(), writes=()):
        i = self.dnext
        self.dnext = (self.dnext + 1) % self.NDMA
        if self.dval[i] > 0:
            self._wait(q, ('dma', i, self.dval[i]))
        self._deps(q, reads, writes, strict=True)
        ins = fn()
        self.dval[i] += 16
        ins.then_inc(self.dsem[i], 16)
        self._commit(('dma', i, self.dval[i]), reads, writes)
        self.n_instr += 1
        return ins

    def barrier(self):
        toks = [('eng', e2, self.cnt[e2]) for e2 in ('pe', 'act', 'dve', 'pool') if self.cnt[e2]]
        toks += [('dma', i, self.dval[i]) for i in range(self.NDMA) if self.dval[i]]
        for e in ('pe', 'act', 'dve', 'pool', 'sp'):
            for t in toks:
                if t[0] == 'eng' and t[1] == e:
                    continue
                self._wait(e, t)

    def finish(self, keys, e='sp'):
        for k in keys:
            self._wait(e, self.lastw.get(k))
        for i in range(self.NDMA):
            if self.dval[i]:
                self._wait(e, ('dma', i, self.dval[i]))


class Shared:
    def __init__(self, nc, st):
        self.nc = nc
        self.fw = FW(nc, st)
        self.PS = [st.enter_context(nc.psum_tensor("ps%d" % i, [128, 512], F32)) for i in range(7)]
        self.psi = 0
        self.uid = 0
        self.scr = {}

    def next_ps(self):
        i = self.psi
        self.psi = (i + 1) % 7
        return i

    def scratch(self, name, shape, dt):
        if name not in self.scr:
            self.scr[name] = self.nc.dram_tensor(name, list(shape), dt).ap()
        return self.scr[name]


def build_layer(prepass, dbg=False, phases=("lru", "att", "sgu", "merge", "moe", "moe8")):
    nc = bass.Bass("TRN2", target_bir_lowering=False)

    def din(name, shape, dt=F32):
        return nc.dram_tensor(name, list(shape), dt, kind="ExternalInput").ap()

    def dout(name, shape, dt=F32):
        return nc.dram_tensor(name, list(shape), dt, kind="ExternalOutput").ap()
    with contextlib.ExitStack() as st0:
        sh = Shared(nc, st0)
        emit_layer(nc, sh, din, dout, prepass, dbg, phases)
        sh.fw.finish([])
    return nc


def emit_layer(nc, sh, din, dout, prepass, dbg=False, phases=("lru", "att", "sgu", "merge", "moe", "moe8"), mode="full", seg=0, lru="compute", mod="compute"):
    xT = din("xT", [128, 8, TLAT])
    cT = din("cT", [128, 8, NCTX])
    cond = din("cond", [128, 8, 2])
    w_mod = din("w_mod", [128, 8, 6 * D])
    b_mod = din("b_mod", [128, 48])
    w_in = din("w_in", [128, 8, NCOL])
    b_in = din("b_in", [128, NCOL // 128])
    edge = din("edge", [128, 2])
    convw = din("convw", [128, 8, 2, 4])
    convb = din("convb", [128, 8, 2])
    wrg = din("wrg", [128, 2, 8, 128])
    wig = din("wig", [128, 2, 8, 128])
    brg = din("brg", [128, 8, 2])
    big = din("big", [128, 8, 2])
    lam = din("lam", [128, 8, 2])
    car = din("car", [128, 8, 2, 3, 2])
    ab_out = dout("ab_out", [128, 8, 2, 2])
    dbg_outs = {}
    if dbg:
        dbg_outs["r_dbg"] = dout("r_dbg", [128, 8, NOWN])
        dbg_outs["a_dbg"] = dout("a_dbg", [128, 4, NOWN])
        dbg_outs["b_dbg"] = dout("b_dbg", [64, 8, NOWN])
        dbg_outs["kt_dbg"] = dout("kt_dbg", [64, 2, TEXT])
        dbg_outs["x1_dbg"] = dout("x1_dbg", [128, 8, NOWN])
        dbg_outs["lg_dbg"] = dout("lg_dbg", [128, 18, 36])
        dbg_outs["sl_dbg"] = dout("sl_dbg", [128, 2, 18], I32)
        dbg_outs["g_dbg"] = dout("g_dbg", [128, 2, 18])
        dbg_outs["wi_dbg"] = dout("wi_dbg", [128, NBLK], I32)
        dbg_outs["vt_dbg"] = dout("vt_dbg", [128, 20, 128])
    if not prepass:
        rope_cos = din("rope_cos", [64, TLAT])
        rope_sin = din("rope_sin", [64, TLAT])
        masks = din("masks", [128, 2, 4, 128])
        sinkx = din("sinkx", [64, 8, 128])
        ones_in = din("ones_in", [128, 64])
        b_va = din("b_va", [128, 128])
        b_qk = din("b_qk", [64, 20])
        b_v = din("b_v", [128, 512])
        sgu_g = din("sgu_g", [128, 512])
        sgu_b = din("sgu_b", [128, 512])
        w_spT = din("w_spT", [128, 4, 128])
        b_sp = din("b_sp", [128, 4, 128])
        w_pa = din("w_pa", [128, 4, D])
        w_pb = din("w_pb", [64, 8, D])
        w_pc = din("w_pc", [128, 8, D])
        w_o = din("w_o", [128, 8, D])
        b_o = din("b_o", [128, 8])
        ln1g = din("ln1g", [128, 8])
        ln1b = din("ln1b", [128, 8])
        ln2g = din("ln2g", [128, 8])
        ln2b = din("ln2b", [128, 8])
        w_gr = din("w_gr", [128, 8, 36])
        b_gr = din("b_gr", [128, 36])
        ident = din("ident", [128, 128])
        ones_f = din("ones_f", [128, 128])
        tri_in = din("tri_in", [128, 128])
        bval_in = din("bval_in", [128, NBLK])
        pidx_in = din("pidx_in", [128, 1])
        wexp = {}
        for nm in ("w1a", "w1b", "w3a", "w3b", "w2a", "w2b"):
            wexp[nm] = din(nm, [NEXP * 128, 2048])
        xout = dout("xout", [128, 8, TOWN])
        cout = dout("cout", [128, 8, NCTX])

    sh.uid += 1
    sfx = "_%d" % sh.uid
    if mode == "premoe":
        phases = ("lru", "att", "sgu", "merge")
    elif mode == "moe":
        phases = ("moe", "moe8")
    GL = mode in ("premoe", "moe")
    T = 66 if mode == "moe" else 18
    SUB = 4 if mode == "moe" else 1
    BS = 128 * SUB
    NB = (33 + NEXP) if mode == "moe" else NBLK
    with contextlib.ExitStack() as st:
        fw = sh.fw

        def sb(name, shape, dt=F32, stack=st):
            return stack.enter_context(nc.sbuf_tensor(name + sfx, list(shape), dt))

        def ps(name, shape, dt=F32, stack=st):
            return stack.enter_context(nc.psum_tensor(name + sfx, list(shape), dt))

        PS = sh.PS
        next_ps = sh.next_ps

        def load(q, dst, src, wkey, rkeys=()):
            eng = {'sp': nc.sync, 'act': nc.scalar, 'pool': nc.gpsimd}[q]
            return fw.dma(q, lambda: eng.dma_start(out=dst, in_=src), reads=list(rkeys), writes=[wkey])

        condt = sb("condt", [128, 8, 2])
        condb = sb("condb", [128, 8, 2], BF16)
        sig = sb("sig", [128, 8, 2])
        bmod = sb("bmod", [128, 48])
        mlat = sb("mlat", [128, 48])
        mctx = sb("mctx", [128, 48])
        bint = sb("bint", [128, NCOL // 128])
        edget = sb("edget", [128, 2])
        cw = sb("cw", [128, 8, 2, 4])
        cb = sb("cb", [128, 8, 2])
        brt = sb("brt", [128, 8, 2])
        bit_ = sb("bit", [128, 8, 2])
        lamt = sb("lamt", [128, 8, 2])
        negc = sb("negc", [128, 8, 2])
        negc2 = sb("negc2", [128, 8, 2])
        cart = sb("cart", [128, 8, 2, 3, 2])
        abt = sb("abt", [128, 8, 2, 2])
        load('sp', condt[:], cond, 'condt')
        load('sp', bmod[:], b_mod, 'bmod')
        load('sp', bint[:], b_in, 'bint')
        load('sp', edget[:], edge, 'edget')
        load('sp', cw[:], convw, 'cw')
        load('sp', cb[:], convb, 'cb')
        load('sp', brt[:], brg, 'brt')
        load('sp', bit_[:], big, 'bit')
        load('sp', lamt[:], lam, 'lamt')
        load('sp', cart[:], car, 'cart')

        fw.op('act', lambda: nc.scalar.activation(out=sig[:], in_=condt[:], func=AF.Sigmoid), reads=['condt'], writes=['sig'])
        fw.op('dve', lambda: nc.vector.tensor_tensor(out=condb[:], in0=condt[:], in1=sig[:], op=ALU.mult), reads=['condt', 'sig'], writes=['condb'])
        fw.op('act', lambda: nc.scalar.activation(out=negc[:], in_=lamt[:], func=AF.Exp, scale=-1.0), reads=['lamt'], writes=['negc'])
        fw.op('act', lambda: nc.scalar.activation(out=negc[:], in_=negc[:], func=AF.Ln, bias=1.0), reads=['negc'], writes=['negc'])
        fw.op('dve', lambda: nc.vector.tensor_scalar(out=negc2[:], in0=negc[:], scalar1=-16.0, scalar2=None, op0=ALU.mult), reads=['negc'], writes=['negc2'])
        fw.op('dve', lambda: nc.vector.tensor_scalar(out=negc[:], in0=negc[:], scalar1=-8.0, scalar2=None, op0=ALU.mult), reads=['negc'], writes=['negc'])

        MOD_d = sh.scratch("MOD_d", [128, 96], F32) if mod != "compute" else None
        if mod == "load":
            load('sp', mlat[:], MOD_d[:, 0:48], 'mlat')
            load('sp', mctx[:], MOD_d[:, 48:96], 'mctx')
        with contextlib.ExitStack() as s1:
            wm = [sb("wm%d" % i, [128, 8, 512], BF16, s1) for i in range(2)]
            if mod == "load":
                wm = None
            pmi = next_ps()
            pm = PS[pmi]
            pmk = 'ps%d' % pmi
            for g in (range(12) if mod != "load" else ()):
                wbuf = wm[g % 2]
                load('pool', wbuf[:], w_mod[:, :, g * 512:(g + 1) * 512], 'wm%d' % (g % 2))
                for jj in range(4):
                    j = g * 4 + jj
                    for kc in range(8):
                        fw.op('pe', lambda: nc.tensor.matmul(pm[:, 2 * j:2 * j + 2], lhsT=wbuf[:, kc, jj * 128:(jj + 1) * 128],
                                                              rhs=condb[:, kc, :], start=(kc == 0), stop=(kc == 7)),
                              reads=['wm%d' % (g % 2), 'condb'], writes=[pmk])
            pmv = pm[:, 0:96].rearrange("p (j t) -> p j t", t=2)
            if mod != "load":
                fw.op('dve', lambda: nc.vector.tensor_tensor(out=mlat[:], in0=pmv[:, :, 0], in1=bmod[:], op=ALU.add), reads=[pmk, 'bmod'], writes=['mlat'])
                fw.op('dve', lambda: nc.vector.tensor_tensor(out=mctx[:], in0=pmv[:, :, 1], in1=bmod[:], op=ALU.add), reads=[pmk, 'bmod'], writes=['mctx'])
            if mod == "store":
                load('sp', MOD_d[:, 0:48], mlat[:], 'MOD_d', ['mlat'])
                load('sp', MOD_d[:, 48:96], mctx[:], 'MOD_d', ['mctx'])
        fw.barrier()
        m1p = sb("m1p", [128, 2, 8])
        m4p = sb("m4p", [128, 2, 8])
        for t_, mv in ((0, mlat), (1, mctx)):
            fw.op('dve', lambda: nc.vector.tensor_scalar(out=m1p[:, t_, :], in0=mv[:, 8:16], scalar1=1.0, scalar2=None, op0=ALU.add), reads=['mlat', 'mctx'], writes=['m1p'])
            fw.op('dve', lambda: nc.vector.tensor_scalar(out=m4p[:, t_, :], in0=mv[:, 32:40], scalar1=1.0, scalar2=None, op0=ALU.add), reads=['mlat', 'mctx'], writes=['m4p'])

        sY = st.enter_context(contextlib.ExitStack())
        yT = sY.enter_context(nc.sbuf_tensor("yT" + sfx, [128, 8, NOWN if not prepass else 8], BF16, side='right'))
        sHT = contextlib.ExitStack()
        hT = sHT.enter_context(nc.sbuf_tensor("hT" + sfx, [128, 8, TEXT], BF16, side='right'))
        with contextlib.ExitStack() as s2:
            xs = [sb("xs%d" % i, [128, TLAT], F32, s2) for i in range(2)]
            for c in (range(8) if mode != "moe" else ()):
                xb_ = xs[c % 2]
                k = 'xs%d' % (c % 2)
                load('sp', xb_[:], xT[:, c, :], k)
                fw.op('act', lambda: nc.scalar.activation(out=hT[:, c, 0:TLAT], in_=xb_[:], func=AF.Identity,
                                                          scale=m1p[:, 0, c:c + 1], bias=mlat[:, c:c + 1]),
                      reads=[k, 'm1p', 'mlat'], writes=['hT%d' % c])
                load('sp', xb_[:, 0:NCTX], cT[:, c, :], k)
                fw.op('act', lambda: nc.scalar.activation(out=hT[:, c, TLAT:TEXT], in_=xb_[:, 0:NCTX], func=AF.Identity,
                                                          scale=m1p[:, 1, c:c + 1], bias=mctx[:, c:c + 1]),
                      reads=[k, 'm1p', 'mctx'], writes=['hT%d' % c])
        fw.barrier()
        hkeys = ['hT%d' % c for c in range(8)]

        def inproj(col0, ncols, tok0, ntok, wkey, wtile, evac):
            if wtile is not None:
                wsl = lambda kc: wtile[:, kc, col0:col0 + ncols]
            else:
                wsl = col0
            return inproj2(wsl, ncols, tok0, ntok, wkey, evac)

        def inproj2(wsl, ncols, tok0, ntok, wkey, evac):
            pi = next_ps()
            pt = PS[pi]
            for kc in range(8):
                fw.op('pe', lambda: nc.tensor.matmul(pt[0:ncols, 0:ntok], lhsT=wsl(kc), rhs=hT[:, kc, tok0:tok0 + ntok],
                                                      start=(kc == 0), stop=(kc == 7)),
                      reads=[wkey, 'hT%d' % kc], writes=['ps%d' % pi])
            evac(pt, 'ps%d' % pi)

        R_d = sh.scratch("R_d", [128, 8, NOWN], BF16)
        A_d = sh.scratch("A_d", [128, 4, NOWN], BF16)
        B_d = sh.scratch("B_d", [64, 8, NOWN], BF16)
        if "lru" in phases:
          with contextlib.ExitStack() as s3:
            rst = sb("rst", [128, NOWN], BF16, s3)
            wxr = [sb("wxr%d" % i, [128, 8, 128], BF16, s3) for i in range(2)]
            wz = [sb("wz%d" % i, [128, 8, 128], BF16, s3) for i in range(2)]
            wr_t = sb("wr_t", [128, 2, 8, 128], BF16, s3)
            wi_t = sb("wi_t", [128, 2, 8, 128], BF16, s3)
            load('pool', wr_t[:], wrg, 'wr_t')
            load('pool', wi_t[:], wig, 'wi_t')
            XRL = sb("XRL", [128, TLAT if lru != "load" else 8], F32, s3)
            XRC = sb("XRC", [128, NCTX + 6], F32, s3)
            GZ = sb("GZ", [128, NOWN if lru != "store" else 8], F32, s3)
            LAU = sh.scratch("LAU", [NSEG, 8, 2, 2, 128, NOWN], F32) if lru != "compute" else None
            if lru == "load":
                TT = TB = RG = [None, None]
                IG = [sb("IG%d" % i, [128, NOWN], F32, s3) for i in range(2)]
                AA = [sb("AA%d" % i, [128, NOWN], F32, s3) for i in range(2)]
            elif lru == "store":
                TT = [sb("TT%d" % i, [128, NOWN], F32, s3) for i in range(2)]
                TB = [sb("TB%d" % i, [128, NOWN], BF16, s3) for i in range(2)]
                RG = [sb("RG%d" % i, [128, NOWN], F32, s3) for i in range(2)]
                IG = [sb("IG%d" % i, [128, NOWN], F32, s3) for i in range(2)]
                AA = [sb("AA%d" % i, [128, NOWN], F32, s3) for i in range(2)]
            else:
                TT = [sb("TT0", [128, NOWN], F32, s3)] * 2
                TB = [sb("TB0", [128, NOWN], BF16, s3)] * 2
                RG = [sb("RG0", [128, NOWN], F32, s3)] * 2
                IG = [sb("IG0", [128, NOWN], F32, s3)] * 2
                AA = [sb("AA0", [128, NOWN], F32, s3)] * 2
            HH = [sb("HH%d" % i, [128, NOWN], F32, s3) for i in range(2)]
            sm = sb("sm", [128, 16], F32, s3)
            fw.op('pool', lambda: nc.gpsimd.memset(XRC[:], 0.0), writes=['XRC'])
            for j in range(8):
                wx = wxr[j % 2]
                wzz = wz[j % 2]
                kx = 'wxr%d' % (j % 2)
                kz = 'wz%d' % (j % 2)
                load('pool', wx[:], w_in[:, :, C_XR + j * 128:C_XR + (j + 1) * 128], kx)
                load('pool', wzz[:], w_in[:, :, C_Z + j * 128:C_Z + (j + 1) * 128], kz)
                bx = bint[:, (C_XR // 128) + j:(C_XR // 128) + j + 1]
                bz = bint[:, (C_Z // 128) + j:(C_Z // 128) + j + 1]
                for ti in (range(5) if lru != "load" else ()):
                    def ev(pt, pk, ti=ti):
                        if ti < 4:
                            fw.op('act', lambda: nc.scalar.activation(out=XRL[:, ti * 512:(ti + 1) * 512], in_=pt[:, 0:512], func=AF.Identity, bias=bx),
                                  reads=[pk, 'bint'], writes=['XRL'])
                        else:
                            fw.op('act', lambda: nc.scalar.activation(out=XRL[:, 2048:2304], in_=pt[:, 0:256], func=AF.Identity, bias=bx),
                                  reads=[pk, 'bint'], writes=['XRL'])
                            fw.op('act', lambda: nc.scalar.activation(out=XRC[:, 3:3 + NCTX], in_=pt[:, 256:512], func=AF.Identity, bias=bx),
                                  reads=[pk, 'bint'], writes=['XRC'])
                    inproj(0, 128, ti * 512, 512, kx, wx, ev)
                if lru != "load":
                  fw.op('dve', lambda: nc.vector.tensor_scalar(out=XRL[:, 0:HALO], in0=XRL[:, 0:HALO], scalar1=edget[:, 0:1], scalar2=None, op0=ALU.mult),
                      reads=['XRL', 'edget'], writes=['XRL'])
                if lru != "load":
                  fw.op('dve', lambda: nc.vector.tensor_scalar(out=XRL[:, HALO + TOWN:TLAT], in0=XRL[:, HALO + TOWN:TLAT], scalar1=edget[:, 1:2], scalar2=None, op0=ALU.mult),
                      reads=['XRL', 'edget'], writes=['XRL'])
                for ti in (range(5) if lru != "store" else ()):
                    def evz(pt, pk, ti=ti):
                        if ti < 4:
                            fw.op('act', lambda: nc.scalar.activation(out=GZ[:, ti * 512:(ti + 1) * 512], in_=pt[:, 0:512], func=AF.Gelu, bias=bz),
                                  reads=[pk, 'bint'], writes=['GZ'])
                        else:
                            fw.op('act', lambda: nc.scalar.activation(out=GZ[:, 2048:2304], in_=pt[:, 0:256], func=AF.Gelu, bias=bz),
                                  reads=[pk, 'bint'], writes=['GZ'])
                    if ti < 4:
                        inproj(0, 128, HALO + ti * 512, 512, kz, wzz, evz)
                    else:
                        inproj(0, 128, TLAT, 256, kz, wzz, evz)
                for d_ in range(2):
                    T_ = TT[d_]; Tb = TB[d_]; Rg = RG[d_]; Ig = IG[d_]; A_ = AA[d_]; H_ = HH[d_]
                    kT, kTb, kR, kI, kA, kH = 'TT', 'TB', 'RG', 'IG', 'AA', 'HH%d' % d_
                    if lru == "store":
                        kT, kTb, kR, kI, kA = 'TT%d' % d_, 'TB%d' % d_, 'RG%d' % d_, 'IG%d' % d_, 'AA%d' % d_
                    if lru == "load":
                        kI, kA = 'IG%d' % d_, 'AA%d' % d_
                        load('sp', A_[:], LAU[seg, j, d_, 0], kA)
                        load('sp', Ig[:], LAU[seg, j, d_, 1], kI)
                    else:
                        for (dst0, n, src, s0, sk) in ((0, TOWN, XRL, HALO, 'XRL'), (TOWN, NCTX, XRC, 3, 'XRC')):
                            for tap in range(4):
                                off = s0 + (tap - 3 if d_ == 0 else tap)
                                wcol = cw[:, j, d_, tap:tap + 1]
                                if tap == 0:
                                    fw.op('dve', lambda: nc.vector.tensor_scalar(out=T_[:, dst0:dst0 + n], in0=src[:, off:off + n], scalar1=wcol,
                                                                                 scalar2=cb[:, j, d_:d_ + 1], op0=ALU.mult, op1=ALU.add),
                                          reads=[sk, 'cw', 'cb'], writes=[kT])
                                else:
                                    fw.op('dve', lambda: nc.vector.scalar_tensor_tensor(out=T_[:, dst0:dst0 + n], in0=src[:, off:off + n], scalar=wcol,
                                                                                        in1=T_[:, dst0:dst0 + n], op0=ALU.mult, op1=ALU.add),
                                          reads=[sk, 'cw', kT], writes=[kT])
                        fw.op('pool', lambda: nc.gpsimd.tensor_copy(out=Tb[:], in_=T_[:]), reads=[kT], writes=[kTb])
                        for ti in range(5):
                            t0 = ti * 512
                            n = 512 if ti < 4 else 256
                            for (wt_, wk, bt_, bk, dst, dk) in ((wr_t, 'wr_t', brt, 'brt', Rg, kR), (wi_t, 'wi_t', bit_, 'bit', Ig, kI)):
                                pi = next_ps()
                                pt = PS[pi]
                                fw.op('pe', lambda: nc.tensor.matmul(pt[:, 0:n], lhsT=wt_[:, d_, j, :], rhs=Tb[:, t0:t0 + n], start=True, stop=True),
                                      reads=[wk, kTb], writes=['ps%d' % pi])
                                fw.op('act', lambda: nc.scalar.activation(out=dst[:, t0:t0 + n], in_=pt[:, 0:n], func=AF.Sigmoid, bias=bt_[:, j, d_:d_ + 1]),
                                      reads=['ps%d' % pi, bk], writes=[dk])
                        fw.op('act', lambda: nc.scalar.activation(out=A_[:], in_=Rg[:], func=AF.Exp, scale=negc[:, j, d_:d_ + 1]), reads=[kR, 'negc'], writes=[kA])
                        fw.op('act', lambda: nc.scalar.activation(out=H_[:], in_=Rg[:], func=AF.Exp, scale=negc2[:, j, d_:d_ + 1]), reads=[kR, 'negc2'], writes=[kH])
                        fw.op('act', lambda: nc.scalar.activation(out=H_[:], in_=H_[:], func=AF.Sqrt, scale=-1.0, bias=1.0), reads=[kH], writes=[kH])
                        fw.op('pool', lambda: nc.gpsimd.tensor_tensor(out=Ig[:], in0=Ig[:], in1=T_[:], op=ALU.mult), reads=[kI, kT], writes=[kI])
                        fw.op('pool', lambda: nc.gpsimd.tensor_tensor(out=Ig[:], in0=Ig[:], in1=H_[:], op=ALU.mult), reads=[kI, kH], writes=[kI])
                        rs = sm[:, 8 + d_:9 + d_]
                        fw.op('dve', lambda: nc.vector.reduce_sum(out=rs, in_=Rg[:, 0:TOWN], axis=AX.X), reads=[kR], writes=['sm_rs%d' % d_])
                        fw.op('act', lambda: nc.scalar.activation(out=abt[:, j, d_, 0:1], in_=rs, func=AF.Exp, scale=negc[:, j, d_:d_ + 1]),
                              reads=['sm_rs%d' % d_, 'negc'], writes=['abt'])
                        if lru == "store":
                            load('sp', LAU[seg, j, d_, 0], A_[:], 'LAU', [kA])
                            load('sp', LAU[seg, j, d_, 1], Ig[:], 'LAU', [kI])
                    if d_ == 0:
                        a_c, u_c, h_c = A_[:, TOWN:NOWN], Ig[:, TOWN:NOWN], H_[:, TOWN:NOWN]
                        a_l, u_l, h_l = A_[:, 0:TOWN], Ig[:, 0:TOWN], H_[:, 0:TOWN]
                        hc_fin = H_[:, NOWN - 1:NOWN]
                        hl_fin = H_[:, TOWN - 1:TOWN]
                    else:
                        a_c, u_c, h_c = A_[:, TOWN:NOWN][:, ::-1], Ig[:, TOWN:NOWN][:, ::-1], H_[:, TOWN:NOWN][:, ::-1]
                        a_l, u_l, h_l = A_[:, 0:TOWN][:, ::-1], Ig[:, 0:TOWN][:, ::-1], H_[:, 0:TOWN][:, ::-1]
                        hc_fin = H_[:, TOWN:TOWN + 1]
                        hl_fin = H_[:, 0:1]
                    fw.op('dve', lambda: nc.vector.tensor_tensor_scan(out=h_c, data0=a_c, data1=u_c, initial=0.0, op0=ALU.mult, op1=ALU.add),
                          reads=[kA, kI], writes=[kH])
                    hin = sm[:, d_ * 4:d_ * 4 + 1]
                    fw.op('dve', lambda: nc.vector.tensor_copy(out=hin, in_=hc_fin), reads=[kH], writes=['hin%d' % d_])
                    for k3 in range(3):
                        fw.op('dve', lambda: nc.vector.scalar_tensor_tensor(out=hin, in0=hin, scalar=cart[:, j, d_, k3, 0:1], in1=cart[:, j, d_, k3, 1:2],
                                                                            op0=ALU.mult, op1=ALU.add),
                              reads=['hin%d' % d_, 'cart'], writes=['hin%d' % d_])
                    fw.op('dve', lambda: nc.vector.tensor_tensor_scan(out=h_l, data0=a_l, data1=u_l, initial=hin, op0=ALU.mult, op1=ALU.add),
                          reads=[kA, kI, 'hin%d' % d_], writes=[kH])
                    tmp = sm[:, d_ * 4 + 1:d_ * 4 + 2]
                    if lru != "load":
                        fw.op('dve', lambda: nc.vector.tensor_tensor(out=tmp, in0=abt[:, j, d_, 0:1], in1=hin, op=ALU.mult), reads=['abt', 'hin%d' % d_], writes=['tmp%d' % d_])
                        fw.op('dve', lambda: nc.vector.tensor_tensor(out=abt[:, j, d_, 1:2], in0=hl_fin, in1=tmp, op=ALU.subtract), reads=[kH, 'tmp%d' % d_], writes=['abt'])
                if lru != "store":
                    fw.op('pool', lambda: nc.gpsimd.tensor_tensor(out=HH[0][:], in0=HH[0][:], in1=HH[1][:], op=ALU.add), reads=['HH0', 'HH1'], writes=['HH0'])
                    fw.op('pool', lambda: nc.gpsimd.tensor_tensor(out=rst[:], in0=HH[0][:], in1=GZ[:], op=ALU.mult), reads=['HH0', 'GZ'], writes=['rst'])
                    load('sp', R_d[:, j, :], rst[:], 'R_d', ['rst'])
          fw.barrier()
          if lru != "load":
              load('sp', ab_out, abt[:], 'ab_out', ['abt'])
        outs = ['ab_out']
        if prepass:
            sHT.close()
            fw.barrier()
            return
        if "att" in phases:
          with contextlib.ExitStack() as s4:
            KT = sb("KT", [64, 2, TEXT], BF16, s4)
            Vtok = sb("Vtok", [128, 20, 128], BF16, s4)
            COS = sb("COS", [64, TLAT], F32, s4)
            SIN = sb("SIN", [64, TLAT], F32, s4)
            MSK = sb("MSK", [128, 2, 4, 128], F32, s4)
            ESX = sb("ESX", [64, 8, 128], F32, s4)
            ONES = sb("ONES", [128, 64], BF16, s4)
            BVA = sb("BVA", [128, 128], F32, s4)
            bqk = sb("bqk", [64, 20], F32, s4)
            wk_t = sb("wk_t", [128, 8, 256], BF16, s4)
            wva_t = sb("wva_t", [128, 8, 128], BF16, s4)
            wq_t = sb("wq_t", [128, 8, 1024], BF16, s4)
            load('sp', COS[:], rope_cos, 'COS')
            load('sp', SIN[:], rope_sin, 'SIN')
            load('sp', MSK[:], masks, 'MSK')
            load('sp', ESX[:], sinkx, 'ESX')
            load('sp', BVA[:], b_va, 'BVA')
            load('sp', bqk[:], b_qk, 'bqk')
            load('pool', ONES[:], ones_in, 'ONES')
            load('pool', wk_t[:], w_in[:, :, C_K:C_K + 256], 'wk_t')
            load('pool', wva_t[:], w_in[:, :, C_VA:C_VA + 128], 'wva_t')
            load('pool', wq_t[:], w_in[:, :, C_Q:C_Q + 1024], 'wq_t')
            fw.op('act', lambda: nc.scalar.activation(out=ESX[:], in_=ESX[:], func=AF.Exp), reads=['ESX'], writes=['ESX'])
            QF = [sb("QF%d" % i, [64, 512], F32, s4) for i in range(2)]
            QS = [sb("QS%d" % i, [64, 512], F32, s4) for i in range(2)]
            rot = [0]

            def roped(wt, wk, c_main, c_sw, bcol_main, bcol_sw, e0, n, dst, dkey, use_rope):
                i = rot[0] % 2
                rot[0] += 1
                qf, qs = QF[i], QS[i]

                def ev1(pt, pk):
                    fw.op('act', lambda: nc.scalar.activation(out=qf[:, 0:n], in_=pt[0:64, 0:n], func=AF.Identity, bias=bqk[:, bcol_main:bcol_main + 1]),
                          reads=[pk, 'bqk'], writes=['QF%d' % i])
                inproj2(lambda kc: wt[:, kc, c_main:c_main + 64], 64, e0, n, wk, ev1)
                if not use_rope:
                    fw.op('dve', lambda: nc.vector.tensor_copy(out=dst, in_=qf[:, 0:n]), reads=['QF%d' % i], writes=[dkey])
                    return

                def ev2(pt, pk):
                    fw.op('act', lambda: nc.scalar.activation(out=qs[:, 0:n], in_=pt[0:64, 0:n], func=AF.Identity, bias=bqk[:, bcol_sw:bcol_sw + 1]),
                          reads=[pk, 'bqk'], writes=['QS%d' % i])
                inproj2(lambda kc: wt[:, kc, c_sw:c_sw + 64], 64, e0, n, wk, ev2)
                fw.op('dve', lambda: nc.vector.tensor_tensor(out=qf[:, 0:n], in0=qf[:, 0:n], in1=COS[:, e0:e0 + n], op=ALU.mult), reads=['QF%d' % i, 'COS'], writes=['QF%d' % i])
                fw.op('pool', lambda: nc.gpsimd.tensor_tensor(out=qs[:, 0:n], in0=qs[:, 0:n], in1=SIN[:, e0:e0 + n], op=ALU.mult), reads=['QS%d' % i, 'SIN'], writes=['QS%d' % i])
                fw.op('dve', lambda: nc.vector.tensor_tensor(out=dst, in0=qf[:, 0:n], in1=qs[:, 0:n], op=ALU.add), reads=['QF%d' % i, 'QS%d' % i], writes=[dkey])

            for kv in range(2):
                for ti in range(5):
                    if ti < 4:
                        roped(wk_t, 'wk_t', kv * 64, 128 + kv * 64, 16 + kv, 18 + kv, ti * 512, 512, KT[:, kv, ti * 512:(ti + 1) * 512], 'KT', True)
                    else:
                        roped(wk_t, 'wk_t', kv * 64, 128 + kv * 64, 16 + kv, 18 + kv, 2048, 256, KT[:, kv, 2048:2304], 'KT', True)
                        roped(wk_t, 'wk_t', kv * 64, 128 + kv * 64, 16 + kv, 18 + kv, TLAT, 256, KT[:, kv, TLAT:TEXT], 'KT', False)
            for tt in range(20):
                pi = next_ps()
                pt = PS[pi]
                for kc in range(8):
                    fw.op('pe', lambda: nc.tensor.matmul(pt[:, 0:128], lhsT=hT[:, kc, tt * 128:(tt + 1) * 128], rhs=wva_t[:, kc, :], start=(kc == 0), stop=(kc == 7)),
                          reads=['wva_t', 'hT%d' % kc], writes=['ps%d' % pi])
                fw.op('dve', lambda: nc.vector.tensor_tensor(out=Vtok[:, tt, :], in0=pt[:, 0:128], in1=BVA[:], op=ALU.add), reads=['ps%d' % pi, 'BVA'], writes=['Vtok'])
            QT = sb("QT", [64, 8, 512], BF16, s4)
            PTS = [sb("PT%d" % i, [128, 4, 128], BF16, s4) for i in range(3)]
            DEN = sb("DEN", [64, 4, 128], F32, s4)
            BTS = [sb("BTS%d" % i, [64, 8, 128], BF16, s4) for i in range(2)]
            pti = [0]
            nqb = 0
            for ti in range(5):
                lat = ti < 4
                n = 512 if lat else 256
                e0 = HALO + ti * 512 if lat else TLAT
                for h in range(8):
                    roped(wq_t, 'wq_t', h * 64, 512 + h * 64, h, 8 + h, e0, n, QT[:, h, 0:n], 'QT', lat)
                for qb in range(n // 128):
                    q0 = qb * 128
                    if lat:
                        bi = ti * 4 + qb
                        ktiles = [(bi, 'p'), (bi + 1, 'o'), (bi + 2, 'n'), (18, 'c'), (19, 'c')]
                        own0 = bi * 128
                    else:
                        bi = -1
                        ktiles = [(18, 'c'), (19, 'c')]
                        own0 = TOWN + qb * 128
                    bts = BTS[nqb % 2]
                    bk = 'BTS%d' % (nqb % 2)
                    nqb += 1
                    for kv in range(2):
                        po_i = next_ps(); pd_i = next_ps()
                        po, pd = PS[po_i], PS[pd_i]
                        for ki, (tt, kind) in enumerate(ktiles):
                            psi_ = next_ps()
                            pss = PS[psi_]
                            fw.op('pe', lambda: nc.tensor.matmul(pss[:, 0:512], lhsT=KT[:, kv, tt * 128:(tt + 1) * 128],
                                                                  rhs=QT[:, 4 * kv:4 * kv + 4, q0:q0 + 128], start=True, stop=True),
                                  reads=['KT', 'QT'], writes=['ps%d' % psi_])
                            pi3 = pti[0] % 3
                            pti[0] += 1
                            P_ = PTS[pi3]
                            pk3 = 'PT%d' % pi3
                            fw.op('act', lambda: nc.scalar.activation(out=P_[:].rearrange("p g q -> p (g q)"), in_=pss[:, 0:512], func=AF.Exp, scale=0.125),
                                  reads=['ps%d' % psi_], writes=[pk3])
                            if kind in ('p', 'n'):
                                mi = 0 if kind == 'p' else 1
                                if (kind == 'p' and bi == 0) or (kind == 'n' and bi == 15):
                                    fw.op('dve', lambda: nc.vector.scalar_tensor_tensor(out=P_[:], in0=P_[:], scalar=edget[:, mi:mi + 1], in1=MSK[:, mi, :, :], op0=ALU.mult, op1=ALU.mult),
                                          reads=[pk3, 'MSK', 'edget'], writes=[pk3])
                                else:
                                    fw.op('dve', lambda: nc.vector.tensor_tensor(out=P_[:], in0=P_[:], in1=MSK[:, mi, :, :], op=ALU.mult), reads=[pk3, 'MSK'], writes=[pk3])
                            last = ki == len(ktiles) - 1
                            fw.op('pe', lambda: nc.tensor.matmul(po[0:64, 0:512], lhsT=Vtok[:, tt, kv * 64:(kv + 1) * 64], rhs=P_[:].rearrange("p g q -> p (g q)"), start=(ki == 0), stop=last),
                                  reads=['Vtok', pk3], writes=['ps%d' % po_i])
                            fw.op('pe', lambda: nc.tensor.matmul(pd[0:64, 0:512], lhsT=ONES[:], rhs=P_[:].rearrange("p g q -> p (g q)"), start=(ki == 0), stop=last),
                                  reads=['ONES', pk3], writes=['ps%d' % pd_i])
                        fw.op('dve', lambda: nc.vector.tensor_tensor(out=DEN[:], in0=pd[0:64, 0:512].rearrange("p (g q) -> p g q", g=4), in1=ESX[:, 4 * kv:4 * kv + 4, :], op=ALU.add),
                              reads=['ps%d' % pd_i, 'ESX'], writes=['DEN'])
                        fw.op('dve', lambda: nc.vector.reciprocal(out=DEN[:], in_=DEN[:]), reads=['DEN'], writes=['DEN'])
                        fw.op('dve', lambda: nc.vector.tensor_tensor(out=bts[:, 4 * kv:4 * kv + 4, :], in0=po[0:64, 0:512].rearrange("p (g q) -> p g q", g=4), in1=DEN[:], op=ALU.mult),
                              reads=['ps%d' % po_i, 'DEN'], writes=[bk])
                    load('sp', B_d[:, :, own0:own0 + 128], bts[:], 'B_d', [bk])

            if dbg:
                ktf = sb("ktf", [64, 2, TEXT], F32, s4)
                vtf = sb("vtf", [128, 20, 128], F32, s4)
                fw.op('dve', lambda: nc.vector.tensor_copy(out=ktf[:], in_=KT[:]), reads=['KT'], writes=['ktf'])
                fw.op('dve', lambda: nc.vector.tensor_copy(out=vtf[:], in_=Vtok[:]), reads=['Vtok'], writes=['vtf'])
                load('sp', dbg_outs['kt_dbg'], ktf[:], 'kt_dbg', ['ktf'])
                load('sp', dbg_outs['vt_dbg'], vtf[:], 'vt_dbg', ['vtf'])
                outs.extend(['kt_dbg', 'vt_dbg'])
        fw.barrier()
        if "sgu" in phases:
          with contextlib.ExitStack() as s5:
            wu_t = sb("wu_t", [128, 8, 512], BF16, s5)
            wv_t = sb("wv_t", [128, 8, 512], BF16, s5)
            BV = sb("BV", [128, 512], F32, s5)
            LNG = sb("LNG", [128, 512], F32, s5)
            LNB = sb("LNB", [128, 512], F32, s5)
            WST = sb("WST", [128, 4, 128], BF16, s5)
            BSP = sb("BSP", [128, 4, 128], F32, s5)
            load('pool', wu_t[:], w_in[:, :, C_U:C_U + 512], 'wu_t')
            load('pool', wv_t[:], w_in[:, :, C_V:C_V + 512], 'wv_t')
            load('pool', WST[:], w_spT, 'WST')
            load('sp', BV[:], b_v, 'BV')
            load('sp', LNG[:], sgu_g, 'LNG')
            load('sp', LNB[:], sgu_b, 'LNB')
            load('sp', BSP[:], b_sp, 'BSP')
            GU = sb("GU", [128, 4, 512], F32, s5)
            V1 = sb("V1", [128, 512], F32, s5)
            GV = sb("GV", [128, 512], F32, s5)
            VNB = sb("VNB", [128, 512], BF16, s5)
            STT = sb("STT", [128, 6], F32, s5)
            MV = sb("MV", [128, 2], F32, s5)
            RSTD = sb("RSTD", [128, 1], F32, s5)
            MX = sb("MX", [128, 4, 128], F32, s5)
            ATS = [sb("ATS%d" % i, [128, 4, 128], BF16, s5) for i in range(2)]
            nch = 0
            for ti in range(5):
                lat = ti < 4
                n = 512 if lat else 256
                e0 = HALO + ti * 512 if lat else TLAT
                for g in range(4):
                    def evu(pt, pk, g=g):
                        fw.op('act', lambda: nc.scalar.activation(out=GU[:, g, 0:n], in_=pt[:, 0:n], func=AF.Gelu, bias=bint[:, C_U // 128 + g:C_U // 128 + g + 1]),
                              reads=[pk, 'bint'], writes=['GU'])
                    inproj2(lambda kc: wu_t[:, kc, g * 128:(g + 1) * 128], 128, e0, n, 'wu_t', evu)
                for sub in range(n // 128):
                    t0 = e0 + sub * 128
                    own0 = (ti * 512 if lat else TOWN) + sub * 128
                    pi = next_ps()
                    pv = PS[pi]
                    for kc in range(8):
                        fw.op('pe', lambda: nc.tensor.matmul(pv[:, 0:512], lhsT=hT[:, kc, t0:t0 + 128], rhs=wv_t[:, kc, :], start=(kc == 0), stop=(kc == 7)),
                              reads=['wv_t', 'hT%d' % kc], writes=['ps%d' % pi])
                    fw.op('dve', lambda: nc.vector.tensor_tensor(out=V1[:], in0=pv[:, 0:512], in1=BV[:], op=ALU.add), reads=['ps%d' % pi, 'BV'], writes=['V1'])
                    fw.op('act', lambda: nc.scalar.activation(out=GV[:], in_=V1[:], func=AF.Gelu), reads=['V1'], writes=['GV'])
                    fw.op('dve', lambda: nc.vector.bn_stats(out=STT[:], in_=GV[:]), reads=['GV'], writes=['STT'])
                    fw.op('dve', lambda: nc.vector.bn_aggr(out=MV[:], in_=STT[:]), reads=['STT'], writes=['MV'])
                    fw.op('act', lambda: nc.scalar.activation(out=RSTD[:], in_=MV[:, 1:2], func=AF.Sqrt, bias=LN_EPS), reads=['MV'], writes=['RSTD'])
                    fw.op('dve', lambda: nc.vector.reciprocal(out=RSTD[:], in_=RSTD[:]), reads=['RSTD'], writes=['RSTD'])
                    fw.op('dve', lambda: nc.vector.tensor_scalar(out=GV[:], in0=GV[:], scalar1=MV[:, 0:1], scalar2=RSTD[:, 0:1], op0=ALU.subtract, op1=ALU.mult),
                          reads=['GV', 'MV', 'RSTD'], writes=['GV'])
                    fw.op('pool', lambda: nc.gpsimd.tensor_tensor(out=GV[:], in0=GV[:], in1=LNG[:], op=ALU.mult), reads=['GV', 'LNG'], writes=['GV'])
                    fw.op('pool', lambda: nc.gpsimd.tensor_tensor(out=VNB[:], in0=GV[:], in1=LNB[:], op=ALU.add), reads=['GV', 'LNB'], writes=['VNB'])
                    pmi_ = next_ps()
                    pmx = PS[pmi_]
                    for g in range(4):
                        fw.op('pe', lambda: nc.tensor.matmul(pmx[:, g * 128:(g + 1) * 128], lhsT=VNB[:, g * 128:(g + 1) * 128], rhs=WST[:, g, :], start=True, stop=True),
                              reads=['VNB', 'WST'], writes=['ps%d' % pmi_])
                    fw.op('dve', lambda: nc.vector.tensor_tensor(out=MX[:], in0=pmx[:, 0:512].rearrange("p (g q) -> p g q", g=4), in1=BSP[:], op=ALU.add),
                          reads=['ps%d' % pmi_, 'BSP'], writes=['MX'])
                    ats = ATS[nch % 2]
                    ak = 'ATS%d' % (nch % 2)
                    nch += 1
                    fw.op('pool', lambda: nc.gpsimd.tensor_tensor(out=ats[:], in0=MX[:], in1=GU[:, :, sub * 128:(sub + 1) * 128], op=ALU.mult), reads=['MX', 'GU'], writes=[ak])
                    load('sp', A_d[:, :, own0:own0 + 128], ats[:], 'A_d', [ak])
        fw.barrier()
        if dbg:
            with contextlib.ExitStack() as sd:
                rb = sb("dbgb", [128, NOWN], BF16, sd)
                rf = sb("dbgf", [128, NOWN], F32, sd)
                for (nm, src_d, npart, nchk) in (("r_dbg", R_d, 128, 8), ("a_dbg", A_d, 128, 4), ("b_dbg", B_d, 64, 8)):
                    o_ = dbg_outs[nm]
                    for j in range(nchk):
                        load('sp', rb[0:npart, :], src_d[:, j, :], 'dbgb', [nm[0].upper() + '_d'])
                        fw.op('dve', lambda: nc.vector.tensor_copy(out=rf[0:npart, :], in_=rb[0:npart, :]), reads=['dbgb'], writes=['dbgf'])
                        load('sp', o_[:, j, :], rf[0:npart, :], nm, ['dbgf'])
                    outs.append(nm)
        def layer_norm(X, xk, n, SQ, sqk, MEAN, mk, MSQ, msk, RSTDW, rk, ONF, ok, gcol, bcol, pvk):
            fw.op('act', lambda: nc.scalar.activation(out=SQ[:, :, 0:n], in_=X[:, :, 0:n], func=AF.Square), reads=[xk], writes=[sqk])
            p1 = next_ps(); p2 = next_ps()
            for c in range(8):
                fw.op('pe', lambda: nc.tensor.matmul(PS[p1][:, 0:n], lhsT=ONF[:], rhs=X[:, c, 0:n], start=(c == 0), stop=(c == 7)), reads=[ok, xk], writes=['ps%d' % p1])
            for c in range(8):
                fw.op('pe', lambda: nc.tensor.matmul(PS[p2][:, 0:n], lhsT=ONF[:], rhs=SQ[:, c, 0:n], start=(c == 0), stop=(c == 7)), reads=[ok, sqk], writes=['ps%d' % p2])
            fw.op('act', lambda: nc.scalar.activation(out=MEAN[:, 0:n], in_=PS[p1][:, 0:n], func=AF.Copy, scale=1.0 / D), reads=['ps%d' % p1], writes=[mk])
            fw.op('dve', lambda: nc.vector.tensor_tensor(out=MSQ[:, 0:n], in0=MEAN[:, 0:n], in1=MEAN[:, 0:n], op=ALU.mult), reads=[mk], writes=[msk])
            fw.op('dve', lambda: nc.vector.scalar_tensor_tensor(out=RSTDW[:, 0:n], in0=PS[p2][:, 0:n], scalar=1.0 / D, in1=MSQ[:, 0:n], op0=ALU.mult, op1=ALU.subtract),
                  reads=['ps%d' % p2, msk], writes=[rk])
            fw.op('act', lambda: nc.scalar.activation(out=RSTDW[:, 0:n], in_=RSTDW[:, 0:n], func=AF.Sqrt, bias=LN_EPS), reads=[rk], writes=[rk])
            fw.op('dve', lambda: nc.vector.reciprocal(out=RSTDW[:, 0:n], in_=RSTDW[:, 0:n]), reads=[rk], writes=[rk])
            for c in range(8):
                fw.op('dve', lambda: nc.vector.tensor_tensor(out=X[:, c, 0:n], in0=X[:, c, 0:n], in1=MEAN[:, 0:n], op=ALU.subtract), reads=[xk, mk], writes=[xk])
                fw.op('pool', lambda: nc.gpsimd.tensor_tensor(out=X[:, c, 0:n], in0=X[:, c, 0:n], in1=RSTDW[:, 0:n], op=ALU.mult), reads=[xk, rk], writes=[xk])
                fw.op('act', lambda: nc.scalar.activation(out=X[:, c, 0:n], in_=X[:, c, 0:n], func=AF.Identity, scale=gcol(c), bias=bcol(c)), reads=[xk, pvk], writes=[xk])

        if GL:
            X1_d = sh.scratch("X1g", [128, 8, 8448], F32)
            H2_d = sh.scratch("H2g", [8448, D], BF16)
            LG_d = sh.scratch("LGg", [128, 66, 36], F32)
        else:
            X1_d = sh.scratch("X1_d", [128, 8, NOWN], F32)
            H2_d = sh.scratch("H2_d", [NOWN, D], BF16)
        LG = sb("LG", [128, T, 36], F32)

        def gbase(o0):
            if not GL:
                return o0
            return seg * TOWN + o0 if o0 < TOWN else SEQ + (o0 - TOWN)

        def gskip(o0):
            return GL and o0 >= TOWN and seg != 0
        OT = [(HALO + i * 512, i * 512, 512) for i in range(4)] + [(TLAT, TOWN, 256)]
        if "merge" in phases:
          with contextlib.ExitStack() as s6:
            ATl = sb("ATl", [128, 4, 1280], BF16, s6)
            BTl = sb("BTl", [64, 8, 1280], BF16, s6)
            RTl = sb("RTl", [128, 8, 1280], BF16, s6)
            WGs = [sb("WG%d" % i, [128, 8, 3, 128], BF16, s6) for i in range(2)]
            WAs = [sb("WA%d" % i, [128, 4, 128], BF16, s6) for i in range(2)]
            WBs = [sb("WB%d" % i, [64, 8, 128], BF16, s6) for i in range(2)]
            WCs = [sb("WC%d" % i, [128, 8, 128], BF16, s6) for i in range(2)]
            GS = [sb("GS%d" % i, [128, 512], F32, s6) for i in range(3)]
            TS = [sb("TS%d" % i, [128, 512], F32, s6) for i in range(3)]
            wi = 0
            for hf in range(2):
                tiles = OT[0:2] if hf == 0 else OT[2:5]
                h0 = tiles[0][1]
                hn = sum(t[2] for t in tiles)
                load('sp', ATl[:, :, 0:hn], A_d[:, :, h0:h0 + hn], 'ATl', ['A_d'])
                load('sp', BTl[:, :, 0:hn], B_d[:, :, h0:h0 + hn], 'BTl', ['B_d'])
                load('sp', RTl[:, :, 0:hn], R_d[:, :, h0:h0 + hn], 'RTl', ['R_d'])
                for oc in range(8):
                    w_ = wi % 2
                    wi += 1
                    WG, WA, WB, WC = WGs[w_], WAs[w_], WBs[w_], WCs[w_]
                    kg, ka, kb, kc_ = 'WG%d' % w_, 'WA%d' % w_, 'WB%d' % w_, 'WC%d' % w_
                    for br in range(3):
                        c0 = C_G + br * 1024 + oc * 128
                        load('pool', WG[:, :, br, :], w_in[:, :, c0:c0 + 128], kg)
                    load('pool', WA[:], w_pa[:, :, oc * 128:(oc + 1) * 128], ka)
                    load('pool', WB[:], w_pb[:, :, oc * 128:(oc + 1) * 128], kb)
                    load('pool', WC[:], w_pc[:, :, oc * 128:(oc + 1) * 128], kc_)
                    for (e0, o0, n) in tiles:
                        l0 = o0 - h0
                        for br in range(3):
                            def evg(pt, pk, br=br):
                                bcol = C_G // 128 + br * 8 + oc
                                fw.op('act', lambda: nc.scalar.activation(out=GS[br][:, 0:n], in_=pt[:, 0:n], func=AF.Sigmoid, bias=bint[:, bcol:bcol + 1]),
                                      reads=[pk, 'bint'], writes=['GS%d' % br])
                            inproj2(lambda kc: WG[:, kc, br, :], 128, e0, n, kg, evg)
                        for br, (Wt, wk, src, sk, nk, npart) in enumerate(((WA, ka, ATl, 'ATl', 4, 128), (WB, kb, BTl, 'BTl', 8, 64), (WC, kc_, RTl, 'RTl', 8, 128))):
                            pi = next_ps()
                            pt = PS[pi]
                            for kk in range(nk):
                                fw.op('pe', lambda: nc.tensor.matmul(pt[:, 0:n], lhsT=Wt[0:npart, kk, :], rhs=src[0:npart, kk, l0:l0 + n], start=(kk == 0), stop=(kk == nk - 1)),
                                      reads=[wk, sk], writes=['ps%d' % pi])
                            fw.op('dve', lambda: nc.vector.tensor_tensor(out=TS[br][:, 0:n], in0=pt[:, 0:n], in1=GS[br][:, 0:n], op=ALU.mult),
                                  reads=['ps%d' % pi, 'GS%d' % br], writes=['TS%d' % br])
                        fw.op('pool', lambda: nc.gpsimd.tensor_tensor(out=TS[0][:, 0:n], in0=TS[0][:, 0:n], in1=TS[1][:, 0:n], op=ALU.add), reads=['TS0', 'TS1'], writes=['TS0'])
                        fw.op('pool', lambda: nc.gpsimd.tensor_tensor(out=yT[:, oc, o0:o0 + n], in0=TS[0][:, 0:n], in1=TS[2][:, 0:n], op=ALU.add), reads=['TS0', 'TS2'], writes=['yT%d' % oc])
          fw.barrier()
          sHT.close()
          with contextlib.ExitStack() as s7:
            WO = sb("WO", [128, 8, D], BF16, s7)
            WGR = sb("WGR", [128, 8, 36], F32, s7)
            BGR = sb("BGR", [128, 36], F32, s7)
            IDN = sb("IDN", [128, 128], F32, s7)
            ONF = sb("ONF", [128, 128], F32, s7)
            pv = sb("pvec", [128, 5, 8], F32, s7)
            bom = sb("bom", [128, 2, 8], F32, s7)
            load('pool', WO[:], w_o, 'WO')
            load('sp', WGR[:], w_gr, 'WGR')
            load('sp', BGR[:], b_gr, 'BGR')
            load('sp', IDN[:], ident, 'IDN')
            load('sp', ONF[:], ones_f, 'ONF')
            load('sp', pv[:, 0, :], b_o, 'pvec')
            load('sp', pv[:, 1, :], ln1g, 'pvec')
            load('sp', pv[:, 2, :], ln1b, 'pvec')
            fw.op('dve', lambda: nc.vector.tensor_tensor(out=bom[:, 0, :], in0=pv[:, 0, :], in1=mlat[:, 16:24], op=ALU.mult), reads=['pvec', 'mlat'], writes=['bom'])
            fw.op('dve', lambda: nc.vector.tensor_tensor(out=bom[:, 1, :], in0=pv[:, 0, :], in1=mctx[:, 16:24], op=ALU.mult), reads=['pvec', 'mctx'], writes=['bom'])
            XT = sb("XT", [128, 8, 512], F32, s7)
            SQ = sb("SQ", [128, 8, 512], F32, s7)
            H2F = sb("H2F", [128, 8, 512], F32, s7)
            MEAN = sb("MEAN", [128, 512], F32, s7)
            MSQ = sb("MSQ", [128, 512], F32, s7)
            RSTDW = sb("RSTDW", [128, 512], F32, s7)
            nsub = 0
            H2B = [sb("H2B%d" % i, [128, D], BF16, s7) for i in range(2)]
            for ti, (e0, o0, n) in enumerate(OT):
                lat = ti < 4
                mv_ = mlat if lat else mctx
                mk_ = 'mlat' if lat else 'mctx'
                li = 0 if lat else 1
                if lat:
                    load('sp', XT[:, :, 0:n], xT[:, :, e0:e0 + n], 'XT')
                else:
                    load('sp', XT[:, :, 0:n], cT[:, :, :], 'XT')
                for oc in range(8):
                    pi = next_ps()
                    pt = PS[pi]
                    for kc in range(8):
                        fw.op('pe', lambda: nc.tensor.matmul(pt[:, 0:n], lhsT=WO[:, kc, oc * 128:(oc + 1) * 128], rhs=yT[:, kc, o0:o0 + n], start=(kc == 0), stop=(kc == 7)),
                              reads=['WO', 'yT%d' % kc], writes=['ps%d' % pi])
                    fw.op('act', lambda: nc.scalar.activation(out=SQ[:, oc, 0:n], in_=pt[:, 0:n], func=AF.Identity, scale=mv_[:, 16 + oc:17 + oc], bias=bom[:, li, oc:oc + 1]),
                          reads=['ps%d' % pi, mk_, 'bom'], writes=['SQ'])
                    fw.op('dve', lambda: nc.vector.scalar_tensor_tensor(out=XT[:, oc, 0:n], in0=XT[:, oc, 0:n], scalar=ALPHA, in1=SQ[:, oc, 0:n], op0=ALU.mult, op1=ALU.add),
                          reads=['XT', 'SQ'], writes=['XT'])

                layer_norm(XT, 'XT', n, SQ, 'SQ', MEAN, 'MEAN', MSQ, 'MSQ', RSTDW, 'RSTDW', ONF, 'ONF', lambda c: pv[:, 1, c:c + 1], lambda c: pv[:, 2, c:c + 1], 'pvec')
                if not gskip(o0):
                    load('sp', X1_d[:, :, gbase(o0):gbase(o0) + n], XT[:, :, 0:n], 'X1_d', ['XT'])
                m4 = m4p[:, li, :]
                for c in range(8):
                    fw.op('act', lambda: nc.scalar.activation(out=H2F[:, c, 0:n], in_=XT[:, c, 0:n], func=AF.Identity, scale=m4p[:, li, c:c + 1], bias=mv_[:, 24 + c:25 + c]),
                          reads=['XT', 'm4p', mk_], writes=['H2F'])
                for sub in range(n // 128):
                    t0 = sub * 128
                    chunk = (o0 + t0) // 128
                    pi = next_ps()
                    pt = PS[pi]
                    for kc in range(8):
                        fw.op('pe', lambda: nc.tensor.matmul(pt[:, 0:36], lhsT=H2F[:, kc, t0:t0 + 128], rhs=WGR[:, kc, :], start=(kc == 0), stop=(kc == 7)),
                              reads=['H2F', 'WGR'], writes=['ps%d' % pi])
                    fw.op('dve', lambda: nc.vector.tensor_tensor(out=LG[:, chunk, :], in0=pt[:, 0:36], in1=BGR[:], op=ALU.add), reads=['ps%d' % pi, 'BGR'], writes=['LG'])
                    hb = H2B[nsub % 2]
                    hk = 'H2B%d' % (nsub % 2)
                    nsub += 1
                    for half in range(2):
                        pi = next_ps()
                        pt = PS[pi]
                        for cc in range(4):
                            c = half * 4 + cc
                            fw.op('pe', lambda: nc.tensor.transpose(out=pt[:, cc * 128:(cc + 1) * 128], in_=H2F[:, c, t0:t0 + 128], identity=IDN[:]),
                                  reads=['H2F', 'IDN'], writes=['ps%d' % pi])
                        if half == 0:
                            fw.op('act', lambda: nc.scalar.copy(out=hb[:, 0:512], in_=pt[:, 0:512]), reads=['ps%d' % pi], writes=[hk])
                        else:
                            fw.op('dve', lambda: nc.vector.tensor_copy(out=hb[:, 512:1024], in_=pt[:, 0:512]), reads=['ps%d' % pi], writes=[hk])
                    if not gskip(o0):
                        load('sp', H2_d[gbase(o0) + t0:gbase(o0) + t0 + 128, :], hb[:], 'H2_d', [hk])
          if mode == "premoe":
              load('sp', LG_d[:, seg * 16:(seg + 1) * 16, :], LG[:, 0:16, :], 'LG_d', ['LG'])
              if seg == 0:
                  load('sp', LG_d[:, 64:66, :], LG[:, 16:18, :], 'LG_d', ['LG'])
          fw.barrier()
          sY.close()
          if mode == "premoe":
              fw.barrier()
              return
          if dbg:
              load('sp', dbg_outs['x1_dbg'], X1_d, 'x1_dbg', ['X1_d'])
              lgf = LG
              load('sp', dbg_outs['lg_dbg'], LG[:], 'lg_dbg', ['LG'])
              outs.extend(['x1_dbg', 'lg_dbg'])
        if mode == "moe":
            sHT.close()
            sY.close()
            load('sp', LG[:], LG_d, 'LG', ['LG_d'])
        if "moe" in phases:
          XB_d = sh.scratch("XBg" if GL else "XB_d", [NB * BS, D], BF16)
          OUT_d = sh.scratch("OUTg" if GL else "OUT_d", [NB * BS, D], F32)
          SL1 = sb("SL1", [128, T], I32)
          SL2 = sb("SL2", [128, T], I32)
          G1 = sb("G1", [128, T], F32)
          G2 = sb("G2", [128, T], F32)
          WIDX = sb("WIDX", [128, NB], I32)
          with contextlib.ExitStack() as s8:
            def t8(name, shape, dt=F32):
                return sb(name, shape, dt, s8)
            TRI = t8("TRI", [128, 128]); ONF2 = t8("ONF2", [128, 128]); BVAL = t8("BVAL", [128, NBLK]); PIDX = t8("PIDX", [128, 1])
            load('sp', TRI[:], tri_in, 'TRI'); load('sp', ONF2[:], ones_f, 'ONF2'); load('sp', BVAL[:], bval_in, 'BVAL'); load('sp', PIDX[:], pidx_in, 'PIDX')
            if SUB != 1:
                fw.op('dve', lambda: nc.vector.tensor_scalar(out=BVAL[:], in0=BVAL[:], scalar1=float(SUB), scalar2=None, op0=ALU.mult), reads=['BVAL'], writes=['BVAL'])
            gmax = t8("gmax", [128, T]); goh = t8("goh", [128, T, 4]); ge = t8("ge", [128, T, 4]); gw = t8("gw", [128, T])
            pen = t8("pen", [128, T, 4]); em = t8("em", [128, T, 32]); em2 = t8("em2", [128, T, 32])
            m1 = t8("m1", [128, T]); m2 = t8("m2", [128, T]); oh1 = t8("oh1", [128, T, 32]); oh2 = t8("oh2", [128, T, 32])
            dm = t8("dm", [128, T]); p1 = t8("p1", [128, T])
            A_ = t8("Aas", [128, T, 32]); CUMA = t8("CUMA", [128, T + 1, 32]); SLT = t8("SLT", [128, T, 32]); TMP = t8("TMP", [128, T, 32])
            cnt = t8("cnt", [128, 32]); cnti = t8("cnti", [128, 32], I32); padd = t8("padd", [128, 32]); pend = t8("pend", [128, 32]); pst = t8("pst", [128, 32])
            one32 = t8("one32", [128, 32]); CMP = t8("CMP", [128, NB, 32]); be = t8("be", [128, NB]); slf = t8("slf", [128, T])

            def V(fn, reads, writes):
                fw.op('dve', fn, reads=reads, writes=writes)
            gl = LG[:, :, 0:4]
            el = LG[:, :, 4:36]
            V(lambda: nc.vector.tensor_reduce(out=gmax[:], in_=gl, op=ALU.max, axis=AX.X), ['LG'], ['gmax'])
            gmb = gmax[:].unsqueeze(2).to_broadcast([128, T, 4])
            V(lambda: nc.vector.tensor_tensor(out=goh[:], in0=gl, in1=gmb, op=ALU.is_equal), ['LG', 'gmax'], ['goh'])
            V(lambda: nc.vector.tensor_tensor(out=ge[:], in0=gl, in1=gmb, op=ALU.subtract), ['LG', 'gmax'], ['ge'])
            fw.op('act', lambda: nc.scalar.activation(out=ge[:], in_=ge[:], func=AF.Exp), reads=['ge'], writes=['ge'])
            V(lambda: nc.vector.tensor_reduce(out=gw[:], in_=ge[:], op=ALU.add, axis=AX.X), ['ge'], ['gw'])
            V(lambda: nc.vector.reciprocal(out=gw[:], in_=gw[:]), ['gw'], ['gw'])
            V(lambda: nc.vector.tensor_scalar(out=pen[:], in0=goh[:], scalar1=1e30, scalar2=-1e30, op0=ALU.mult, op1=ALU.add), ['goh'], ['pen'])
            V(lambda: nc.vector.tensor_tensor(out=em[:].rearrange("p t (g e) -> p t g e", g=4), in0=el.rearrange("p t (g e) -> p t g e", g=4),
                                              in1=pen[:].unsqueeze(3).to_broadcast([128, T, 4, 8]), op=ALU.add), ['LG', 'pen'], ['em'])
            V(lambda: nc.vector.tensor_reduce(out=m1[:], in_=em[:], op=ALU.max, axis=AX.X), ['em'], ['m1'])
            V(lambda: nc.vector.tensor_tensor(out=oh1[:], in0=em[:], in1=m1[:].unsqueeze(2).to_broadcast([128, T, 32]), op=ALU.is_equal), ['em', 'm1'], ['oh1'])
            V(lambda: nc.vector.scalar_tensor_tensor(out=em2[:], in0=oh1[:], scalar=-1e30, in1=em[:], op0=ALU.mult, op1=ALU.add), ['oh1', 'em'], ['em2'])
            V(lambda: nc.vector.tensor_reduce(out=m2[:], in_=em2[:], op=ALU.max, axis=AX.X), ['em2'], ['m2'])
            V(lambda: nc.vector.tensor_tensor(out=oh2[:], in0=em2[:], in1=m2[:].unsqueeze(2).to_broadcast([128, T, 32]), op=ALU.is_equal), ['em2', 'm2'], ['oh2'])
            V(lambda: nc.vector.tensor_tensor(out=dm[:], in0=m2[:], in1=m1[:], op=ALU.subtract), ['m1', 'm2'], ['dm'])
            fw.op('act', lambda: nc.scalar.activation(out=dm[:], in_=dm[:], func=AF.Exp), reads=['dm'], writes=['dm'])
            V(lambda: nc.vector.tensor_scalar(out=p1[:], in0=dm[:], scalar1=1.0, scalar2=None, op0=ALU.add), ['dm'], ['p1'])
            V(lambda: nc.vector.reciprocal(out=p1[:], in_=p1[:]), ['p1'], ['p1'])
            V(lambda: nc.vector.tensor_tensor(out=G1[:], in0=gw[:], in1=p1[:], op=ALU.mult), ['gw', 'p1'], ['G1'])
            V(lambda: nc.vector.tensor_tensor(out=dm[:], in0=dm[:], in1=p1[:], op=ALU.mult), ['dm', 'p1'], ['dm'])
            V(lambda: nc.vector.tensor_tensor(out=G2[:], in0=gw[:], in1=dm[:], op=ALU.mult), ['gw', 'dm'], ['G2'])
            V(lambda: nc.vector.tensor_tensor(out=A_[:], in0=oh1[:], in1=oh2[:], op=ALU.add), ['oh1', 'oh2'], ['A'])
            V(lambda: nc.vector.memset(CUMA[:, 0, :], 0.0), [], ['CUMA'])
            for t in range(T):
                V(lambda: nc.vector.tensor_tensor(out=CUMA[:, t + 1, :], in0=CUMA[:, t, :], in1=A_[:, t, :], op=ALU.add), ['CUMA', 'A'], ['CUMA'])
            pc_ = next_ps()
            fw.op('pe', lambda: nc.tensor.matmul(PS[pc_][:, 0:32], lhsT=ONF2[:], rhs=CUMA[:, T, :], start=True, stop=True), reads=['ONF2', 'CUMA'], writes=['ps%d' % pc_])
            V(lambda: nc.vector.tensor_copy(out=cnt[:], in_=PS[pc_][:, 0:32]), ['ps%d' % pc_], ['cnt'])
            CM2 = t8("CM2", [128, 32, 40])
            V(lambda: nc.vector.tensor_tensor(out=CM2[:], in0=cnt[:].unsqueeze(2).to_broadcast([128, 32, 40]), in1=BVAL[:, 0:40].unsqueeze(1).to_broadcast([128, 32, 40]), op=ALU.is_gt),
              ['cnt', 'BVAL'], ['CM2'])
            V(lambda: nc.vector.tensor_reduce(out=padd[:], in_=CM2[:], op=ALU.add, axis=AX.X), ['CM2'], ['padd'])
            V(lambda: nc.vector.tensor_scalar(out=padd[:], in0=padd[:], scalar1=float(BS), scalar2=None, op0=ALU.mult), ['padd'], ['padd'])
            V(lambda: nc.vector.memset(one32[:], 1.0), [], ['one32'])
            V(lambda: nc.vector.tensor_tensor_scan(out=pend[:], data0=one32[:], data1=padd[:], initial=0.0, op0=ALU.mult, op1=ALU.add), ['one32', 'padd'], ['pend'])
            V(lambda: nc.vector.tensor_tensor(out=pst[:], in0=pend[:], in1=padd[:], op=ALU.subtract), ['pend', 'padd'], ['pst'])
            psb = pst[:].unsqueeze(1)
            for g0_ in range(0, T, 16):
                ng_ = min(16, T - g0_)
                pr_ = next_ps()
                pk = 'ps%d' % pr_
                for t in range(g0_, g0_ + ng_):
                    tt_ = t - g0_
                    fw.op('pe', lambda: nc.tensor.matmul(PS[pr_][:, tt_ * 32:(tt_ + 1) * 32], lhsT=TRI[:], rhs=A_[:, t, :], start=True, stop=False), reads=['TRI', 'A'], writes=[pk])
                    fw.op('pe', lambda: nc.tensor.matmul(PS[pr_][:, tt_ * 32:(tt_ + 1) * 32], lhsT=ONF2[:], rhs=CUMA[:, t, :], start=False, stop=True), reads=['ONF2', 'CUMA'], writes=[pk])
                V(lambda: nc.vector.tensor_tensor(out=SLT[:, g0_:g0_ + ng_, :], in0=PS[pr_][:, 0:ng_ * 32].rearrange("p (t e) -> p t e", e=32), in1=psb.to_broadcast([128, ng_, 32]), op=ALU.add),
                  [pk, 'pst'], ['SLT'])
            for (oh, ok, SL) in ((oh1, 'oh1', SL1), (oh2, 'oh2', SL2)):
                V(lambda: nc.vector.tensor_tensor(out=TMP[:], in0=SLT[:], in1=oh[:], op=ALU.mult), ['SLT', ok], ['TMP'])
                V(lambda: nc.vector.tensor_reduce(out=slf[:], in_=TMP[:], op=ALU.add, axis=AX.X), ['TMP'], ['slf'])
                V(lambda: nc.vector.tensor_copy(out=SL[:], in_=slf[:]), ['slf'], ['SL'])
            V(lambda: nc.vector.tensor_tensor(out=CMP[:], in0=pend[:].unsqueeze(1).to_broadcast([128, NB, 32]), in1=BVAL[:, 0:NB].unsqueeze(2).to_broadcast([128, NB, 32]), op=ALU.is_le),
              ['pend', 'BVAL'], ['CMP'])
            V(lambda: nc.vector.tensor_reduce(out=be[:], in_=CMP[:], op=ALU.add, axis=AX.X), ['CMP'], ['be'])
            V(lambda: nc.vector.tensor_scalar(out=be[:], in0=be[:], scalar1=float(NEXP - 1), scalar2=128.0, op0=ALU.min, op1=ALU.mult), ['be'], ['be'])
            V(lambda: nc.vector.tensor_scalar(out=WIDX[:], in0=be[:], scalar1=PIDX[:, 0:1], scalar2=None, op0=ALU.add), ['be', 'PIDX'], ['WIDX'])
            if dbg:
                load('sp', dbg_outs['sl_dbg'][:, 0, :], SL1[:], 'sl_dbg', ['SL'])
                load('sp', dbg_outs['sl_dbg'][:, 1, :], SL2[:], 'sl_dbg', ['SL'])
                load('sp', dbg_outs['g_dbg'][:, 0, :], G1[:], 'g_dbg', ['G1'])
                load('sp', dbg_outs['g_dbg'][:, 1, :], G2[:], 'g_dbg', ['G2'])
                load('sp', dbg_outs['wi_dbg'], WIDX[:], 'wi_dbg', ['WIDX'])
                outs.extend(['sl_dbg', 'g_dbg', 'wi_dbg'])
          fw.barrier()
          if 'moe8' not in phases and dbg:
              sHT.close()
              fw.barrier()
              return
          with contextlib.ExitStack() as s8b:
            HBs = [sb("HBs%d" % i, [128, D], BF16, s8b) for i in range(2)]
            for t in range(T):
                hb_ = HBs[t % 2]
                hk_ = 'HBs%d' % (t % 2)
                load('sp', hb_[:], H2_d[t * 128:(t + 1) * 128, :], hk_, ['H2_d'])
                for SL in (SL1, SL2):
                    fw.dma('pool', lambda: nc.gpsimd.indirect_dma_start(out=XB_d[:, :], out_offset=bass.IndirectOffsetOnAxis(ap=SL[:, t:t + 1], axis=0),
                                                                        in_=hb_[:], in_offset=None), reads=[hk_, 'SL'], writes=['XB_d'])
          fw.barrier()
          with contextlib.ExitStack() as s9:
            IDB = sb("IDB", [128, 128], BF16, s9)
            load('pool', IDB[:], ident, 'IDB')
            W1B = [sb("W1B%d" % i, [128, 4096], BF16, s9) for i in range(2)]
            W3B = [sb("W3B%d" % i, [128, 4096], BF16, s9) for i in range(2)]
            W2B = [sb("W2B%d" % i, [128, 4096], BF16, s9) for i in range(2)]
            NBUF = 3
            XBs = [sb("XBs%d" % i, [128, D], BF16, s9) for i in range(NBUF)]
            XTs = [sb("XTs%d" % i, [128, 8, 128], BF16, s9) for i in range(NBUF)]
            S1s = [sb("S1s%d" % i, [128, 512], F32, s9) for i in range(NBUF)]
            Gbs = [sb("Gbs%d" % i, [128, 512], BF16, s9) for i in range(NBUF)]
            GTs = [sb("GTs%d" % i, [128, 4, 128], BF16, s9) for i in range(NBUF)]
            OBs = [sb("OBs%d" % i, [128, D], F32, s9) for i in range(NBUF)]
            items = [(b_, s_) for b_ in range(NB) for s_ in range(SUB)]
            NI = len(items)

            def loadw(b_):
                i2 = b_ % 2
                for (wt, wk, srcn) in ((W1B[i2], 'W1B%d' % i2, "w1"), (W3B[i2], 'W3B%d' % i2, "w3"), (W2B[i2], 'W2B%d' % i2, "w2")):
                    for a_, hsfx in enumerate(("a", "b")):
                        fw.dma('pool', lambda: nc.gpsimd.indirect_dma_start(out=wt[:, a_ * 2048:(a_ + 1) * 2048], out_offset=None, in_=wexp[srcn + hsfx][:, :],
                                                                            in_offset=bass.IndirectOffsetOnAxis(ap=WIDX[:, b_:b_ + 1], axis=0)),
                               reads=['WIDX'], writes=[wk])

            def stA(i):
                b_, s_ = items[i]
                j = i % NBUF
                r0 = b_ * BS + s_ * 128
                xb = XBs[j]
                load('sp', xb[:], XB_d[r0:r0 + 128, :], 'XBs%d' % j, ['XB_d'])
                pi_ = next_ps()
                pv_ = PS[pi_][:].bitcast(BF16)
                for c in range(8):
                    fw.op('pe', lambda: nc.tensor.transpose(out=pv_[:, c * 128:(c + 1) * 128], in_=xb[:, c * 128:(c + 1) * 128], identity=IDB[:]),
                          reads=['XBs%d' % j, 'IDB'], writes=['ps%d' % pi_])
                fw.op('act', lambda: nc.scalar.copy(out=XTs[j][:, 0:4, :], in_=pv_[:, 0:512].rearrange("p (c s) -> p c s", c=4)), reads=['ps%d' % pi_], writes=['XTs%d' % j])
                fw.op('dve', lambda: nc.vector.tensor_copy(out=XTs[j][:, 4:8, :], in_=pv_[:, 512:1024].rearrange("p (c s) -> p c s", c=4)), reads=['ps%d' % pi_], writes=['XTs%d' % j])

            def stB(i):
                b_, s_ = items[i]
                j = i % NBUF
                i2 = b_ % 2
                p1i = next_ps(); p3i = next_ps()
                for (pi_, wt, wk) in ((p1i, W1B[i2], 'W1B%d' % i2), (p3i, W3B[i2], 'W3B%d' % i2)):
                    for kc in range(8):
                        fw.op('pe', lambda: nc.tensor.matmul(PS[pi_][:, 0:512], lhsT=XTs[j][:, kc, :], rhs=wt[:, kc * 512:(kc + 1) * 512], start=(kc == 0), stop=(kc == 7)),
                              reads=['XTs%d' % j, wk], writes=['ps%d' % pi_])
                fw.op('act', lambda: nc.scalar.activation(out=S1s[j][:], in_=PS[p1i][:, 0:512], func=AF.Silu), reads=['ps%d' % p1i], writes=['S1s%d' % j])
                fw.op('dve', lambda: nc.vector.tensor_tensor(out=Gbs[j][:], in0=PS[p3i][:, 0:512], in1=S1s[j][:], op=ALU.mult), reads=['ps%d' % p3i, 'S1s%d' % j], writes=['Gbs%d' % j])

            def stC(i):
                j = i % NBUF
                pi_ = next_ps()
                pv_ = PS[pi_][:].bitcast(BF16)
                for fc in range(4):
                    fw.op('pe', lambda: nc.tensor.transpose(out=pv_[:, fc * 128:(fc + 1) * 128], in_=Gbs[j][:, fc * 128:(fc + 1) * 128], identity=IDB[:]),
                          reads=['Gbs%d' % j, 'IDB'], writes=['ps%d' % pi_])
                fw.op('dve', lambda: nc.vector.tensor_copy(out=GTs[j][:], in_=pv_[:, 0:512].rearrange("p (c s) -> p c s", c=4)), reads=['ps%d' % pi_], writes=['GTs%d' % j])

            def stD(i):
                b_, s_ = items[i]
                j = i % NBUF
                i2 = b_ % 2
                r0 = b_ * BS + s_ * 128
                ob = OBs[j]
                for half in range(2):
                    pi_ = next_ps()
                    for fc in range(4):
                        fw.op('pe', lambda: nc.tensor.matmul(PS[pi_][:, 0:512], lhsT=GTs[j][:, fc, :], rhs=W2B[i2][:, fc * 1024 + half * 512:fc * 1024 + (half + 1) * 512], start=(fc == 0), stop=(fc == 3)),
                              reads=['GTs%d' % j, 'W2B%d' % i2], writes=['ps%d' % pi_])
                    if half == 0:
                        fw.op('act', lambda: nc.scalar.copy(out=ob[:, 0:512], in_=PS[pi_][:, 0:512]), reads=['ps%d' % pi_], writes=['OBs%d' % j])
                    else:
                        fw.op('dve', lambda: nc.vector.tensor_copy(out=ob[:, 512:1024], in_=PS[pi_][:, 0:512]), reads=['ps%d' % pi_], writes=['OBs%d' % j])
                load('sp', OUT_d[r0:r0 + 128, :], ob[:], 'OUT_d', ['OBs%d' % j])

            loadw(0)
            if NB > 1:
                loadw(1)
            for step in range(NI + 2):
                if step < NI:
                    stA(step)
                if 0 <= step - 1 < NI:
                    stB(step - 1)
                if 0 <= step - 2 < NI:
                    stD(step - 2)
                    bb, ss = items[step - 2]
                    if ss == SUB - 1 and bb + 2 < NB:
                        loadw(bb + 2)
                if 0 <= step - 1 < NI:
                    stC(step - 1)
          fw.barrier()
          with contextlib.ExitStack() as s10:
            IDN2 = sb("IDN2", [128, 128], F32, s10)
            ONF3 = sb("ONF3", [128, 128], F32, s10)
            pv2 = sb("pvec2", [128, 2, 8], F32, s10)
            load('sp', IDN2[:], ident, 'IDN2')
            load('sp', ONF3[:], ones_f, 'ONF3')
            load('sp', pv2[:, 0, :], ln2g, 'pvec2')
            load('sp', pv2[:, 1, :], ln2b, 'pvec2')
            XT2 = sb("XT2", [128, 8, 512], F32, s10)
            SQ2 = sb("SQ2", [128, 8, 512], F32, s10)
            MEAN2 = sb("MEAN2", [128, 512], F32, s10)
            MSQ2 = sb("MSQ2", [128, 512], F32, s10)
            RSTD2 = sb("RSTD2", [128, 512], F32, s10)
            O1s = [sb("O1s%d" % i, [128, D], F32, s10) for i in range(2)]
            O2s = [sb("O2s%d" % i, [128, D], F32, s10) for i in range(2)]
            ng = 0
            if mode == "moe":
                CTL = [(g_ * 512, 512, True) for g_ in range(16)] + [(SEQ, NCTX, False)]
            else:
                CTL = [(o0_, n_, i_ < 4) for i_, (e0_, o0_, n_) in enumerate(OT)]
            for (o0, n, lat) in CTL:
                mv_ = mlat if lat else mctx
                mk_ = 'mlat' if lat else 'mctx'
                load('sp', XT2[:, :, 0:n], X1_d[:, :, o0:o0 + n], 'XT2', ['X1_d'])
                for sub in range(n // 128):
                    t = (o0 + sub * 128) // 128
                    o1, o2 = O1s[ng % 2], O2s[ng % 2]
                    ko1, ko2 = 'O1s%d' % (ng % 2), 'O2s%d' % (ng % 2)
                    ng += 1
                    for (o_, ko_, SL) in ((o1, ko1, SL1), (o2, ko2, SL2)):
                        fw.dma('pool', lambda: nc.gpsimd.indirect_dma_start(out=o_[:], out_offset=None, in_=OUT_d[:, :],
                                                                            in_offset=bass.IndirectOffsetOnAxis(ap=SL[:, t:t + 1], axis=0)),
                               reads=['OUT_d', 'SL'], writes=[ko_])
                    fw.op('dve', lambda: nc.vector.tensor_scalar(out=o1[:], in0=o1[:], scalar1=G1[:, t:t + 1], scalar2=None, op0=ALU.mult), reads=[ko1, 'G1'], writes=[ko1])
                    fw.op('dve', lambda: nc.vector.scalar_tensor_tensor(out=o1[:], in0=o2[:], scalar=G2[:, t:t + 1], in1=o1[:], op0=ALU.mult, op1=ALU.add), reads=[ko1, ko2, 'G2'], writes=[ko1])
                    for half in range(2):
                        pi_ = next_ps()
                        for cc in range(4):
                            c = half * 4 + cc
                            fw.op('pe', lambda: nc.tensor.transpose(out=PS[pi_][:, cc * 128:(cc + 1) * 128], in_=o1[:, c * 128:(c + 1) * 128], identity=IDN2[:]),
                                  reads=[ko1, 'IDN2'], writes=['ps%d' % pi_])
                        for cc in range(4):
                            c = half * 4 + cc
                            fw.op('act', lambda: nc.scalar.activation(out=SQ2[:, c, sub * 128:(sub + 1) * 128], in_=PS[pi_][:, cc * 128:(cc + 1) * 128], func=AF.Copy, scale=mv_[:, 40 + c:41 + c]),
                                  reads=['ps%d' % pi_, mk_], writes=['SQ2'])
                fw.op('dve', lambda: nc.vector.scalar_tensor_tensor(out=XT2[:, :, 0:n], in0=XT2[:, :, 0:n], scalar=ALPHA, in1=SQ2[:, :, 0:n], op0=ALU.mult, op1=ALU.add),
                      reads=['XT2', 'SQ2'], writes=['XT2'])
                layer_norm(XT2, 'XT2', n, SQ2, 'SQ2', MEAN2, 'MEAN2', MSQ2, 'MSQ2', RSTD2, 'RSTD2', ONF3, 'ONF3',
                           lambda c: pv2[:, 0, c:c + 1], lambda c: pv2[:, 1, c:c + 1], 'pvec2')
                if lat:
                    load('sp', xout[:, :, o0:o0 + n], XT2[:, :, 0:n], 'xout', ['XT2'])
                else:
                    load('sp', cout[:, :, :], XT2[:, :, 0:n], 'cout', ['XT2'])
            outs.extend(['xout', 'cout'])
        sHT.close()
        fw.barrier()


def _fm(a):
    return np.ascontiguousarray(a.T.reshape(8, 128, a.shape[0]).transpose(1, 0, 2))


def _pp(v):
    return np.ascontiguousarray(v.reshape(-1, 128).T)


def _kc(w):
    return np.ascontiguousarray(w.reshape(8, 128, w.shape[1]).transpose(1, 0, 2))


def _partner_cols(nh):
    idx = []
    for h in range(nh):
        for d in range(64):
            half, i = d // 32, d % 32
            p = i + 16 if i < 16 else i - 16
            idx.append(h * 64 + half * 32 + p)
    return np.array(idx)


def prep_layer_weights(inp, l):
    w_in = inp['w_in'][l]
    b_in = inp['b_in'][l]
    o = np.cumsum([0, 512, 512, 512, 3072, 1024, 128, 128, 1024])
    q, u, v, g, z, k, va, xr = [slice(o[i], o[i + 1]) for i in range(8)]
    pq = _partner_cols(8)
    pk = _partner_cols(2)

    def cols(a):
        return np.concatenate([a[..., q], a[..., q][..., pq], a[..., k], a[..., k][..., pk], a[..., u], a[..., v],
                               a[..., va], a[..., g], a[..., z], a[..., xr]], axis=-1)
    W = {}
    W['w_in'] = _kc(cols(w_in))
    W['b_in'] = _pp(cols(b_in))
    W['w_mod'] = _kc(inp['w_mod'][l])
    W['b_mod'] = _pp(inp['b_mod'][l])
    W['convw'] = np.ascontiguousarray(inp['conv_w'][l].reshape(2, 4, 8, 128).transpose(3, 2, 0, 1))
    W['convb'] = np.ascontiguousarray(inp['conv_b'][l].reshape(2, 8, 128).transpose(2, 1, 0))
    W['wrg'] = np.ascontiguousarray(inp['w_rgate'][l].transpose(2, 0, 1, 3))
    W['wig'] = np.ascontiguousarray(inp['w_igate'][l].transpose(2, 0, 1, 3))
    W['brg'] = np.ascontiguousarray(inp['b_rgate'][l].reshape(2, 8, 128).transpose(2, 1, 0))
    W['big'] = np.ascontiguousarray(inp['b_igate'][l].reshape(2, 8, 128).transpose(2, 1, 0))
    W['lam'] = np.ascontiguousarray(inp['lru_lambda'][l].reshape(2, 8, 128).transpose(2, 1, 0))
    bq, bqs, bk, bks = b_in[q], b_in[q][pq], b_in[k], b_in[k][pk]
    W['b_qk'] = np.ascontiguousarray(np.concatenate([bq.reshape(8, 64), bqs.reshape(8, 64), bk.reshape(2, 64), bks.reshape(2, 64)], 0).T)
    W['b_va'] = np.ascontiguousarray(np.broadcast_to(b_in[va][None, :], (128, 128)))
    W['b_v'] = np.ascontiguousarray(np.broadcast_to(b_in[v][None, :], (128, 512)))
    W['sinkx'] = np.ascontiguousarray(np.broadcast_to(inp['attn_sink'][l][None, :, None], (64, 8, 128)))
    W['sgu_g'] = np.ascontiguousarray(np.broadcast_to(inp['sgu_ln_g'][l][None, :], (128, 512)))
    W['sgu_b'] = np.ascontiguousarray(np.broadcast_to(inp['sgu_ln_b'][l][None, :], (128, 512)))
    W['w_spT'] = np.ascontiguousarray(inp['w_spatial'][l].transpose(2, 0, 1))
    W['b_sp'] = np.ascontiguousarray(np.broadcast_to(inp['b_spatial'][l][None, :, :], (128, 4, 128)))
    W['w_pa'] = np.ascontiguousarray(inp['w_proj_a'][l].reshape(4, 128, D).transpose(1, 0, 2))
    W['w_pb'] = np.ascontiguousarray(inp['w_proj_b'][l].reshape(8, 64, D).transpose(1, 0, 2))
    W['w_pc'] = _kc(inp['w_proj_c'][l])
    W['w_o'] = _kc(inp['w_out'][l])
    W['b_o'] = _pp(inp['b_out'][l])
    W['ln1g'] = _pp(inp['ln1_g'][l]); W['ln1b'] = _pp(inp['ln1_b'][l])
    W['ln2g'] = _pp(inp['ln2_g'][l]); W['ln2b'] = _pp(inp['ln2_b'][l])
    W['w_gr'] = _kc(np.concatenate([inp['w_group'][l], inp['w_router'][l]], 1))
    w1r = inp['w1'][l].reshape(NEXP, 8, 128, 512).transpose(0, 2, 1, 3).reshape(NEXP * 128, 4096)
    w3r = inp['w3'][l].reshape(NEXP, 8, 128, 512).transpose(0, 2, 1, 3).reshape(NEXP * 128, 4096)
    w2r = inp['w2'][l].reshape(NEXP, 4, 128, 1024).transpose(0, 2, 1, 3).reshape(NEXP * 128, 4096)
    for nm, arr in (("w1", w1r), ("w3", w3r), ("w2", w2r)):
        W[nm + 'a'] = np.ascontiguousarray(arr[:, :2048])
        W[nm + 'b'] = np.ascontiguousarray(arr[:, 2048:])
    W['b_gr'] = np.ascontiguousarray(np.broadcast_to(np.concatenate([inp['b_group'][l], inp['b_router'][l]])[None, :], (128, 36)))
    return W


def const_inputs(core):
    s = core % NSEG
    pos = np.arange(TLAT) + s * TOWN - HALO
    row = (pos // 64).astype(np.float32)
    colp = (pos % 64).astype(np.float32)
    inv = (10000.0 ** (-np.arange(0, 32, 2, dtype=np.float32) / 32)).astype(np.float32)
    cos = np.zeros((64, TLAT), np.float32)
    sin = np.zeros((64, TLAT), np.float32)
    for d in range(64):
        half, i = d // 32, d % 32
        ang = (row if half == 0 else colp) * inv[i % 16]
        cos[d] = np.cos(ang)
        sin[d] = np.sin(ang) * (-1.0 if i < 16 else 1.0)
    kk = np.arange(128)[:, None]
    qq = np.arange(128)[None, :]
    m = np.zeros((128, 2, 4, 128), np.float32)
    m[:, 0] = (kk >= qq).astype(np.float32)[:, None, :]
    m[:, 1] = (kk <= qq).astype(np.float32)[:, None, :]
    return {'rope_cos': cos, 'rope_sin': sin, 'masks': m, 'ones_in': np.ones((128, 64), np.float32),
            'ident': np.eye(128, dtype=np.float32), 'tri_in': np.triu(np.ones((128, 128), np.float32), 1),
            'bval_in': np.ascontiguousarray(np.broadcast_to((np.arange(NBLK, dtype=np.float32) * 128)[None, :], (128, NBLK))),
            'pidx_in': np.arange(128, dtype=np.float32)[:, None].copy(), 'ones_f': np.ones((128, 128), np.float32)}


def core_inputs(x, ctx, c, c_ctx, core):
    b, s = core // NSEG, core % NSEG
    lo = s * TOWN - HALO
    ext = np.zeros((TLAT, D), np.float32)
    a, e = max(lo, 0), min(lo + TLAT, SEQ)
    ext[a - lo:e - lo] = x[b, a:e]
    m = {'xT': _fm(ext), 'cT': _fm(ctx[b])}
    m['cond'] = np.ascontiguousarray(np.stack([_pp(c[b]), _pp(c_ctx)], -1))
    ed = np.zeros((128, 2), np.float32)
    ed[:, 0] = 1.0 if s > 0 else 0.0
    ed[:, 1] = 1.0 if s < NSEG - 1 else 0.0
    m['edge'] = ed
    return m


def carry_input(ab, core):
    car = np.zeros((128, 8, 2, 3, 2), np.float32)
    car[..., 0] = 1.0
    if ab is None:
        return car
    b, s = core // NSEG, core % NSEG
    fwd = [b * NSEG + j for j in range(0, s)]
    bwd = [b * NSEG + j for j in range(NSEG - 1, s, -1)]
    for k, cc in enumerate(fwd):
        car[:, :, 0, k, :] = ab[cc][:, :, 0, :]
    for k, cc in enumerate(bwd):
        car[:, :, 1, k, :] = ab[cc][:, :, 1, :]
    return car


PRE_NAMES = ['w_mod', 'b_mod', 'w_in', 'b_in', 'convw', 'convb', 'wrg', 'wig', 'brg', 'big', 'lam']
XPAD = SEQ + 2 * HALO


def all_consts():
    pos = np.arange(XPAD) - HALO
    row = (pos // 64).astype(np.float32)
    colp = (pos % 64).astype(np.float32)
    inv = (10000.0 ** (-np.arange(0, 32, 2, dtype=np.float32) / 32)).astype(np.float32)
    cos = np.zeros((64, XPAD), np.float32)
    sin = np.zeros((64, XPAD), np.float32)
    for d in range(64):
        half, i = d // 32, d % 32
        ang = (row if half == 0 else colp) * inv[i % 16]
        cos[d] = np.cos(ang)
        sin[d] = np.sin(ang) * (-1.0 if i < 16 else 1.0)
    C = const_inputs(0)
    C.pop('rope_cos')
    C.pop('rope_sin')
    C['rope_cos_all'] = cos
    C['rope_sin_all'] = sin
    ed = np.ones((NSEG, 128, 2), np.float32)
    ed[0, :, 0] = 0.0
    ed[NSEG - 1, :, 1] = 0.0
    C['edge_all'] = ed
    C['car_id'] = carry_input(None, 0)
    return C


def build_fused(w_shapes, c_shapes):
    nc = bass.Bass("TRN2", target_bir_lowering=False)

    def ein(name, shape, dt=F32):
        return nc.dram_tensor(name, list(shape), dt, kind="ExternalInput").ap()
    X0 = ein("X0", [128, 8, XPAD])
    C0 = ein("C0", [128, 8, NCTX])
    cond = ein("cond", [128, 8, 2])
    EXPN = ("w1a", "w1b", "w3a", "w3b", "w2a", "w2b")
    Wall = {k: ein(k, [DEPTH] + list(shp)) for k, shp in w_shapes.items() if k not in EXPN}
    Wexp = {k: [ein("%s_%d" % (k, l_), list(w_shapes[k])) for l_ in range(DEPTH)] for k in EXPN}
    Call = {k: ein(k, shp) for k, shp in c_shapes.items()}
    yout = nc.dram_tensor("yout", [128, 8, SEQ], F32, kind="ExternalOutput").ap()
    XS = [nc.dram_tensor("XS%d" % i, [128, 8, XPAD], F32).ap() for i in range(2)]
    CS = [nc.dram_tensor("CS%d" % i, [128, 8, NCTX], F32).ap() for i in range(3)]
    AB_d = nc.dram_tensor("AB_d", [NSEG, 128, 8, 2, 2], F32).ap()
    CAR_d = nc.dram_tensor("CAR_d", [128, 8, 2, 3, 2], F32).ap()
    with contextlib.ExitStack() as st0:
        sh = Shared(nc, st0)
        fw = sh.fw
        with contextlib.ExitStack() as sz:
            zt = sz.enter_context(nc.sbuf_tensor("zt", [128, 8, HALO], F32))
            fw.op('dve', lambda: nc.vector.memset(zt[:], 0.0), writes=['zt'])
            for i in range(2):
                fw.dma('sp', lambda: nc.sync.dma_start(out=XS[i][:, :, 0:HALO], in_=zt[:]), reads=['zt'], writes=['XS'])
                fw.dma('sp', lambda: nc.sync.dma_start(out=XS[i][:, :, HALO + SEQ:XPAD], in_=zt[:]), reads=['zt'], writes=['XS'])
            fw.barrier()
        for l in range(DEPTH):
            Xsrc = X0 if l == 0 else XS[(l - 1) % 2]
            Xdst = XS[l % 2]
            Csrc = C0 if l == 0 else CS[(l - 1) % 2]
            Cdst = CS[l % 2]
            for prepass in (True, False):
                for seg in range(NSEG):
                    if not prepass:
                        fwd = list(range(0, seg))
                        bwd = list(range(NSEG - 1, seg, -1))
                        for d_, lst in ((0, fwd), (1, bwd)):
                            for k in range(3):
                                if k < len(lst):
                                    src_ = AB_d[lst[k]][:, :, d_, :]
                                else:
                                    src_ = Call['car_id'][:, :, d_, k, :]
                                fw.dma('sp', lambda: nc.sync.dma_start(out=CAR_d[:, :, d_, k, :], in_=src_), writes=['CAR_d'])
                        fw.barrier()

                    def getin(name, shape, dt=F32, seg=seg, prepass=prepass):
                        if name == 'xT':
                            return Xsrc[:, :, seg * TOWN:seg * TOWN + TLAT]
                        if name == 'cT':
                            return Csrc
                        if name == 'cond':
                            return cond
                        if name == 'edge':
                            return Call['edge_all'][seg]
                        if name == 'car':
                            return Call['car_id'] if prepass else CAR_d
                        if name == 'rope_cos':
                            return Call['rope_cos_all'][:, seg * TOWN:seg * TOWN + TLAT]
                        if name == 'rope_sin':
                            return Call['rope_sin_all'][:, seg * TOWN:seg * TOWN + TLAT]
                        if name in Call:
                            return Call[name]
                        if name in Wexp:
                            return Wexp[name][l]
                        return Wall[name][l]

                    def getout(name, shape, dt=F32, seg=seg):
                        if name == 'ab_out':
                            return AB_d[seg]
                        if name == 'xout':
                            return XS[l % 2][:, :, HALO:HALO + TOWN]
                        if name == 'cout':
                            return CS[2]
                        raise KeyError(name)
                    if prepass:
                        emit_layer(nc, sh, getin, getout, True, seg=seg, lru="store", mod=("store" if seg == 0 else "load"))
                    else:
                        emit_layer(nc, sh, getin, getout, False, mode="premoe", seg=seg, lru="load", mod="load")

            def getout_m(name, shape, dt=F32):
                if name == 'xout':
                    return yout if l == DEPTH - 1 else Xdst[:, :, HALO:HALO + SEQ]
                if name == 'cout':
                    return Cdst
                return CS[2]
            emit_layer(nc, sh, getin, getout_m, False, mode="moe", mod="load")
        fw.finish([])
        print("fused instructions:", fw.n_instr)
    return nc


_PROG = {}


def _unfm(a):
    return np.ascontiguousarray(a.transpose(2, 1, 0).reshape(a.shape[2], D))


def kernel(**inputs):
    inp = {k: np.asarray(v) for k, v in inputs.items()}
    x = np.ascontiguousarray(inp['x'], dtype=np.float32)
    ctx = np.ascontiguousarray(inp['ctx'], dtype=np.float32)
    c, c_ctx = inp['c'].astype(np.float32), inp['c_ctx'].astype(np.float32)
    Ws = [prep_layer_weights(inp, l) for l in range(DEPTH)]
    EXPN = ("w1a", "w1b", "w3a", "w3b", "w2a", "w2b")
    Wst = {k: np.stack([Ws[l][k] for l in range(DEPTH)], 0) for k in Ws[0] if k not in EXPN}
    Wex = {"%s_%d" % (k, l): Ws[l][k] for k in EXPN for l in range(DEPTH)}
    wshapes = {k: Ws[0][k].shape for k in Ws[0]}
    del Ws
    C = all_consts()
    if 'fused' not in _PROG:
        _PROG['fused'] = build_fused(wshapes, {k: v.shape for k, v in C.items()})
    nc = _PROG['fused']
    cores = list(range(8))
    per_batch = []
    for b in range(2):
        xp = np.zeros((XPAD, D), np.float32)
        xp[HALO:HALO + SEQ] = x[b]
        m = {'X0': _fm(xp), 'C0': _fm(ctx[b]), 'cond': np.ascontiguousarray(np.stack([_pp(c[b]), _pp(c_ctx)], -1))}
        m.update(Wst)
        m.update(Wex)
        m.update(C)
        per_batch.append(m)
    maps = [per_batch[cc // NSEG] for cc in cores]
    res = run_bass_kernel_spmd(nc, maps, core_ids=cores)
    out = np.empty_like(x)
    for b in range(2):
        out[b] = _unfm(np.asarray(res.results[b * NSEG]['yout']))
    return out
```

```python
import contextlib
import numpy as np
import concourse.bass as bass
import concourse.mybir as mybir
from concourse.bass_utils import run_bass_kernel_spmd

F32 = mybir.dt.float32
BF16 = mybir.dt.bfloat16
I32 = mybir.dt.int32
AF = mybir.ActivationFunctionType
ALU = mybir.AluOpType
AX = mybir.AxisListType

D = 1024
DEPTH = 4
SEQ = 8192
NCTX = 256
NSEG = 4
TOWN = 2048
HALO = 128
TLAT = TOWN + 2 * HALO
TEXT = TLAT + NCTX
NOWN = TOWN + NCTX
ALPHA = (2.0 * DEPTH) ** 0.25
LN_EPS = 1e-6
NEXP = 32
NBLK = (NOWN * 2) // 128 + NEXP
BVN = 128
C_Q, C_QS, C_K, C_KS, C_U, C_V, C_VA, C_G, C_Z, C_XR = 0, 512, 1024, 1152, 1280, 1792, 2304, 2432, 5504, 6528
NCOL = 7552


class FW:
    NDMA = 24

    def __init__(self, nc, stack):
        self.nc = nc
        self.E = {'pe': nc.tensor, 'act': nc.scalar, 'dve': nc.vector, 'pool': nc.gpsimd, 'sp': nc.sync}
        self.sem = {}
        self.cnt = {}
        for e in ('pe', 'act', 'dve', 'pool'):
            self.sem[e] = stack.enter_context(nc.semaphore('s_' + e))
            self.cnt[e] = 0
        self.dsem = [stack.enter_context(nc.semaphore('d%d' % i)) for i in range(self.NDMA)]
        self.dval = [0] * self.NDMA
        self.dnext = 0
        self.waited = {}
        self.lastw = {}
        self.readers = {}
        self.n_instr = 0

    def _wait(self, e, tok):
        if tok is None:
            return
        kind, sk, val = tok
        k = (e, kind, sk)
        if self.waited.get(k, 0) >= val:
            return
        self.waited[k] = val
        s = self.sem[sk] if kind == 'eng' else self.dsem[sk]
        self.E[e].wait_ge(s, val)

    def _deps(self, e, reads, writes, strict=False):
        for r in reads:
            t = self.lastw.get(r)
            if t is not None:
                if t[0] == 'eng' and t[1] == e and e == 'pe':
                    continue
                self._wait(e, t)
        for w in writes:
            t = self.lastw.get(w)
            if t is not None and (strict or not (t[0] == 'eng' and t[1] == e)):
                self._wait(e, t)
            for t in self.readers.get(w, ()):
                if strict or not (t[0] == 'eng' and t[1] == e):
                    self._wait(e, t)

    def _commit(self, tok, reads, writes):
        for r in reads:
            self.readers.setdefault(r, []).append(tok)
        for w in writes:
            self.lastw[w] = tok
            self.readers[w] = []

    def op(self, e, fn, reads=(), writes=()):
        self._deps(e, reads, writes)
        ins = fn()
        self.cnt[e] += 1
        ins.then_inc(self.sem[e], 1)
        self._commit(('eng', e, self.cnt[e]), reads, writes)
        self.n_instr += 1
        return ins

    def dma(self, q, fn, reads=(), writes=()):
        i = self.dnext
        self.dnext = (self.dnext + 1) % self.NDMA
        if self.dval[i] > 0:
            self._wait(q, ('dma', i, self.dval[i]))
        self._deps(q, reads, writes, strict=True)
        ins = fn()
        self.dval[i] += 16
        ins.then_inc(self.dsem[i], 16)
        self._commit(('dma', i, self.dval[i]), reads, writes)
        self.n_instr += 1
        return ins

    def barrier(self):
        toks = [('eng', e2, self.cnt[e2]) for e2 in ('pe', 'act', 'dve', 'pool') if self.cnt[e2]]
        toks += [('dma', i, self.dval[i]) for i in range(self.NDMA) if self.dval[i]]
        for e in ('pe', 'act', 'dve', 'pool', 'sp'):
            for t in toks:
                if t[0] == 'eng' and t[1] == e:
                    continue
                self._wait(e, t)

    def finish(self, keys, e='sp'):
        for k in keys:
            self._wait(e, self.lastw.get(k))
        for i in range(self.NDMA):
            if self.dval[i]:
                self._wait(e, ('dma', i, self.dval[i]))


class Shared:
    def __init__(self, nc, st):
        self.nc = nc
        self.fw = FW(nc, st)
        self.PS = [st.enter_context(nc.psum_tensor("ps%d" % i, [128, 512], F32)) for i in range(7)]
        self.psi = 0
        self.uid = 0
        self.scr = {}

    def next_ps(self):
        i = self.psi
        self.psi = (i + 1) % 7
        return i

    def scratch(self, name, shape, dt):
        if name not in self.scr:
            self.scr[name] = self.nc.dram_tensor(name, list(shape), dt).ap()
        return self.scr[name]


def build_layer(prepass, dbg=False, phases=("lru", "att", "sgu", "merge", "moe", "moe8")):
    nc = bass.Bass("TRN2", target_bir_lowering=False)

    def din(name, shape, dt=F32):
        return nc.dram_tensor(name, list(shape), dt, kind="ExternalInput").ap()

    def dout(name, shape, dt=F32):
        return nc.dram_tensor(name, list(shape), dt, kind="ExternalOutput").ap()
    with contextlib.ExitStack() as st0:
        sh = Shared(nc, st0)
        emit_layer(nc, sh, din, dout, prepass, dbg, phases)
        sh.fw.finish([])
    return nc


def emit_layer(nc, sh, din, dout, prepass, dbg=False, phases=("lru", "att", "sgu", "merge", "moe", "moe8"), mode="full", seg=0, lru="compute", mod="compute"):
    xT = din("xT", [128, 8, TLAT])
    cT = din("cT", [128, 8, NCTX])
    cond = din("cond", [128, 8, 2])
    w_mod = din("w_mod", [128, 8, 6 * D])
    b_mod = din("b_mod", [128, 48])
    w_in = din("w_in", [128, 8, NCOL])
    b_in = din("b_in", [128, NCOL // 128])
    edge = din("edge", [128, 2])
    convw = din("convw", [128, 8, 2, 4])
    convb = din("convb", [128, 8, 2])
    wrg = din("wrg", [128, 2, 8, 128])
    wig = din("wig", [128, 2, 8, 128])
    brg = din("brg", [128, 8, 2])
    big = din("big", [128, 8, 2])
    lam = din("lam", [128, 8, 2])
    car = din("car", [128, 8, 2, 3, 2])
    ab_out = dout("ab_out", [128, 8, 2, 2])
    dbg_outs = {}
    if dbg:
        dbg_outs["r_dbg"] = dout("r_dbg", [128, 8, NOWN])
        dbg_outs["a_dbg"] = dout("a_dbg", [128, 4, NOWN])
        dbg_outs["b_dbg"] = dout("b_dbg", [64, 8, NOWN])
        dbg_outs["kt_dbg"] = dout("kt_dbg", [64, 2, TEXT])
        dbg_outs["x1_dbg"] = dout("x1_dbg", [128, 8, NOWN])
        dbg_outs["lg_dbg"] = dout("lg_dbg", [128, 18, 36])
        dbg_outs["sl_dbg"] = dout("sl_dbg", [128, 2, 18], I32)
        dbg_outs["g_dbg"] = dout("g_dbg", [128, 2, 18])
        dbg_outs["wi_dbg"] = dout("wi_dbg", [128, NBLK], I32)
        dbg_outs["vt_dbg"] = dout("vt_dbg", [128, 20, 128])
    if not prepass:
        rope_cos = din("rope_cos", [64, TLAT])
        rope_sin = din("rope_sin", [64, TLAT])
        masks = din("masks", [128, 2, 4, 128])
        sinkx = din("sinkx", [64, 8, 128])
        ones_in = din("ones_in", [128, 64])
        b_va = din("b_va", [128, 128])
        b_qk = din("b_qk", [64, 20])
        b_v = din("b_v", [128, 512])
        sgu_g = din("sgu_g", [128, 512])
        sgu_b = din("sgu_b", [128, 512])
        w_spT = din("w_spT", [128, 4, 128])
        b_sp = din("b_sp", [128, 4, 128])
        w_pa = din("w_pa", [128, 4, D])
        w_pb = din("w_pb", [64, 8, D])
        w_pc = din("w_pc", [128, 8, D])
        w_o = din("w_o", [128, 8, D])
        b_o = din("b_o", [128, 8])
        ln1g = din("ln1g", [128, 8])
        ln1b = din("ln1b", [128, 8])
        ln2g = din("ln2g", [128, 8])
        ln2b = din("ln2b", [128, 8])
        w_gr = din("w_gr", [128, 8, 36])
        b_gr = din("b_gr", [128, 36])
        ident = din("ident", [128, 128])
        ones_f = din("ones_f", [128, 128])
        tri_in = din("tri_in", [128, 128])
        bval_in = din("bval_in", [128, BVN])
        pidx_in = din("pidx_in", [128, 1])
        wexp = {}
        for nm in ("w1a", "w1b", "w3a", "w3b", "w2a", "w2b"):
            wexp[nm] = din(nm, [NEXP * 128, 2048])
        xout = dout("xout", [128, 8, TOWN])
        cout = dout("cout", [128, 8, NCTX])

    sh.uid += 1
    sfx = "_%d" % sh.uid
    if mode == "premoe":
        phases = ("lru", "att", "sgu", "merge")
    elif mode == "moe":
        phases = ("moe", "moe8")
    GL = mode in ("premoe", "moe")
    T = 66 if mode == "moe" else 18
    SUB = 2 if mode == "moe" else 1
    BS = 128 * SUB
    NB = (66 + NEXP) if mode == "moe" else NBLK
    with contextlib.ExitStack() as st:
        fw = sh.fw

        def sb(name, shape, dt=F32, stack=st):
            return stack.enter_context(nc.sbuf_tensor(name + sfx, list(shape), dt))

        def ps(name, shape, dt=F32, stack=st):
            return stack.enter_context(nc.psum_tensor(name + sfx, list(shape), dt))

        PS = sh.PS
        next_ps = sh.next_ps

        def load(q, dst, src, wkey, rkeys=()):
            eng = {'sp': nc.sync, 'act': nc.scalar, 'pool': nc.gpsimd}[q]
            return fw.dma(q, lambda: eng.dma_start(out=dst, in_=src), reads=list(rkeys), writes=[wkey])

        condt = sb("condt", [128, 8, 2])
        condb = sb("condb", [128, 8, 2], BF16)
        sig = sb("sig", [128, 8, 2])
        bmod = sb("bmod", [128, 48])
        mlat = sb("mlat", [128, 48])
        mctx = sb("mctx", [128, 48])
        bint = sb("bint", [128, NCOL // 128])
        edget = sb("edget", [128, 2])
        cw = sb("cw", [128, 8, 2, 4])
        cb = sb("cb", [128, 8, 2])
        brt = sb("brt", [128, 8, 2])
        bit_ = sb("bit", [128, 8, 2])
        lamt = sb("lamt", [128, 8, 2])
        negc = sb("negc", [128, 8, 2])
        negc2 = sb("negc2", [128, 8, 2])
        cart = sb("cart", [128, 8, 2, 3, 2])
        abt = sb("abt", [128, 8, 2, 2])
        load('sp', condt[:], cond, 'condt')
        load('sp', bmod[:], b_mod, 'bmod')
        load('sp', bint[:], b_in, 'bint')
        load('sp', edget[:], edge, 'edget')
        load('sp', cw[:], convw, 'cw')
        load('sp', cb[:], convb, 'cb')
        load('sp', brt[:], brg, 'brt')
        load('sp', bit_[:], big, 'bit')
        load('sp', lamt[:], lam, 'lamt')
        load('sp', cart[:], car, 'cart')

        fw.op('act', lambda: nc.scalar.activation(out=sig[:], in_=condt[:], func=AF.Sigmoid), reads=['condt'], writes=['sig'])
        fw.op('dve', lambda: nc.vector.tensor_tensor(out=condb[:], in0=condt[:], in1=sig[:], op=ALU.mult), reads=['condt', 'sig'], writes=['condb'])
        fw.op('act', lambda: nc.scalar.activation(out=negc[:], in_=lamt[:], func=AF.Exp, scale=-1.0), reads=['lamt'], writes=['negc'])
        fw.op('act', lambda: nc.scalar.activation(out=negc[:], in_=negc[:], func=AF.Ln, bias=1.0), reads=['negc'], writes=['negc'])
        fw.op('dve', lambda: nc.vector.tensor_scalar(out=negc2[:], in0=negc[:], scalar1=-16.0, scalar2=None, op0=ALU.mult), reads=['negc'], writes=['negc2'])
        fw.op('dve', lambda: nc.vector.tensor_scalar(out=negc[:], in0=negc[:], scalar1=-8.0, scalar2=None, op0=ALU.mult), reads=['negc'], writes=['negc'])

        MOD_d = sh.scratch("MOD_d", [128, 96], F32) if mod != "compute" else None
        if mod == "load":
            load('sp', mlat[:], MOD_d[:, 0:48], 'mlat')
            load('sp', mctx[:], MOD_d[:, 48:96], 'mctx')
        with contextlib.ExitStack() as s1:
            wm = [sb("wm%d" % i, [128, 8, 512], BF16, s1) for i in range(2)]
            if mod == "load":
                wm = None
            pmi = next_ps()
            pm = PS[pmi]
            pmk = 'ps%d' % pmi
            for g in (range(12) if mod != "load" else ()):
                wbuf = wm[g % 2]
                load('pool', wbuf[:], w_mod[:, :, g * 512:(g + 1) * 512], 'wm%d' % (g % 2))
                for jj in range(4):
                    j = g * 4 + jj
                    for kc in range(8):
                        fw.op('pe', lambda: nc.tensor.matmul(pm[:, 2 * j:2 * j + 2], lhsT=wbuf[:, kc, jj * 128:(jj + 1) * 128],
                                                              rhs=condb[:, kc, :], start=(kc == 0), stop=(kc == 7)),
                              reads=['wm%d' % (g % 2), 'condb'], writes=[pmk])
            pmv = pm[:, 0:96].rearrange("p (j t) -> p j t", t=2)
            if mod != "load":
                fw.op('dve', lambda: nc.vector.tensor_tensor(out=mlat[:], in0=pmv[:, :, 0], in1=bmod[:], op=ALU.add), reads=[pmk, 'bmod'], writes=['mlat'])
                fw.op('dve', lambda: nc.vector.tensor_tensor(out=mctx[:], in0=pmv[:, :, 1], in1=bmod[:], op=ALU.add), reads=[pmk, 'bmod'], writes=['mctx'])
            if mod == "store":
                load('sp', MOD_d[:, 0:48], mlat[:], 'MOD_d', ['mlat'])
                load('sp', MOD_d[:, 48:96], mctx[:], 'MOD_d', ['mctx'])
        fw.barrier()
        m1p = sb("m1p", [128, 2, 8])
        m4p = sb("m4p", [128, 2, 8])
        for t_, mv in ((0, mlat), (1, mctx)):
            fw.op('dve', lambda: nc.vector.tensor_scalar(out=m1p[:, t_, :], in0=mv[:, 8:16], scalar1=1.0, scalar2=None, op0=ALU.add), reads=['mlat', 'mctx'], writes=['m1p'])
            fw.op('dve', lambda: nc.vector.tensor_scalar(out=m4p[:, t_, :], in0=mv[:, 32:40], scalar1=1.0, scalar2=None, op0=ALU.add), reads=['mlat', 'mctx'], writes=['m4p'])

        sY = st.enter_context(contextlib.ExitStack())
        yT = sY.enter_context(nc.sbuf_tensor("yT" + sfx, [128, 8, NOWN if not prepass else 8], BF16, side='right'))
        sHT = contextlib.ExitStack()
        hT = sHT.enter_context(nc.sbuf_tensor("hT" + sfx, [128, 8, TEXT], BF16, side='right'))
        with contextlib.ExitStack() as s2:
            xs = [sb("xs%d" % i, [128, TLAT], F32, s2) for i in range(2)]
            for c in (range(8) if mode != "moe" else ()):
                xb_ = xs[c % 2]
                k = 'xs%d' % (c % 2)
                load('sp', xb_[:], xT[:, c, :], k)
                fw.op('act', lambda: nc.scalar.activation(out=hT[:, c, 0:TLAT], in_=xb_[:], func=AF.Identity,
                                                          scale=m1p[:, 0, c:c + 1], bias=mlat[:, c:c + 1]),
                      reads=[k, 'm1p', 'mlat'], writes=['hT%d' % c])
                load('sp', xb_[:, 0:NCTX], cT[:, c, :], k)
                fw.op('act', lambda: nc.scalar.activation(out=hT[:, c, TLAT:TEXT], in_=xb_[:, 0:NCTX], func=AF.Identity,
                                                          scale=m1p[:, 1, c:c + 1], bias=mctx[:, c:c + 1]),
                      reads=[k, 'm1p', 'mctx'], writes=['hT%d' % c])
        fw.barrier()
        hkeys = ['hT%d' % c for c in range(8)]

        def inproj(col0, ncols, tok0, ntok, wkey, wtile, evac):
            if wtile is not None:
                wsl = lambda kc: wtile[:, kc, col0:col0 + ncols]
            else:
                wsl = col0
            return inproj2(wsl, ncols, tok0, ntok, wkey, evac)

        def inproj2(wsl, ncols, tok0, ntok, wkey, evac):
            pi = next_ps()
            pt = PS[pi]
            for kc in range(8):
                fw.op('pe', lambda: nc.tensor.matmul(pt[0:ncols, 0:ntok], lhsT=wsl(kc), rhs=hT[:, kc, tok0:tok0 + ntok],
                                                      start=(kc == 0), stop=(kc == 7)),
                      reads=[wkey, 'hT%d' % kc], writes=['ps%d' % pi])
            evac(pt, 'ps%d' % pi)

        R_d = sh.scratch("R_d", [128, 8, NOWN], BF16)
        A_d = sh.scratch("A_d", [128, 4, NOWN], BF16)
        B_d = sh.scratch("B_d", [64, 8, NOWN], BF16)
        if "lru" in phases:
          with contextlib.ExitStack() as s3:
            rst = sb("rst", [128, NOWN], BF16, s3)
            wxr = [sb("wxr%d" % i, [128, 8, 128], BF16, s3) for i in range(2)]
            wz = [sb("wz%d" % i, [128, 8, 128], BF16, s3) for i in range(2)]
            wr_t = sb("wr_t", [128, 2, 8, 128], BF16, s3)
            wi_t = sb("wi_t", [128, 2, 8, 128], BF16, s3)
            load('pool', wr_t[:], wrg, 'wr_t')
            load('pool', wi_t[:], wig, 'wi_t')
            XRL = sb("XRL", [128, TLAT if lru != "load" else 8], F32, s3)
            XRC = sb("XRC", [128, NCTX + 6], F32, s3)
            GZ = sb("GZ", [128, NOWN if lru != "store" else 8], F32, s3)
            LAU = sh.scratch("LAU", [NSEG, 8, 2, 2, 128, NOWN], F32) if lru != "compute" else None
            if lru == "load":
                TT = TB = RG = [None, None]
                IG = [sb("IG%d" % i, [128, NOWN], F32, s3) for i in range(2)]
                AA = [sb("AA%d" % i, [128, NOWN], F32, s3) for i in range(2)]
            elif lru == "store":
                TT = [sb("TT%d" % i, [128, NOWN], F32, s3) for i in range(2)]
                TB = [sb("TB%d" % i, [128, NOWN], BF16, s3) for i in range(2)]
                RG = [sb("RG%d" % i, [128, NOWN], F32, s3) for i in range(2)]
                IG = [sb("IG%d" % i, [128, NOWN], F32, s3) for i in range(2)]
                AA = [sb("AA%d" % i, [128, NOWN], F32, s3) for i in range(2)]
            else:
                TT = [sb("TT0", [128, NOWN], F32, s3)] * 2
                TB = [sb("TB0", [128, NOWN], BF16, s3)] * 2
                RG = [sb("RG0", [128, NOWN], F32, s3)] * 2
                IG = [sb("IG0", [128, NOWN], F32, s3)] * 2
                AA = [sb("AA0", [128, NOWN], F32, s3)] * 2
            HH = [sb("HH%d" % i, [128, NOWN], F32, s3) for i in range(2)]
            sm = sb("sm", [128, 16], F32, s3)
            fw.op('pool', lambda: nc.gpsimd.memset(XRC[:], 0.0), writes=['XRC'])
            for j in range(8):
                wx = wxr[j % 2]
                wzz = wz[j % 2]
                kx = 'wxr%d' % (j % 2)
                kz = 'wz%d' % (j % 2)
                load('pool', wx[:], w_in[:, :, C_XR + j * 128:C_XR + (j + 1) * 128], kx)
                load('pool', wzz[:], w_in[:, :, C_Z + j * 128:C_Z + (j + 1) * 128], kz)
                bx = bint[:, (C_XR // 128) + j:(C_XR // 128) + j + 1]
                bz = bint[:, (C_Z // 128) + j:(C_Z // 128) + j + 1]
                for ti in (range(5) if lru != "load" else ()):
                    def ev(pt, pk, ti=ti):
                        if ti < 4:
                            fw.op('act', lambda: nc.scalar.activation(out=XRL[:, ti * 512:(ti + 1) * 512], in_=pt[:, 0:512], func=AF.Identity, bias=bx),
                                  reads=[pk, 'bint'], writes=['XRL'])
                        else:
                            fw.op('act', lambda: nc.scalar.activation(out=XRL[:, 2048:2304], in_=pt[:, 0:256], func=AF.Identity, bias=bx),
                                  reads=[pk, 'bint'], writes=['XRL'])
                            fw.op('act', lambda: nc.scalar.activation(out=XRC[:, 3:3 + NCTX], in_=pt[:, 256:512], func=AF.Identity, bias=bx),
                                  reads=[pk, 'bint'], writes=['XRC'])
                    inproj(0, 128, ti * 512, 512, kx, wx, ev)
                if lru != "load":
                  fw.op('dve', lambda: nc.vector.tensor_scalar(out=XRL[:, 0:HALO], in0=XRL[:, 0:HALO], scalar1=edget[:, 0:1], scalar2=None, op0=ALU.mult),
                      reads=['XRL', 'edget'], writes=['XRL'])
                if lru != "load":
                  fw.op('dve', lambda: nc.vector.tensor_scalar(out=XRL[:, HALO + TOWN:TLAT], in0=XRL[:, HALO + TOWN:TLAT], scalar1=edget[:, 1:2], scalar2=None, op0=ALU.mult),
                      reads=['XRL', 'edget'], writes=['XRL'])
                for ti in (range(5) if lru != "store" else ()):
                    def evz(pt, pk, ti=ti):
                        if ti < 4:
                            fw.op('act', lambda: nc.scalar.activation(out=GZ[:, ti * 512:(ti + 1) * 512], in_=pt[:, 0:512], func=AF.Gelu, bias=bz),
                                  reads=[pk, 'bint'], writes=['GZ'])
                        else:
                            fw.op('act', lambda: nc.scalar.activation(out=GZ[:, 2048:2304], in_=pt[:, 0:256], func=AF.Gelu, bias=bz),
                                  reads=[pk, 'bint'], writes=['GZ'])
                    if ti < 4:
                        inproj(0, 128, HALO + ti * 512, 512, kz, wzz, evz)
                    else:
                        inproj(0, 128, TLAT, 256, kz, wzz, evz)
                for d_ in range(2):
                    T_ = TT[d_]; Tb = TB[d_]; Rg = RG[d_]; Ig = IG[d_]; A_ = AA[d_]; H_ = HH[d_]
                    kT, kTb, kR, kI, kA, kH = 'TT', 'TB', 'RG', 'IG', 'AA', 'HH%d' % d_
                    if lru == "store":
                        kT, kTb, kR, kI, kA = 'TT%d' % d_, 'TB%d' % d_, 'RG%d' % d_, 'IG%d' % d_, 'AA%d' % d_
                    if lru == "load":
                        kI, kA = 'IG%d' % d_, 'AA%d' % d_
                        load('sp', A_[:], LAU[seg, j, d_, 0], kA)
                        load('sp', Ig[:], LAU[seg, j, d_, 1], kI)
                    else:
                        for (dst0, n, src, s0, sk) in ((0, TOWN, XRL, HALO, 'XRL'), (TOWN, NCTX, XRC, 3, 'XRC')):
                            for tap in range(4):
                                off = s0 + (tap - 3 if d_ == 0 else tap)
                                wcol = cw[:, j, d_, tap:tap + 1]
                                if tap == 0:
                                    fw.op('dve', lambda: nc.vector.tensor_scalar(out=T_[:, dst0:dst0 + n], in0=src[:, off:off + n], scalar1=wcol,
                                                                                 scalar2=cb[:, j, d_:d_ + 1], op0=ALU.mult, op1=ALU.add),
                                          reads=[sk, 'cw', 'cb'], writes=[kT])
                                else:
                                    fw.op('dve', lambda: nc.vector.scalar_tensor_tensor(out=T_[:, dst0:dst0 + n], in0=src[:, off:off + n], scalar=wcol,
                                                                                        in1=T_[:, dst0:dst0 + n], op0=ALU.mult, op1=ALU.add),
                                          reads=[sk, 'cw', kT], writes=[kT])
                        fw.op('pool', lambda: nc.gpsimd.tensor_copy(out=Tb[:], in_=T_[:]), reads=[kT], writes=[kTb])
                        for ti in range(5):
                            t0 = ti * 512
                            n = 512 if ti < 4 else 256
                            for (wt_, wk, bt_, bk, dst, dk) in ((wr_t, 'wr_t', brt, 'brt', Rg, kR), (wi_t, 'wi_t', bit_, 'bit', Ig, kI)):
                                pi = next_ps()
                                pt = PS[pi]
                                fw.op('pe', lambda: nc.tensor.matmul(pt[:, 0:n], lhsT=wt_[:, d_, j, :], rhs=Tb[:, t0:t0 + n], start=True, stop=True),
                                      reads=[wk, kTb], writes=['ps%d' % pi])
                                fw.op('act', lambda: nc.scalar.activation(out=dst[:, t0:t0 + n], in_=pt[:, 0:n], func=AF.Sigmoid, bias=bt_[:, j, d_:d_ + 1]),
                                      reads=['ps%d' % pi, bk], writes=[dk])
                        fw.op('act', lambda: nc.scalar.activation(out=A_[:], in_=Rg[:], func=AF.Exp, scale=negc[:, j, d_:d_ + 1]), reads=[kR, 'negc'], writes=[kA])
                        fw.op('act', lambda: nc.scalar.activation(out=H_[:], in_=Rg[:], func=AF.Exp, scale=negc2[:, j, d_:d_ + 1]), reads=[kR, 'negc2'], writes=[kH])
                        fw.op('act', lambda: nc.scalar.activation(out=H_[:], in_=H_[:], func=AF.Sqrt, scale=-1.0, bias=1.0), reads=[kH], writes=[kH])
                        fw.op('pool', lambda: nc.gpsimd.tensor_tensor(out=Ig[:], in0=Ig[:], in1=T_[:], op=ALU.mult), reads=[kI, kT], writes=[kI])
                        fw.op('pool', lambda: nc.gpsimd.tensor_tensor(out=Ig[:], in0=Ig[:], in1=H_[:], op=ALU.mult), reads=[kI, kH], writes=[kI])
                        rs = sm[:, 8 + d_:9 + d_]
                        fw.op('dve', lambda: nc.vector.reduce_sum(out=rs, in_=Rg[:, 0:TOWN], axis=AX.X), reads=[kR], writes=['sm_rs%d' % d_])
                        fw.op('act', lambda: nc.scalar.activation(out=abt[:, j, d_, 0:1], in_=rs, func=AF.Exp, scale=negc[:, j, d_:d_ + 1]),
                              reads=['sm_rs%d' % d_, 'negc'], writes=['abt'])
                        if lru == "store":
                            load('sp', LAU[seg, j, d_, 0], A_[:], 'LAU', [kA])
                            load('sp', LAU[seg, j, d_, 1], Ig[:], 'LAU', [kI])
                    if d_ == 0:
                        a_c, u_c, h_c = A_[:, TOWN:NOWN], Ig[:, TOWN:NOWN], H_[:, TOWN:NOWN]
                        a_l, u_l, h_l = A_[:, 0:TOWN], Ig[:, 0:TOWN], H_[:, 0:TOWN]
                        hc_fin = H_[:, NOWN - 1:NOWN]
                        hl_fin = H_[:, TOWN - 1:TOWN]
                    else:
                        a_c, u_c, h_c = A_[:, TOWN:NOWN][:, ::-1], Ig[:, TOWN:NOWN][:, ::-1], H_[:, TOWN:NOWN][:, ::-1]
                        a_l, u_l, h_l = A_[:, 0:TOWN][:, ::-1], Ig[:, 0:TOWN][:, ::-1], H_[:, 0:TOWN][:, ::-1]
                        hc_fin = H_[:, TOWN:TOWN + 1]
                        hl_fin = H_[:, 0:1]
                    fw.op('dve', lambda: nc.vector.tensor_tensor_scan(out=h_c, data0=a_c, data1=u_c, initial=0.0, op0=ALU.mult, op1=ALU.add),
                          reads=[kA, kI], writes=[kH])
                    hin = sm[:, d_ * 4:d_ * 4 + 1]
                    fw.op('dve', lambda: nc.vector.tensor_copy(out=hin, in_=hc_fin), reads=[kH], writes=['hin%d' % d_])
                    for k3 in range(3):
                        fw.op('dve', lambda: nc.vector.scalar_tensor_tensor(out=hin, in0=hin, scalar=cart[:, j, d_, k3, 0:1], in1=cart[:, j, d_, k3, 1:2],
                                                                            op0=ALU.mult, op1=ALU.add),
                              reads=['hin%d' % d_, 'cart'], writes=['hin%d' % d_])
                    fw.op('dve', lambda: nc.vector.tensor_tensor_scan(out=h_l, data0=a_l, data1=u_l, initial=hin, op0=ALU.mult, op1=ALU.add),
                          reads=[kA, kI, 'hin%d' % d_], writes=[kH])
                    tmp = sm[:, d_ * 4 + 1:d_ * 4 + 2]
                    if lru != "load":
                        fw.op('dve', lambda: nc.vector.tensor_tensor(out=tmp, in0=abt[:, j, d_, 0:1], in1=hin, op=ALU.mult), reads=['abt', 'hin%d' % d_], writes=['tmp%d' % d_])
                        fw.op('dve', lambda: nc.vector.tensor_tensor(out=abt[:, j, d_, 1:2], in0=hl_fin, in1=tmp, op=ALU.subtract), reads=[kH, 'tmp%d' % d_], writes=['abt'])
                if lru != "store":
                    fw.op('pool', lambda: nc.gpsimd.tensor_tensor(out=HH[0][:], in0=HH[0][:], in1=HH[1][:], op=ALU.add), reads=['HH0', 'HH1'], writes=['HH0'])
                    fw.op('pool', lambda: nc.gpsimd.tensor_tensor(out=rst[:], in0=HH[0][:], in1=GZ[:], op=ALU.mult), reads=['HH0', 'GZ'], writes=['rst'])
                    load('sp', R_d[:, j, :], rst[:], 'R_d', ['rst'])
          fw.barrier()
          if lru != "load":
              load('sp', ab_out, abt[:], 'ab_out', ['abt'])
        outs = ['ab_out']
        if prepass:
            sHT.close()
            fw.barrier()
            return
        if "att" in phases:
          with contextlib.ExitStack() as s4:
            KT = sb("KT", [64, 2, TEXT], BF16, s4)
            Vtok = sb("Vtok", [128, 20, 128], BF16, s4)
            COS = sb("COS", [64, TLAT], F32, s4)
            SIN = sb("SIN", [64, TLAT], F32, s4)
            MSK = sb("MSK", [128, 2, 4, 128], F32, s4)
            ESX = sb("ESX", [64, 8, 128], F32, s4)
            ONES = sb("ONES", [128, 64], BF16, s4)
            BVA = sb("BVA", [128, 128], F32, s4)
            bqk = sb("bqk", [64, 20], F32, s4)
            wk_t = sb("wk_t", [128, 8, 256], BF16, s4)
            wva_t = sb("wva_t", [128, 8, 128], BF16, s4)
            wq_t = sb("wq_t", [128, 8, 1024], BF16, s4)
            load('sp', COS[:], rope_cos, 'COS')
            load('sp', SIN[:], rope_sin, 'SIN')
            load('sp', MSK[:], masks, 'MSK')
            load('sp', ESX[:], sinkx, 'ESX')
            load('sp', BVA[:], b_va, 'BVA')
            load('sp', bqk[:], b_qk, 'bqk')
            load('pool', ONES[:], ones_in, 'ONES')
            load('pool', wk_t[:], w_in[:, :, C_K:C_K + 256], 'wk_t')
            load('pool', wva_t[:], w_in[:, :, C_VA:C_VA + 128], 'wva_t')
            load('pool', wq_t[:], w_in[:, :, C_Q:C_Q + 1024], 'wq_t')
            fw.op('act', lambda: nc.scalar.activation(out=ESX[:], in_=ESX[:], func=AF.Exp), reads=['ESX'], writes=['ESX'])
            QF = [sb("QF%d" % i, [64, 512], F32, s4) for i in range(2)]
            QS = [sb("QS%d" % i, [64, 512], F32, s4) for i in range(2)]
            rot = [0]

            def roped(wt, wk, c_main, c_sw, bcol_main, bcol_sw, e0, n, dst, dkey, use_rope):
                i = rot[0] % 2
                rot[0] += 1
                qf, qs = QF[i], QS[i]

                def ev1(pt, pk):
                    fw.op('act', lambda: nc.scalar.activation(out=qf[:, 0:n], in_=pt[0:64, 0:n], func=AF.Identity, bias=bqk[:, bcol_main:bcol_main + 1]),
                          reads=[pk, 'bqk'], writes=['QF%d' % i])
                inproj2(lambda kc: wt[:, kc, c_main:c_main + 64], 64, e0, n, wk, ev1)
                if not use_rope:
                    fw.op('dve', lambda: nc.vector.tensor_copy(out=dst, in_=qf[:, 0:n]), reads=['QF%d' % i], writes=[dkey])
                    return

                def ev2(pt, pk):
                    fw.op('act', lambda: nc.scalar.activation(out=qs[:, 0:n], in_=pt[0:64, 0:n], func=AF.Identity, bias=bqk[:, bcol_sw:bcol_sw + 1]),
                          reads=[pk, 'bqk'], writes=['QS%d' % i])
                inproj2(lambda kc: wt[:, kc, c_sw:c_sw + 64], 64, e0, n, wk, ev2)
                fw.op('dve', lambda: nc.vector.tensor_tensor(out=qf[:, 0:n], in0=qf[:, 0:n], in1=COS[:, e0:e0 + n], op=ALU.mult), reads=['QF%d' % i, 'COS'], writes=['QF%d' % i])
                fw.op('pool', lambda: nc.gpsimd.tensor_tensor(out=qs[:, 0:n], in0=qs[:, 0:n], in1=SIN[:, e0:e0 + n], op=ALU.mult), reads=['QS%d' % i, 'SIN'], writes=['QS%d' % i])
                fw.op('dve', lambda: nc.vector.tensor_tensor(out=dst, in0=qf[:, 0:n], in1=qs[:, 0:n], op=ALU.add), reads=['QF%d' % i, 'QS%d' % i], writes=[dkey])

            for kv in range(2):
                for ti in range(5):
                    if ti < 4:
                        roped(wk_t, 'wk_t', kv * 64, 128 + kv * 64, 16 + kv, 18 + kv, ti * 512, 512, KT[:, kv, ti * 512:(ti + 1) * 512], 'KT', True)
                    else:
                        roped(wk_t, 'wk_t', kv * 64, 128 + kv * 64, 16 + kv, 18 + kv, 2048, 256, KT[:, kv, 2048:2304], 'KT', True)
                        roped(wk_t, 'wk_t', kv * 64, 128 + kv * 64, 16 + kv, 18 + kv, TLAT, 256, KT[:, kv, TLAT:TEXT], 'KT', False)
            for tt in range(20):
                pi = next_ps()
                pt = PS[pi]
                for kc in range(8):
                    fw.op('pe', lambda: nc.tensor.matmul(pt[:, 0:128], lhsT=hT[:, kc, tt * 128:(tt + 1) * 128], rhs=wva_t[:, kc, :], start=(kc == 0), stop=(kc == 7)),
                          reads=['wva_t', 'hT%d' % kc], writes=['ps%d' % pi])
                fw.op('dve', lambda: nc.vector.tensor_tensor(out=Vtok[:, tt, :], in0=pt[:, 0:128], in1=BVA[:], op=ALU.add), reads=['ps%d' % pi, 'BVA'], writes=['Vtok'])
            QT = sb("QT", [64, 8, 512], BF16, s4)
            PTS = [sb("PT%d" % i, [128, 4, 128], BF16, s4) for i in range(3)]
            DEN = sb("DEN", [64, 4, 128], F32, s4)
            BTS = [sb("BTS%d" % i, [64, 8, 128], BF16, s4) for i in range(2)]
            pti = [0]
            nqb = 0
            for ti in range(5):
                lat = ti < 4
                n = 512 if lat else 256
                e0 = HALO + ti * 512 if lat else TLAT
                for h in range(8):
                    roped(wq_t, 'wq_t', h * 64, 512 + h * 64, h, 8 + h, e0, n, QT[:, h, 0:n], 'QT', lat)
                for qb in range(n // 128):
                    q0 = qb * 128
                    if lat:
                        bi = ti * 4 + qb
                        ktiles = [(bi, 'p'), (bi + 1, 'o'), (bi + 2, 'n'), (18, 'c'), (19, 'c')]
                        own0 = bi * 128
                    else:
                        bi = -1
                        ktiles = [(18, 'c'), (19, 'c')]
                        own0 = TOWN + qb * 128
                    bts = BTS[nqb % 2]
                    bk = 'BTS%d' % (nqb % 2)
                    nqb += 1
                    for kv in range(2):
                        po_i = next_ps(); pd_i = next_ps()
                        po, pd = PS[po_i], PS[pd_i]
                        for ki, (tt, kind) in enumerate(ktiles):
                            psi_ = next_ps()
                            pss = PS[psi_]
                            fw.op('pe', lambda: nc.tensor.matmul(pss[:, 0:512], lhsT=KT[:, kv, tt * 128:(tt + 1) * 128],
                                                                  rhs=QT[:, 4 * kv:4 * kv + 4, q0:q0 + 128], start=True, stop=True),
                                  reads=['KT', 'QT'], writes=['ps%d' % psi_])
                            pi3 = pti[0] % 3
                            pti[0] += 1
                            P_ = PTS[pi3]
                            pk3 = 'PT%d' % pi3
                            fw.op('act', lambda: nc.scalar.activation(out=P_[:].rearrange("p g q -> p (g q)"), in_=pss[:, 0:512], func=AF.Exp, scale=0.125),
                                  reads=['ps%d' % psi_], writes=[pk3])
                            if kind in ('p', 'n'):
                                mi = 0 if kind == 'p' else 1
                                if (kind == 'p' and bi == 0) or (kind == 'n' and bi == 15):
                                    fw.op('dve', lambda: nc.vector.scalar_tensor_tensor(out=P_[:], in0=P_[:], scalar=edget[:, mi:mi + 1], in1=MSK[:, mi, :, :], op0=ALU.mult, op1=ALU.mult),
                                          reads=[pk3, 'MSK', 'edget'], writes=[pk3])
                                else:
                                    fw.op('dve', lambda: nc.vector.tensor_tensor(out=P_[:], in0=P_[:], in1=MSK[:, mi, :, :], op=ALU.mult), reads=[pk3, 'MSK'], writes=[pk3])
                            last = ki == len(ktiles) - 1
                            fw.op('pe', lambda: nc.tensor.matmul(po[0:64, 0:512], lhsT=Vtok[:, tt, kv * 64:(kv + 1) * 64], rhs=P_[:].rearrange("p g q -> p (g q)"), start=(ki == 0), stop=last),
                                  reads=['Vtok', pk3], writes=['ps%d' % po_i])
                            fw.op('pe', lambda: nc.tensor.matmul(pd[0:64, 0:512], lhsT=ONES[:], rhs=P_[:].rearrange("p g q -> p (g q)"), start=(ki == 0), stop=last),
                                  reads=['ONES', pk3], writes=['ps%d' % pd_i])
                        fw.op('dve', lambda: nc.vector.tensor_tensor(out=DEN[:], in0=pd[0:64, 0:512].rearrange("p (g q) -> p g q", g=4), in1=ESX[:, 4 * kv:4 * kv + 4, :], op=ALU.add),
                              reads=['ps%d' % pd_i, 'ESX'], writes=['DEN'])
                        fw.op('dve', lambda: nc.vector.reciprocal(out=DEN[:], in_=DEN[:]), reads=['DEN'], writes=['DEN'])
                        fw.op('dve', lambda: nc.vector.tensor_tensor(out=bts[:, 4 * kv:4 * kv + 4, :], in0=po[0:64, 0:512].rearrange("p (g q) -> p g q", g=4), in1=DEN[:], op=ALU.mult),
                              reads=['ps%d' % po_i, 'DEN'], writes=[bk])
                    load('sp', B_d[:, :, own0:own0 + 128], bts[:], 'B_d', [bk])

            if dbg:
                ktf = sb("ktf", [64, 2, TEXT], F32, s4)
                vtf = sb("vtf", [128, 20, 128], F32, s4)
                fw.op('dve', lambda: nc.vector.tensor_copy(out=ktf[:], in_=KT[:]), reads=['KT'], writes=['ktf'])
                fw.op('dve', lambda: nc.vector.tensor_copy(out=vtf[:], in_=Vtok[:]), reads=['Vtok'], writes=['vtf'])
                load('sp', dbg_outs['kt_dbg'], ktf[:], 'kt_dbg', ['ktf'])
                load('sp', dbg_outs['vt_dbg'], vtf[:], 'vt_dbg', ['vtf'])
                outs.extend(['kt_dbg', 'vt_dbg'])
        fw.barrier()
        if "sgu" in phases:
          with contextlib.ExitStack() as s5:
            wu_t = sb("wu_t", [128, 8, 512], BF16, s5)
            wv_t = sb("wv_t", [128, 8, 512], BF16, s5)
            BV = sb("BV", [128, 512], F32, s5)
            LNG = sb("LNG", [128, 512], F32, s5)
            LNB = sb("LNB", [128, 512], F32, s5)
            WST = sb("WST", [128, 4, 128], BF16, s5)
            BSP = sb("BSP", [128, 4, 128], F32, s5)
            load('pool', wu_t[:], w_in[:, :, C_U:C_U + 512], 'wu_t')
            load('pool', wv_t[:], w_in[:, :, C_V:C_V + 512], 'wv_t')
            load('pool', WST[:], w_spT, 'WST')
            load('sp', BV[:], b_v, 'BV')
            load('sp', LNG[:], sgu_g, 'LNG')
            load('sp', LNB[:], sgu_b, 'LNB')
            load('sp', BSP[:], b_sp, 'BSP')
            GU = sb("GU", [128, 4, 512], F32, s5)
            V1 = sb("V1", [128, 512], F32, s5)
            GV = sb("GV", [128, 512], F32, s5)
            VNB = sb("VNB", [128, 512], BF16, s5)
            STT = sb("STT", [128, 6], F32, s5)
            MV = sb("MV", [128, 2], F32, s5)
            RSTD = sb("RSTD", [128, 1], F32, s5)
            MX = sb("MX", [128, 4, 128], F32, s5)
            ATS = [sb("ATS%d" % i, [128, 4, 128], BF16, s5) for i in range(2)]
            nch = 0
            for ti in range(5):
                lat = ti < 4
                n = 512 if lat else 256
                e0 = HALO + ti * 512 if lat else TLAT
                for g in range(4):
                    def evu(pt, pk, g=g):
                        fw.op('act', lambda: nc.scalar.activation(out=GU[:, g, 0:n], in_=pt[:, 0:n], func=AF.Gelu, bias=bint[:, C_U // 128 + g:C_U // 128 + g + 1]),
                              reads=[pk, 'bint'], writes=['GU'])
                    inproj2(lambda kc: wu_t[:, kc, g * 128:(g + 1) * 128], 128, e0, n, 'wu_t', evu)
                for sub in range(n // 128):
                    t0 = e0 + sub * 128
                    own0 = (ti * 512 if lat else TOWN) + sub * 128
                    pi = next_ps()
                    pv = PS[pi]
                    for kc in range(8):
                        fw.op('pe', lambda: nc.tensor.matmul(pv[:, 0:512], lhsT=hT[:, kc, t0:t0 + 128], rhs=wv_t[:, kc, :], start=(kc == 0), stop=(kc == 7)),
                              reads=['wv_t', 'hT%d' % kc], writes=['ps%d' % pi])
                    fw.op('dve', lambda: nc.vector.tensor_tensor(out=V1[:], in0=pv[:, 0:512], in1=BV[:], op=ALU.add), reads=['ps%d' % pi, 'BV'], writes=['V1'])
                    fw.op('act', lambda: nc.scalar.activation(out=GV[:], in_=V1[:], func=AF.Gelu), reads=['V1'], writes=['GV'])
                    fw.op('dve', lambda: nc.vector.bn_stats(out=STT[:], in_=GV[:]), reads=['GV'], writes=['STT'])
                    fw.op('dve', lambda: nc.vector.bn_aggr(out=MV[:], in_=STT[:]), reads=['STT'], writes=['MV'])
                    fw.op('act', lambda: nc.scalar.activation(out=RSTD[:], in_=MV[:, 1:2], func=AF.Sqrt, bias=LN_EPS), reads=['MV'], writes=['RSTD'])
                    fw.op('dve', lambda: nc.vector.reciprocal(out=RSTD[:], in_=RSTD[:]), reads=['RSTD'], writes=['RSTD'])
                    fw.op('dve', lambda: nc.vector.tensor_scalar(out=GV[:], in0=GV[:], scalar1=MV[:, 0:1], scalar2=RSTD[:, 0:1], op0=ALU.subtract, op1=ALU.mult),
                          reads=['GV', 'MV', 'RSTD'], writes=['GV'])
                    fw.op('pool', lambda: nc.gpsimd.tensor_tensor(out=GV[:], in0=GV[:], in1=LNG[:], op=ALU.mult), reads=['GV', 'LNG'], writes=['GV'])
                    fw.op('pool', lambda: nc.gpsimd.tensor_tensor(out=VNB[:], in0=GV[:], in1=LNB[:], op=ALU.add), reads=['GV', 'LNB'], writes=['VNB'])
                    pmi_ = next_ps()
                    pmx = PS[pmi_]
                    for g in range(4):
                        fw.op('pe', lambda: nc.tensor.matmul(pmx[:, g * 128:(g + 1) * 128], lhsT=VNB[:, g * 128:(g + 1) * 128], rhs=WST[:, g, :], start=True, stop=True),
                              reads=['VNB', 'WST'], writes=['ps%d' % pmi_])
                    fw.op('dve', lambda: nc.vector.tensor_tensor(out=MX[:], in0=pmx[:, 0:512].rearrange("p (g q) -> p g q", g=4), in1=BSP[:], op=ALU.add),
                          reads=['ps%d' % pmi_, 'BSP'], writes=['MX'])
                    ats = ATS[nch % 2]
                    ak = 'ATS%d' % (nch % 2)
                    nch += 1
                    fw.op('pool', lambda: nc.gpsimd.tensor_tensor(out=ats[:], in0=MX[:], in1=GU[:, :, sub * 128:(sub + 1) * 128], op=ALU.mult), reads=['MX', 'GU'], writes=[ak])
                    load('sp', A_d[:, :, own0:own0 + 128], ats[:], 'A_d', [ak])
        fw.barrier()
        if dbg:
            with contextlib.ExitStack() as sd:
                rb = sb("dbgb", [128, NOWN], BF16, sd)
                rf = sb("dbgf", [128, NOWN], F32, sd)
                for (nm, src_d, npart, nchk) in (("r_dbg", R_d, 128, 8), ("a_dbg", A_d, 128, 4), ("b_dbg", B_d, 64, 8)):
                    o_ = dbg_outs[nm]
                    for j in range(nchk):
                        load('sp', rb[0:npart, :], src_d[:, j, :], 'dbgb', [nm[0].upper() + '_d'])
                        fw.op('dve', lambda: nc.vector.tensor_copy(out=rf[0:npart, :], in_=rb[0:npart, :]), reads=['dbgb'], writes=['dbgf'])
                        load('sp', o_[:, j, :], rf[0:npart, :], nm, ['dbgf'])
                    outs.append(nm)
        def layer_norm(X, xk, n, SQ, sqk, MEAN, mk, MSQ, msk, RSTDW, rk, ONF, ok, gcol, bcol, pvk):
            fw.op('act', lambda: nc.scalar.activation(out=SQ[:, :, 0:n], in_=X[:, :, 0:n], func=AF.Square), reads=[xk], writes=[sqk])
            p1 = next_ps(); p2 = next_ps()
            for c in range(8):
                fw.op('pe', lambda: nc.tensor.matmul(PS[p1][:, 0:n], lhsT=ONF[:], rhs=X[:, c, 0:n], start=(c == 0), stop=(c == 7)), reads=[ok, xk], writes=['ps%d' % p1])
            for c in range(8):
                fw.op('pe', lambda: nc.tensor.matmul(PS[p2][:, 0:n], lhsT=ONF[:], rhs=SQ[:, c, 0:n], start=(c == 0), stop=(c == 7)), reads=[ok, sqk], writes=['ps%d' % p2])
            fw.op('act', lambda: nc.scalar.activation(out=MEAN[:, 0:n], in_=PS[p1][:, 0:n], func=AF.Copy, scale=1.0 / D), reads=['ps%d' % p1], writes=[mk])
            fw.op('dve', lambda: nc.vector.tensor_tensor(out=MSQ[:, 0:n], in0=MEAN[:, 0:n], in1=MEAN[:, 0:n], op=ALU.mult), reads=[mk], writes=[msk])
            fw.op('dve', lambda: nc.vector.scalar_tensor_tensor(out=RSTDW[:, 0:n], in0=PS[p2][:, 0:n], scalar=1.0 / D, in1=MSQ[:, 0:n], op0=ALU.mult, op1=ALU.subtract),
                  reads=['ps%d' % p2, msk], writes=[rk])
            fw.op('act', lambda: nc.scalar.activation(out=RSTDW[:, 0:n], in_=RSTDW[:, 0:n], func=AF.Sqrt, bias=LN_EPS), reads=[rk], writes=[rk])
            fw.op('dve', lambda: nc.vector.reciprocal(out=RSTDW[:, 0:n], in_=RSTDW[:, 0:n]), reads=[rk], writes=[rk])
            for c in range(8):
                fw.op('dve', lambda: nc.vector.tensor_tensor(out=X[:, c, 0:n], in0=X[:, c, 0:n], in1=MEAN[:, 0:n], op=ALU.subtract), reads=[xk, mk], writes=[xk])
                fw.op('pool', lambda: nc.gpsimd.tensor_tensor(out=X[:, c, 0:n], in0=X[:, c, 0:n], in1=RSTDW[:, 0:n], op=ALU.mult), reads=[xk, rk], writes=[xk])
                fw.op('act', lambda: nc.scalar.activation(out=X[:, c, 0:n], in_=X[:, c, 0:n], func=AF.Identity, scale=gcol(c), bias=bcol(c)), reads=[xk, pvk], writes=[xk])

        if GL:
            X1_d = sh.scratch("X1g", [128, 8, 8448], F32)
            H2_d = sh.scratch("H2g", [8448, D], BF16)
            LG_d = sh.scratch("LGg", [128, 66, 36], F32)
        else:
            X1_d = sh.scratch("X1_d", [128, 8, NOWN], F32)
            H2_d = sh.scratch("H2_d", [NOWN, D], BF16)
        LG = sb("LG", [128, T, 36], F32)

        def gbase(o0):
            if not GL:
                return o0
            return seg * TOWN + o0 if o0 < TOWN else SEQ + (o0 - TOWN)

        def gskip(o0):
            return GL and o0 >= TOWN and seg != 0
        OT = [(HALO + i * 512, i * 512, 512) for i in range(4)] + [(TLAT, TOWN, 256)]
        if "merge" in phases:
          with contextlib.ExitStack() as s6:
            ATl = sb("ATl", [128, 4, 1280], BF16, s6)
            BTl = sb("BTl", [64, 8, 1280], BF16, s6)
            RTl = sb("RTl", [128, 8, 1280], BF16, s6)
            WGs = [sb("WG%d" % i, [128, 8, 3, 128], BF16, s6) for i in range(2)]
            WAs = [sb("WA%d" % i, [128, 4, 128], BF16, s6) for i in range(2)]
            WBs = [sb("WB%d" % i, [64, 8, 128], BF16, s6) for i in range(2)]
            WCs = [sb("WC%d" % i, [128, 8, 128], BF16, s6) for i in range(2)]
            GS = [sb("GS%d" % i, [128, 512], F32, s6) for i in range(3)]
            TS = [sb("TS%d" % i, [128, 512], F32, s6) for i in range(3)]
            wi = 0
            for hf in range(2):
                tiles = OT[0:2] if hf == 0 else OT[2:5]
                h0 = tiles[0][1]
                hn = sum(t[2] for t in tiles)
                load('sp', ATl[:, :, 0:hn], A_d[:, :, h0:h0 + hn], 'ATl', ['A_d'])
                load('sp', BTl[:, :, 0:hn], B_d[:, :, h0:h0 + hn], 'BTl', ['B_d'])
                load('sp', RTl[:, :, 0:hn], R_d[:, :, h0:h0 + hn], 'RTl', ['R_d'])
                for oc in range(8):
                    w_ = wi % 2
                    wi += 1
                    WG, WA, WB, WC = WGs[w_], WAs[w_], WBs[w_], WCs[w_]
                    kg, ka, kb, kc_ = 'WG%d' % w_, 'WA%d' % w_, 'WB%d' % w_, 'WC%d' % w_
                    for br in range(3):
                        c0 = C_G + br * 1024 + oc * 128
                        load('pool', WG[:, :, br, :], w_in[:, :, c0:c0 + 128], kg)
                    load('pool', WA[:], w_pa[:, :, oc * 128:(oc + 1) * 128], ka)
                    load('pool', WB[:], w_pb[:, :, oc * 128:(oc + 1) * 128], kb)
                    load('pool', WC[:], w_pc[:, :, oc * 128:(oc + 1) * 128], kc_)
                    for (e0, o0, n) in tiles:
                        l0 = o0 - h0
                        for br in range(3):
                            def evg(pt, pk, br=br):
                                bcol = C_G // 128 + br * 8 + oc
                                fw.op('act', lambda: nc.scalar.activation(out=GS[br][:, 0:n], in_=pt[:, 0:n], func=AF.Sigmoid, bias=bint[:, bcol:bcol + 1]),
                                      reads=[pk, 'bint'], writes=['GS%d' % br])
                            inproj2(lambda kc: WG[:, kc, br, :], 128, e0, n, kg, evg)
                        for br, (Wt, wk, src, sk, nk, npart) in enumerate(((WA, ka, ATl, 'ATl', 4, 128), (WB, kb, BTl, 'BTl', 8, 64), (WC, kc_, RTl, 'RTl', 8, 128))):
                            pi = next_ps()
                            pt = PS[pi]
                            for kk in range(nk):
                                fw.op('pe', lambda: nc.tensor.matmul(pt[:, 0:n], lhsT=Wt[0:npart, kk, :], rhs=src[0:npart, kk, l0:l0 + n], start=(kk == 0), stop=(kk == nk - 1)),
                                      reads=[wk, sk], writes=['ps%d' % pi])
                            fw.op('dve', lambda: nc.vector.tensor_tensor(out=TS[br][:, 0:n], in0=pt[:, 0:n], in1=GS[br][:, 0:n], op=ALU.mult),
                                  reads=['ps%d' % pi, 'GS%d' % br], writes=['TS%d' % br])
                        fw.op('pool', lambda: nc.gpsimd.tensor_tensor(out=TS[0][:, 0:n], in0=TS[0][:, 0:n], in1=TS[1][:, 0:n], op=ALU.add), reads=['TS0', 'TS1'], writes=['TS0'])
                        fw.op('pool', lambda: nc.gpsimd.tensor_tensor(out=yT[:, oc, o0:o0 + n], in0=TS[0][:, 0:n], in1=TS[2][:, 0:n], op=ALU.add), reads=['TS0', 'TS2'], writes=['yT%d' % oc])
          fw.barrier()
          sHT.close()
          with contextlib.ExitStack() as s7:
            WO = sb("WO", [128, 8, D], BF16, s7)
            WGR = sb("WGR", [128, 8, 36], F32, s7)
            BGR = sb("BGR", [128, 36], F32, s7)
            IDN = sb("IDN", [128, 128], F32, s7)
            ONF = sb("ONF", [128, 128], F32, s7)
            pv = sb("pvec", [128, 5, 8], F32, s7)
            bom = sb("bom", [128, 2, 8], F32, s7)
            load('pool', WO[:], w_o, 'WO')
            load('sp', WGR[:], w_gr, 'WGR')
            load('sp', BGR[:], b_gr, 'BGR')
            load('sp', IDN[:], ident, 'IDN')
            load('sp', ONF[:], ones_f, 'ONF')
            load('sp', pv[:, 0, :], b_o, 'pvec')
            load('sp', pv[:, 1, :], ln1g, 'pvec')
            load('sp', pv[:, 2, :], ln1b, 'pvec')
            fw.op('dve', lambda: nc.vector.tensor_tensor(out=bom[:, 0, :], in0=pv[:, 0, :], in1=mlat[:, 16:24], op=ALU.mult), reads=['pvec', 'mlat'], writes=['bom'])
            fw.op('dve', lambda: nc.vector.tensor_tensor(out=bom[:, 1, :], in0=pv[:, 0, :], in1=mctx[:, 16:24], op=ALU.mult), reads=['pvec', 'mctx'], writes=['bom'])
            XT = sb("XT", [128, 8, 512], F32, s7)
            SQ = sb("SQ", [128, 8, 512], F32, s7)
            H2F = sb("H2F", [128, 8, 512], F32, s7)
            MEAN = sb("MEAN", [128, 512], F32, s7)
            MSQ = sb("MSQ", [128, 512], F32, s7)
            RSTDW = sb("RSTDW", [128, 512], F32, s7)
            nsub = 0
            H2B = [sb("H2B%d" % i, [128, D], BF16, s7) for i in range(2)]
            for ti, (e0, o0, n) in enumerate(OT):
                lat = ti < 4
                mv_ = mlat if lat else mctx
                mk_ = 'mlat' if lat else 'mctx'
                li = 0 if lat else 1
                if lat:
                    load('sp', XT[:, :, 0:n], xT[:, :, e0:e0 + n], 'XT')
                else:
                    load('sp', XT[:, :, 0:n], cT[:, :, :], 'XT')
                for oc in range(8):
                    pi = next_ps()
                    pt = PS[pi]
                    for kc in range(8):
                        fw.op('pe', lambda: nc.tensor.matmul(pt[:, 0:n], lhsT=WO[:, kc, oc * 128:(oc + 1) * 128], rhs=yT[:, kc, o0:o0 + n], start=(kc == 0), stop=(kc == 7)),
                              reads=['WO', 'yT%d' % kc], writes=['ps%d' % pi])
                    fw.op('act', lambda: nc.scalar.activation(out=SQ[:, oc, 0:n], in_=pt[:, 0:n], func=AF.Identity, scale=mv_[:, 16 + oc:17 + oc], bias=bom[:, li, oc:oc + 1]),
                          reads=['ps%d' % pi, mk_, 'bom'], writes=['SQ'])
                    fw.op('dve', lambda: nc.vector.scalar_tensor_tensor(out=XT[:, oc, 0:n], in0=XT[:, oc, 0:n], scalar=ALPHA, in1=SQ[:, oc, 0:n], op0=ALU.mult, op1=ALU.add),
                          reads=['XT', 'SQ'], writes=['XT'])

                layer_norm(XT, 'XT', n, SQ, 'SQ', MEAN, 'MEAN', MSQ, 'MSQ', RSTDW, 'RSTDW', ONF, 'ONF', lambda c: pv[:, 1, c:c + 1], lambda c: pv[:, 2, c:c + 1], 'pvec')
                if not gskip(o0):
                    load('sp', X1_d[:, :, gbase(o0):gbase(o0) + n], XT[:, :, 0:n], 'X1_d', ['XT'])
                m4 = m4p[:, li, :]
                for c in range(8):
                    fw.op('act', lambda: nc.scalar.activation(out=H2F[:, c, 0:n], in_=XT[:, c, 0:n], func=AF.Identity, scale=m4p[:, li, c:c + 1], bias=mv_[:, 24 + c:25 + c]),
                          reads=['XT', 'm4p', mk_], writes=['H2F'])
                for sub in range(n // 128):
                    t0 = sub * 128
                    chunk = (o0 + t0) // 128
                    pi = next_ps()
                    pt = PS[pi]
                    for kc in range(8):
                        fw.op('pe', lambda: nc.tensor.matmul(pt[:, 0:36], lhsT=H2F[:, kc, t0:t0 + 128], rhs=WGR[:, kc, :], start=(kc == 0), stop=(kc == 7)),
                              reads=['H2F', 'WGR'], writes=['ps%d' % pi])
                    fw.op('dve', lambda: nc.vector.tensor_tensor(out=LG[:, chunk, :], in0=pt[:, 0:36], in1=BGR[:], op=ALU.add), reads=['ps%d' % pi, 'BGR'], writes=['LG'])
                    hb = H2B[nsub % 2]
                    hk = 'H2B%d' % (nsub % 2)
                    nsub += 1
                    for half in range(2):
                        pi = next_ps()
                        pt = PS[pi]
                        for cc in range(4):
                            c = half * 4 + cc
                            fw.op('pe', lambda: nc.tensor.transpose(out=pt[:, cc * 128:(cc + 1) * 128], in_=H2F[:, c, t0:t0 + 128], identity=IDN[:]),
                                  reads=['H2F', 'IDN'], writes=['ps%d' % pi])
                        if half == 0:
                            fw.op('act', lambda: nc.scalar.copy(out=hb[:, 0:512], in_=pt[:, 0:512]), reads=['ps%d' % pi], writes=[hk])
                        else:
                            fw.op('dve', lambda: nc.vector.tensor_copy(out=hb[:, 512:1024], in_=pt[:, 0:512]), reads=['ps%d' % pi], writes=[hk])
                    if not gskip(o0):
                        load('sp', H2_d[gbase(o0) + t0:gbase(o0) + t0 + 128, :], hb[:], 'H2_d', [hk])
          if mode == "premoe":
              load('sp', LG_d[:, seg * 16:(seg + 1) * 16, :], LG[:, 0:16, :], 'LG_d', ['LG'])
              if seg == 0:
                  load('sp', LG_d[:, 64:66, :], LG[:, 16:18, :], 'LG_d', ['LG'])
          fw.barrier()
          sY.close()
          if mode == "premoe":
              fw.barrier()
              return
          if dbg:
              load('sp', dbg_outs['x1_dbg'], X1_d, 'x1_dbg', ['X1_d'])
              lgf = LG
              load('sp', dbg_outs['lg_dbg'], LG[:], 'lg_dbg', ['LG'])
              outs.extend(['x1_dbg', 'lg_dbg'])
        if mode == "moe":
            sHT.close()
            sY.close()
            load('sp', LG[:], LG_d, 'LG', ['LG_d'])
        if "moe" in phases:
          XB_d = sh.scratch("XBg" if GL else "XB_d", [NB * BS, D], BF16)
          OUT_d = sh.scratch("OUTg" if GL else "OUT_d", [NB * BS, D], F32)
          SL1 = sb("SL1", [128, T], I32)
          SL2 = sb("SL2", [128, T], I32)
          G1 = sb("G1", [128, T], F32)
          G2 = sb("G2", [128, T], F32)
          WIDX = sb("WIDX", [128, NB], I32)
          with contextlib.ExitStack() as s8:
            def t8(name, shape, dt=F32):
                return sb(name, shape, dt, s8)
            TRI = t8("TRI", [128, 128]); ONF2 = t8("ONF2", [128, 128]); BVAL = t8("BVAL", [128, BVN]); PIDX = t8("PIDX", [128, 1])
            load('sp', TRI[:], tri_in, 'TRI'); load('sp', ONF2[:], ones_f, 'ONF2'); load('sp', BVAL[:], bval_in, 'BVAL'); load('sp', PIDX[:], pidx_in, 'PIDX')
            if SUB != 1:
                fw.op('dve', lambda: nc.vector.tensor_scalar(out=BVAL[:], in0=BVAL[:], scalar1=float(SUB), scalar2=None, op0=ALU.mult), reads=['BVAL'], writes=['BVAL'])
            gmax = t8("gmax", [128, T]); goh = t8("goh", [128, T, 4]); ge = t8("ge", [128, T, 4]); gw = t8("gw", [128, T])
            pen = t8("pen", [128, T, 4]); em = t8("em", [128, T, 32]); em2 = t8("em2", [128, T, 32])
            m1 = t8("m1", [128, T]); m2 = t8("m2", [128, T]); oh1 = t8("oh1", [128, T, 32]); oh2 = t8("oh2", [128, T, 32])
            dm = t8("dm", [128, T]); p1 = t8("p1", [128, T])
            A_ = t8("Aas", [128, T, 32]); CUMA = t8("CUMA", [128, T + 1, 32]); SLT = t8("SLT", [128, T, 32]); TMP = t8("TMP", [128, T, 32])
            cnt = t8("cnt", [128, 32]); cnti = t8("cnti", [128, 32], I32); padd = t8("padd", [128, 32]); pend = t8("pend", [128, 32]); pst = t8("pst", [128, 32])
            one32 = t8("one32", [128, 32]); CMP = t8("CMP", [128, NB, 32]); be = t8("be", [128, NB]); slf = t8("slf", [128, T])

            def V(fn, reads, writes):
                fw.op('dve', fn, reads=reads, writes=writes)
            gl = LG[:, :, 0:4]
            el = LG[:, :, 4:36]
            V(lambda: nc.vector.tensor_reduce(out=gmax[:], in_=gl, op=ALU.max, axis=AX.X), ['LG'], ['gmax'])
            gmb = gmax[:].unsqueeze(2).to_broadcast([128, T, 4])
            V(lambda: nc.vector.tensor_tensor(out=goh[:], in0=gl, in1=gmb, op=ALU.is_equal), ['LG', 'gmax'], ['goh'])
            V(lambda: nc.vector.tensor_tensor(out=ge[:], in0=gl, in1=gmb, op=ALU.subtract), ['LG', 'gmax'], ['ge'])
            fw.op('act', lambda: nc.scalar.activation(out=ge[:], in_=ge[:], func=AF.Exp), reads=['ge'], writes=['ge'])
            V(lambda: nc.vector.tensor_reduce(out=gw[:], in_=ge[:], op=ALU.add, axis=AX.X), ['ge'], ['gw'])
            V(lambda: nc.vector.reciprocal(out=gw[:], in_=gw[:]), ['gw'], ['gw'])
            V(lambda: nc.vector.tensor_scalar(out=pen[:], in0=goh[:], scalar1=1e30, scalar2=-1e30, op0=ALU.mult, op1=ALU.add), ['goh'], ['pen'])
            V(lambda: nc.vector.tensor_tensor(out=em[:].rearrange("p t (g e) -> p t g e", g=4), in0=el.rearrange("p t (g e) -> p t g e", g=4),
                                              in1=pen[:].unsqueeze(3).to_broadcast([128, T, 4, 8]), op=ALU.add), ['LG', 'pen'], ['em'])
            V(lambda: nc.vector.tensor_reduce(out=m1[:], in_=em[:], op=ALU.max, axis=AX.X), ['em'], ['m1'])
            V(lambda: nc.vector.tensor_tensor(out=oh1[:], in0=em[:], in1=m1[:].unsqueeze(2).to_broadcast([128, T, 32]), op=ALU.is_equal), ['em', 'm1'], ['oh1'])
            V(lambda: nc.vector.scalar_tensor_tensor(out=em2[:], in0=oh1[:], scalar=-1e30, in1=em[:], op0=ALU.mult, op1=ALU.add), ['oh1', 'em'], ['em2'])
            V(lambda: nc.vector.tensor_reduce(out=m2[:], in_=em2[:], op=ALU.max, axis=AX.X), ['em2'], ['m2'])
            V(lambda: nc.vector.tensor_tensor(out=oh2[:], in0=em2[:], in1=m2[:].unsqueeze(2).to_broadcast([128, T, 32]), op=ALU.is_equal), ['em2', 'm2'], ['oh2'])
            V(lambda: nc.vector.tensor_tensor(out=dm[:], in0=m2[:], in1=m1[:], op=ALU.subtract), ['m1', 'm2'], ['dm'])
            fw.op('act', lambda: nc.scalar.activation(out=dm[:], in_=dm[:], func=AF.Exp), reads=['dm'], writes=['dm'])
            V(lambda: nc.vector.tensor_scalar(out=p1[:], in0=dm[:], scalar1=1.0, scalar2=None, op0=ALU.add), ['dm'], ['p1'])
            V(lambda: nc.vector.reciprocal(out=p1[:], in_=p1[:]), ['p1'], ['p1'])
            V(lambda: nc.vector.tensor_tensor(out=G1[:], in0=gw[:], in1=p1[:], op=ALU.mult), ['gw', 'p1'], ['G1'])
            V(lambda: nc.vector.tensor_tensor(out=dm[:], in0=dm[:], in1=p1[:], op=ALU.mult), ['dm', 'p1'], ['dm'])
            V(lambda: nc.vector.tensor_tensor(out=G2[:], in0=gw[:], in1=dm[:], op=ALU.mult), ['gw', 'dm'], ['G2'])
            V(lambda: nc.vector.tensor_tensor(out=A_[:], in0=oh1[:], in1=oh2[:], op=ALU.add), ['oh1', 'oh2'], ['A'])
            V(lambda: nc.vector.memset(CUMA[:, 0, :], 0.0), [], ['CUMA'])
            for t in range(T):
                V(lambda: nc.vector.tensor_tensor(out=CUMA[:, t + 1, :], in0=CUMA[:, t, :], in1=A_[:, t, :], op=ALU.add), ['CUMA', 'A'], ['CUMA'])
            pc_ = next_ps()
            fw.op('pe', lambda: nc.tensor.matmul(PS[pc_][:, 0:32], lhsT=ONF2[:], rhs=CUMA[:, T, :], start=True, stop=True), reads=['ONF2', 'CUMA'], writes=['ps%d' % pc_])
            V(lambda: nc.vector.tensor_copy(out=cnt[:], in_=PS[pc_][:, 0:32]), ['ps%d' % pc_], ['cnt'])
            NK = 72
            CM2 = t8("CM2", [128, 32, NK])
            V(lambda: nc.vector.tensor_tensor(out=CM2[:], in0=cnt[:].unsqueeze(2).to_broadcast([128, 32, NK]), in1=BVAL[:, 0:NK].unsqueeze(1).to_broadcast([128, 32, NK]), op=ALU.is_gt),
              ['cnt', 'BVAL'], ['CM2'])
            V(lambda: nc.vector.tensor_reduce(out=padd[:], in_=CM2[:], op=ALU.add, axis=AX.X), ['CM2'], ['padd'])
            V(lambda: nc.vector.tensor_scalar(out=padd[:], in0=padd[:], scalar1=float(BS), scalar2=None, op0=ALU.mult), ['padd'], ['padd'])
            V(lambda: nc.vector.memset(one32[:], 1.0), [], ['one32'])
            V(lambda: nc.vector.tensor_tensor_scan(out=pend[:], data0=one32[:], data1=padd[:], initial=0.0, op0=ALU.mult, op1=ALU.add), ['one32', 'padd'], ['pend'])
            V(lambda: nc.vector.tensor_tensor(out=pst[:], in0=pend[:], in1=padd[:], op=ALU.subtract), ['pend', 'padd'], ['pst'])
            psb = pst[:].unsqueeze(1)
            for g0_ in range(0, T, 16):
                ng_ = min(16, T - g0_)
                pr_ = next_ps()
                pk = 'ps%d' % pr_
                for t in range(g0_, g0_ + ng_):
                    tt_ = t - g0_
                    fw.op('pe', lambda: nc.tensor.matmul(PS[pr_][:, tt_ * 32:(tt_ + 1) * 32], lhsT=TRI[:], rhs=A_[:, t, :], start=True, stop=False), reads=['TRI', 'A'], writes=[pk])
                    fw.op('pe', lambda: nc.tensor.matmul(PS[pr_][:, tt_ * 32:(tt_ + 1) * 32], lhsT=ONF2[:], rhs=CUMA[:, t, :], start=False, stop=True), reads=['ONF2', 'CUMA'], writes=[pk])
                V(lambda: nc.vector.tensor_tensor(out=SLT[:, g0_:g0_ + ng_, :], in0=PS[pr_][:, 0:ng_ * 32].rearrange("p (t e) -> p t e", e=32), in1=psb.to_broadcast([128, ng_, 32]), op=ALU.add),
                  [pk, 'pst'], ['SLT'])
            for (oh, ok, SL) in ((oh1, 'oh1', SL1), (oh2, 'oh2', SL2)):
                V(lambda: nc.vector.tensor_tensor(out=TMP[:], in0=SLT[:], in1=oh[:], op=ALU.mult), ['SLT', ok], ['TMP'])
                V(lambda: nc.vector.tensor_reduce(out=slf[:], in_=TMP[:], op=ALU.add, axis=AX.X), ['TMP'], ['slf'])
                V(lambda: nc.vector.tensor_copy(out=SL[:], in_=slf[:]), ['slf'], ['SL'])
            V(lambda: nc.vector.tensor_tensor(out=CMP[:], in0=pend[:].unsqueeze(1).to_broadcast([128, NB, 32]), in1=BVAL[:, 0:NB].unsqueeze(2).to_broadcast([128, NB, 32]), op=ALU.is_le),
              ['pend', 'BVAL'], ['CMP'])
            V(lambda: nc.vector.tensor_reduce(out=be[:], in_=CMP[:], op=ALU.add, axis=AX.X), ['CMP'], ['be'])
            V(lambda: nc.vector.tensor_scalar(out=be[:], in0=be[:], scalar1=float(NEXP - 1), scalar2=128.0, op0=ALU.min, op1=ALU.mult), ['be'], ['be'])
            V(lambda: nc.vector.tensor_scalar(out=WIDX[:], in0=be[:], scalar1=PIDX[:, 0:1], scalar2=None, op0=ALU.add), ['be', 'PIDX'], ['WIDX'])
            if dbg:
                load('sp', dbg_outs['sl_dbg'][:, 0, :], SL1[:], 'sl_dbg', ['SL'])
                load('sp', dbg_outs['sl_dbg'][:, 1, :], SL2[:], 'sl_dbg', ['SL'])
                load('sp', dbg_outs['g_dbg'][:, 0, :], G1[:], 'g_dbg', ['G1'])
                load('sp', dbg_outs['g_dbg'][:, 1, :], G2[:], 'g_dbg', ['G2'])
                load('sp', dbg_outs['wi_dbg'], WIDX[:], 'wi_dbg', ['WIDX'])
                outs.extend(['sl_dbg', 'g_dbg', 'wi_dbg'])
          fw.barrier()
          if 'moe8' not in phases and dbg:
              sHT.close()
              fw.barrier()
              return
          with contextlib.ExitStack() as s8b:
            HBs = [sb("HBs%d" % i, [128, D], BF16, s8b) for i in range(2)]
            for t in range(T):
                hb_ = HBs[t % 2]
                hk_ = 'HBs%d' % (t % 2)
                load('sp', hb_[:], H2_d[t * 128:(t + 1) * 128, :], hk_, ['H2_d'])
                for SL in (SL1, SL2):
                    fw.dma('pool', lambda: nc.gpsimd.indirect_dma_start(out=XB_d[:, :], out_offset=bass.IndirectOffsetOnAxis(ap=SL[:, t:t + 1], axis=0),
                                                                        in_=hb_[:], in_offset=None), reads=[hk_, 'SL'], writes=['XB_d'])
          fw.barrier()
          with contextlib.ExitStack() as s9:
            IDB = sb("IDB", [128, 128], BF16, s9)
            load('pool', IDB[:], ident, 'IDB')
            W1B = [sb("W1B%d" % i, [128, 4096], BF16, s9) for i in range(2)]
            W3B = [sb("W3B%d" % i, [128, 4096], BF16, s9) for i in range(2)]
            W2B = [sb("W2B%d" % i, [128, 4096], BF16, s9) for i in range(2)]
            NBUF = 3
            XBs = [sb("XBs%d" % i, [128, D], BF16, s9) for i in range(NBUF)]
            XTs = [sb("XTs%d" % i, [128, 8, 128], BF16, s9) for i in range(NBUF)]
            S1s = [sb("S1s%d" % i, [128, 512], F32, s9) for i in range(NBUF)]
            Gbs = [sb("Gbs%d" % i, [128, 512], BF16, s9) for i in range(NBUF)]
            GTs = [sb("GTs%d" % i, [128, 4, 128], BF16, s9) for i in range(NBUF)]
            OBs = [sb("OBs%d" % i, [128, D], F32, s9) for i in range(NBUF)]
            items = [(b_, s_) for b_ in range(NB) for s_ in range(SUB)]
            NI = len(items)

            def loadw(b_):
                i2 = b_ % 2
                for (wt, wk, srcn) in ((W1B[i2], 'W1B%d' % i2, "w1"), (W3B[i2], 'W3B%d' % i2, "w3"), (W2B[i2], 'W2B%d' % i2, "w2")):
                    for a_, hsfx in enumerate(("a", "b")):
                        fw.dma('pool', lambda: nc.gpsimd.indirect_dma_start(out=wt[:, a_ * 2048:(a_ + 1) * 2048], out_offset=None, in_=wexp[srcn + hsfx][:, :],
                                                                            in_offset=bass.IndirectOffsetOnAxis(ap=WIDX[:, b_:b_ + 1], axis=0)),
                               reads=['WIDX'], writes=[wk])

            def stA(i):
                b_, s_ = items[i]
                j = i % NBUF
                r0 = b_ * BS + s_ * 128
                xb = XBs[j]
                load('sp', xb[:], XB_d[r0:r0 + 128, :], 'XBs%d' % j, ['XB_d'])
                pi_ = next_ps()
                pv_ = PS[pi_][:].bitcast(BF16)
                for c in range(8):
                    fw.op('pe', lambda: nc.tensor.transpose(out=pv_[:, c * 128:(c + 1) * 128], in_=xb[:, c * 128:(c + 1) * 128], identity=IDB[:]),
                          reads=['XBs%d' % j, 'IDB'], writes=['ps%d' % pi_])
                fw.op('act', lambda: nc.scalar.copy(out=XTs[j][:, 0:4, :], in_=pv_[:, 0:512].rearrange("p (c s) -> p c s", c=4)), reads=['ps%d' % pi_], writes=['XTs%d' % j])
                fw.op('dve', lambda: nc.vector.tensor_copy(out=XTs[j][:, 4:8, :], in_=pv_[:, 512:1024].rearrange("p (c s) -> p c s", c=4)), reads=['ps%d' % pi_], writes=['XTs%d' % j])

            def stB(i):
                b_, s_ = items[i]
                j = i % NBUF
                i2 = b_ % 2
                p1i = next_ps(); p3i = next_ps()
                for (pi_, wt, wk) in ((p1i, W1B[i2], 'W1B%d' % i2), (p3i, W3B[i2], 'W3B%d' % i2)):
                    for kc in range(8):
                        fw.op('pe', lambda: nc.tensor.matmul(PS[pi_][:, 0:512], lhsT=XTs[j][:, kc, :], rhs=wt[:, kc * 512:(kc + 1) * 512], start=(kc == 0), stop=(kc == 7)),
                              reads=['XTs%d' % j, wk], writes=['ps%d' % pi_])
                fw.op('act', lambda: nc.scalar.activation(out=S1s[j][:], in_=PS[p1i][:, 0:512], func=AF.Silu), reads=['ps%d' % p1i], writes=['S1s%d' % j])
                fw.op('dve', lambda: nc.vector.tensor_tensor(out=Gbs[j][:], in0=PS[p3i][:, 0:512], in1=S1s[j][:], op=ALU.mult), reads=['ps%d' % p3i, 'S1s%d' % j], writes=['Gbs%d' % j])

            def stC(i):
                j = i % NBUF
                pi_ = next_ps()
                pv_ = PS[pi_][:].bitcast(BF16)
                for fc in range(4):
                    fw.op('pe', lambda: nc.tensor.transpose(out=pv_[:, fc * 128:(fc + 1) * 128], in_=Gbs[j][:, fc * 128:(fc + 1) * 128], identity=IDB[:]),
                          reads=['Gbs%d' % j, 'IDB'], writes=['ps%d' % pi_])
                fw.op('dve', lambda: nc.vector.tensor_copy(out=GTs[j][:], in_=pv_[:, 0:512].rearrange("p (c s) -> p c s", c=4)), reads=['ps%d' % pi_], writes=['GTs%d' % j])

            def stD(i):
                b_, s_ = items[i]
                j = i % NBUF
                i2 = b_ % 2
                r0 = b_ * BS + s_ * 128
                ob = OBs[j]
                for half in range(2):
                    pi_ = next_ps()
                    for fc in range(4):
                        fw.op('pe', lambda: nc.tensor.matmul(PS[pi_][:, 0:512], lhsT=GTs[j][:, fc, :], rhs=W2B[i2][:, fc * 1024 + half * 512:fc * 1024 + (half + 1) * 512], start=(fc == 0), stop=(fc == 3)),
                              reads=['GTs%d' % j, 'W2B%d' % i2], writes=['ps%d' % pi_])
                    if half == 0:
                        fw.op('act', lambda: nc.scalar.copy(out=ob[:, 0:512], in_=PS[pi_][:, 0:512]), reads=['ps%d' % pi_], writes=['OBs%d' % j])
                    else:
                        fw.op('dve', lambda: nc.vector.tensor_copy(out=ob[:, 512:1024], in_=PS[pi_][:, 0:512]), reads=['ps%d' % pi_], writes=['OBs%d' % j])
                load('sp', OUT_d[r0:r0 + 128, :], ob[:], 'OUT_d', ['OBs%d' % j])

            loadw(0)
            if NB > 1:
                loadw(1)
            for step in range(NI + 2):
                if step < NI:
                    stA(step)
                if 0 <= step - 1 < NI:
                    stB(step - 1)
                if 0 <= step - 2 < NI:
                    stD(step - 2)
                    bb, ss = items[step - 2]
                    if ss == SUB - 1 and bb + 2 < NB:
                        loadw(bb + 2)
                if 0 <= step - 1 < NI:
                    stC(step - 1)
          fw.barrier()
          with contextlib.ExitStack() as s10:
            IDN2 = sb("IDN2", [128, 128], F32, s10)
            ONF3 = sb("ONF3", [128, 128], F32, s10)
            pv2 = sb("pvec2", [128, 2, 8], F32, s10)
            load('sp', IDN2[:], ident, 'IDN2')
            load('sp', ONF3[:], ones_f, 'ONF3')
            load('sp', pv2[:, 0, :], ln2g, 'pvec2')
            load('sp', pv2[:, 1, :], ln2b, 'pvec2')
            XT2 = sb("XT2", [128, 8, 512], F32, s10)
            SQ2 = sb("SQ2", [128, 8, 512], F32, s10)
            MEAN2 = sb("MEAN2", [128, 512], F32, s10)
            MSQ2 = sb("MSQ2", [128, 512], F32, s10)
            RSTD2 = sb("RSTD2", [128, 512], F32, s10)
            O1s = [sb("O1s%d" % i, [128, D], F32, s10) for i in range(2)]
            O2s = [sb("O2s%d" % i, [128, D], F32, s10) for i in range(2)]
            ng = 0
            if mode == "moe":
                CTL = [(g_ * 512, 512, True) for g_ in range(16)] + [(SEQ, NCTX, False)]
            else:
                CTL = [(o0_, n_, i_ < 4) for i_, (e0_, o0_, n_) in enumerate(OT)]
            for (o0, n, lat) in CTL:
                mv_ = mlat if lat else mctx
                mk_ = 'mlat' if lat else 'mctx'
                load('sp', XT2[:, :, 0:n], X1_d[:, :, o0:o0 + n], 'XT2', ['X1_d'])
                for sub in range(n // 128):
                    t = (o0 + sub * 128) // 128
                    o1, o2 = O1s[ng % 2], O2s[ng % 2]
                    ko1, ko2 = 'O1s%d' % (ng % 2), 'O2s%d' % (ng % 2)
                    ng += 1
                    for (o_, ko_, SL) in ((o1, ko1, SL1), (o2, ko2, SL2)):
                        fw.dma('pool', lambda: nc.gpsimd.indirect_dma_start(out=o_[:], out_offset=None, in_=OUT_d[:, :],
                                                                            in_offset=bass.IndirectOffsetOnAxis(ap=SL[:, t:t + 1], axis=0)),
                               reads=['OUT_d', 'SL'], writes=[ko_])
                    fw.op('dve', lambda: nc.vector.tensor_scalar(out=o1[:], in0=o1[:], scalar1=G1[:, t:t + 1], scalar2=None, op0=ALU.mult), reads=[ko1, 'G1'], writes=[ko1])
                    fw.op('dve', lambda: nc.vector.scalar_tensor_tensor(out=o1[:], in0=o2[:], scalar=G2[:, t:t + 1], in1=o1[:], op0=ALU.mult, op1=ALU.add), reads=[ko1, ko2, 'G2'], writes=[ko1])
                    for half in range(2):
                        pi_ = next_ps()
                        for cc in range(4):
                            c = half * 4 + cc
                            fw.op('pe', lambda: nc.tensor.transpose(out=PS[pi_][:, cc * 128:(cc + 1) * 128], in_=o1[:, c * 128:(c + 1) * 128], identity=IDN2[:]),
                                  reads=[ko1, 'IDN2'], writes=['ps%d' % pi_])
                        for cc in range(4):
                            c = half * 4 + cc
                            fw.op('act', lambda: nc.scalar.activation(out=SQ2[:, c, sub * 128:(sub + 1) * 128], in_=PS[pi_][:, cc * 128:(cc + 1) * 128], func=AF.Copy, scale=mv_[:, 40 + c:41 + c]),
                                  reads=['ps%d' % pi_, mk_], writes=['SQ2'])
                fw.op('dve', lambda: nc.vector.scalar_tensor_tensor(out=XT2[:, :, 0:n], in0=XT2[:, :, 0:n], scalar=ALPHA, in1=SQ2[:, :, 0:n], op0=ALU.mult, op1=ALU.add),
                      reads=['XT2', 'SQ2'], writes=['XT2'])
                layer_norm(XT2, 'XT2', n, SQ2, 'SQ2', MEAN2, 'MEAN2', MSQ2, 'MSQ2', RSTD2, 'RSTD2', ONF3, 'ONF3',
                           lambda c: pv2[:, 0, c:c + 1], lambda c: pv2[:, 1, c:c + 1], 'pvec2')
                if lat:
                    load('sp', xout[:, :, o0:o0 + n], XT2[:, :, 0:n], 'xout', ['XT2'])
                else:
                    load('sp', cout[:, :, :], XT2[:, :, 0:n], 'cout', ['XT2'])
            outs.extend(['xout', 'cout'])
        sHT.close()
        fw.barrier()


def _fm(a):
    return np.ascontiguousarray(a.T.reshape(8, 128, a.shape[0]).transpose(1, 0, 2))


def _pp(v):
    return np.ascontiguousarray(v.reshape(-1, 128).T)


def _kc(w):
    return np.ascontiguousarray(w.reshape(8, 128, w.shape[1]).transpose(1, 0, 2))


def _partner_cols(nh):
    idx = []
    for h in range(nh):
        for d in range(64):
            half, i = d // 32, d % 32
            p = i + 16 if i < 16 else i - 16
            idx.append(h * 64 + half * 32 + p)
    return np.array(idx)


def prep_layer_weights(inp, l):
    w_in = inp['w_in'][l]
    b_in = inp['b_in'][l]
    o = np.cumsum([0, 512, 512, 512, 3072, 1024, 128, 128, 1024])
    q, u, v, g, z, k, va, xr = [slice(o[i], o[i + 1]) for i in range(8)]
    pq = _partner_cols(8)
    pk = _partner_cols(2)

    def cols(a):
        return np.concatenate([a[..., q], a[..., q][..., pq], a[..., k], a[..., k][..., pk], a[..., u], a[..., v],
                               a[..., va], a[..., g], a[..., z], a[..., xr]], axis=-1)
    W = {}
    W['w_in'] = _kc(cols(w_in))
    W['b_in'] = _pp(cols(b_in))
    W['w_mod'] = _kc(inp['w_mod'][l])
    W['b_mod'] = _pp(inp['b_mod'][l])
    W['convw'] = np.ascontiguousarray(inp['conv_w'][l].reshape(2, 4, 8, 128).transpose(3, 2, 0, 1))
    W['convb'] = np.ascontiguousarray(inp['conv_b'][l].reshape(2, 8, 128).transpose(2, 1, 0))
    W['wrg'] = np.ascontiguousarray(inp['w_rgate'][l].transpose(2, 0, 1, 3))
    W['wig'] = np.ascontiguousarray(inp['w_igate'][l].transpose(2, 0, 1, 3))
    W['brg'] = np.ascontiguousarray(inp['b_rgate'][l].reshape(2, 8, 128).transpose(2, 1, 0))
    W['big'] = np.ascontiguousarray(inp['b_igate'][l].reshape(2, 8, 128).transpose(2, 1, 0))
    W['lam'] = np.ascontiguousarray(inp['lru_lambda'][l].reshape(2, 8, 128).transpose(2, 1, 0))
    bq, bqs, bk, bks = b_in[q], b_in[q][pq], b_in[k], b_in[k][pk]
    W['b_qk'] = np.ascontiguousarray(np.concatenate([bq.reshape(8, 64), bqs.reshape(8, 64), bk.reshape(2, 64), bks.reshape(2, 64)], 0).T)
    W['b_va'] = np.ascontiguousarray(np.broadcast_to(b_in[va][None, :], (128, 128)))
    W['b_v'] = np.ascontiguousarray(np.broadcast_to(b_in[v][None, :], (128, 512)))
    W['sinkx'] = np.ascontiguousarray(np.broadcast_to(inp['attn_sink'][l][None, :, None], (64, 8, 128)))
    W['sgu_g'] = np.ascontiguousarray(np.broadcast_to(inp['sgu_ln_g'][l][None, :], (128, 512)))
    W['sgu_b'] = np.ascontiguousarray(np.broadcast_to(inp['sgu_ln_b'][l][None, :], (128, 512)))
    W['w_spT'] = np.ascontiguousarray(inp['w_spatial'][l].transpose(2, 0, 1))
    W['b_sp'] = np.ascontiguousarray(np.broadcast_to(inp['b_spatial'][l][None, :, :], (128, 4, 128)))
    W['w_pa'] = np.ascontiguousarray(inp['w_proj_a'][l].reshape(4, 128, D).transpose(1, 0, 2))
    W['w_pb'] = np.ascontiguousarray(inp['w_proj_b'][l].reshape(8, 64, D).transpose(1, 0, 2))
    W['w_pc'] = _kc(inp['w_proj_c'][l])
    W['w_o'] = _kc(inp['w_out'][l])
    W['b_o'] = _pp(inp['b_out'][l])
    W['ln1g'] = _pp(inp['ln1_g'][l]); W['ln1b'] = _pp(inp['ln1_b'][l])
    W['ln2g'] = _pp(inp['ln2_g'][l]); W['ln2b'] = _pp(inp['ln2_b'][l])
    W['w_gr'] = _kc(np.concatenate([inp['w_group'][l], inp['w_router'][l]], 1))
    w1r = inp['w1'][l].reshape(NEXP, 8, 128, 512).transpose(0, 2, 1, 3).reshape(NEXP * 128, 4096)
    w3r = inp['w3'][l].reshape(NEXP, 8, 128, 512).transpose(0, 2, 1, 3).reshape(NEXP * 128, 4096)
    w2r = inp['w2'][l].reshape(NEXP, 4, 128, 1024).transpose(0, 2, 1, 3).reshape(NEXP * 128, 4096)
    for nm, arr in (("w1", w1r), ("w3", w3r), ("w2", w2r)):
        W[nm + 'a'] = np.ascontiguousarray(arr[:, :2048])
        W[nm + 'b'] = np.ascontiguousarray(arr[:, 2048:])
    W['b_gr'] = np.ascontiguousarray(np.broadcast_to(np.concatenate([inp['b_group'][l], inp['b_router'][l]])[None, :], (128, 36)))
    return W


def const_inputs(core):
    s = core % NSEG
    pos = np.arange(TLAT) + s * TOWN - HALO
    row = (pos // 64).astype(np.float32)
    colp = (pos % 64).astype(np.float32)
    inv = (10000.0 ** (-np.arange(0, 32, 2, dtype=np.float32) / 32)).astype(np.float32)
    cos = np.zeros((64, TLAT), np.float32)
    sin = np.zeros((64, TLAT), np.float32)
    for d in range(64):
        half, i = d // 32, d % 32
        ang = (row if half == 0 else colp) * inv[i % 16]
        cos[d] = np.cos(ang)
        sin[d] = np.sin(ang) * (-1.0 if i < 16 else 1.0)
    kk = np.arange(128)[:, None]
    qq = np.arange(128)[None, :]
    m = np.zeros((128, 2, 4, 128), np.float32)
    m[:, 0] = (kk >= qq).astype(np.float32)[:, None, :]
    m[:, 1] = (kk <= qq).astype(np.float32)[:, None, :]
    return {'rope_cos': cos, 'rope_sin': sin, 'masks': m, 'ones_in': np.ones((128, 64), np.float32),
            'ident': np.eye(128, dtype=np.float32), 'tri_in': np.triu(np.ones((128, 128), np.float32), 1),
            'bval_in': np.ascontiguousarray(np.broadcast_to((np.arange(BVN, dtype=np.float32) * 128)[None, :], (128, BVN))),
            'pidx_in': np.arange(128, dtype=np.float32)[:, None].copy(), 'ones_f': np.ones((128, 128), np.float32)}


def core_inputs(x, ctx, c, c_ctx, core):
    b, s = core // NSEG, core % NSEG
    lo = s * TOWN - HALO
    ext = np.zeros((TLAT, D), np.float32)
    a, e = max(lo, 0), min(lo + TLAT, SEQ)
    ext[a - lo:e - lo] = x[b, a:e]
    m = {'xT': _fm(ext), 'cT': _fm(ctx[b])}
    m['cond'] = np.ascontiguousarray(np.stack([_pp(c[b]), _pp(c_ctx)], -1))
    ed = np.zeros((128, 2), np.float32)
    ed[:, 0] = 1.0 if s > 0 else 0.0
    ed[:, 1] = 1.0 if s < NSEG - 1 else 0.0
    m['edge'] = ed
    return m


def carry_input(ab, core):
    car = np.zeros((128, 8, 2, 3, 2), np.float32)
    car[..., 0] = 1.0
    if ab is None:
        return car
    b, s = core // NSEG, core % NSEG
    fwd = [b * NSEG + j for j in range(0, s)]
    bwd = [b * NSEG + j for j in range(NSEG - 1, s, -1)]
    for k, cc in enumerate(fwd):
        car[:, :, 0, k, :] = ab[cc][:, :, 0, :]
    for k, cc in enumerate(bwd):
        car[:, :, 1, k, :] = ab[cc][:, :, 1, :]
    return car


PRE_NAMES = ['w_mod', 'b_mod', 'w_in', 'b_in', 'convw', 'convb', 'wrg', 'wig', 'brg', 'big', 'lam']
XPAD = SEQ + 2 * HALO


def all_consts():
    pos = np.arange(XPAD) - HALO
    row = (pos // 64).astype(np.float32)
    colp = (pos % 64).astype(np.float32)
    inv = (10000.0 ** (-np.arange(0, 32, 2, dtype=np.float32) / 32)).astype(np.float32)
    cos = np.zeros((64, XPAD), np.float32)
    sin = np.zeros((64, XPAD), np.float32)
    for d in range(64):
        half, i = d // 32, d % 32
        ang = (row if half == 0 else colp) * inv[i % 16]
        cos[d] = np.cos(ang)
        sin[d] = np.sin(ang) * (-1.0 if i < 16 else 1.0)
    C = const_inputs(0)
    C.pop('rope_cos')
    C.pop('rope_sin')
    C['rope_cos_all'] = cos
    C['rope_sin_all'] = sin
    ed = np.ones((NSEG, 128, 2), np.float32)
    ed[0, :, 0] = 0.0
    ed[NSEG - 1, :, 1] = 0.0
    C['edge_all'] = ed
    C['car_id'] = carry_input(None, 0)
    return C


def build_fused(w_shapes, c_shapes):
    nc = bass.Bass("TRN2", target_bir_lowering=False)

    def ein(name, shape, dt=F32):
        return nc.dram_tensor(name, list(shape), dt, kind="ExternalInput").ap()
    X0 = ein("X0", [128, 8, XPAD])
    C0 = ein("C0", [128, 8, NCTX])
    cond = ein("cond", [128, 8, 2])
    EXPN = ("w1a", "w1b", "w3a", "w3b", "w2a", "w2b")
    Wall = {k: ein(k, [DEPTH] + list(shp)) for k, shp in w_shapes.items() if k not in EXPN}
    Wexp = {k: [ein("%s_%d" % (k, l_), list(w_shapes[k])) for l_ in range(DEPTH)] for k in EXPN}
    Call = {k: ein(k, shp) for k, shp in c_shapes.items()}
    yout = nc.dram_tensor("yout", [128, 8, SEQ], F32, kind="ExternalOutput").ap()
    XS = [nc.dram_tensor("XS%d" % i, [128, 8, XPAD], F32).ap() for i in range(2)]
    CS = [nc.dram_tensor("CS%d" % i, [128, 8, NCTX], F32).ap() for i in range(3)]
    AB_d = nc.dram_tensor("AB_d", [NSEG, 128, 8, 2, 2], F32).ap()
    CAR_d = nc.dram_tensor("CAR_d", [128, 8, 2, 3, 2], F32).ap()
    with contextlib.ExitStack() as st0:
        sh = Shared(nc, st0)
        fw = sh.fw
        with contextlib.ExitStack() as sz:
            zt = sz.enter_context(nc.sbuf_tensor("zt", [128, 8, HALO], F32))
            fw.op('dve', lambda: nc.vector.memset(zt[:], 0.0), writes=['zt'])
            for i in range(2):
                fw.dma('sp', lambda: nc.sync.dma_start(out=XS[i][:, :, 0:HALO], in_=zt[:]), reads=['zt'], writes=['XS'])
                fw.dma('sp', lambda: nc.sync.dma_start(out=XS[i][:, :, HALO + SEQ:XPAD], in_=zt[:]), reads=['zt'], writes=['XS'])
            fw.barrier()
        for l in range(DEPTH):
            Xsrc = X0 if l == 0 else XS[(l - 1) % 2]
            Xdst = XS[l % 2]
            Csrc = C0 if l == 0 else CS[(l - 1) % 2]
            Cdst = CS[l % 2]
            for prepass in (True, False):
                for seg in range(NSEG):
                    if not prepass:
                        fwd = list(range(0, seg))
                        bwd = list(range(NSEG - 1, seg, -1))
                        for d_, lst in ((0, fwd), (1, bwd)):
                            for k in range(3):
                                if k < len(lst):
                                    src_ = AB_d[lst[k]][:, :, d_, :]
                                else:
                                    src_ = Call['car_id'][:, :, d_, k, :]
                                fw.dma('sp', lambda: nc.sync.dma_start(out=CAR_d[:, :, d_, k, :], in_=src_), writes=['CAR_d'])
                        fw.barrier()

                    def getin(name, shape, dt=F32, seg=seg, prepass=prepass):
                        if name == 'xT':
                            return Xsrc[:, :, seg * TOWN:seg * TOWN + TLAT]
                        if name == 'cT':
                            return Csrc
                        if name == 'cond':
                            return cond
                        if name == 'edge':
                            return Call['edge_all'][seg]
                        if name == 'car':
                            return Call['car_id'] if prepass else CAR_d
                        if name == 'rope_cos':
                            return Call['rope_cos_all'][:, seg * TOWN:seg * TOWN + TLAT]
                        if name == 'rope_sin':
                            return Call['rope_sin_all'][:, seg * TOWN:seg * TOWN + TLAT]
                        if name in Call:
                            return Call[name]
                        if name in Wexp:
                            return Wexp[name][l]
                        return Wall[name][l]

                    def getout(name, shape, dt=F32, seg=seg):
                        if name == 'ab_out':
                            return AB_d[seg]
                        if name == 'xout':
                            return XS[l % 2][:, :, HALO:HALO + TOWN]
                        if name == 'cout':
                            return CS[2]
                        raise KeyError(name)
                    if prepass:
                        emit_layer(nc, sh, getin, getout, True, seg=seg, lru="store", mod=("store" if seg == 0 else "load"))
                    else:
                        emit_layer(nc, sh, getin, getout, False, mode="premoe", seg=seg, lru="load", mod="load")

            def getout_m(name, shape, dt=F32):
                if name == 'xout':
                    return yout if l == DEPTH - 1 else Xdst[:, :, HALO:HALO + SEQ]
                if name == 'cout':
                    return Cdst
                return CS[2]
            emit_layer(nc, sh, getin, getout_m, False, mode="moe", mod="load")
        fw.finish([])
        print("fused instructions:", fw.n_instr)
    return nc


_PROG = {}


def _unfm(a):
    return np.ascontiguousarray(a.transpose(2, 1, 0).reshape(a.shape[2], D))


def kernel(**inputs):
    inp = {k: np.asarray(v) for k, v in inputs.items()}
    x = np.ascontiguousarray(inp['x'], dtype=np.float32)
    ctx = np.ascontiguousarray(inp['ctx'], dtype=np.float32)
    c, c_ctx = inp['c'].astype(np.float32), inp['c_ctx'].astype(np.float32)
    Ws = [prep_layer_weights(inp, l) for l in range(DEPTH)]
    EXPN = ("w1a", "w1b", "w3a", "w3b", "w2a", "w2b")
    Wst = {k: np.stack([Ws[l][k] for l in range(DEPTH)], 0) for k in Ws[0] if k not in EXPN}
    Wex = {"%s_%d" % (k, l): Ws[l][k] for k in EXPN for l in range(DEPTH)}
    wshapes = {k: Ws[0][k].shape for k in Ws[0]}
    del Ws
    C = all_consts()
    if 'fused' not in _PROG:
        _PROG['fused'] = build_fused(wshapes, {k: v.shape for k, v in C.items()})
    nc = _PROG['fused']
    cores = list(range(8))
    per_batch = []
    for b in range(2):
        xp = np.zeros((XPAD, D), np.float32)
        xp[HALO:HALO + SEQ] = x[b]
        m = {'X0': _fm(xp), 'C0': _fm(ctx[b]), 'cond': np.ascontiguousarray(np.stack([_pp(c[b]), _pp(c_ctx)], -1))}
        m.update(Wst)
        m.update(Wex)
        m.update(C)
        per_batch.append(m)
    maps = [per_batch[cc // NSEG] for cc in cores]
    res = run_bass_kernel_spmd(nc, maps, core_ids=cores)
    out = np.empty_like(x)
    for b in range(2):
        out[b] = _unfm(np.asarray(res.results[b * NSEG]['yout']))
    return out
```
